# Optimizing a Trainium2 kernel written in Bass

```python
import math
import jax, jax.numpy as jnp
from jax import lax
import numpy as np

D_MODEL = 1024
BATCH = 8
SEQ = 4096
DEPTH = 2

CHUNK = 64

CONV_WIDTH = D_MODEL // 2
CONV_GROUPS = 8
CONV_K = 3
SGU_WIDTH = D_MODEL // 2
SGU_HEADS = 8
SGU_HEAD_DIM = SGU_WIDTH // SGU_HEADS
SGU_BLOCK = 128
IN_COLS = 3 * CONV_WIDTH + 2 * SGU_WIDTH

SB_HEADS = 16
SB_HEAD_DIM = D_MODEL // SB_HEADS
SB_QBLOCK = 128

N_GROUPS = 4
EXPERTS_PER_GROUP = 4
TOP_K_IN_GROUP = 2
D_EXPERT = D_MODEL // 2

DN_ALPHA = (2 * DEPTH) ** 0.25
DN_BETA = (8 * DEPTH) ** -0.25
LN_EPS = 1e-5

N_EVEN = (DEPTH + 1) // 2
N_ODD = DEPTH // 2

kernel_name = "hybrid_conv_sgu_stickbreak_hmoe_deepnorm"


def layer_norm(x, g, b):
    xf = x.astype(jnp.float32)
    mu = jnp.mean(xf, axis=-1, keepdims=True)
    var = jnp.mean(jnp.square(xf - mu), axis=-1, keepdims=True)
    y = (xf - mu) * lax.rsqrt(var + LN_EPS) * g.astype(jnp.float32) + b.astype(jnp.float32)
    return y.astype(x.dtype)


def short_conv_mix(b_gate, c_gate, h, conv_w):
    u = c_gate * h
    y = lax.conv_general_dilated(
        u, conv_w[:, None, :].astype(u.dtype),
        window_strides=(1,), padding=[(CONV_K - 1, 0)],
        dimension_numbers=("NWC", "WIO", "NWC"),
        feature_group_count=CONV_WIDTH)
    return b_gate * y


def spatial_gating(z_u, z_v, ln_g, ln_b, w_s, b_s):
    bsz, seq, _ = z_v.shape
    v = layer_norm(z_v, ln_g, ln_b)
    vb = v.reshape(bsz, seq // SGU_BLOCK, SGU_BLOCK, SGU_HEADS, SGU_HEAD_DIM)
    pos = jnp.arange(SGU_BLOCK)
    chunk_causal = (pos[None, :] // CHUNK) <= (pos[:, None] // CHUNK)
    w = jnp.where(chunk_causal[None], w_s, jnp.zeros((), w_s.dtype))
    mixed = jnp.einsum("hts,bnshc->bnthc", w, vb) + jnp.transpose(b_s)[None, None, :, :, None]
    return z_u * mixed.reshape(bsz, seq, SGU_WIDTH)


def conv_sgu_mixer(x, w_in, conv_w, ln_g, ln_b, w_s, b_s, w_out):
    proj = x @ w_in
    b_gate, c_gate, h, z_sgu = jnp.split(
        proj, [CONV_WIDTH, 2 * CONV_WIDTH, 3 * CONV_WIDTH], axis=-1)
    z_u, z_v = jnp.split(jax.nn.gelu(z_sgu), 2, axis=-1)
    y_a = short_conv_mix(b_gate, c_gate, h, conv_w)
    y_b = spatial_gating(z_u, z_v, ln_g, ln_b, w_s, b_s)
    return jnp.concatenate([y_a, y_b], axis=-1) @ w_out


def stick_breaking_attention(q, k, v):
    seq = q.shape[2]
    qf = q.astype(jnp.float32) * (SB_HEAD_DIM ** -0.5)
    kf = k.astype(jnp.float32)
    vf = v.astype(jnp.float32)
    outs = []
    for i in range(seq // SB_QBLOCK):
        start, end = i * SB_QBLOCK, (i + 1) * SB_QBLOCK
        z = jnp.einsum("bhtd,bhsd->bhts", qf[:, :, start:end], kf[:, :, :end])
        t_pos = start + jnp.arange(SB_QBLOCK)
        s_pos = jnp.arange(end)
        strict = s_pos[None, :] < t_pos[:, None]
        log_1m = jnp.where(strict, jax.nn.log_sigmoid(-z), 0.0)
        between = lax.cumsum(log_1m, axis=3, reverse=True) - log_1m
        att = jnp.where(strict, jnp.exp(jax.nn.log_sigmoid(z) + between), 0.0)
        outs.append(jnp.einsum("bhts,bhsd->bhtd", att, vf[:, :, :end]))
    return jnp.concatenate(outs, axis=2).astype(v.dtype)


def stick_breaking_mixer(x, w_qkv, w_out):
    bsz, seq, _ = x.shape
    qkv = (x @ w_qkv).reshape(bsz, seq, 3, SB_HEADS, SB_HEAD_DIM)
    q = jnp.transpose(qkv[:, :, 0], (0, 2, 1, 3))
    k = jnp.transpose(qkv[:, :, 1], (0, 2, 1, 3))
    v = jnp.transpose(qkv[:, :, 2], (0, 2, 1, 3))
    o = stick_breaking_attention(q, k, v)
    return jnp.transpose(o, (0, 2, 1, 3)).reshape(bsz, seq, D_MODEL) @ w_out


def hierarchical_moe(x, w_group, b_group, w_router, b_router, w1, w3, w2):
    bsz, seq, d = x.shape
    xt = x.reshape(-1, d)
    g_prob = jax.nn.softmax((xt @ w_group).astype(jnp.float32) + b_group.astype(jnp.float32), axis=-1)
    g_top, g_idx = lax.top_k(g_prob, 1)
    e_logits_all = jnp.einsum("td,gde->tge", xt, w_router).astype(jnp.float32) + b_router.astype(jnp.float32)
    e_logits = jnp.take_along_axis(e_logits_all, g_idx[:, :, None], axis=1)[:, 0]
    e_top, e_idx = lax.top_k(jax.nn.softmax(e_logits, axis=-1), TOP_K_IN_GROUP)
    e_w = e_top / jnp.sum(e_top, axis=-1, keepdims=True)
    w_in_group = jnp.sum(jax.nn.one_hot(e_idx, EXPERTS_PER_GROUP) * e_w[..., None], axis=1)
    combine = jax.nn.one_hot(g_idx[:, 0], N_GROUPS)[:, :, None] * (g_top * w_in_group)[:, None, :]
    y = jnp.zeros((xt.shape[0], d), jnp.float32)
    for g in range(N_GROUPS):
        hid = jax.nn.silu(jnp.einsum("td,edf->tef", xt, w1[g])) * jnp.einsum("td,edf->tef", xt, w3[g])
        y = y + jnp.einsum("tef,efd->td", hid * combine[:, g, :, None].astype(hid.dtype), w2[g])
    return y.astype(x.dtype).reshape(bsz, seq, d)


def setup_inputs(seed: int = 0) -> dict:
    key = jax.random.key(seed)
    ks = jax.random.split(key, 24)
    f32 = jnp.float32

    def nrm(k, shape, scale):
        return jax.random.normal(k, shape, f32) * scale

    return {
        "x": nrm(ks[0], (BATCH, SEQ, D_MODEL), 1.0),
        "even_w_in": nrm(ks[1], (N_EVEN, D_MODEL, IN_COLS), D_MODEL ** -0.5),
        "even_conv_w": nrm(ks[2], (N_EVEN, CONV_K, CONV_WIDTH), CONV_K ** -0.5),
        "even_sgu_ln_g": 1.0 + nrm(ks[3], (N_EVEN, SGU_WIDTH), 0.02),
        "even_sgu_ln_b": nrm(ks[4], (N_EVEN, SGU_WIDTH), 0.02),
        "even_sgu_w_s": nrm(ks[5], (N_EVEN, SGU_HEADS, SGU_BLOCK, SGU_BLOCK), SGU_BLOCK ** -0.5),
        "even_sgu_b_s": 1.0 + nrm(ks[6], (N_EVEN, SGU_HEADS, SGU_BLOCK), 0.02),
        "even_w_out": nrm(ks[7], (N_EVEN, D_MODEL, D_MODEL), DN_BETA * D_MODEL ** -0.5),
        "odd_w_qkv": nrm(ks[8], (N_ODD, D_MODEL, 3 * D_MODEL), D_MODEL ** -0.5),
        "odd_w_out": nrm(ks[9], (N_ODD, D_MODEL, D_MODEL), DN_BETA * D_MODEL ** -0.5),
        "mix_ln_g": 1.0 + nrm(ks[10], (DEPTH, D_MODEL), 0.02),
        "mix_ln_b": nrm(ks[11], (DEPTH, D_MODEL), 0.02),
        "moe_w_group": nrm(ks[12], (DEPTH, D_MODEL, N_GROUPS), D_MODEL ** -0.5),
        "moe_b_group": nrm(ks[13], (DEPTH, N_GROUPS), 0.01),
        "moe_w_router": nrm(ks[14], (DEPTH, N_GROUPS, D_MODEL, EXPERTS_PER_GROUP), D_MODEL ** -0.5),
        "moe_b_router": nrm(ks[15], (DEPTH, N_GROUPS, EXPERTS_PER_GROUP), 0.01),
        "moe_w1": nrm(ks[16], (DEPTH, N_GROUPS, EXPERTS_PER_GROUP, D_MODEL, D_EXPERT), D_MODEL ** -0.5),
        "moe_w3": nrm(ks[17], (DEPTH, N_GROUPS, EXPERTS_PER_GROUP, D_MODEL, D_EXPERT), D_MODEL ** -0.5),
        "moe_w2": nrm(ks[18], (DEPTH, N_GROUPS, EXPERTS_PER_GROUP, D_EXPERT, D_MODEL), DN_BETA * D_EXPERT ** -0.5),
        "ffn_ln_g": 1.0 + nrm(ks[19], (DEPTH, D_MODEL), 0.02),
        "ffn_ln_b": nrm(ks[20], (DEPTH, D_MODEL), 0.02),
    }


def reference(x, even_w_in, even_conv_w, even_sgu_ln_g, even_sgu_ln_b, even_sgu_w_s,
              even_sgu_b_s, even_w_out, odd_w_qkv, odd_w_out, mix_ln_g, mix_ln_b,
              moe_w_group, moe_b_group, moe_w_router, moe_b_router, moe_w1, moe_w3,
              moe_w2, ffn_ln_g, ffn_ln_b):
    for layer in range(DEPTH):
        i = layer // 2
        if layer % 2 == 0:
            mix = conv_sgu_mixer(x, even_w_in[i], even_conv_w[i], even_sgu_ln_g[i],
                                 even_sgu_ln_b[i], even_sgu_w_s[i], even_sgu_b_s[i], even_w_out[i])
        else:
            mix = stick_breaking_mixer(x, odd_w_qkv[i], odd_w_out[i])
        x = layer_norm(DN_ALPHA * x + mix, mix_ln_g[layer], mix_ln_b[layer])
        ffn = hierarchical_moe(x, moe_w_group[layer], moe_b_group[layer], moe_w_router[layer],
                               moe_b_router[layer], moe_w1[layer], moe_w3[layer], moe_w2[layer])
        x = layer_norm(DN_ALPHA * x + ffn, ffn_ln_g[layer], ffn_ln_b[layer])
    return x
```

```python
import contextlib
import numpy as np
import concourse.bass as bass
import concourse.mybir as mybir
from concourse.bass_utils import run_bass_kernel_spmd

F32 = mybir.dt.float32
BF16 = mybir.dt.bfloat16
AF = mybir.ActivationFunctionType
ALU = mybir.AluOpType

T = 4096
D = 1024
NT = 32
ALPHA = float(4 ** 0.25)
EPS = 1e-5
NCORES = 8


class B:
    __slots__ = ("t", "w", "r", "dsem", "dcnt")

    def __init__(self, t=None):
        self.t = t
        self.w = None
        self.r = {}
        self.dsem = None
        self.dcnt = 0


class Eng:
    pass


class KB:
    def __init__(self):
        self.nc = bass.Bass("TRN2", target_bir_lowering=False)
        self.es = contextlib.ExitStack()
        self.nsem = 0
        self.dma_bufs = []

    def sem(self, name):
        self.nsem += 1
        return self.es.enter_context(self.nc.semaphore(name))

    def sb(self, name, shape, dt, stack=None):
        st = stack if stack is not None else self.es
        self.nt = getattr(self, "nt", 0) + 1
        return B(st.enter_context(self.nc.sbuf_tensor("%s_%d" % (name, self.nt), shape, dt)))

    def ps(self, name, shape, dt, stack=None):
        st = stack if stack is not None else self.es
        self.nt = getattr(self, "nt", 0) + 1
        return B(st.enter_context(self.nc.psum_tensor("%s_%d" % (name, self.nt), shape, dt)))

    def start(self):
        nc = self.nc
        self.E = {}
        for name, eng in (("pe", nc.tensor), ("act", nc.scalar), ("dve", nc.vector),
                          ("pool", nc.gpsimd), ("sp", nc.sync)):
            e = Eng()
            e.name = name
            e.eng = eng
            e.sem = self.sem("s_" + name)
            e.count = 0
            e.seen = {}
            self.E[name] = e

    def _waits(self, E, reads, writes):
        need = {}

        def acc(tok):
            k = id(tok[0])
            if k not in need or need[k][1] < tok[1]:
                need[k] = tok

        for b in reads:
            if b.w is not None:
                acc(b.w)
        for b in writes:
            if b.w is not None:
                acc(b.w)
            for tok in b.r.values():
                acc(tok)
        for k, (sem, val) in need.items():
            if E.name == "pe" and sem is E.sem:
                continue
            if E.seen.get(k, 0) >= val:
                continue
            E.eng.wait_ge(sem, val)
            E.seen[k] = val

    def _commit(self, tok, reads, writes):
        k = id(tok[0])
        for b in reads:
            b.r[k] = tok
        for b in writes:
            b.w = tok
            b.r = {}

    def op(self, en, fn, reads=(), writes=(), mark=True):
        E = self.E[en]
        self._waits(E, reads, writes)
        inst = fn(E.eng)
        if mark:
            E.count += 1
            inst.then_inc(E.sem, 1)
            tok = (E.sem, E.count)
        else:
            tok = (E.sem, E.count + 1)
        self._commit(tok, reads, writes)
        return inst

    def dma(self, q, out_ap, in_ap, sbufB, reads=(), writes=()):
        E = self.E[q]
        self._waits(E, reads, writes)
        if sbufB.dsem is None:
            sbufB.dsem = self.sem("d%d" % self.nsem)
            self.dma_bufs.append(sbufB)
        sbufB.dcnt += 16
        E.eng.dma_start(out=out_ap, in_=in_ap).then_inc(sbufB.dsem, 16)
        tok = (sbufB.dsem, sbufB.dcnt)
        self._commit(tok, reads, writes)

    def barrier(self):
        toks = []
        for e in self.E.values():
            if e.count > 0:
                toks.append((e.sem, e.count))
        for b in self.dma_bufs:
            if b.dcnt > 0:
                toks.append((b.dsem, b.dcnt))
        for E in self.E.values():
            for (sem, val) in toks:
                if sem is E.sem:
                    continue
                k = id(sem)
                if E.seen.get(k, 0) >= val:
                    continue
                E.eng.wait_ge(sem, val)
                E.seen[k] = val

    def mmg(self, outB, out_ap, pairs, reads):
        n = len(pairs)
        for i, (l, r) in enumerate(pairs):
            self.op("pe", lambda e, l=l, r=r, i=i: e.matmul(out_ap, lhsT=l, rhs=r, start=(i == 0), stop=(i == n - 1)),
                    reads=reads, writes=[outB], mark=(i == n - 1))

    def act(self, out_ap, in_ap, func, reads, writes, bias=None, scale=None):
        kw = {}
        if bias is not None:
            kw["bias"] = bias
        if scale is not None:
            kw["scale"] = scale
        self.op("act", lambda e: e.activation(out=out_ap, in_=in_ap, func=func, **kw), reads=reads, writes=writes)

    def copy(self, en, out_ap, in_ap, reads, writes):
        if en == "act":
            self.op("act", lambda e: e.copy(out=out_ap, in_=in_ap), reads=reads, writes=writes)
        else:
            self.op(en, lambda e: e.tensor_copy(out=out_ap, in_=in_ap), reads=reads, writes=writes)

    def tt(self, en, out_ap, a_ap, b_ap, op, reads, writes):
        self.op(en, lambda e: e.tensor_tensor(out=out_ap, in0=a_ap, in1=b_ap, op=op), reads=reads, writes=writes)

    def ts(self, en, out_ap, in_ap, s1, s2, op0, op1, reads, writes):
        if op1 is None:
            self.op(en, lambda e: e.tensor_scalar(out=out_ap, in0=in_ap, scalar1=s1, scalar2=None, op0=op0),
                    reads=reads, writes=writes)
        else:
            self.op(en, lambda e: e.tensor_scalar(out=out_ap, in0=in_ap, scalar1=s1, scalar2=s2, op0=op0, op1=op1),
                    reads=reads, writes=writes)

    def stt(self, en, out_ap, in0, scalar, in1, op0, op1, reads, writes):
        self.op(en, lambda e: e.scalar_tensor_tensor(out=out_ap, in0=in0, scalar=scalar, in1=in1, op0=op0, op1=op1),
                reads=reads, writes=writes)


def build(stop_after=None, dbg=False):
    kb = KB()
    dk = {"kind": "ExternalOutput"} if dbg else {}
    nc = kb.nc

    def din(name, shape):
        return nc.dram_tensor(name, shape, F32, kind="ExternalInput").ap()

    x_d = din("x", [T, D])
    w_in_d = din("w_in", [D, 2560])
    convw_d = din("convw", [128, 12])
    sgu_g_d = din("sgu_g", [1, 512])
    sgu_b_d = din("sgu_b", [1, 512])
    wsT_d = din("wsT", [8, 128, 128])
    bs_d = din("bs", [1, 1024])
    w_out0_d = din("w_out0", [D, D])
    w_qkv_d = din("w_qkv", [D, 3072])
    w_out1_d = din("w_out1", [D, D])
    mix_g_d = din("mix_g", [2, D])
    mix_b_d = din("mix_b", [2, D])
    wr_d = din("wr", [2, D, 20])
    br_d = din("br", [2, 20])
    w1_d = din("w1", [2, 16, D, 512])
    w3_d = din("w3", [2, 16, D, 512])
    w2_d = din("w2", [2, 16, 512, D])
    ffn_g_d = din("ffn_g", [2, D])
    ffn_b_d = din("ffn_b", [2, D])
    out_d = nc.dram_tensor("out", [T, D], F32, kind="ExternalOutput").ap()

    xa_d = nc.dram_tensor("xa_s", [T, D], F32, **dk).ap()
    xb_d = nc.dram_tensor("xb_s", [T, D], F32, **dk).ap()
    xT_d = nc.dram_tensor("xT_s", [128, 8, T], BF16, **dk).ap()
    qT_d = nc.dram_tensor("qT_s", [8, 128, T], BF16, **dk).ap()
    kT_d = nc.dram_tensor("kT_s", [8, 128, T], BF16, **dk).ap()
    v_d = nc.dram_tensor("v_s", [T, D], BF16, **dk).ap()

    xa_B = [B() for _ in range(NT)]
    xb_B = [B() for _ in range(NT)]
    xT_B = [B() for _ in range(8)]
    q_B = [B() for _ in range(8)]
    k_B = [B() for _ in range(8)]
    v_B = [B() for _ in range(8)]
    out_B = [B() for _ in range(NT)]

    kb.start()
    es = kb.es

    ident = kb.sb("ident", [128, 128], BF16)
    c_all = kb.sb("c_all", [128, NT, 16], F32)

    block = es.enter_context(nc.Block())

    @block.sync
    def _(sync):
        kb.op("pool", lambda e: e.memset(ident.t[:], 0.0), writes=[ident])
        kb.op("pool", lambda e: e.affine_select(out=ident.t[:], in_=ident.t[:], pattern=[[-1, 128]],
                                                compare_op=ALU.not_equal, fill=1.0, base=0, channel_multiplier=1),
              reads=[ident], writes=[ident])

        def layernorm(ps_, r, D_, g_bc, b_bc, outB, out_ap, stats, mv, tmp, mul_eng="pool"):
            nch = D_ // 512
            for c in range(nch):
                kb.op("dve", lambda e, c=c: e.bn_stats(out=stats.t[:, c * 6:(c + 1) * 6], in_=r.t[:, c * 512:(c + 1) * 512]),
                      reads=[r], writes=[stats])
            kb.op("dve", lambda e: e.bn_aggr(out=mv.t[:, 0:2], in_=stats.t[:, 0:nch * 6]), reads=[stats], writes=[mv])
            kb.act(tmp.t[:, 0:1], mv.t[:, 1:2], AF.Ln, [mv], [tmp], bias=EPS, scale=1.0)
            kb.act(tmp.t[:, 1:2], tmp.t[:, 0:1], AF.Exp, [tmp], [tmp], scale=-0.5)
            kb.ts("dve", r.t[:, 0:D_], r.t[:, 0:D_], mv.t[:, 0:1], tmp.t[:, 1:2], ALU.subtract, ALU.mult, [r, mv, tmp], [r])
            kb.tt(mul_eng, r.t[:, 0:D_], r.t[:, 0:D_], g_bc.t[:, 0:D_], ALU.mult, [r, g_bc], [r])
            kb.tt(mul_eng, out_ap, r.t[:, 0:D_], b_bc.t[:, 0:D_], ALU.add, [r, b_bc], [outB])

        def bcast_load(dst, src_row_ap):
            kb.dma("sp", dst.t[:], src_row_ap.partition_broadcast(128), dst, writes=[dst])

        def post_mixer(ps_, lyr, gt, x_res_ap, x_resB, pso, yT, yT_cols, Wo, P):
            for dh in range(2):
                kb.mmg(pso[dh], pso[dh].t[:, :],
                       [(yT.t[:, k, yT_cols], Wo.t[:, k, dh * 512:(dh + 1) * 512]) for k in range(8)],
                       reads=[yT, Wo])
            r = P["r"][gt % 2]
            for dh in range(2):
                kb.stt("dve", r.t[:, dh * 512:(dh + 1) * 512], x_res_ap[:, dh * 512:(dh + 1) * 512], ALPHA,
                       pso[dh].t[:, :], ALU.mult, ALU.add, [x_resB, pso[dh]], [r])
            x1 = P["x1"][gt % 2]
            layernorm(ps_, r, D, P["mg"], P["mb"], x1, x1.t[:, :], P["stats"], P["mv"], P["tmp"])
            kb.dma("pool", xa_d[gt * 128:(gt + 1) * 128, :], x1.t[:, :], x1, reads=[x1], writes=[xa_B[gt]])
            x1b = P["x1b"]
            kb.copy("pool", x1b.t[:, :], x1.t[:, :], [x1], [x1b])
            pT = P["pT"]
            for k in range(8):
                kb.op("pe", lambda e, k=k: e.transpose(pT.t[:, k * 128:(k + 1) * 128], x1b.t[:, k * 128:(k + 1) * 128], ident.t[:]),
                      reads=[x1b, ident], writes=[pT], mark=(k == 7))
            x1T = P["x1T"][(gt // 4) % 2]
            s = gt % 4
            kb.copy("act", x1T.t[:, :, s * 128:(s + 1) * 128], pT.t[:, :].rearrange("p (k t) -> p k t", k=8), [pT], [x1T])
            prt = P["prt"]
            kb.mmg(prt, prt.t[:, 0:20], [(x1T.t[:, k, s * 128:(s + 1) * 128], P["Wr"].t[:, k, :]) for k in range(8)],
                   reads=[x1T, P["Wr"]])
            rt = P["rt"]
            lg = rt.t[:, 0:20]
            kb.tt("dve", lg, prt.t[:, 0:20], P["brb"].t[:, :], ALU.add, [prt, P["brb"]], [rt])
            R_ = [rt]
            kb.op("dve", lambda e: e.reduce_max(out=rt.t[:, 20:21], in_=rt.t[:, 0:4], axis=mybir.AxisListType.X), reads=R_, writes=R_)
            kb.ts("dve", rt.t[:, 24:28], rt.t[:, 0:4], rt.t[:, 20:21], None, ALU.is_equal, None, R_, R_)
            kb.ts("dve", rt.t[:, 21:22], rt.t[:, 20:21], -1.0, None, ALU.mult, None, R_, R_)
            kb.act(rt.t[:, 28:32], rt.t[:, 0:4], AF.Exp, R_, R_, bias=rt.t[:, 21:22], scale=1.0)
            kb.op("dve", lambda e: e.reduce_sum(out=rt.t[:, 22:23], in_=rt.t[:, 28:32], axis=mybir.AxisListType.X), reads=R_, writes=R_)
            kb.ts("dve", rt.t[:, 32:36], rt.t[:, 4:8], rt.t[:, 24:25], None, ALU.mult, None, R_, R_)
            for g in range(1, 4):
                kb.stt("dve", rt.t[:, 32:36], rt.t[:, 4 + 4 * g:8 + 4 * g], rt.t[:, 24 + g:25 + g], rt.t[:, 32:36],
                       ALU.mult, ALU.add, R_, R_)
            kb.op("dve", lambda e: e.reduce_max(out=rt.t[:, 36:37], in_=rt.t[:, 32:36], axis=mybir.AxisListType.X), reads=R_, writes=R_)
            kb.ts("dve", rt.t[:, 40:44], rt.t[:, 32:36], rt.t[:, 36:37], None, ALU.is_equal, None, R_, R_)
            kb.ts("dve", rt.t[:, 37:38], rt.t[:, 36:37], -1.0, None, ALU.mult, None, R_, R_)
            kb.act(rt.t[:, 44:48], rt.t[:, 32:36], AF.Exp, R_, R_, bias=rt.t[:, 37:38], scale=1.0)
            kb.op("dve", lambda e: e.reduce_max(out=rt.t[:, 38:39], in_=rt.t[:, 44:48], axis=mybir.AxisListType.X), reads=R_, writes=R_)
            kb.tt("dve", rt.t[:, 48:52], rt.t[:, 44:48], rt.t[:, 40:44], ALU.mult, R_, R_)
            kb.tt("dve", rt.t[:, 48:52], rt.t[:, 44:48], rt.t[:, 48:52], ALU.subtract, R_, R_)
            kb.op("dve", lambda e: e.reduce_max(out=rt.t[:, 39:40], in_=rt.t[:, 48:52], axis=mybir.AxisListType.X), reads=R_, writes=R_)
            kb.ts("dve", rt.t[:, 52:56], rt.t[:, 44:48], rt.t[:, 39:40], None, ALU.is_ge, None, R_, R_)
            kb.tt("dve", rt.t[:, 52:56], rt.t[:, 52:56], rt.t[:, 44:48], ALU.mult, R_, R_)
            kb.tt("dve", rt.t[:, 56:57], rt.t[:, 38:39], rt.t[:, 39:40], ALU.add, R_, R_)
            kb.tt("dve", rt.t[:, 56:57], rt.t[:, 56:57], rt.t[:, 22:23], ALU.mult, R_, R_)
            kb.op("dve", lambda e: e.reciprocal(out=rt.t[:, 57:58], in_=rt.t[:, 56:57]), reads=R_, writes=R_)
            kb.ts("dve", rt.t[:, 60:64], rt.t[:, 24:28], rt.t[:, 57:58], None, ALU.mult, None, R_, R_)
            for g in range(4):
                kb.ts("dve", c_all.t[:, gt, 4 * g:4 * g + 4], rt.t[:, 52:56], rt.t[:, 60 + g:61 + g], None, ALU.mult, None,
                      R_, [c_all])
            if s == 3:
                mt = gt // 4
                kb.dma("pool", xT_d[:, :, mt * 512:(mt + 1) * 512], x1T.t[:, :, :], x1T, reads=[x1T], writes=[xT_B[mt]])

        def post_mixer_allocs(ps_, lyr):
            P = {}
            P["r"] = [kb.sb("pm_r%d" % i, [128, D], F32, ps_) for i in range(2)]
            P["x1"] = [kb.sb("pm_x1%d" % i, [128, D], F32, ps_) for i in range(2)]
            P["x1b"] = kb.sb("pm_x1b", [128, D], BF16, ps_)
            P["x1T"] = [kb.sb("pm_x1T", [128, 8, 512], BF16, ps_)] * 2
            P["stats"] = kb.sb("pm_stats", [128, 12], F32, ps_)
            P["mv"] = kb.sb("pm_mv", [128, 2], F32, ps_)
            P["tmp"] = kb.sb("pm_tmp", [128, 2], F32, ps_)
            P["rt"] = kb.sb("pm_rt", [128, 64], F32, ps_)
            P["mg"] = kb.sb("pm_mg", [128, D], F32, ps_)
            P["mb"] = kb.sb("pm_mb", [128, D], F32, ps_)
            P["brb"] = kb.sb("pm_brb", [128, 20], F32, ps_)
            P["Wr"] = kb.sb("pm_Wr", [128, 8, 20], BF16, ps_)
            P["pT"] = kb.ps("pm_pT", [128, 1024], BF16, ps_)
            P["prt"] = kb.ps("pm_prt", [128, 512], F32, ps_)
            bcast_load(P["mg"], mix_g_d[lyr:lyr + 1, :])
            bcast_load(P["mb"], mix_b_d[lyr:lyr + 1, :])
            bcast_load(P["brb"], br_d[lyr:lyr + 1, :])
            kb.dma("pool", P["Wr"].t[:, :, :], wr_d[lyr].rearrange("(k p) n -> p k n", p=128), P["Wr"], writes=[P["Wr"]])
            return P

        def load_w(ps_, name, src_ap, ncols):
            W = kb.sb(name, [128, 8, ncols], BF16, ps_)
            v = src_ap.rearrange("(k p) n -> p k n", p=128)
            for k in range(8):
                kb.dma("pool", W.t[:, k, :], v[:, k, :], W, writes=[W])
            return W

        def load_xT(xsrc_d, xsrc_B, mt, xin, xbf, pT, xT):
            rd = [xsrc_B[mt * 4 + s] for s in range(4)] if xsrc_B is not None else []
            kb.dma("sp", xin.t[:, :, :], xsrc_d[mt * 512:(mt + 1) * 512, :].rearrange("(s p) d -> p s d", p=128), xin,
                   reads=rd, writes=[xin])
            for s in range(4):
                xb = xbf[s % 2]
                kb.copy("pool", xb.t[:, :], xin.t[:, s, :], [xin], [xb])
                for k in range(8):
                    kb.op("pe", lambda e, k=k, xb=xb: e.transpose(pT.t[:, k * 128:(k + 1) * 128], xb.t[:, k * 128:(k + 1) * 128], ident.t[:]),
                          reads=[xb, ident], writes=[pT], mark=(k == 7))
                kb.copy("act", xT.t[:, :, s * 128:(s + 1) * 128], pT.t[:, :].rearrange("p (k t) -> p k t", k=8), [pT], [xT])

        def mixer0():
            with contextlib.ExitStack() as ps_:
                P = post_mixer_allocs(ps_, 0)
                Wi = load_w(ps_, "m0_Wi", w_in_d, 2560)
                Wo = load_w(ps_, "m0_Wo", w_out0_d, 1024)
                WsT = kb.sb("m0_WsT", [128, 8, 128], BF16, ps_)
                kb.dma("pool", WsT.t[:, :, :], wsT_d.rearrange("h s t -> s h t"), WsT, writes=[WsT])
                kb.op("pool", lambda e: e.memset(WsT.t[64:128, :, 0:64], 0.0), reads=[WsT], writes=[WsT])
                bsr = kb.sb("m0_bsr", [1, 1024], BF16, ps_)
                kb.dma("pool", bsr.t[:, :], bs_d[:, :], bsr, writes=[bsr])
                onesr = kb.sb("m0_ones", [1, 128], BF16, ps_)
                kb.op("pool", lambda e: e.memset(onesr.t[:, :], 1.0), writes=[onesr])
                cw = kb.sb("m0_cw", [128, 12], F32, ps_)
                kb.dma("sp", cw.t[:, :], convw_d[:, :], cw, writes=[cw])
                sg = kb.sb("m0_sg", [128, 512], F32, ps_)
                sbb = kb.sb("m0_sbb", [128, 512], F32, ps_)
                bcast_load(sg, sgu_g_d[0:1, :])
                bcast_load(sbb, sgu_b_d[0:1, :])
                xin = kb.sb("m0_xin", [128, 4, D], F32, ps_)
                xbf = [kb.sb("m0_xbf%d" % i, [128, D], BF16, ps_) for i in range(2)]
                xres = [kb.sb("m0_xres%d" % i, [128, D], F32, ps_) for i in range(2)]
                xT = kb.sb("m0_xT", [128, 8, 512], BF16, ps_)
                yT = kb.sb("m0_yT", [128, 8, 512], BF16, ps_)
                ub = kb.sb("m0_ub", [128, 4, 514], F32, ps_)
                Cs = kb.sb("m0_Cs", [128, 512], F32, ps_)
                acc = kb.sb("m0_acc", [128, 512], F32, ps_)
                zu = kb.sb("m0_zu", [128, 4, 512], F32, ps_)
                g1 = kb.sb("m0_g1", [128, 512], F32, ps_)
                g2 = kb.sb("m0_g2", [128, 512], F32, ps_)
                gv = kb.sb("m0_gv", [128, 512], F32, ps_)
                vt = kb.sb("m0_vt", [128, 4, 512], BF16, ps_)
                st2 = kb.sb("m0_st2", [128, 6], F32, ps_)
                mv2 = kb.sb("m0_mv2", [128, 2], F32, ps_)
                tmp2 = kb.sb("m0_tmp2", [128, 2], F32, ps_)
                pp = [kb.ps("m0_pp%d" % i, [128, 512], F32, ps_) for i in range(2)]
                psg = [kb.ps("m0_psg%d" % i, [128, 512], F32, ps_) for i in range(2)]
                pso = [kb.ps("m0_pso%d" % i, [128, 512], F32, ps_) for i in range(2)]
                pT = P["pT"]
                kb.op("pool", lambda e: e.memset(ub.t[:, :, :], 0.0), writes=[ub])
                ppi = [0]

                def proj_fm(c, xT):
                    p = pp[ppi[0] % 2]
                    ppi[0] += 1
                    kb.mmg(p, p.t[:, :], [(Wi.t[:, k, c * 128:(c + 1) * 128], xT.t[:, k, :]) for k in range(8)], reads=[Wi, xT])
                    return p

                def gelu(p, p_ap, outB, out_ap, n):
                    kb.act(g1.t[:, 0:n], p_ap, AF.Square, [p], [g1])
                    kb.ts("pool", g1.t[:, 0:n], g1.t[:, 0:n], 0.044715, 1.0, ALU.mult, ALU.add, [g1], [g1])
                    kb.tt("dve", g2.t[:, 0:n], g1.t[:, 0:n], p_ap, ALU.mult, [g1, p], [g2])
                    kb.act(g1.t[:, 0:n], g2.t[:, 0:n], AF.Exp, [g2], [g1], scale=-1.5957691216057308)
                    kb.ts("pool", g1.t[:, 0:n], g1.t[:, 0:n], 1.0, None, ALU.add, None, [g1], [g1])
                    kb.op("dve", lambda e: e.reciprocal(out=g2.t[:, 0:n], in_=g1.t[:, 0:n]), reads=[g1], writes=[g2])
                    kb.tt("dve", out_ap, g2.t[:, 0:n], p_ap, ALU.mult, [g2, p], [outB])

                for mt in range(8):
                    load_xT(x_d, None, mt, xin, xbf, pT, xT)
                    u = ub
                    for j in range(4):
                        pC = proj_fm(4 + j, xT)
                        kb.copy("act", Cs.t[:, :], pC.t[:, :], [pC], [Cs])
                        pH = proj_fm(8 + j, xT)
                        kb.copy("dve", u.t[:, j, 0:2], u.t[:, j, 512:514], [u], [u])
                        kb.tt("dve", u.t[:, j, 2:514], Cs.t[:, :], pH.t[:, :], ALU.mult, [Cs, pH], [u])
                        kb.ts("dve", acc.t[:, :], u.t[:, j, 2:514], cw.t[:, j * 3 + 2:j * 3 + 3], None, ALU.mult, None, [u, cw], [acc])
                        kb.stt("dve", acc.t[:, :], u.t[:, j, 1:513], cw.t[:, j * 3 + 1:j * 3 + 2], acc.t[:, :], ALU.mult, ALU.add, [u, cw, acc], [acc])
                        kb.stt("dve", acc.t[:, :], u.t[:, j, 0:512], cw.t[:, j * 3:j * 3 + 1], acc.t[:, :], ALU.mult, ALU.add, [u, cw, acc], [acc])
                        pB = proj_fm(j, xT)
                        kb.tt("dve", yT.t[:, j, :], acc.t[:, :], pB.t[:, :], ALU.mult, [acc, pB], [yT])
                    for j in range(4):
                        pZ = proj_fm(12 + j, xT)
                        gelu(pZ, pZ.t[:, :], zu, zu.t[:, j, :], 512)
                    for s in range(4):
                        p = pp[ppi[0] % 2]
                        ppi[0] += 1
                        kb.mmg(p, p.t[:, :], [(xT.t[:, k, s * 128:(s + 1) * 128], Wi.t[:, k, 2048:2560]) for k in range(8)], reads=[Wi, xT])
                        gelu(p, p.t[:, :], gv, gv.t[:, :], 512)
                        layernorm(ps_, gv, 512, sg, sbb, vt, vt.t[:, s, :], st2, mv2, tmp2, mul_eng="pool")
                    for hp in range(4):
                        for j in range(2):
                            h = 2 * hp + j
                            pg = psg[j]
                            for s in range(4):
                                kb.op("pe", lambda e, s=s, pg=pg, h=h, hp=hp: e.matmul(pg.t[:, s * 128:(s + 1) * 128], lhsT=vt.t[:, s, hp * 128:(hp + 1) * 128],
                                                                         rhs=WsT.t[:, h, :], start=True, stop=False),
                                      reads=[vt, WsT], writes=[pg], mark=False)
                                kb.op("pe", lambda e, s=s, pg=pg, h=h: e.matmul(pg.t[:, s * 128:(s + 1) * 128], lhsT=onesr.t[0:1, :],
                                                                   rhs=bsr.t[0:1, h * 128:(h + 1) * 128], start=False, stop=True),
                                      reads=[onesr, bsr], writes=[pg], mark=(s == 3))
                            kb.tt("dve", yT.t[j * 64:(j + 1) * 64, 4 + hp, :], zu.t[j * 64:(j + 1) * 64, hp, :], pg.t[j * 64:(j + 1) * 64, :],
                                  ALU.mult, [zu, pg], [yT])
                    for s in range(4):
                        gt = mt * 4 + s
                        xr = xres[gt % 2]
                        kb.dma("sp", xr.t[:, :], x_d[gt * 128:(gt + 1) * 128, :], xr, writes=[xr])
                        post_mixer(ps_, 0, gt, xr.t[:, :], xr, pso, yT, slice(s * 128, (s + 1) * 128), Wo, P)
                kb.barrier()

        def moe(lyr, dst_d, dst_B):
            with contextlib.ExitStack() as ps_:
                fg = kb.sb("f_g", [128, D], F32, ps_)
                fb = kb.sb("f_b", [128, D], F32, ps_)
                bcast_load(fg, ffn_g_d[lyr:lyr + 1, :])
                bcast_load(fb, ffn_b_d[lyr:lyr + 1, :])
                xTh = kb.sb("f_xTh", [128, 8, 2048], BF16, ps_)
                yacc = kb.sb("f_yacc", [128, 16, D], F32, ps_)
                w1s = [kb.sb("f_w1_%d" % i, [128, 8, 512], BF16, ps_) for i in range(2)]
                w3s = [kb.sb("f_w3_%d" % i, [128, 8, 512], BF16, ps_) for i in range(2)]
                w2s = [kb.sb("f_w2_%d" % i, [128, 4, D], BF16, ps_) for i in range(2)]
                hid = [kb.sb("f_hid%d" % i, [128, 4, 512], BF16, ps_) for i in range(2)]
                sl = [kb.sb("f_sl%d" % i, [128, 512], BF16, ps_) for i in range(2)]
                x1l = [kb.sb("f_x1l%d" % i, [128, D], F32, ps_) for i in range(2)]
                rr = [kb.sb("f_r%d" % i, [128, D], F32, ps_) for i in range(2)]
                st = kb.sb("f_st", [128, 12], F32, ps_)
                mv = kb.sb("f_mv", [128, 2], F32, ps_)
                tmp = kb.sb("f_tmp", [128, 2], F32, ps_)
                ph1 = [kb.ps("f_ph1_%d" % i, [128, 512], F32, ps_) for i in range(2)]
                ph3 = [kb.ps("f_ph3_%d" % i, [128, 512], F32, ps_) for i in range(2)]
                py = [kb.ps("f_py%d" % i, [128, 512], F32, ps_) for i in range(2)]

                def load_expert(e):
                    sl_ = e % 2
                    kb.dma("pool", w1s[sl_].t[:, :, :], w1_d[lyr, e].rearrange("(k p) f -> p k f", p=128), w1s[sl_], writes=[w1s[sl_]])
                    kb.dma("pool", w3s[sl_].t[:, :, :], w3_d[lyr, e].rearrange("(k p) f -> p k f", p=128), w3s[sl_], writes=[w3s[sl_]])
                    kb.dma("pool", w2s[sl_].t[:, :, :], w2_d[lyr, e].rearrange("(c p) d -> p c d", p=128), w2s[sl_], writes=[w2s[sl_]])

                cnt = [0]
                for hf in range(2):
                    kb.dma("sp", xTh.t[:, :, :], xT_d[:, :, hf * 2048:(hf + 1) * 2048], xTh,
                           reads=[xT_B[hf * 4 + i] for i in range(4)], writes=[xTh])
                    load_expert(0)
                    for e in range(16):
                        if e + 1 < 16:
                            load_expert(e + 1)
                        w1 = w1s[e % 2]
                        w3 = w3s[e % 2]
                        w2 = w2s[e % 2]
                        for mt in range(4):
                            hd = hid[(e * 4 + mt) % 2]
                            for fc in range(4):
                                i2 = cnt[0] % 2
                                cnt[0] += 1
                                xs = xTh.t[:, :, mt * 512:(mt + 1) * 512]
                                kb.mmg(ph1[i2], ph1[i2].t[:, :], [(w1.t[:, k, fc * 128:(fc + 1) * 128], xTh.t[:, k, mt * 512:(mt + 1) * 512]) for k in range(8)],
                                       reads=[w1, xTh])
                                kb.mmg(ph3[i2], ph3[i2].t[:, :], [(w3.t[:, k, fc * 128:(fc + 1) * 128], xTh.t[:, k, mt * 512:(mt + 1) * 512]) for k in range(8)],
                                       reads=[w3, xTh])
                                kb.act(sl[i2].t[:, :], ph1[i2].t[:, :], AF.Silu, [ph1[i2]], [sl[i2]])
                                kb.tt("dve", hd.t[:, fc, :], sl[i2].t[:, :], ph3[i2].t[:, :], ALU.mult, [sl[i2], ph3[i2]], [hd])
                            for s in range(4):
                                ti = mt * 4 + s
                                gt = hf * 16 + ti
                                for dh in range(2):
                                    p = py[dh]
                                    kb.mmg(p, p.t[:, :], [(hd.t[:, fc, s * 128:(s + 1) * 128], w2.t[:, fc, dh * 512:(dh + 1) * 512]) for fc in range(4)],
                                           reads=[hd, w2])
                                    ya = yacc.t[:, ti, dh * 512:(dh + 1) * 512]
                                    if e == 0:
                                        kb.ts("dve", ya, p.t[:, :], c_all.t[:, gt, e:e + 1], None, ALU.mult, None, [p, c_all], [yacc])
                                    else:
                                        kb.stt("dve", ya, p.t[:, :], c_all.t[:, gt, e:e + 1], ya, ALU.mult, ALU.add, [p, c_all, yacc], [yacc])
                    for ti in range(16):
                        gt = hf * 16 + ti
                        xl = x1l[ti % 2]
                        r = rr[ti % 2]
                        kb.dma("sp", xl.t[:, :], xa_d[gt * 128:(gt + 1) * 128, :], xl, reads=[xa_B[gt]], writes=[xl])
                        kb.stt("dve", r.t[:, :], xl.t[:, :], ALPHA, yacc.t[:, ti, :], ALU.mult, ALU.add, [xl, yacc], [r])
                        layernorm(ps_, r, D, fg, fb, xl, xl.t[:, :], st, mv, tmp)
                        kb.dma("pool", dst_d[gt * 128:(gt + 1) * 128, :], xl.t[:, :], xl, reads=[xl], writes=[dst_B[gt]])
                kb.barrier()

        def attention():
            with contextlib.ExitStack() as ps_:
                Wq = load_w(ps_, "a_Wqkv", w_qkv_d, 3072)
                xin = kb.sb("a_xin", [128, 4, D], F32, ps_)
                xbf = [kb.sb("a_xbf%d" % i, [128, D], BF16, ps_) for i in range(2)]
                xT = kb.sb("a_xT", [128, 8, 512], BF16, ps_)
                qm = [kb.sb("a_qm%d" % i, [128, 8, 512], BF16, ps_) for i in range(2)]
                km = [kb.sb("a_km%d" % i, [128, 8, 512], BF16, ps_) for i in range(2)]
                vm = [kb.sb("a_vm%d" % i, [128, 4, D], BF16, ps_) for i in range(2)]
                pT = kb.ps("a_pT", [128, 1024], BF16, ps_)
                pp = [kb.ps("a_pp%d" % i, [128, 512], F32, ps_) for i in range(4)]
                ppi = 0
                for mt in range(8):
                    load_xT(xb_d, xb_B, mt, xin, xbf, pT, xT)
                    q_ = qm[mt % 2]
                    k_ = km[mt % 2]
                    v_ = vm[mt % 2]
                    for hp in range(8):
                        p = pp[ppi % 4]; ppi += 1
                        kb.mmg(p, p.t[:, :], [(Wq.t[:, k, hp * 128:(hp + 1) * 128], xT.t[:, k, :]) for k in range(8)], reads=[Wq, xT])
                        kb.op("act", lambda e, p=p, hp=hp, q_=q_: e.mul(out=q_.t[:, hp, :], in_=p.t[:, :], mul=0.125), reads=[p], writes=[q_])
                        p = pp[ppi % 4]; ppi += 1
                        kb.mmg(p, p.t[:, :], [(Wq.t[:, k, 1024 + hp * 128:1024 + (hp + 1) * 128], xT.t[:, k, :]) for k in range(8)], reads=[Wq, xT])
                        kb.copy("dve", k_.t[:, hp, :], p.t[:, :], [p], [k_])
                    for s in range(4):
                        for dh in range(2):
                            p = pp[ppi % 4]; ppi += 1
                            kb.mmg(p, p.t[:, :], [(xT.t[:, k, s * 128:(s + 1) * 128], Wq.t[:, k, 2048 + dh * 512:2048 + (dh + 1) * 512]) for k in range(8)],
                                   reads=[Wq, xT])
                            kb.copy("dve" if dh == 0 else "act", v_.t[:, s, dh * 512:(dh + 1) * 512], p.t[:, :], [p], [v_])
                    cs = slice(mt * 512, (mt + 1) * 512)
                    kb.dma("pool", qT_d.rearrange("h p t -> p h t")[:, :, cs], q_.t[:, :, :], q_, reads=[q_], writes=[q_B[mt]])
                    kb.dma("pool", kT_d.rearrange("h p t -> p h t")[:, :, cs], k_.t[:, :, :], k_, reads=[k_], writes=[k_B[mt]])
                    kb.dma("pool", v_d[mt * 512:(mt + 1) * 512, :].rearrange("(s p) d -> p s d", p=128), v_.t[:, :, :], v_, reads=[v_], writes=[v_B[mt]])
                kb.barrier()

            with contextlib.ExitStack() as ps_:
                oT = kb.sb("a_oT", [128, 8, T], BF16, ps_)
                masks = kb.sb("a_mask", [128, 4, 512], BF16, ps_)
                negU = kb.sb("a_negU", [128, 128], BF16, ps_)
                negO = kb.sb("a_negO", [128, 128], BF16, ps_)
                zer = kb.sb("a_zero", [128, 128], BF16, ps_)
                kb.op("pool", lambda e: e.memset(masks.t[:, :, :], 1.0), writes=[masks])
                for m in range(4):
                    kb.op("pool", lambda e, m=m: e.affine_select(out=masks.t[:, m, :], in_=masks.t[:, m, :], pattern=[[1, 512]],
                                                                compare_op=ALU.is_gt, fill=0.0, base=-128 * m, channel_multiplier=-1),
                          reads=[masks], writes=[masks])
                kb.op("pool", lambda e: e.memset(negU.t[:, :], -1.0), writes=[negU])
                kb.op("pool", lambda e: e.affine_select(out=negU.t[:, :], in_=negU.t[:, :], pattern=[[-1, 128]],
                                                        compare_op=ALU.is_ge, fill=0.0, base=0, channel_multiplier=1),
                      reads=[negU], writes=[negU])
                kb.op("pool", lambda e: e.memset(negO.t[:, :], -1.0), writes=[negO])
                kb.op("pool", lambda e: e.memset(zer.t[:, :], 0.0), writes=[zer])
                with contextlib.ExitStack() as ps2:
                    qh = [kb.sb("a_qh%d" % i, [128, T], BF16, ps2) for i in range(2)]
                    kh = [kb.sb("a_kh%d" % i, [128, T], BF16, ps2) for i in range(2)]
                    vh = [kb.sb("a_vh%d" % i, [128, NT, 128], BF16, ps2) for i in range(2)]
                    ex = [kb.sb("a_ex%d" % i, [128, 512], F32, ps2) for i in range(2)]
                    lnu = [kb.sb("a_lnu%d" % i, [128, 512], BF16, ps2) for i in range(3)]
                    Sb = [kb.sb("a_S%d" % i, [128, 512], BF16, ps2) for i in range(2)]
                    att = [kb.sb("a_att%d" % i, [128, 512], BF16, ps2) for i in range(2)]
                    pz = [kb.ps("a_pz%d" % i, [128, 512], F32, ps2) for i in range(2)]
                    pM = [kb.ps("a_pM%d" % i, [128, 512], F32, ps2) for i in range(2)]
                    pc = [kb.ps("a_pc%d" % i, [128, 512], F32, ps2) for i in range(2)]

                    def load_hp(hp):
                        i2 = hp % 2
                        kb.dma("sp", qh[i2].t[:, :], qT_d[hp], qh[i2], reads=q_B, writes=[qh[i2]])
                        kb.dma("sp", kh[i2].t[:, :], kT_d[hp], kh[i2], reads=k_B, writes=[kh[i2]])
                        kb.dma("sp", vh[i2].t[:, :, :], v_d.rearrange("(b p) d -> p b d", p=128)[:, :, hp * 128:(hp + 1) * 128], vh[i2],
                               reads=v_B, writes=[vh[i2]])

                    load_hp(0)
                    it = 0
                    pci = 0
                    for hp in range(8):
                        if hp + 1 < 8:
                            load_hp(hp + 1)
                        q_ = qh[hp % 2]
                        k_ = kh[hp % 2]
                        v_ = vh[hp % 2]
                        for j in range(2):
                            pr = slice(j * 64, (j + 1) * 64)
                            for i in range(8):
                                pcb = pc[pci % 2]
                                pci += 1
                                kb.op("pe", lambda e, pcb=pcb: e.matmul(pcb.t[:, :], lhsT=zer.t[:, :], rhs=masks.t[:, 0, :], start=True, stop=False),
                                      reads=[zer, masks], writes=[pcb], mark=False)
                                first = True
                                Sprev = None
                                for b in range(4 * i + 3, -1, -1):
                                    m = b - 4 * i
                                    qlo = 128 * m if m > 0 else 0
                                    n = 512 - qlo
                                    qs = slice(i * 512 + qlo, (i + 1) * 512)
                                    ks = slice(b * 128, (b + 1) * 128)
                                    z = pz[it % 2]
                                    M = pM[it % 2]
                                    e_ = ex[it % 2]
                                    l_ = lnu[it % 3]
                                    a_ = att[it % 2]
                                    it += 1
                                    kb.mmg(z, z.t[:, 0:n], [(k_.t[pr, ks], q_.t[pr, qs])], reads=[k_, q_])
                                    kb.act(e_.t[:, 0:n], z.t[:, 0:n], AF.Exp, [z], [e_])
                                    kb.act(l_.t[:, 0:n], e_.t[:, 0:n], AF.Ln, [e_], [l_], bias=1.0, scale=1.0)
                                    if m >= 0:
                                        kb.tt("pool", l_.t[:, 0:n], l_.t[:, 0:n], masks.t[:, m, qlo:512], ALU.mult, [l_, masks], [l_])
                                    prs = [(k_.t[pr, ks], q_.t[pr, qs]), (negU.t[:, :], l_.t[:, 0:n])]
                                    rds = [k_, q_, negU, l_]
                                    if not first:
                                        prs.append((negO.t[:, :], Sprev.t[:, qlo:512]))
                                        rds.append(Sprev)
                                    kb.mmg(M, M.t[:, 0:n], prs, reads=rds)
                                    kb.act(a_.t[:, 0:n], M.t[:, 0:n], AF.Exp, [M], [a_])
                                    if m >= 0:
                                        kb.tt("pool", a_.t[:, 0:n], a_.t[:, 0:n], masks.t[:, m, qlo:512], ALU.mult, [a_, masks], [a_])
                                    if b > 0:
                                        Sn = Sb[0] if Sprev is not Sb[0] else Sb[1]
                                        if first:
                                            if qlo > 0:
                                                kb.op("dve", lambda e, Sn=Sn, qlo=qlo: e.memset(Sn.t[:, 0:qlo], 0.0), writes=[Sn])
                                            kb.copy("dve", Sn.t[:, qlo:512], l_.t[:, 0:n], [l_], [Sn])
                                        else:
                                            if qlo > 0:
                                                kb.copy("dve", Sn.t[:, 0:qlo], Sprev.t[:, 0:qlo], [Sprev], [Sn])
                                            kb.tt("dve", Sn.t[:, qlo:512], Sprev.t[:, qlo:512], l_.t[:, 0:n], ALU.add, [Sprev, l_], [Sn])
                                        Sprev = Sn
                                    kb.op("pe", lambda e, pcb=pcb, v_=v_, b=b, a_=a_, n=n, qlo=qlo: e.matmul(
                                        pcb.t[:, qlo:512], lhsT=v_.t[:, b, :], rhs=a_.t[:, 0:n], start=False, stop=(b == 0)),
                                        reads=[v_, a_], writes=[pcb], mark=True)
                                    first = False
                                kb.copy("dve", oT.t[pr, hp, i * 512:(i + 1) * 512], pcb.t[pr, :], [pcb], [oT])
                    kb.barrier()
                with contextlib.ExitStack() as ps3:
                    P = post_mixer_allocs(ps3, 1)
                    Wo = load_w(ps3, "a_Wo", w_out1_d, 1024)
                    xres = [kb.sb("a_xres%d" % i, [128, D], F32, ps3) for i in range(2)]
                    pso = [kb.ps("a_pso%d" % i, [128, 512], F32, ps3) for i in range(2)]
                    for gt in range(NT):
                        xr = xres[gt % 2]
                        kb.dma("sp", xr.t[:, :], xb_d[gt * 128:(gt + 1) * 128, :], xr, reads=[xb_B[gt]], writes=[xr])
                        post_mixer(ps3, 1, gt, xr.t[:, :], xr, pso, oT, slice(gt * 128, (gt + 1) * 128), Wo, P)
                    kb.barrier()

        mixer0()
        if stop_after != "m0":
            moe(0, xb_d, xb_B)
            if stop_after != "moe0":
                attention()
                if stop_after != "attn":
                    moe(1, out_d, out_B)
        kb.barrier()

    es.close()
    return nc


_NC_CACHE = {}


def kernel(x, even_w_in, even_conv_w, even_sgu_ln_g, even_sgu_ln_b, even_sgu_w_s, even_sgu_b_s, even_w_out,
           odd_w_qkv, odd_w_out, mix_ln_g, mix_ln_b, moe_w_group, moe_b_group, moe_w_router, moe_b_router,
           moe_w1, moe_w3, moe_w2, ffn_ln_g, ffn_ln_b):
    f = lambda a: np.ascontiguousarray(np.asarray(a, dtype=np.float32))
    convw = f(np.asarray(even_conv_w)[0].reshape(3, 4, 128).transpose(2, 1, 0).reshape(128, 12))
    wsT = f(np.asarray(even_sgu_w_s)[0].transpose(0, 2, 1))
    bs = f(np.asarray(even_sgu_b_s)[0].reshape(1, 1024))
    wr = f(np.concatenate([np.asarray(moe_w_group),
                           np.asarray(moe_w_router).transpose(0, 2, 1, 3).reshape(2, D, 16)], axis=2))
    br = f(np.concatenate([np.asarray(moe_b_group), np.asarray(moe_b_router).reshape(2, 16)], axis=1))
    shared = {
        "w_in": f(even_w_in[0]), "convw": convw, "sgu_g": f(even_sgu_ln_g), "sgu_b": f(even_sgu_ln_b),
        "wsT": wsT, "bs": bs, "w_out0": f(even_w_out[0]), "w_qkv": f(odd_w_qkv[0]), "w_out1": f(odd_w_out[0]),
        "mix_g": f(mix_ln_g), "mix_b": f(mix_ln_b), "wr": wr, "br": br,
        "w1": f(np.asarray(moe_w1).reshape(2, 16, D, 512)), "w3": f(np.asarray(moe_w3).reshape(2, 16, D, 512)),
        "w2": f(np.asarray(moe_w2).reshape(2, 16, 512, D)), "ffn_g": f(ffn_ln_g), "ffn_b": f(ffn_ln_b),
    }
    xs = f(x)
    if "nc" not in _NC_CACHE:
        _NC_CACHE["nc"] = build()
    nc = _NC_CACHE["nc"]
    in_maps = [dict(shared, x=xs[c]) for c in range(NCORES)]
    res = run_bass_kernel_spmd(nc, in_maps, core_ids=list(range(NCORES)))
    return np.stack([res.results[c]["out"] for c in range(NCORES)], axis=0)
```

```python
import contextlib
import numpy as np
import concourse.bass as bass
import concourse.mybir as mybir
from concourse.bass_utils import run_bass_kernel_spmd

F32 = mybir.dt.float32
BF16 = mybir.dt.bfloat16
AF = mybir.ActivationFunctionType
ALU = mybir.AluOpType

T = 4096
D = 1024
NT = 32
ALPHA = float(4 ** 0.25)
EPS = 1e-5
NCORES = 8


class B:
    __slots__ = ("t", "w", "r", "dsem", "dcnt")

    def __init__(self, t=None):
        self.t = t
        self.w = None
        self.r = {}
        self.dsem = None
        self.dcnt = 0


class Eng:
    pass


class KB:
    def __init__(self):
        self.nc = bass.Bass("TRN2", target_bir_lowering=False)
        self.es = contextlib.ExitStack()
        self.nsem = 0
        self.dma_bufs = []

    def sem(self, name):
        self.nsem += 1
        return self.es.enter_context(self.nc.semaphore(name))

    def sb(self, name, shape, dt, stack=None):
        st = stack if stack is not None else self.es
        self.nt = getattr(self, "nt", 0) + 1
        return B(st.enter_context(self.nc.sbuf_tensor("%s_%d" % (name, self.nt), shape, dt)))

    def ps(self, name, shape, dt, stack=None):
        st = stack if stack is not None else self.es
        self.nt = getattr(self, "nt", 0) + 1
        return B(st.enter_context(self.nc.psum_tensor("%s_%d" % (name, self.nt), shape, dt)))

    def start(self):
        nc = self.nc
        self.E = {}
        for name, eng in (("pe", nc.tensor), ("act", nc.scalar), ("dve", nc.vector),
                          ("pool", nc.gpsimd), ("sp", nc.sync)):
            e = Eng()
            e.name = name
            e.eng = eng
            e.sem = self.sem("s_" + name)
            e.count = 0
            e.seen = {}
            self.E[name] = e

    def _waits(self, E, reads, writes):
        need = {}

        def acc(tok):
            k = id(tok[0])
            if k not in need or need[k][1] < tok[1]:
                need[k] = tok

        for b in reads:
            if b.w is not None:
                acc(b.w)
        for b in writes:
            if b.w is not None:
                acc(b.w)
            for tok in b.r.values():
                acc(tok)
        for k, (sem, val) in need.items():
            if E.name == "pe" and sem is E.sem:
                continue
            if E.seen.get(k, 0) >= val:
                continue
            E.eng.wait_ge(sem, val)
            E.seen[k] = val

    def _commit(self, tok, reads, writes):
        k = id(tok[0])
        for b in reads:
            b.r[k] = tok
        for b in writes:
            b.w = tok
            b.r = {}

    def op(self, en, fn, reads=(), writes=(), mark=True):
        E = self.E[en]
        self._waits(E, reads, writes)
        inst = fn(E.eng)
        if mark:
            E.count += 1
            inst.then_inc(E.sem, 1)
            tok = (E.sem, E.count)
        else:
            tok = (E.sem, E.count + 1)
        self._commit(tok, reads, writes)
        return inst

    def dma(self, q, out_ap, in_ap, sbufB, reads=(), writes=()):
        E = self.E[q]
        self._waits(E, reads, writes)
        if sbufB.dsem is None:
            sbufB.dsem = self.sem("d%d" % self.nsem)
            self.dma_bufs.append(sbufB)
        sbufB.dcnt += 16
        E.eng.dma_start(out=out_ap, in_=in_ap).then_inc(sbufB.dsem, 16)
        tok = (sbufB.dsem, sbufB.dcnt)
        self._commit(tok, reads, writes)

    def barrier(self):
        toks = []
        for e in self.E.values():
            if e.count > 0:
                toks.append((e.sem, e.count))
        for b in self.dma_bufs:
            if b.dcnt > 0:
                toks.append((b.dsem, b.dcnt))
        for E in self.E.values():
            for (sem, val) in toks:
                if sem is E.sem:
                    continue
                k = id(sem)
                if E.seen.get(k, 0) >= val:
                    continue
                E.eng.wait_ge(sem, val)
                E.seen[k] = val

    def mmg(self, outB, out_ap, pairs, reads):
        n = len(pairs)
        for i, (l, r) in enumerate(pairs):
            self.op("pe", lambda e, l=l, r=r, i=i: e.matmul(out_ap, lhsT=l, rhs=r, start=(i == 0), stop=(i == n - 1)),
                    reads=reads, writes=[outB], mark=(i == n - 1))

    def act(self, out_ap, in_ap, func, reads, writes, bias=None, scale=None):
        kw = {}
        if bias is not None:
            kw["bias"] = bias
        if scale is not None:
            kw["scale"] = scale
        self.op("act", lambda e: e.activation(out=out_ap, in_=in_ap, func=func, **kw), reads=reads, writes=writes)

    def copy(self, en, out_ap, in_ap, reads, writes):
        if en == "act":
            self.op("act", lambda e: e.copy(out=out_ap, in_=in_ap), reads=reads, writes=writes)
        else:
            self.op(en, lambda e: e.tensor_copy(out=out_ap, in_=in_ap), reads=reads, writes=writes)

    def tt(self, en, out_ap, a_ap, b_ap, op, reads, writes):
        self.op(en, lambda e: e.tensor_tensor(out=out_ap, in0=a_ap, in1=b_ap, op=op), reads=reads, writes=writes)

    def ts(self, en, out_ap, in_ap, s1, s2, op0, op1, reads, writes):
        if op1 is None:
            self.op(en, lambda e: e.tensor_scalar(out=out_ap, in0=in_ap, scalar1=s1, scalar2=None, op0=op0),
                    reads=reads, writes=writes)
        else:
            self.op(en, lambda e: e.tensor_scalar(out=out_ap, in0=in_ap, scalar1=s1, scalar2=s2, op0=op0, op1=op1),
                    reads=reads, writes=writes)

    def stt(self, en, out_ap, in0, scalar, in1, op0, op1, reads, writes):
        self.op(en, lambda e: e.scalar_tensor_tensor(out=out_ap, in0=in0, scalar=scalar, in1=in1, op0=op0, op1=op1),
                reads=reads, writes=writes)


def build(stop_after=None, dbg=False):
    kb = KB()
    dk = {"kind": "ExternalOutput"} if dbg else {}
    nc = kb.nc

    def din(name, shape):
        return nc.dram_tensor(name, shape, F32, kind="ExternalInput").ap()

    x_d = din("x", [T, D])
    w_in_d = din("w_in", [D, 2560])
    convw_d = din("convw", [128, 12])
    sgu_g_d = din("sgu_g", [1, 512])
    sgu_b_d = din("sgu_b", [1, 512])
    wsT_d = din("wsT", [8, 128, 128])
    bs_d = din("bs", [1, 1024])
    w_out0_d = din("w_out0", [D, D])
    w_qkv_d = din("w_qkv", [D, 3072])
    w_out1_d = din("w_out1", [D, D])
    mix_g_d = din("mix_g", [2, D])
    mix_b_d = din("mix_b", [2, D])
    wr_d = din("wr", [2, D, 20])
    br_d = din("br", [2, 20])
    w1_d = din("w1", [2, 16, D, 512])
    w3_d = din("w3", [2, 16, D, 512])
    w2_d = din("w2", [2, 16, 512, D])
    ffn_g_d = din("ffn_g", [2, D])
    ffn_b_d = din("ffn_b", [2, D])
    out_d = nc.dram_tensor("out", [T, D], F32, kind="ExternalOutput").ap()

    xa_d = nc.dram_tensor("xa_s", [T, D], F32, **dk).ap()
    xb_d = nc.dram_tensor("xb_s", [T, D], F32, **dk).ap()
    xT_d = nc.dram_tensor("xT_s", [128, 8, T], BF16, **dk).ap()
    qT_d = nc.dram_tensor("qT_s", [8, 128, T], BF16, **dk).ap()
    kT_d = nc.dram_tensor("kT_s", [8, 128, T], BF16, **dk).ap()
    v_d = nc.dram_tensor("v_s", [T, D], BF16, **dk).ap()

    xa_B = [B() for _ in range(NT)]
    xb_B = [B() for _ in range(NT)]
    xT_B = [B() for _ in range(8)]
    q_B = [B() for _ in range(8)]
    k_B = [B() for _ in range(8)]
    v_B = [B() for _ in range(8)]
    out_B = [B() for _ in range(NT)]

    kb.start()
    es = kb.es

    ident = kb.sb("ident", [128, 128], BF16)
    c_all = kb.sb("c_all", [128, NT, 16], F32)

    block = es.enter_context(nc.Block())

    @block.sync
    def _(sync):
        kb.op("pool", lambda e: e.memset(ident.t[:], 0.0), writes=[ident])
        kb.op("pool", lambda e: e.affine_select(out=ident.t[:], in_=ident.t[:], pattern=[[-1, 128]],
                                                compare_op=ALU.not_equal, fill=1.0, base=0, channel_multiplier=1),
              reads=[ident], writes=[ident])

        def layernorm(ps_, r, D_, g_bc, b_bc, outB, out_ap, stats, mv, tmp, mul_eng="pool"):
            nch = D_ // 512
            for c in range(nch):
                kb.op("dve", lambda e, c=c: e.bn_stats(out=stats.t[:, c * 6:(c + 1) * 6], in_=r.t[:, c * 512:(c + 1) * 512]),
                      reads=[r], writes=[stats])
            kb.op("dve", lambda e: e.bn_aggr(out=mv.t[:, 0:2], in_=stats.t[:, 0:nch * 6]), reads=[stats], writes=[mv])
            kb.act(tmp.t[:, 0:1], mv.t[:, 1:2], AF.Ln, [mv], [tmp], bias=EPS, scale=1.0)
            kb.act(tmp.t[:, 1:2], tmp.t[:, 0:1], AF.Exp, [tmp], [tmp], scale=-0.5)
            kb.ts("dve", r.t[:, 0:D_], r.t[:, 0:D_], mv.t[:, 0:1], tmp.t[:, 1:2], ALU.subtract, ALU.mult, [r, mv, tmp], [r])
            kb.tt(mul_eng, r.t[:, 0:D_], r.t[:, 0:D_], g_bc.t[:, 0:D_], ALU.mult, [r, g_bc], [r])
            kb.tt(mul_eng, out_ap, r.t[:, 0:D_], b_bc.t[:, 0:D_], ALU.add, [r, b_bc], [outB])

        def bcast_load(dst, src_row_ap):
            kb.dma("sp", dst.t[:], src_row_ap.partition_broadcast(128), dst, writes=[dst])

        def post_mixer(ps_, lyr, gt, x_res_ap, x_resB, pso, yT, yT_cols, Wo, P):
            for dh in range(2):
                kb.mmg(pso[dh], pso[dh].t[:, :],
                       [(yT.t[:, k, yT_cols], Wo.t[:, k, dh * 512:(dh + 1) * 512]) for k in range(8)],
                       reads=[yT, Wo])
            r = P["r"][gt % 2]
            for dh in range(2):
                kb.stt("dve", r.t[:, dh * 512:(dh + 1) * 512], x_res_ap[:, dh * 512:(dh + 1) * 512], ALPHA,
                       pso[dh].t[:, :], ALU.mult, ALU.add, [x_resB, pso[dh]], [r])
            x1 = P["x1"][gt % 2]
            layernorm(ps_, r, D, P["mg"], P["mb"], x1, x1.t[:, :], P["stats"], P["mv"], P["tmp"])
            kb.dma("pool", xa_d[gt * 128:(gt + 1) * 128, :], x1.t[:, :], x1, reads=[x1], writes=[xa_B[gt]])
            x1b = P["x1b"]
            kb.copy("pool", x1b.t[:, :], x1.t[:, :], [x1], [x1b])
            pT = P["pT"]
            for k in range(8):
                kb.op("pe", lambda e, k=k: e.transpose(pT.t[:, k * 128:(k + 1) * 128], x1b.t[:, k * 128:(k + 1) * 128], ident.t[:]),
                      reads=[x1b, ident], writes=[pT], mark=(k == 7))
            x1T = P["x1T"][(gt // 4) % 2]
            s = gt % 4
            kb.copy("act", x1T.t[:, :, s * 128:(s + 1) * 128], pT.t[:, :].rearrange("p (k t) -> p k t", k=8), [pT], [x1T])
            prt = P["prt"]
            kb.mmg(prt, prt.t[:, 0:20], [(x1T.t[:, k, s * 128:(s + 1) * 128], P["Wr"].t[:, k, :]) for k in range(8)],
                   reads=[x1T, P["Wr"]])
            rt = P["rt"]
            lg = rt.t[:, 0:20]
            kb.tt("dve", lg, prt.t[:, 0:20], P["brb"].t[:, :], ALU.add, [prt, P["brb"]], [rt])
            R_ = [rt]
            kb.op("dve", lambda e: e.reduce_max(out=rt.t[:, 20:21], in_=rt.t[:, 0:4], axis=mybir.AxisListType.X), reads=R_, writes=R_)
            kb.ts("dve", rt.t[:, 24:28], rt.t[:, 0:4], rt.t[:, 20:21], None, ALU.is_equal, None, R_, R_)
            kb.ts("dve", rt.t[:, 21:22], rt.t[:, 20:21], -1.0, None, ALU.mult, None, R_, R_)
            kb.act(rt.t[:, 28:32], rt.t[:, 0:4], AF.Exp, R_, R_, bias=rt.t[:, 21:22], scale=1.0)
            kb.op("dve", lambda e: e.reduce_sum(out=rt.t[:, 22:23], in_=rt.t[:, 28:32], axis=mybir.AxisListType.X), reads=R_, writes=R_)
            kb.ts("dve", rt.t[:, 32:36], rt.t[:, 4:8], rt.t[:, 24:25], None, ALU.mult, None, R_, R_)
            for g in range(1, 4):
                kb.stt("dve", rt.t[:, 32:36], rt.t[:, 4 + 4 * g:8 + 4 * g], rt.t[:, 24 + g:25 + g], rt.t[:, 32:36],
                       ALU.mult, ALU.add, R_, R_)
            kb.op("dve", lambda e: e.reduce_max(out=rt.t[:, 36:37], in_=rt.t[:, 32:36], axis=mybir.AxisListType.X), reads=R_, writes=R_)
            kb.ts("dve", rt.t[:, 40:44], rt.t[:, 32:36], rt.t[:, 36:37], None, ALU.is_equal, None, R_, R_)
            kb.ts("dve", rt.t[:, 37:38], rt.t[:, 36:37], -1.0, None, ALU.mult, None, R_, R_)
            kb.act(rt.t[:, 44:48], rt.t[:, 32:36], AF.Exp, R_, R_, bias=rt.t[:, 37:38], scale=1.0)
            kb.op("dve", lambda e: e.reduce_max(out=rt.t[:, 38:39], in_=rt.t[:, 44:48], axis=mybir.AxisListType.X), reads=R_, writes=R_)
            kb.tt("dve", rt.t[:, 48:52], rt.t[:, 44:48], rt.t[:, 40:44], ALU.mult, R_, R_)
            kb.tt("dve", rt.t[:, 48:52], rt.t[:, 44:48], rt.t[:, 48:52], ALU.subtract, R_, R_)
            kb.op("dve", lambda e: e.reduce_max(out=rt.t[:, 39:40], in_=rt.t[:, 48:52], axis=mybir.AxisListType.X), reads=R_, writes=R_)
            kb.ts("dve", rt.t[:, 52:56], rt.t[:, 44:48], rt.t[:, 39:40], None, ALU.is_ge, None, R_, R_)
            kb.tt("dve", rt.t[:, 52:56], rt.t[:, 52:56], rt.t[:, 44:48], ALU.mult, R_, R_)
            kb.tt("dve", rt.t[:, 56:57], rt.t[:, 38:39], rt.t[:, 39:40], ALU.add, R_, R_)
            kb.tt("dve", rt.t[:, 56:57], rt.t[:, 56:57], rt.t[:, 22:23], ALU.mult, R_, R_)
            kb.op("dve", lambda e: e.reciprocal(out=rt.t[:, 57:58], in_=rt.t[:, 56:57]), reads=R_, writes=R_)
            kb.ts("dve", rt.t[:, 60:64], rt.t[:, 24:28], rt.t[:, 57:58], None, ALU.mult, None, R_, R_)
            for g in range(4):
                kb.ts("dve", c_all.t[:, gt, 4 * g:4 * g + 4], rt.t[:, 52:56], rt.t[:, 60 + g:61 + g], None, ALU.mult, None,
                      R_, [c_all])
            if s == 3:
                mt = gt // 4
                kb.dma("pool", xT_d[:, :, mt * 512:(mt + 1) * 512], x1T.t[:, :, :], x1T, reads=[x1T], writes=[xT_B[mt]])

        def post_mixer_allocs(ps_, lyr):
            P = {}
            P["r"] = [kb.sb("pm_r%d" % i, [128, D], F32, ps_) for i in range(2)]
            P["x1"] = [kb.sb("pm_x1%d" % i, [128, D], F32, ps_) for i in range(2)]
            P["x1b"] = kb.sb("pm_x1b", [128, D], BF16, ps_)
            P["x1T"] = [kb.sb("pm_x1T", [128, 8, 512], BF16, ps_)] * 2
            P["stats"] = kb.sb("pm_stats", [128, 12], F32, ps_)
            P["mv"] = kb.sb("pm_mv", [128, 2], F32, ps_)
            P["tmp"] = kb.sb("pm_tmp", [128, 2], F32, ps_)
            P["rt"] = kb.sb("pm_rt", [128, 64], F32, ps_)
            P["mg"] = kb.sb("pm_mg", [128, D], F32, ps_)
            P["mb"] = kb.sb("pm_mb", [128, D], F32, ps_)
            P["brb"] = kb.sb("pm_brb", [128, 20], F32, ps_)
            P["Wr"] = kb.sb("pm_Wr", [128, 8, 20], BF16, ps_)
            P["pT"] = kb.ps("pm_pT", [128, 1024], BF16, ps_)
            P["prt"] = kb.ps("pm_prt", [128, 512], F32, ps_)
            bcast_load(P["mg"], mix_g_d[lyr:lyr + 1, :])
            bcast_load(P["mb"], mix_b_d[lyr:lyr + 1, :])
            bcast_load(P["brb"], br_d[lyr:lyr + 1, :])
            kb.dma("pool", P["Wr"].t[:, :, :], wr_d[lyr].rearrange("(k p) n -> p k n", p=128), P["Wr"], writes=[P["Wr"]])
            return P

        def load_w(ps_, name, src_ap, ncols):
            W = kb.sb(name, [128, 8, ncols], BF16, ps_)
            v = src_ap.rearrange("(k p) n -> p k n", p=128)
            for k in range(8):
                kb.dma("pool", W.t[:, k, :], v[:, k, :], W, writes=[W])
            return W

        def load_xT(xsrc_d, xsrc_B, mt, xin, xbf, pT, xT):
            rd = [xsrc_B[mt * 4 + s] for s in range(4)] if xsrc_B is not None else []
            kb.dma("sp", xin.t[:, :, :], xsrc_d[mt * 512:(mt + 1) * 512, :].rearrange("(s p) d -> p s d", p=128), xin,
                   reads=rd, writes=[xin])
            for s in range(4):
                xb = xbf[s % 2]
                kb.copy("pool", xb.t[:, :], xin.t[:, s, :], [xin], [xb])
                for k in range(8):
                    kb.op("pe", lambda e, k=k, xb=xb: e.transpose(pT.t[:, k * 128:(k + 1) * 128], xb.t[:, k * 128:(k + 1) * 128], ident.t[:]),
                          reads=[xb, ident], writes=[pT], mark=(k == 7))
                kb.copy("act", xT.t[:, :, s * 128:(s + 1) * 128], pT.t[:, :].rearrange("p (k t) -> p k t", k=8), [pT], [xT])

        def mixer0():
            with contextlib.ExitStack() as ps_:
                P = post_mixer_allocs(ps_, 0)
                Wi = load_w(ps_, "m0_Wi", w_in_d, 2560)
                Wo = load_w(ps_, "m0_Wo", w_out0_d, 1024)
                WsT = kb.sb("m0_WsT", [128, 8, 128], BF16, ps_)
                kb.dma("pool", WsT.t[:, :, :], wsT_d.rearrange("h s t -> s h t"), WsT, writes=[WsT])
                kb.op("pool", lambda e: e.memset(WsT.t[64:128, :, 0:64], 0.0), reads=[WsT], writes=[WsT])
                bsr = kb.sb("m0_bsr", [1, 1024], BF16, ps_)
                kb.dma("pool", bsr.t[:, :], bs_d[:, :], bsr, writes=[bsr])
                onesr = kb.sb("m0_ones", [1, 128], BF16, ps_)
                kb.op("pool", lambda e: e.memset(onesr.t[:, :], 1.0), writes=[onesr])
                cw = kb.sb("m0_cw", [128, 12], F32, ps_)
                kb.dma("sp", cw.t[:, :], convw_d[:, :], cw, writes=[cw])
                sg = kb.sb("m0_sg", [128, 512], F32, ps_)
                sbb = kb.sb("m0_sbb", [128, 512], F32, ps_)
                bcast_load(sg, sgu_g_d[0:1, :])
                bcast_load(sbb, sgu_b_d[0:1, :])
                xin = kb.sb("m0_xin", [128, 4, D], F32, ps_)
                xbf = [kb.sb("m0_xbf%d" % i, [128, D], BF16, ps_) for i in range(2)]
                xres = [kb.sb("m0_xres%d" % i, [128, D], F32, ps_) for i in range(2)]
                xT = kb.sb("m0_xT", [128, 8, 512], BF16, ps_)
                yT = kb.sb("m0_yT", [128, 8, 512], BF16, ps_)
                ub = kb.sb("m0_ub", [128, 4, 514], F32, ps_)
                Cs = kb.sb("m0_Cs", [128, 512], F32, ps_)
                acc = kb.sb("m0_acc", [128, 512], F32, ps_)
                zu = kb.sb("m0_zu", [128, 4, 512], F32, ps_)
                g1 = kb.sb("m0_g1", [128, 512], F32, ps_)
                g2 = kb.sb("m0_g2", [128, 512], F32, ps_)
                gv = kb.sb("m0_gv", [128, 512], F32, ps_)
                vt = kb.sb("m0_vt", [128, 4, 512], BF16, ps_)
                st2 = kb.sb("m0_st2", [128, 6], F32, ps_)
                mv2 = kb.sb("m0_mv2", [128, 2], F32, ps_)
                tmp2 = kb.sb("m0_tmp2", [128, 2], F32, ps_)
                pp = [kb.ps("m0_pp%d" % i, [128, 512], F32, ps_) for i in range(2)]
                psg = [kb.ps("m0_psg%d" % i, [128, 512], F32, ps_) for i in range(2)]
                pso = [kb.ps("m0_pso%d" % i, [128, 512], F32, ps_) for i in range(2)]
                pT = P["pT"]
                kb.op("pool", lambda e: e.memset(ub.t[:, :, :], 0.0), writes=[ub])
                ppi = [0]

                def proj_fm(c, xT):
                    p = pp[ppi[0] % 2]
                    ppi[0] += 1
                    kb.mmg(p, p.t[:, :], [(Wi.t[:, k, c * 128:(c + 1) * 128], xT.t[:, k, :]) for k in range(8)], reads=[Wi, xT])
                    return p

                def gelu(p, p_ap, outB, out_ap, n):
                    kb.act(g1.t[:, 0:n], p_ap, AF.Square, [p], [g1])
                    kb.ts("pool", g1.t[:, 0:n], g1.t[:, 0:n], 0.044715, 1.0, ALU.mult, ALU.add, [g1], [g1])
                    kb.tt("dve", g2.t[:, 0:n], g1.t[:, 0:n], p_ap, ALU.mult, [g1, p], [g2])
                    kb.act(g1.t[:, 0:n], g2.t[:, 0:n], AF.Exp, [g2], [g1], scale=-1.5957691216057308)
                    kb.ts("pool", g1.t[:, 0:n], g1.t[:, 0:n], 1.0, None, ALU.add, None, [g1], [g1])
                    kb.op("dve", lambda e: e.reciprocal(out=g2.t[:, 0:n], in_=g1.t[:, 0:n]), reads=[g1], writes=[g2])
                    kb.tt("dve", out_ap, g2.t[:, 0:n], p_ap, ALU.mult, [g2, p], [outB])

                for mt in range(8):
                    load_xT(x_d, None, mt, xin, xbf, pT, xT)
                    u = ub
                    for j in range(4):
                        pC = proj_fm(4 + j, xT)
                        kb.copy("act", Cs.t[:, :], pC.t[:, :], [pC], [Cs])
                        pH = proj_fm(8 + j, xT)
                        kb.copy("dve", u.t[:, j, 0:2], u.t[:, j, 512:514], [u], [u])
                        kb.tt("dve", u.t[:, j, 2:514], Cs.t[:, :], pH.t[:, :], ALU.mult, [Cs, pH], [u])
                        kb.ts("dve", acc.t[:, :], u.t[:, j, 2:514], cw.t[:, j * 3 + 2:j * 3 + 3], None, ALU.mult, None, [u, cw], [acc])
                        kb.stt("dve", acc.t[:, :], u.t[:, j, 1:513], cw.t[:, j * 3 + 1:j * 3 + 2], acc.t[:, :], ALU.mult, ALU.add, [u, cw, acc], [acc])
                        kb.stt("dve", acc.t[:, :], u.t[:, j, 0:512], cw.t[:, j * 3:j * 3 + 1], acc.t[:, :], ALU.mult, ALU.add, [u, cw, acc], [acc])
                        pB = proj_fm(j, xT)
                        kb.tt("dve", yT.t[:, j, :], acc.t[:, :], pB.t[:, :], ALU.mult, [acc, pB], [yT])
                    for j in range(4):
                        pZ = proj_fm(12 + j, xT)
                        gelu(pZ, pZ.t[:, :], zu, zu.t[:, j, :], 512)
                    for s in range(4):
                        p = pp[ppi[0] % 2]
                        ppi[0] += 1
                        kb.mmg(p, p.t[:, :], [(xT.t[:, k, s * 128:(s + 1) * 128], Wi.t[:, k, 2048:2560]) for k in range(8)], reads=[Wi, xT])
                        gelu(p, p.t[:, :], gv, gv.t[:, :], 512)
                        layernorm(ps_, gv, 512, sg, sbb, vt, vt.t[:, s, :], st2, mv2, tmp2, mul_eng="pool")
                    for hp in range(4):
                        for j in range(2):
                            h = 2 * hp + j
                            pg = psg[j]
                            for s in range(4):
                                kb.op("pe", lambda e, s=s, pg=pg, h=h, hp=hp: e.matmul(pg.t[:, s * 128:(s + 1) * 128], lhsT=vt.t[:, s, hp * 128:(hp + 1) * 128],
                                                                         rhs=WsT.t[:, h, :], start=True, stop=False),
                                      reads=[vt, WsT], writes=[pg], mark=False)
                                kb.op("pe", lambda e, s=s, pg=pg, h=h: e.matmul(pg.t[:, s * 128:(s + 1) * 128], lhsT=onesr.t[0:1, :],
                                                                   rhs=bsr.t[0:1, h * 128:(h + 1) * 128], start=False, stop=True),
                                      reads=[onesr, bsr], writes=[pg], mark=(s == 3))
                            kb.tt("dve", yT.t[j * 64:(j + 1) * 64, 4 + hp, :], zu.t[j * 64:(j + 1) * 64, hp, :], pg.t[j * 64:(j + 1) * 64, :],
                                  ALU.mult, [zu, pg], [yT])
                    for s in range(4):
                        gt = mt * 4 + s
                        xr = xres[gt % 2]
                        kb.dma("sp", xr.t[:, :], x_d[gt * 128:(gt + 1) * 128, :], xr, writes=[xr])
                        post_mixer(ps_, 0, gt, xr.t[:, :], xr, pso, yT, slice(s * 128, (s + 1) * 128), Wo, P)
                kb.barrier()

        def moe(lyr, dst_d, dst_B):
            with contextlib.ExitStack() as ps_:
                fg = kb.sb("f_g", [128, D], F32, ps_)
                fb = kb.sb("f_b", [128, D], F32, ps_)
                bcast_load(fg, ffn_g_d[lyr:lyr + 1, :])
                bcast_load(fb, ffn_b_d[lyr:lyr + 1, :])
                xTh = kb.sb("f_xTh", [128, 8, 2048], BF16, ps_)
                yacc = kb.sb("f_yacc", [128, 16, D], F32, ps_)
                w1s = [kb.sb("f_w1_%d" % i, [128, 8, 512], BF16, ps_) for i in range(2)]
                w3s = [kb.sb("f_w3_%d" % i, [128, 8, 512], BF16, ps_) for i in range(2)]
                w2s = [kb.sb("f_w2_%d" % i, [128, 4, D], BF16, ps_) for i in range(2)]
                hid = [kb.sb("f_hid%d" % i, [128, 4, 512], BF16, ps_) for i in range(2)]
                sl = [kb.sb("f_sl%d" % i, [128, 512], BF16, ps_) for i in range(2)]
                x1l = [kb.sb("f_x1l%d" % i, [128, D], F32, ps_) for i in range(2)]
                rr = [kb.sb("f_r%d" % i, [128, D], F32, ps_) for i in range(2)]
                st = kb.sb("f_st", [128, 12], F32, ps_)
                mv = kb.sb("f_mv", [128, 2], F32, ps_)
                tmp = kb.sb("f_tmp", [128, 2], F32, ps_)
                ph1 = [kb.ps("f_ph1_%d" % i, [128, 512], F32, ps_) for i in range(2)]
                ph3 = [kb.ps("f_ph3_%d" % i, [128, 512], F32, ps_) for i in range(2)]
                py = [kb.ps("f_py%d" % i, [128, 512], F32, ps_) for i in range(2)]

                def load_expert(e):
                    sl_ = e % 2
                    kb.dma("pool", w1s[sl_].t[:, :, :], w1_d[lyr, e].rearrange("(k p) f -> p k f", p=128), w1s[sl_], writes=[w1s[sl_]])
                    kb.dma("pool", w3s[sl_].t[:, :, :], w3_d[lyr, e].rearrange("(k p) f -> p k f", p=128), w3s[sl_], writes=[w3s[sl_]])
                    kb.dma("pool", w2s[sl_].t[:, :, :], w2_d[lyr, e].rearrange("(c p) d -> p c d", p=128), w2s[sl_], writes=[w2s[sl_]])

                cnt = [0]
                for hf in range(2):
                    kb.dma("sp", xTh.t[:, :, :], xT_d[:, :, hf * 2048:(hf + 1) * 2048], xTh,
                           reads=[xT_B[hf * 4 + i] for i in range(4)], writes=[xTh])
                    load_expert(0)
                    for e in range(16):
                        if e + 1 < 16:
                            load_expert(e + 1)
                        w1 = w1s[e % 2]
                        w3 = w3s[e % 2]
                        w2 = w2s[e % 2]
                        for mt in range(4):
                            hd = hid[(e * 4 + mt) % 2]
                            for fc in range(4):
                                i2 = cnt[0] % 2
                                cnt[0] += 1
                                xs = xTh.t[:, :, mt * 512:(mt + 1) * 512]
                                kb.mmg(ph1[i2], ph1[i2].t[:, :], [(w1.t[:, k, fc * 128:(fc + 1) * 128], xTh.t[:, k, mt * 512:(mt + 1) * 512]) for k in range(8)],
                                       reads=[w1, xTh])
                                kb.mmg(ph3[i2], ph3[i2].t[:, :], [(w3.t[:, k, fc * 128:(fc + 1) * 128], xTh.t[:, k, mt * 512:(mt + 1) * 512]) for k in range(8)],
                                       reads=[w3, xTh])
                                kb.act(sl[i2].t[:, :], ph1[i2].t[:, :], AF.Silu, [ph1[i2]], [sl[i2]])
                                kb.tt("dve", hd.t[:, fc, :], sl[i2].t[:, :], ph3[i2].t[:, :], ALU.mult, [sl[i2], ph3[i2]], [hd])
                            for s in range(4):
                                ti = mt * 4 + s
                                gt = hf * 16 + ti
                                for dh in range(2):
                                    p = py[dh]
                                    kb.mmg(p, p.t[:, :], [(hd.t[:, fc, s * 128:(s + 1) * 128], w2.t[:, fc, dh * 512:(dh + 1) * 512]) for fc in range(4)],
                                           reads=[hd, w2])
                                    ya = yacc.t[:, ti, dh * 512:(dh + 1) * 512]
                                    if e == 0:
                                        kb.ts("dve", ya, p.t[:, :], c_all.t[:, gt, e:e + 1], None, ALU.mult, None, [p, c_all], [yacc])
                                    else:
                                        kb.stt("dve", ya, p.t[:, :], c_all.t[:, gt, e:e + 1], ya, ALU.mult, ALU.add, [p, c_all, yacc], [yacc])
                    for ti in range(16):
                        gt = hf * 16 + ti
                        xl = x1l[ti % 2]
                        r = rr[ti % 2]
                        kb.dma("sp", xl.t[:, :], xa_d[gt * 128:(gt + 1) * 128, :], xl, reads=[xa_B[gt]], writes=[xl])
                        kb.stt("dve", r.t[:, :], xl.t[:, :], ALPHA, yacc.t[:, ti, :], ALU.mult, ALU.add, [xl, yacc], [r])
                        layernorm(ps_, r, D, fg, fb, xl, xl.t[:, :], st, mv, tmp)
                        kb.dma("pool", dst_d[gt * 128:(gt + 1) * 128, :], xl.t[:, :], xl, reads=[xl], writes=[dst_B[gt]])
                kb.barrier()

        def attention():
            with contextlib.ExitStack() as ps_:
                Wq = load_w(ps_, "a_Wqkv", w_qkv_d, 3072)
                xin = kb.sb("a_xin", [128, 4, D], F32, ps_)
                xbf = [kb.sb("a_xbf%d" % i, [128, D], BF16, ps_) for i in range(2)]
                xT = kb.sb("a_xT", [128, 8, 512], BF16, ps_)
                qm = [kb.sb("a_qm%d" % i, [128, 8, 512], BF16, ps_) for i in range(2)]
                km = [kb.sb("a_km%d" % i, [128, 8, 512], BF16, ps_) for i in range(2)]
                vm = [kb.sb("a_vm%d" % i, [128, 4, D], BF16, ps_) for i in range(2)]
                pT = kb.ps("a_pT", [128, 1024], BF16, ps_)
                pp = [kb.ps("a_pp%d" % i, [128, 512], F32, ps_) for i in range(4)]
                ppi = 0
                for mt in range(8):
                    load_xT(xb_d, xb_B, mt, xin, xbf, pT, xT)
                    q_ = qm[mt % 2]
                    k_ = km[mt % 2]
                    v_ = vm[mt % 2]
                    for hp in range(8):
                        p = pp[ppi % 4]; ppi += 1
                        kb.mmg(p, p.t[:, :], [(Wq.t[:, k, hp * 128:(hp + 1) * 128], xT.t[:, k, :]) for k in range(8)], reads=[Wq, xT])
                        kb.op("act", lambda e, p=p, hp=hp, q_=q_: e.mul(out=q_.t[:, hp, :], in_=p.t[:, :], mul=0.125), reads=[p], writes=[q_])
                        p = pp[ppi % 4]; ppi += 1
                        kb.mmg(p, p.t[:, :], [(Wq.t[:, k, 1024 + hp * 128:1024 + (hp + 1) * 128], xT.t[:, k, :]) for k in range(8)], reads=[Wq, xT])
                        kb.copy("dve", k_.t[:, hp, :], p.t[:, :], [p], [k_])
                    for s in range(4):
                        for dh in range(2):
                            p = pp[ppi % 4]; ppi += 1
                            kb.mmg(p, p.t[:, :], [(xT.t[:, k, s * 128:(s + 1) * 128], Wq.t[:, k, 2048 + dh * 512:2048 + (dh + 1) * 512]) for k in range(8)],
                                   reads=[Wq, xT])
                            kb.copy("dve" if dh == 0 else "act", v_.t[:, s, dh * 512:(dh + 1) * 512], p.t[:, :], [p], [v_])
                    cs = slice(mt * 512, (mt + 1) * 512)
                    kb.dma("pool", qT_d.rearrange("h p t -> p h t")[:, :, cs], q_.t[:, :, :], q_, reads=[q_], writes=[q_B[mt]])
                    kb.dma("pool", kT_d.rearrange("h p t -> p h t")[:, :, cs], k_.t[:, :, :], k_, reads=[k_], writes=[k_B[mt]])
                    kb.dma("pool", v_d[mt * 512:(mt + 1) * 512, :].rearrange("(s p) d -> p s d", p=128), v_.t[:, :, :], v_, reads=[v_], writes=[v_B[mt]])
                kb.barrier()

            with contextlib.ExitStack() as ps_:
                oT = kb.sb("a_oT", [128, 8, T], BF16, ps_)
                masks = kb.sb("a_mask", [128, 4, 512], BF16, ps_)
                negU = kb.sb("a_negU", [128, 128], BF16, ps_)
                negO = kb.sb("a_negO", [128, 128], BF16, ps_)
                zer = kb.sb("a_zero", [128, 128], BF16, ps_)
                kb.op("pool", lambda e: e.memset(masks.t[:, :, :], 1.0), writes=[masks])
                for m in range(4):
                    kb.op("pool", lambda e, m=m: e.affine_select(out=masks.t[:, m, :], in_=masks.t[:, m, :], pattern=[[1, 512]],
                                                                compare_op=ALU.is_gt, fill=0.0, base=-128 * m, channel_multiplier=-1),
                          reads=[masks], writes=[masks])
                kb.op("pool", lambda e: e.memset(negU.t[:, :], -1.0), writes=[negU])
                kb.op("pool", lambda e: e.affine_select(out=negU.t[:, :], in_=negU.t[:, :], pattern=[[-1, 128]],
                                                        compare_op=ALU.is_ge, fill=0.0, base=0, channel_multiplier=1),
                      reads=[negU], writes=[negU])
                kb.op("pool", lambda e: e.memset(negO.t[:, :], -1.0), writes=[negO])
                kb.op("pool", lambda e: e.memset(zer.t[:, :], 0.0), writes=[zer])
                with contextlib.ExitStack() as ps2:
                    qh = [kb.sb("a_qh%d" % i, [128, T], BF16, ps2) for i in range(2)]
                    kh = [kb.sb("a_kh%d" % i, [128, T], BF16, ps2) for i in range(2)]
                    vh = [kb.sb("a_vh%d" % i, [128, NT, 128], BF16, ps2) for i in range(2)]
                    ex = [[kb.sb("a_ex", [128, 512], F32, ps2) for _ in range(2)] for c in range(2)]
                    lnu = [[kb.sb("a_lnu", [128, 512], BF16, ps2) for _ in range(3)] for c in range(2)]
                    Sb = [[kb.sb("a_S", [128, 512], BF16, ps2) for _ in range(4)] for c in range(2)]
                    att = [[kb.sb("a_att", [128, 512], BF16, ps2) for _ in range(2)] for c in range(2)]
                    pz = [[kb.ps("a_pz", [128, 512], F32, ps2) for _ in range(2)] for c in range(2)]
                    pM = [kb.ps("a_pM", [128, 512], F32, ps2) for c in range(2)]
                    pc = [kb.ps("a_pc", [128, 512], F32, ps2) for c in range(2)]

                    def load_hp(hp):
                        i2 = hp % 2
                        kb.dma("sp", qh[i2].t[:, :], qT_d[hp], qh[i2], reads=q_B, writes=[qh[i2]])
                        kb.dma("sp", kh[i2].t[:, :], kT_d[hp], kh[i2], reads=k_B, writes=[kh[i2]])
                        kb.dma("sp", vh[i2].t[:, :, :], v_d.rearrange("(b p) d -> p b d", p=128)[:, :, hp * 128:(hp + 1) * 128], vh[i2],
                               reads=v_B, writes=[vh[i2]])

                    class It:
                        pass

                    nctr = [0, 0]

                    def make_items(hp, c):
                        items = []
                        for i in range(8):
                            for b in range(4 * i + 3, -1, -1):
                                it = It()
                                it.c = c
                                it.hp = hp
                                it.i = i
                                it.b = b
                                it.first = (b == 4 * i + 3)
                                it.last = (b == 0)
                                it.m = b - 4 * i
                                it.qlo = 128 * it.m if it.m > 0 else 0
                                it.n = nctr[c]
                                nctr[c] += 1
                                items.append(it)
                        return items

                    def stageA(it):
                        c = it.c
                        q_ = qh[it.hp % 2]
                        k_ = kh[it.hp % 2]
                        pr = slice(c * 64, (c + 1) * 64)
                        qlo = it.qlo
                        n = 512 - qlo
                        qs = slice(it.i * 512 + qlo, (it.i + 1) * 512)
                        ks = slice(it.b * 128, (it.b + 1) * 128)
                        z = pz[c][it.n % 2]
                        e_ = ex[c][it.n % 2]
                        l_ = lnu[c][it.n % 3]
                        kb.mmg(z, z.t[:, 0:n], [(k_.t[pr, ks], q_.t[pr, qs])], reads=[k_, q_])
                        kb.act(e_.t[:, 0:n], z.t[:, 0:n], AF.Exp, [z], [e_])
                        kb.act(l_.t[:, 0:n], e_.t[:, 0:n], AF.Ln, [e_], [l_], bias=1.0, scale=1.0)
                        if it.m >= 0:
                            kb.tt("pool", l_.t[:, 0:n], l_.t[:, 0:n], masks.t[:, it.m, qlo:512], ALU.mult, [l_, masks], [l_])
                        if not it.last:
                            Sn = Sb[c][it.n % 4]
                            if it.first:
                                if qlo > 0:
                                    kb.op("dve", lambda e: e.memset(Sn.t[:, 0:qlo], 0.0), writes=[Sn])
                                kb.copy("dve", Sn.t[:, qlo:512], l_.t[:, 0:n], [l_], [Sn])
                            else:
                                Sp = Sb[c][(it.n - 1) % 4]
                                if qlo > 0:
                                    kb.copy("dve", Sn.t[:, 0:qlo], Sp.t[:, 0:qlo], [Sp], [Sn])
                                kb.tt("dve", Sn.t[:, qlo:512], Sp.t[:, qlo:512], l_.t[:, 0:n], ALU.add, [Sp, l_], [Sn])

                    def stageB(it):
                        c = it.c
                        q_ = qh[it.hp % 2]
                        k_ = kh[it.hp % 2]
                        pr = slice(c * 64, (c + 1) * 64)
                        qlo = it.qlo
                        n = 512 - qlo
                        qs = slice(it.i * 512 + qlo, (it.i + 1) * 512)
                        ks = slice(it.b * 128, (it.b + 1) * 128)
                        l_ = lnu[c][it.n % 3]
                        a_ = att[c][it.n % 2]
                        M = pM[c]
                        prs = [(k_.t[pr, ks], q_.t[pr, qs]), (negU.t[:, :], l_.t[:, 0:n])]
                        rds = [k_, q_, negU, l_]
                        if not it.first:
                            Sp = Sb[c][(it.n - 1) % 4]
                            prs.append((negO.t[:, :], Sp.t[:, qlo:512]))
                            rds.append(Sp)
                        kb.mmg(M, M.t[:, 0:n], prs, reads=rds)
                        kb.act(a_.t[:, 0:n], M.t[:, 0:n], AF.Exp, [M], [a_])
                        if it.m >= 0:
                            kb.tt("pool", a_.t[:, 0:n], a_.t[:, 0:n], masks.t[:, it.m, qlo:512], ALU.mult, [a_, masks], [a_])

                    def stageC(it):
                        c = it.c
                        v_ = vh[it.hp % 2]
                        pr = slice(c * 64, (c + 1) * 64)
                        qlo = it.qlo
                        n = 512 - qlo
                        a_ = att[c][it.n % 2]
                        pcb = pc[c]
                        if it.first:
                            kb.op("pe", lambda e: e.matmul(pcb.t[:, :], lhsT=zer.t[:, :], rhs=masks.t[:, 0, :], start=True, stop=False),
                                  reads=[zer, masks], writes=[pcb], mark=False)
                        kb.op("pe", lambda e: e.matmul(pcb.t[:, qlo:512], lhsT=v_.t[:, it.b, :], rhs=a_.t[:, 0:n], start=False, stop=it.last),
                              reads=[v_, a_], writes=[pcb], mark=True)
                        if it.last:
                            kb.copy("dve", oT.t[pr, it.hp, it.i * 512:(it.i + 1) * 512], pcb.t[pr, :], [pcb], [oT])

                    load_hp(0)
                    for hp in range(8):
                        if hp + 1 < 8:
                            load_hp(hp + 1)
                        l0 = make_items(hp, 0)
                        l1 = make_items(hp, 1)
                        L = []
                        for a, b_ in zip(l0, l1):
                            L.append(a)
                            L.append(b_)
                        for g in range(len(L) + 4):
                            if g < len(L):
                                stageA(L[g])
                            if 0 <= g - 2 < len(L):
                                stageB(L[g - 2])
                            if 0 <= g - 4 < len(L):
                                stageC(L[g - 4])
                    kb.barrier()
                with contextlib.ExitStack() as ps3:
                    P = post_mixer_allocs(ps3, 1)
                    Wo = load_w(ps3, "a_Wo", w_out1_d, 1024)
                    xres = [kb.sb("a_xres%d" % i, [128, D], F32, ps3) for i in range(2)]
                    pso = [kb.ps("a_pso%d" % i, [128, 512], F32, ps3) for i in range(2)]
                    for gt in range(NT):
                        xr = xres[gt % 2]
                        kb.dma("sp", xr.t[:, :], xb_d[gt * 128:(gt + 1) * 128, :], xr, reads=[xb_B[gt]], writes=[xr])
                        post_mixer(ps3, 1, gt, xr.t[:, :], xr, pso, oT, slice(gt * 128, (gt + 1) * 128), Wo, P)
                    kb.barrier()

        mixer0()
        if stop_after != "m0":
            moe(0, xb_d, xb_B)
            if stop_after != "moe0":
                attention()
                if stop_after != "attn":
                    moe(1, out_d, out_B)
        kb.barrier()

    es.close()
    return nc


_NC_CACHE = {}


def kernel(x, even_w_in, even_conv_w, even_sgu_ln_g, even_sgu_ln_b, even_sgu_w_s, even_sgu_b_s, even_w_out,
           odd_w_qkv, odd_w_out, mix_ln_g, mix_ln_b, moe_w_group, moe_b_group, moe_w_router, moe_b_router,
           moe_w1, moe_w3, moe_w2, ffn_ln_g, ffn_ln_b):
    f = lambda a: np.ascontiguousarray(np.asarray(a, dtype=np.float32))
    convw = f(np.asarray(even_conv_w)[0].reshape(3, 4, 128).transpose(2, 1, 0).reshape(128, 12))
    wsT = f(np.asarray(even_sgu_w_s)[0].transpose(0, 2, 1))
    bs = f(np.asarray(even_sgu_b_s)[0].reshape(1, 1024))
    wr = f(np.concatenate([np.asarray(moe_w_group),
                           np.asarray(moe_w_router).transpose(0, 2, 1, 3).reshape(2, D, 16)], axis=2))
    br = f(np.concatenate([np.asarray(moe_b_group), np.asarray(moe_b_router).reshape(2, 16)], axis=1))
    shared = {
        "w_in": f(even_w_in[0]), "convw": convw, "sgu_g": f(even_sgu_ln_g), "sgu_b": f(even_sgu_ln_b),
        "wsT": wsT, "bs": bs, "w_out0": f(even_w_out[0]), "w_qkv": f(odd_w_qkv[0]), "w_out1": f(odd_w_out[0]),
        "mix_g": f(mix_ln_g), "mix_b": f(mix_ln_b), "wr": wr, "br": br,
        "w1": f(np.asarray(moe_w1).reshape(2, 16, D, 512)), "w3": f(np.asarray(moe_w3).reshape(2, 16, D, 512)),
        "w2": f(np.asarray(moe_w2).reshape(2, 16, 512, D)), "ffn_g": f(ffn_ln_g), "ffn_b": f(ffn_ln_b),
    }
    xs = f(x)
    if "nc" not in _NC_CACHE:
        _NC_CACHE["nc"] = build()
    nc = _NC_CACHE["nc"]
    in_maps = [dict(shared, x=xs[c]) for c in range(NCORES)]
    res = run_bass_kernel_spmd(nc, in_maps, core_ids=list(range(NCORES)))
    return np.stack([res.results[c]["out"] for c in range(NCORES)], axis=0)
```

```python
import contextlib
import numpy as np
import concourse.bass as bass
import concourse.mybir as mybir
from concourse.bass_utils import run_bass_kernel_spmd

F32 = mybir.dt.float32
BF16 = mybir.dt.bfloat16
I32 = mybir.dt.int32
OOB = 1 << 20
AF = mybir.ActivationFunctionType
ALU = mybir.AluOpType

T = 4096
D = 1024
NT = 32
ALPHA = float(4 ** 0.25)
EPS = 1e-5
NCORES = 8
SPARSE = True


class B:
    __slots__ = ("t", "w", "r", "dsem", "dcnt")

    def __init__(self, t=None):
        self.t = t
        self.w = None
        self.r = {}
        self.dsem = None
        self.dcnt = 0


class Eng:
    pass


class KB:
    def __init__(self):
        self.nc = bass.Bass("TRN2", target_bir_lowering=False)
        self.es = contextlib.ExitStack()
        self.nsem = 0
        self.dma_bufs = []
        self.rstack = []
        self.bregs = {}

    def sem(self, name):
        self.nsem += 1
        return self.es.enter_context(self.nc.semaphore(name))

    def sb(self, name, shape, dt, stack=None):
        st = stack if stack is not None else self.es
        self.nt = getattr(self, "nt", 0) + 1
        return B(st.enter_context(self.nc.sbuf_tensor("%s_%d" % (name, self.nt), shape, dt)))

    def ps(self, name, shape, dt, stack=None):
        st = stack if stack is not None else self.es
        self.nt = getattr(self, "nt", 0) + 1
        return B(st.enter_context(self.nc.psum_tensor("%s_%d" % (name, self.nt), shape, dt)))

    def start(self):
        nc = self.nc
        self.E = {}
        for name, eng in (("pe", nc.tensor), ("act", nc.scalar), ("dve", nc.vector),
                          ("pool", nc.gpsimd), ("sp", nc.sync)):
            e = Eng()
            e.name = name
            e.eng = eng
            e.sem = self.sem("s_" + name)
            e.count = 0
            e.seen = {}
            self.E[name] = e
        ET = mybir.EngineType
        self.etmap = {ET.PE: "pe", ET.Activation: "act", ET.DVE: "dve", ET.Pool: "pool", ET.SP: "sp"}
        self.mregs = nc.alloc_registers("mr", [ET.PE, ET.Activation, ET.DVE, ET.Pool, ET.SP])

    def _waits(self, E, reads, writes):
        need = {}

        def acc(tok):
            k = id(tok[0])
            if k not in need or need[k][1] < tok[1]:
                need[k] = tok

        for b in reads:
            if b.w is not None:
                acc(b.w)
        for b in writes:
            if b.w is not None:
                acc(b.w)
            for tok in b.r.values():
                acc(tok)
        for k, (sem, val) in need.items():
            if E.name == "pe" and sem is E.sem:
                continue
            if E.seen.get(k, 0) >= val:
                continue
            E.eng.wait_ge(sem, val)
            E.seen[k] = val

    def _commit(self, tok, reads, writes):
        k = id(tok[0])
        for b in reads:
            b.r[k] = tok
        for b in writes:
            b.w = tok
            b.r = {}

    def op(self, en, fn, reads=(), writes=(), mark=True):
        E = self.E[en]
        self._waits(E, reads, writes)
        inst = fn(E.eng)
        if mark:
            E.count += 1
            inst.then_inc(E.sem, 1)
            tok = (E.sem, E.count)
        else:
            tok = (E.sem, E.count + 1)
        self._commit(tok, reads, writes)
        return inst

    def dma(self, q, out_ap, in_ap, sbufB, reads=(), writes=()):
        E = self.E[q]
        self._waits(E, reads, writes)
        if sbufB.dsem is None:
            sbufB.dsem = self.sem("d%d" % self.nsem)
            self.dma_bufs.append(sbufB)
        for rd in self.rstack:
            rec = rd[q].setdefault(id(sbufB.dsem), [sbufB.dsem, sbufB.dcnt, 0])
            rec[2] += 16
        sbufB.dcnt += 16
        E.eng.dma_start(out=out_ap, in_=in_ap).then_inc(sbufB.dsem, 16)
        tok = (sbufB.dsem, sbufB.dcnt)
        self._commit(tok, reads, writes)

    def idma(self, out_ap, in_ap, idx_ap, scatter, bound, sbufB, reads=(), writes=()):
        E = self.E["pool"]
        self._waits(E, reads, writes)
        if sbufB.dsem is None:
            sbufB.dsem = self.sem("d%d" % self.nsem)
            self.dma_bufs.append(sbufB)
        for rd in self.rstack:
            rec = rd["pool"].setdefault(id(sbufB.dsem), [sbufB.dsem, sbufB.dcnt, 0])
            rec[2] += 16
        sbufB.dcnt += 16
        off = bass.IndirectOffsetOnAxis(ap=idx_ap, axis=0)
        if bound not in self.bregs:
            r = self.es.enter_context(E.eng.register("rb%d" % bound))
            E.eng.reg_mov(r, bound)
            self.bregs[bound] = r
        bound = self.bregs[bound]
        if scatter:
            E.eng.indirect_dma_start(out=out_ap, out_offset=off, in_=in_ap, in_offset=None,
                                     bounds_check=bound, oob_is_err=False).then_inc(sbufB.dsem, 16)
        else:
            E.eng.indirect_dma_start(out=out_ap, out_offset=None, in_=in_ap, in_offset=off,
                                     bounds_check=bound, oob_is_err=False).then_inc(sbufB.dsem, 16)
        tok = (sbufB.dsem, sbufB.dcnt)
        self._commit(tok, reads, writes)

    def region(self, thr, body):
        engs = list(self.E.values())
        snap = {E.name: dict(E.seen) for E in engs}
        before = {E.name: E.count for E in engs}
        rd = {E.name: {} for E in engs}
        self.rstack.append(rd)
        with self.nc.If_cmp(self.mregs, thr, "IS_LT"):
            body()
        self.rstack.pop()
        with self.nc.Else():
            for E in engs:
                nm = E.count - before[E.name]
                if nm > 0:
                    E.eng.wait_ge(E.sem, before[E.name])
                    E.eng.sem_inc(E.sem, nm)
                for (sem, bt, add) in rd[E.name].values():
                    E.eng.wait_ge(sem, bt)
                    E.eng.sem_inc(sem, add)
        for E in engs:
            E.seen = snap[E.name]

    def barrier(self):
        toks = []
        for e in self.E.values():
            if e.count > 0:
                toks.append((e.sem, e.count))
        for b in self.dma_bufs:
            if b.dcnt > 0:
                toks.append((b.dsem, b.dcnt))
        for E in self.E.values():
            for (sem, val) in toks:
                if sem is E.sem:
                    continue
                k = id(sem)
                if E.seen.get(k, 0) >= val:
                    continue
                E.eng.wait_ge(sem, val)
                E.seen[k] = val

    def mmg(self, outB, out_ap, pairs, reads):
        n = len(pairs)
        for i, (l, r) in enumerate(pairs):
            self.op("pe", lambda e, l=l, r=r, i=i: e.matmul(out_ap, lhsT=l, rhs=r, start=(i == 0), stop=(i == n - 1)),
                    reads=reads, writes=[outB], mark=(i == n - 1))

    def act(self, out_ap, in_ap, func, reads, writes, bias=None, scale=None):
        kw = {}
        if bias is not None:
            kw["bias"] = bias
        if scale is not None:
            kw["scale"] = scale
        self.op("act", lambda e: e.activation(out=out_ap, in_=in_ap, func=func, **kw), reads=reads, writes=writes)

    def copy(self, en, out_ap, in_ap, reads, writes):
        if en == "act":
            self.op("act", lambda e: e.copy(out=out_ap, in_=in_ap), reads=reads, writes=writes)
        else:
            self.op(en, lambda e: e.tensor_copy(out=out_ap, in_=in_ap), reads=reads, writes=writes)

    def tt(self, en, out_ap, a_ap, b_ap, op, reads, writes):
        self.op(en, lambda e: e.tensor_tensor(out=out_ap, in0=a_ap, in1=b_ap, op=op), reads=reads, writes=writes)

    def ts(self, en, out_ap, in_ap, s1, s2, op0, op1, reads, writes):
        if op1 is None:
            self.op(en, lambda e: e.tensor_scalar(out=out_ap, in0=in_ap, scalar1=s1, scalar2=None, op0=op0),
                    reads=reads, writes=writes)
        else:
            self.op(en, lambda e: e.tensor_scalar(out=out_ap, in0=in_ap, scalar1=s1, scalar2=s2, op0=op0, op1=op1),
                    reads=reads, writes=writes)

    def stt(self, en, out_ap, in0, scalar, in1, op0, op1, reads, writes):
        self.op(en, lambda e: e.scalar_tensor_tensor(out=out_ap, in0=in0, scalar=scalar, in1=in1, op0=op0, op1=op1),
                reads=reads, writes=writes)


def build(stop_after=None, dbg=False):
    kb = KB()
    dk = {"kind": "ExternalOutput"} if dbg else {}
    nc = kb.nc

    def din(name, shape):
        return nc.dram_tensor(name, shape, F32, kind="ExternalInput").ap()

    x_d = din("x", [T, D])
    w_in_d = din("w_in", [D, 2560])
    convw_d = din("convw", [128, 12])
    sgu_g_d = din("sgu_g", [1, 512])
    sgu_b_d = din("sgu_b", [1, 512])
    wsT_d = din("wsT", [8, 128, 128])
    bs_d = din("bs", [1, 1024])
    w_out0_d = din("w_out0", [D, D])
    w_qkv_d = din("w_qkv", [D, 3072])
    w_out1_d = din("w_out1", [D, D])
    mix_g_d = din("mix_g", [2, D])
    mix_b_d = din("mix_b", [2, D])
    wr_d = din("wr", [2, D, 20])
    br_d = din("br", [2, 20])
    w1_d = din("w1", [2, 16, D, 512])
    w3_d = din("w3", [2, 16, D, 512])
    w2_d = din("w2", [2, 16, 512, D])
    ffn_g_d = din("ffn_g", [2, D])
    ffn_b_d = din("ffn_b", [2, D])
    out_d = nc.dram_tensor("out", [T, D], F32, kind="ExternalOutput").ap()

    xa_d = nc.dram_tensor("xa_s", [T, D], F32, **dk).ap()
    xb_d = nc.dram_tensor("xb_s", [T, D], F32, **dk).ap()
    xT_d = nc.dram_tensor("xT_s", [128, 8, T], BF16, **dk).ap()
    qT_d = nc.dram_tensor("qT_s", [8, 128, T], BF16, **dk).ap()
    kT_d = nc.dram_tensor("kT_s", [8, 128, T], BF16, **dk).ap()
    v_d = nc.dram_tensor("v_s", [T, D], BF16, **dk).ap()

    x1b_d = nc.dram_tensor("x1b_s", [T, D], BF16, **dk).ap()
    list_d = nc.dram_tensor("list_s", [16 * 4096, 2], I32, **dk).ap()
    y2_d = nc.dram_tensor("y2_s", [2 * T, D], F32, **dk).ap()
    x1bd_B = [B() for _ in range(NT)]
    xa_B = [B() for _ in range(NT)]
    xb_B = [B() for _ in range(NT)]
    xT_B = [B() for _ in range(8)]
    q_B = [B() for _ in range(8)]
    k_B = [B() for _ in range(8)]
    v_B = [B() for _ in range(8)]
    out_B = [B() for _ in range(NT)]

    kb.start()
    es = kb.es

    ident = kb.sb("ident", [128, 128], BF16)
    c_all = kb.sb("c_all", [128, NT, 16], F32)
    cAB = kb.sb("cAB", [128, NT, 2], F32)
    off_t = kb.sb("off_t", [128, 16], F32)
    ebase1 = kb.sb("ebase1", [128, 16], F32)
    Lstr = kb.sb("Lstr", [128, 128], BF16)
    onesb = kb.sb("onesb", [128, 128], BF16)
    cnt_i = kb.sb("cnt_i", [128, 16], I32)
    tl2 = kb.sb("tl2", [128, NT, 2, 2], I32)
    oobt = kb.sb("oobt", [128, 1024], I32)

    block = es.enter_context(nc.Block())

    @block.sync
    def _(sync):
        kb.op("pool", lambda e: e.memset(ident.t[:], 0.0), writes=[ident])
        kb.op("pool", lambda e: e.affine_select(out=ident.t[:], in_=ident.t[:], pattern=[[-1, 128]],
                                                compare_op=ALU.not_equal, fill=1.0, base=0, channel_multiplier=1),
              reads=[ident], writes=[ident])

        kb.op("pool", lambda e: e.memset(onesb.t[:], 1.0), writes=[onesb])
        kb.op("pool", lambda e: e.memset(Lstr.t[:], 1.0), writes=[Lstr])
        kb.op("pool", lambda e: e.affine_select(out=Lstr.t[:], in_=Lstr.t[:], pattern=[[1, 128]],
                                                compare_op=ALU.is_gt, fill=0.0, base=0, channel_multiplier=-1),
              reads=[Lstr], writes=[Lstr])
        kb.op("pool", lambda e: e.iota(ebase1.t[:], [[4096, 16]], base=1, channel_multiplier=0,
                                       allow_small_or_imprecise_dtypes=True), writes=[ebase1])
        kb.op("pool", lambda e: e.memset(oobt.t[:], OOB), writes=[oobt])
        for r_ in range(2):
            kb.op("pool", lambda e, r_=r_: e.iota(tl2.t[:, :, r_, 0], [[128, NT]], base=0, channel_multiplier=1), writes=[tl2])
            kb.op("pool", lambda e, r_=r_: e.iota(tl2.t[:, :, r_, 1], [[256, NT]], base=r_, channel_multiplier=2), writes=[tl2])

        def layernorm(ps_, r, D_, g_bc, b_bc, outB, out_ap, stats, mv, tmp, mul_eng="pool"):
            nch = D_ // 512
            for c in range(nch):
                kb.op("dve", lambda e, c=c: e.bn_stats(out=stats.t[:, c * 6:(c + 1) * 6], in_=r.t[:, c * 512:(c + 1) * 512]),
                      reads=[r], writes=[stats])
            kb.op("dve", lambda e: e.bn_aggr(out=mv.t[:, 0:2], in_=stats.t[:, 0:nch * 6]), reads=[stats], writes=[mv])
            kb.act(tmp.t[:, 0:1], mv.t[:, 1:2], AF.Ln, [mv], [tmp], bias=EPS, scale=1.0)
            kb.act(tmp.t[:, 1:2], tmp.t[:, 0:1], AF.Exp, [tmp], [tmp], scale=-0.5)
            kb.ts("dve", r.t[:, 0:D_], r.t[:, 0:D_], mv.t[:, 0:1], tmp.t[:, 1:2], ALU.subtract, ALU.mult, [r, mv, tmp], [r])
            kb.tt(mul_eng, r.t[:, 0:D_], r.t[:, 0:D_], g_bc.t[:, 0:D_], ALU.mult, [r, g_bc], [r])
            kb.tt(mul_eng, out_ap, r.t[:, 0:D_], b_bc.t[:, 0:D_], ALU.add, [r, b_bc], [outB])

        def bcast_load(dst, src_row_ap):
            kb.dma("sp", dst.t[:], src_row_ap.partition_broadcast(128), dst, writes=[dst])

        def post_mixer(ps_, lyr, gt, x_res_ap, x_resB, pso, yT, yT_cols, Wo, P):
            for dh in range(2):
                kb.mmg(pso[dh], pso[dh].t[:, :],
                       [(yT.t[:, k, yT_cols], Wo.t[:, k, dh * 512:(dh + 1) * 512]) for k in range(8)],
                       reads=[yT, Wo])
            r = P["r"][gt % 2]
            for dh in range(2):
                kb.stt("dve", r.t[:, dh * 512:(dh + 1) * 512], x_res_ap[:, dh * 512:(dh + 1) * 512], ALPHA,
                       pso[dh].t[:, :], ALU.mult, ALU.add, [x_resB, pso[dh]], [r])
            x1 = P["x1"][gt % 2]
            layernorm(ps_, r, D, P["mg"], P["mb"], x1, x1.t[:, :], P["stats"], P["mv"], P["tmp"])
            kb.dma("pool", xa_d[gt * 128:(gt + 1) * 128, :], x1.t[:, :], x1, reads=[x1], writes=[xa_B[gt]])
            x1b = P["x1b"][gt % 2]
            kb.copy("pool", x1b.t[:, :], x1.t[:, :], [x1], [x1b])
            kb.dma("pool", x1b_d[gt * 128:(gt + 1) * 128, :], x1b.t[:, :], x1b, reads=[x1b], writes=[x1bd_B[gt]])
            pT = P["pT"]
            for k in range(8):
                kb.op("pe", lambda e, k=k: e.transpose(pT.t[:, k * 128:(k + 1) * 128], x1b.t[:, k * 128:(k + 1) * 128], ident.t[:]),
                      reads=[x1b, ident], writes=[pT], mark=(k == 7))
            x1T = P["x1T"][(gt // 4) % 2]
            s = gt % 4
            kb.copy("act", x1T.t[:, :, s * 128:(s + 1) * 128], pT.t[:, :].rearrange("p (k t) -> p k t", k=8), [pT], [x1T])
            prt = P["prt"]
            kb.mmg(prt, prt.t[:, 0:20], [(x1T.t[:, k, s * 128:(s + 1) * 128], P["Wr"].t[:, k, :]) for k in range(8)],
                   reads=[x1T, P["Wr"]])
            rt = P["rt"]
            lg = rt.t[:, 0:20]
            kb.tt("dve", lg, prt.t[:, 0:20], P["brb"].t[:, :], ALU.add, [prt, P["brb"]], [rt])
            R_ = [rt]
            kb.op("dve", lambda e: e.reduce_max(out=rt.t[:, 20:21], in_=rt.t[:, 0:4], axis=mybir.AxisListType.X), reads=R_, writes=R_)
            kb.ts("dve", rt.t[:, 24:28], rt.t[:, 0:4], rt.t[:, 20:21], None, ALU.is_equal, None, R_, R_)
            kb.ts("dve", rt.t[:, 21:22], rt.t[:, 20:21], -1.0, None, ALU.mult, None, R_, R_)
            kb.act(rt.t[:, 28:32], rt.t[:, 0:4], AF.Exp, R_, R_, bias=rt.t[:, 21:22], scale=1.0)
            kb.op("dve", lambda e: e.reduce_sum(out=rt.t[:, 22:23], in_=rt.t[:, 28:32], axis=mybir.AxisListType.X), reads=R_, writes=R_)
            kb.ts("dve", rt.t[:, 32:36], rt.t[:, 4:8], rt.t[:, 24:25], None, ALU.mult, None, R_, R_)
            for g in range(1, 4):
                kb.stt("dve", rt.t[:, 32:36], rt.t[:, 4 + 4 * g:8 + 4 * g], rt.t[:, 24 + g:25 + g], rt.t[:, 32:36],
                       ALU.mult, ALU.add, R_, R_)
            kb.op("dve", lambda e: e.reduce_max(out=rt.t[:, 36:37], in_=rt.t[:, 32:36], axis=mybir.AxisListType.X), reads=R_, writes=R_)
            kb.ts("dve", rt.t[:, 40:44], rt.t[:, 32:36], rt.t[:, 36:37], None, ALU.is_equal, None, R_, R_)
            kb.ts("dve", rt.t[:, 37:38], rt.t[:, 36:37], -1.0, None, ALU.mult, None, R_, R_)
            kb.act(rt.t[:, 44:48], rt.t[:, 32:36], AF.Exp, R_, R_, bias=rt.t[:, 37:38], scale=1.0)
            kb.op("dve", lambda e: e.reduce_max(out=rt.t[:, 38:39], in_=rt.t[:, 44:48], axis=mybir.AxisListType.X), reads=R_, writes=R_)
            kb.tt("dve", rt.t[:, 48:52], rt.t[:, 44:48], rt.t[:, 40:44], ALU.mult, R_, R_)
            kb.tt("dve", rt.t[:, 48:52], rt.t[:, 44:48], rt.t[:, 48:52], ALU.subtract, R_, R_)
            kb.op("dve", lambda e: e.reduce_max(out=rt.t[:, 39:40], in_=rt.t[:, 48:52], axis=mybir.AxisListType.X), reads=R_, writes=R_)
            kb.ts("dve", rt.t[:, 52:56], rt.t[:, 44:48], rt.t[:, 39:40], None, ALU.is_ge, None, R_, R_)
            kb.tt("dve", rt.t[:, 52:56], rt.t[:, 52:56], rt.t[:, 44:48], ALU.mult, R_, R_)
            kb.tt("dve", rt.t[:, 56:57], rt.t[:, 38:39], rt.t[:, 39:40], ALU.add, R_, R_)
            kb.tt("dve", rt.t[:, 56:57], rt.t[:, 56:57], rt.t[:, 22:23], ALU.mult, R_, R_)
            kb.op("dve", lambda e: e.reciprocal(out=rt.t[:, 57:58], in_=rt.t[:, 56:57]), reads=R_, writes=R_)
            kb.ts("dve", rt.t[:, 60:64], rt.t[:, 24:28], rt.t[:, 57:58], None, ALU.mult, None, R_, R_)
            for g in range(4):
                kb.ts("dve", c_all.t[:, gt, 4 * g:4 * g + 4], rt.t[:, 52:56], rt.t[:, 60 + g:61 + g], None, ALU.mult, None,
                      R_, [c_all])
            if s == 3 and not SPARSE:
                mt = gt // 4
                kb.dma("pool", xT_d[:, :, mt * 512:(mt + 1) * 512], x1T.t[:, :, :], x1T, reads=[x1T], writes=[xT_B[mt]])
            if SPARSE:
                rt2 = P["rt2"]
                mkb = P["mkb"]
                idx2 = P["idx2"][gt % 2]
                Q_ = [rt2]
                cg = c_all.t[:, gt, :]
                kb.ts("dve", rt2.t[:, 0:16], cg, 0.0, None, ALU.is_gt, None, [c_all], Q_)
                kb.copy("dve", mkb.t[:, :], rt2.t[:, 0:16], Q_, [mkb])
                kb.mmg(prt, prt.t[:, 32:48], [(Lstr.t[:, :], mkb.t[:, :])], reads=[Lstr, mkb])
                kb.mmg(prt, prt.t[:, 48:64], [(onesb.t[:, :], mkb.t[:, :])], reads=[onesb, mkb])
                kb.tt("dve", rt2.t[:, 16:32], prt.t[:, 32:48], ebase1.t[:, :], ALU.add, [prt, ebase1], Q_)
                kb.tt("dve", rt2.t[:, 16:32], rt2.t[:, 16:32], off_t.t[:, :], ALU.add, Q_ + [off_t], Q_)
                kb.tt("dve", rt2.t[:, 16:32], rt2.t[:, 16:32], rt2.t[:, 0:16], ALU.mult, Q_, Q_)
                kb.tt("dve", off_t.t[:, :], off_t.t[:, :], prt.t[:, 48:64], ALU.add, [off_t, prt], [off_t])
                kb.op("dve", lambda e: e.reduce_max(out=rt2.t[:, 64:65], in_=rt2.t[:, 16:32], axis=mybir.AxisListType.X), reads=Q_, writes=Q_)
                kb.ts("dve", rt2.t[:, 32:48], rt2.t[:, 16:32], rt2.t[:, 64:65], None, ALU.is_equal, None, Q_, Q_)
                kb.tt("dve", rt2.t[:, 48:64], rt2.t[:, 32:48], cg, ALU.mult, Q_ + [c_all], Q_)
                kb.op("dve", lambda e: e.reduce_sum(out=cAB.t[:, gt, 0:1], in_=rt2.t[:, 48:64], axis=mybir.AxisListType.X), reads=Q_, writes=[cAB])
                kb.op("dve", lambda e: e.reduce_sum(out=rt2.t[:, 66:67], in_=cg, axis=mybir.AxisListType.X), reads=[c_all], writes=Q_)
                kb.tt("dve", cAB.t[:, gt, 1:2], rt2.t[:, 66:67], cAB.t[:, gt, 0:1], ALU.subtract, Q_ + [cAB], [cAB])
                kb.tt("dve", rt2.t[:, 48:64], rt2.t[:, 16:32], rt2.t[:, 32:48], ALU.mult, Q_, Q_)
                kb.tt("dve", rt2.t[:, 48:64], rt2.t[:, 16:32], rt2.t[:, 48:64], ALU.subtract, Q_, Q_)
                kb.op("dve", lambda e: e.reduce_max(out=rt2.t[:, 67:68], in_=rt2.t[:, 48:64], axis=mybir.AxisListType.X), reads=Q_, writes=Q_)
                kb.ts("dve", rt2.t[:, 68:69], rt2.t[:, 67:68], 0.0, float(OOB), ALU.is_equal, ALU.mult, Q_, Q_)
                kb.tt("dve", rt2.t[:, 67:68], rt2.t[:, 67:68], rt2.t[:, 68:69], ALU.add, Q_, Q_)
                kb.ts("dve", idx2.t[:, 0:1], rt2.t[:, 64:65], -1.0, None, ALU.add, None, Q_, [idx2])
                kb.ts("dve", idx2.t[:, 1:2], rt2.t[:, 67:68], -1.0, None, ALU.add, None, Q_, [idx2])
                for r_ in range(2):
                    kb.idma(list_d[:, :], tl2.t[:, gt, r_, :], idx2.t[:, r_:r_ + 1], True, 16 * 4096 - 1, idx2,
                            reads=[idx2, tl2, P["listB"]])
                if gt == NT - 1:
                    kb.ts("dve", cnt_i.t[:, :], off_t.t[:, :], -1.0, 4096.0, ALU.mult, ALU.add, [off_t], [cnt_i])

        def post_mixer_allocs(ps_, lyr):
            P = {}
            P["r"] = [kb.sb("pm_r%d" % i, [128, D], F32, ps_) for i in range(2)]
            P["x1"] = [kb.sb("pm_x1%d" % i, [128, D], F32, ps_) for i in range(2)]
            P["x1b"] = [kb.sb("pm_x1b", [128, D], BF16, ps_) for _ in range(2)]
            P["rt2"] = kb.sb("pm_rt2", [128, 128], F32, ps_)
            P["mkb"] = kb.sb("pm_mkb", [128, 16], BF16, ps_)
            P["idx2"] = [kb.sb("pm_idx2", [128, 2], I32, ps_) for _ in range(2)]
            P["listB"] = B()
            kb.op("dve", lambda e: e.memset(off_t.t[:], 0.0), writes=[off_t])
            kb.dma("pool", list_d.rearrange("(p a) c -> p (a c)", p=128), oobt.t[:, :], oobt, reads=[oobt], writes=[P["listB"]])
            P["x1T"] = [kb.sb("pm_x1T", [128, 8, 512], BF16, ps_)] * 2
            P["stats"] = kb.sb("pm_stats", [128, 12], F32, ps_)
            P["mv"] = kb.sb("pm_mv", [128, 2], F32, ps_)
            P["tmp"] = kb.sb("pm_tmp", [128, 2], F32, ps_)
            P["rt"] = kb.sb("pm_rt", [128, 64], F32, ps_)
            P["mg"] = kb.sb("pm_mg", [128, D], F32, ps_)
            P["mb"] = kb.sb("pm_mb", [128, D], F32, ps_)
            P["brb"] = kb.sb("pm_brb", [128, 20], F32, ps_)
            P["Wr"] = kb.sb("pm_Wr", [128, 8, 20], BF16, ps_)
            P["pT"] = kb.ps("pm_pT", [128, 1024], BF16, ps_)
            P["prt"] = kb.ps("pm_prt", [128, 512], F32, ps_)
            bcast_load(P["mg"], mix_g_d[lyr:lyr + 1, :])
            bcast_load(P["mb"], mix_b_d[lyr:lyr + 1, :])
            bcast_load(P["brb"], br_d[lyr:lyr + 1, :])
            kb.dma("pool", P["Wr"].t[:, :, :], wr_d[lyr].rearrange("(k p) n -> p k n", p=128), P["Wr"], writes=[P["Wr"]])
            return P

        def load_w(ps_, name, src_ap, ncols):
            W = kb.sb(name, [128, 8, ncols], BF16, ps_)
            v = src_ap.rearrange("(k p) n -> p k n", p=128)
            for k in range(8):
                kb.dma("pool", W.t[:, k, :], v[:, k, :], W, writes=[W])
            return W

        def load_xT(xsrc_d, xsrc_B, mt, xin, xbf, pT, xT):
            rd = [xsrc_B[mt * 4 + s] for s in range(4)] if xsrc_B is not None else []
            kb.dma("sp", xin.t[:, :, :], xsrc_d[mt * 512:(mt + 1) * 512, :].rearrange("(s p) d -> p s d", p=128), xin,
                   reads=rd, writes=[xin])
            for s in range(4):
                xb = xbf[s % 2]
                kb.copy("pool", xb.t[:, :], xin.t[:, s, :], [xin], [xb])
                for k in range(8):
                    kb.op("pe", lambda e, k=k, xb=xb: e.transpose(pT.t[:, k * 128:(k + 1) * 128], xb.t[:, k * 128:(k + 1) * 128], ident.t[:]),
                          reads=[xb, ident], writes=[pT], mark=(k == 7))
                kb.copy("act", xT.t[:, :, s * 128:(s + 1) * 128], pT.t[:, :].rearrange("p (k t) -> p k t", k=8), [pT], [xT])

        def mixer0():
            with contextlib.ExitStack() as ps_:
                P = post_mixer_allocs(ps_, 0)
                Wi = load_w(ps_, "m0_Wi", w_in_d, 2560)
                Wo = load_w(ps_, "m0_Wo", w_out0_d, 1024)
                WsT = kb.sb("m0_WsT", [128, 8, 128], BF16, ps_)
                kb.dma("pool", WsT.t[:, :, :], wsT_d.rearrange("h s t -> s h t"), WsT, writes=[WsT])
                kb.op("pool", lambda e: e.memset(WsT.t[64:128, :, 0:64], 0.0), reads=[WsT], writes=[WsT])
                bsr = kb.sb("m0_bsr", [1, 1024], BF16, ps_)
                kb.dma("pool", bsr.t[:, :], bs_d[:, :], bsr, writes=[bsr])
                onesr = kb.sb("m0_ones", [1, 128], BF16, ps_)
                kb.op("pool", lambda e: e.memset(onesr.t[:, :], 1.0), writes=[onesr])
                cw = kb.sb("m0_cw", [128, 12], F32, ps_)
                kb.dma("sp", cw.t[:, :], convw_d[:, :], cw, writes=[cw])
                sg = kb.sb("m0_sg", [128, 512], F32, ps_)
                sbb = kb.sb("m0_sbb", [128, 512], F32, ps_)
                bcast_load(sg, sgu_g_d[0:1, :])
                bcast_load(sbb, sgu_b_d[0:1, :])
                xin = kb.sb("m0_xin", [128, 4, D], F32, ps_)
                xbf = [kb.sb("m0_xbf%d" % i, [128, D], BF16, ps_) for i in range(2)]
                xres = [kb.sb("m0_xres%d" % i, [128, D], F32, ps_) for i in range(2)]
                xT = kb.sb("m0_xT", [128, 8, 512], BF16, ps_)
                yT = kb.sb("m0_yT", [128, 8, 512], BF16, ps_)
                ub = kb.sb("m0_ub", [128, 4, 514], F32, ps_)
                Cs = kb.sb("m0_Cs", [128, 512], F32, ps_)
                acc = kb.sb("m0_acc", [128, 512], F32, ps_)
                zu = kb.sb("m0_zu", [128, 4, 512], F32, ps_)
                g1 = kb.sb("m0_g1", [128, 512], F32, ps_)
                g2 = kb.sb("m0_g2", [128, 512], F32, ps_)
                gv = kb.sb("m0_gv", [128, 512], F32, ps_)
                vt = kb.sb("m0_vt", [128, 4, 512], BF16, ps_)
                st2 = kb.sb("m0_st2", [128, 6], F32, ps_)
                mv2 = kb.sb("m0_mv2", [128, 2], F32, ps_)
                tmp2 = kb.sb("m0_tmp2", [128, 2], F32, ps_)
                pp = [kb.ps("m0_pp%d" % i, [128, 512], F32, ps_) for i in range(2)]
                psg = [kb.ps("m0_psg%d" % i, [128, 512], F32, ps_) for i in range(2)]
                pso = [kb.ps("m0_pso%d" % i, [128, 512], F32, ps_) for i in range(2)]
                pT = P["pT"]
                kb.op("pool", lambda e: e.memset(ub.t[:, :, :], 0.0), writes=[ub])
                ppi = [0]

                def proj_fm(c, xT):
                    p = pp[ppi[0] % 2]
                    ppi[0] += 1
                    kb.mmg(p, p.t[:, :], [(Wi.t[:, k, c * 128:(c + 1) * 128], xT.t[:, k, :]) for k in range(8)], reads=[Wi, xT])
                    return p

                def gelu(p, p_ap, outB, out_ap, n):
                    kb.act(g1.t[:, 0:n], p_ap, AF.Square, [p], [g1])
                    kb.ts("pool", g1.t[:, 0:n], g1.t[:, 0:n], 0.044715, 1.0, ALU.mult, ALU.add, [g1], [g1])
                    kb.tt("dve", g2.t[:, 0:n], g1.t[:, 0:n], p_ap, ALU.mult, [g1, p], [g2])
                    kb.act(g1.t[:, 0:n], g2.t[:, 0:n], AF.Exp, [g2], [g1], scale=-1.5957691216057308)
                    kb.ts("pool", g1.t[:, 0:n], g1.t[:, 0:n], 1.0, None, ALU.add, None, [g1], [g1])
                    kb.op("dve", lambda e: e.reciprocal(out=g2.t[:, 0:n], in_=g1.t[:, 0:n]), reads=[g1], writes=[g2])
                    kb.tt("dve", out_ap, g2.t[:, 0:n], p_ap, ALU.mult, [g2, p], [outB])

                for mt in range(8):
                    load_xT(x_d, None, mt, xin, xbf, pT, xT)
                    u = ub
                    for j in range(4):
                        pC = proj_fm(4 + j, xT)
                        kb.copy("act", Cs.t[:, :], pC.t[:, :], [pC], [Cs])
                        pH = proj_fm(8 + j, xT)
                        kb.copy("dve", u.t[:, j, 0:2], u.t[:, j, 512:514], [u], [u])
                        kb.tt("dve", u.t[:, j, 2:514], Cs.t[:, :], pH.t[:, :], ALU.mult, [Cs, pH], [u])
                        kb.ts("dve", acc.t[:, :], u.t[:, j, 2:514], cw.t[:, j * 3 + 2:j * 3 + 3], None, ALU.mult, None, [u, cw], [acc])
                        kb.stt("dve", acc.t[:, :], u.t[:, j, 1:513], cw.t[:, j * 3 + 1:j * 3 + 2], acc.t[:, :], ALU.mult, ALU.add, [u, cw, acc], [acc])
                        kb.stt("dve", acc.t[:, :], u.t[:, j, 0:512], cw.t[:, j * 3:j * 3 + 1], acc.t[:, :], ALU.mult, ALU.add, [u, cw, acc], [acc])
                        pB = proj_fm(j, xT)
                        kb.tt("dve", yT.t[:, j, :], acc.t[:, :], pB.t[:, :], ALU.mult, [acc, pB], [yT])
                    for j in range(4):
                        pZ = proj_fm(12 + j, xT)
                        gelu(pZ, pZ.t[:, :], zu, zu.t[:, j, :], 512)
                    for s in range(4):
                        p = pp[ppi[0] % 2]
                        ppi[0] += 1
                        kb.mmg(p, p.t[:, :], [(xT.t[:, k, s * 128:(s + 1) * 128], Wi.t[:, k, 2048:2560]) for k in range(8)], reads=[Wi, xT])
                        gelu(p, p.t[:, :], gv, gv.t[:, :], 512)
                        layernorm(ps_, gv, 512, sg, sbb, vt, vt.t[:, s, :], st2, mv2, tmp2, mul_eng="pool")
                    for hp in range(4):
                        for j in range(2):
                            h = 2 * hp + j
                            pg = psg[j]
                            for s in range(4):
                                kb.op("pe", lambda e, s=s, pg=pg, h=h, hp=hp: e.matmul(pg.t[:, s * 128:(s + 1) * 128], lhsT=vt.t[:, s, hp * 128:(hp + 1) * 128],
                                                                         rhs=WsT.t[:, h, :], start=True, stop=False),
                                      reads=[vt, WsT], writes=[pg], mark=False)
                                kb.op("pe", lambda e, s=s, pg=pg, h=h: e.matmul(pg.t[:, s * 128:(s + 1) * 128], lhsT=onesr.t[0:1, :],
                                                                   rhs=bsr.t[0:1, h * 128:(h + 1) * 128], start=False, stop=True),
                                      reads=[onesr, bsr], writes=[pg], mark=(s == 3))
                            kb.tt("dve", yT.t[j * 64:(j + 1) * 64, 4 + hp, :], zu.t[j * 64:(j + 1) * 64, hp, :], pg.t[j * 64:(j + 1) * 64, :],
                                  ALU.mult, [zu, pg], [yT])
                    for s in range(4):
                        gt = mt * 4 + s
                        xr = xres[gt % 2]
                        kb.dma("sp", xr.t[:, :], x_d[gt * 128:(gt + 1) * 128, :], xr, writes=[xr])
                        post_mixer(ps_, 0, gt, xr.t[:, :], xr, pso, yT, slice(s * 128, (s + 1) * 128), Wo, P)
                kb.barrier()

        def moe(lyr, dst_d, dst_B):
            with contextlib.ExitStack() as ps_:
                fg = kb.sb("f_g", [128, D], F32, ps_)
                fb = kb.sb("f_b", [128, D], F32, ps_)
                bcast_load(fg, ffn_g_d[lyr:lyr + 1, :])
                bcast_load(fb, ffn_b_d[lyr:lyr + 1, :])
                xTh = kb.sb("f_xTh", [128, 8, 2048], BF16, ps_)
                yacc = kb.sb("f_yacc", [128, 16, D], F32, ps_)
                w1s = [kb.sb("f_w1_%d" % i, [128, 8, 512], BF16, ps_) for i in range(2)]
                w3s = [kb.sb("f_w3_%d" % i, [128, 8, 512], BF16, ps_) for i in range(2)]
                w2s = [kb.sb("f_w2_%d" % i, [128, 4, D], BF16, ps_) for i in range(2)]
                hid = [kb.sb("f_hid%d" % i, [128, 4, 512], BF16, ps_) for i in range(2)]
                sl = [kb.sb("f_sl%d" % i, [128, 512], BF16, ps_) for i in range(2)]
                x1l = [kb.sb("f_x1l%d" % i, [128, D], F32, ps_) for i in range(2)]
                rr = [kb.sb("f_r%d" % i, [128, D], F32, ps_) for i in range(2)]
                st = kb.sb("f_st", [128, 12], F32, ps_)
                mv = kb.sb("f_mv", [128, 2], F32, ps_)
                tmp = kb.sb("f_tmp", [128, 2], F32, ps_)
                ph1 = [kb.ps("f_ph1_%d" % i, [128, 512], F32, ps_) for i in range(2)]
                ph3 = [kb.ps("f_ph3_%d" % i, [128, 512], F32, ps_) for i in range(2)]
                py = [kb.ps("f_py%d" % i, [128, 512], F32, ps_) for i in range(2)]

                def load_expert(e):
                    sl_ = e % 2
                    kb.dma("pool", w1s[sl_].t[:, :, :], w1_d[lyr, e].rearrange("(k p) f -> p k f", p=128), w1s[sl_], writes=[w1s[sl_]])
                    kb.dma("pool", w3s[sl_].t[:, :, :], w3_d[lyr, e].rearrange("(k p) f -> p k f", p=128), w3s[sl_], writes=[w3s[sl_]])
                    kb.dma("pool", w2s[sl_].t[:, :, :], w2_d[lyr, e].rearrange("(c p) d -> p c d", p=128), w2s[sl_], writes=[w2s[sl_]])

                cnt = [0]
                for hf in range(2):
                    kb.dma("sp", xTh.t[:, :, :], xT_d[:, :, hf * 2048:(hf + 1) * 2048], xTh,
                           reads=[xT_B[hf * 4 + i] for i in range(4)], writes=[xTh])
                    load_expert(0)
                    for e in range(16):
                        if e + 1 < 16:
                            load_expert(e + 1)
                        w1 = w1s[e % 2]
                        w3 = w3s[e % 2]
                        w2 = w2s[e % 2]
                        for mt in range(4):
                            hd = hid[(e * 4 + mt) % 2]
                            for fc in range(4):
                                i2 = cnt[0] % 2
                                cnt[0] += 1
                                xs = xTh.t[:, :, mt * 512:(mt + 1) * 512]
                                kb.mmg(ph1[i2], ph1[i2].t[:, :], [(w1.t[:, k, fc * 128:(fc + 1) * 128], xTh.t[:, k, mt * 512:(mt + 1) * 512]) for k in range(8)],
                                       reads=[w1, xTh])
                                kb.mmg(ph3[i2], ph3[i2].t[:, :], [(w3.t[:, k, fc * 128:(fc + 1) * 128], xTh.t[:, k, mt * 512:(mt + 1) * 512]) for k in range(8)],
                                       reads=[w3, xTh])
                                kb.act(sl[i2].t[:, :], ph1[i2].t[:, :], AF.Silu, [ph1[i2]], [sl[i2]])
                                kb.tt("dve", hd.t[:, fc, :], sl[i2].t[:, :], ph3[i2].t[:, :], ALU.mult, [sl[i2], ph3[i2]], [hd])
                            for s in range(4):
                                ti = mt * 4 + s
                                gt = hf * 16 + ti
                                for dh in range(2):
                                    p = py[dh]
                                    kb.mmg(p, p.t[:, :], [(hd.t[:, fc, s * 128:(s + 1) * 128], w2.t[:, fc, dh * 512:(dh + 1) * 512]) for fc in range(4)],
                                           reads=[hd, w2])
                                    ya = yacc.t[:, ti, dh * 512:(dh + 1) * 512]
                                    if e == 0:
                                        kb.ts("dve", ya, p.t[:, :], c_all.t[:, gt, e:e + 1], None, ALU.mult, None, [p, c_all], [yacc])
                                    else:
                                        kb.stt("dve", ya, p.t[:, :], c_all.t[:, gt, e:e + 1], ya, ALU.mult, ALU.add, [p, c_all, yacc], [yacc])
                    for ti in range(16):
                        gt = hf * 16 + ti
                        xl = x1l[ti % 2]
                        r = rr[ti % 2]
                        kb.dma("sp", xl.t[:, :], xa_d[gt * 128:(gt + 1) * 128, :], xl, reads=[xa_B[gt]], writes=[xl])
                        kb.stt("dve", r.t[:, :], xl.t[:, :], ALPHA, yacc.t[:, ti, :], ALU.mult, ALU.add, [xl, yacc], [r])
                        layernorm(ps_, r, D, fg, fb, xl, xl.t[:, :], st, mv, tmp)
                        kb.dma("pool", dst_d[gt * 128:(gt + 1) * 128, :], xl.t[:, :], xl, reads=[xl], writes=[dst_B[gt]])
                kb.barrier()

        def moe_sparse(lyr, dst_d, dst_B):
            with contextlib.ExitStack() as ps_:
                fg = kb.sb("f_g", [128, D], F32, ps_)
                fb = kb.sb("f_b", [128, D], F32, ps_)
                bcast_load(fg, ffn_g_d[lyr:lyr + 1, :])
                bcast_load(fb, ffn_b_d[lyr:lyr + 1, :])
                w1s = [kb.sb("f_w1", [128, 8, 512], BF16, ps_) for i in range(2)]
                w3s = [kb.sb("f_w3", [128, 8, 512], BF16, ps_) for i in range(2)]
                w2s = [kb.sb("f_w2", [128, 4, D], BF16, ps_) for i in range(2)]
                xg = [kb.sb("f_xg", [128, D], BF16, ps_) for i in range(3)]
                lst = [kb.sb("f_lst", [128, 2], I32, ps_) for i in range(3)]
                xgT = [kb.sb("f_xgT", [128, 8, 128], BF16, ps_) for i in range(2)]
                sl = [kb.sb("f_sl", [128, 512], BF16, ps_) for i in range(2)]
                hid = [kb.sb("f_hid", [128, 4, 128], BF16, ps_) for i in range(2)]
                ysb = [kb.sb("f_ysb", [128, D], F32, ps_) for i in range(2)]
                y2l = [kb.sb("f_y2l", [128, 2, D], F32, ps_) for i in range(2)]
                x1l = [kb.sb("f_x1l", [128, D], F32, ps_) for i in range(2)]
                rr = [kb.sb("f_r", [128, D], F32, ps_) for i in range(2)]
                st = kb.sb("f_st", [128, 12], F32, ps_)
                mv = kb.sb("f_mv", [128, 2], F32, ps_)
                tmp = kb.sb("f_tmp", [128, 2], F32, ps_)
                pT = kb.ps("f_pT", [128, 1024], BF16, ps_)
                ph1 = [kb.ps("f_ph1", [128, 512], F32, ps_) for i in range(2)]
                ph3 = [kb.ps("f_ph3", [128, 512], F32, ps_) for i in range(2)]
                py = [kb.ps("f_py", [128, 512], F32, ps_) for i in range(2)]
                for i in range(3):
                    kb.op("dve", lambda e, i=i: e.memset(xg[i].t[:, :], 0.0), writes=[xg[i]])

                def load_expert(e):
                    sl_ = e % 2
                    kb.dma("pool", w1s[sl_].t[:, :, :], w1_d[lyr, e].rearrange("(k p) f -> p k f", p=128), w1s[sl_], writes=[w1s[sl_]])
                    kb.dma("pool", w3s[sl_].t[:, :, :], w3_d[lyr, e].rearrange("(k p) f -> p k f", p=128), w3s[sl_], writes=[w3s[sl_]])
                    kb.dma("pool", w2s[sl_].t[:, :, :], w2_d[lyr, e].rearrange("(c p) d -> p c d", p=128), w2s[sl_], writes=[w2s[sl_]])

                load_expert(0)
                for e in range(16):
                    if e + 1 < 16:
                        load_expert(e + 1)
                    w1 = w1s[e % 2]
                    w3 = w3s[e % 2]
                    w2 = w2s[e % 2]
                    for reg in kb.mregs:
                        E_ = kb.E[kb.etmap[reg.engine]]
                        kb._waits(E_, [cnt_i], [])
                        E_.eng.reg_load(reg, cnt_i.t[0:1, e:e + 1])

                    def fetch(j, e=e):
                        i2 = j % 3
                        base = e * 4096 + j * 128
                        kb.dma("sp", lst[i2].t[:, :], list_d[base:base + 128, :], lst[i2], writes=[lst[i2]])
                        kb.idma(xg[i2].t[:, :], x1b_d[:, :], lst[i2].t[:, 0:1], False, T - 1, xg[i2], reads=[lst[i2]], writes=[xg[i2]])

                    def transp(j):
                        i2 = j % 2
                        i3 = j % 3
                        for k in range(8):
                            kb.op("pe", lambda e_, k=k: e_.transpose(pT.t[:, k * 128:(k + 1) * 128], xg[i3].t[:, k * 128:(k + 1) * 128], ident.t[:]),
                                  reads=[xg[i3], ident], writes=[pT], mark=(k == 7))
                        kb.copy("act", xgT[i2].t[:, :, :], pT.t[:, :].rearrange("p (k t) -> p k t", k=8), [pT], [xgT[i2]])

                    def slot(j, w1=w1, w3=w3, w2=w2):
                        i2 = j % 2
                        i3 = j % 3
                        if j + 2 < NT:
                            fetch(j + 2)
                        for fc in range(4):
                            kb.mmg(ph1[i2], ph1[i2].t[:, fc * 128:(fc + 1) * 128],
                                   [(w1.t[:, k, fc * 128:(fc + 1) * 128], xgT[i2].t[:, k, :]) for k in range(8)], reads=[w1, xgT[i2]])
                        for fc in range(4):
                            kb.mmg(ph3[i2], ph3[i2].t[:, fc * 128:(fc + 1) * 128],
                                   [(w3.t[:, k, fc * 128:(fc + 1) * 128], xgT[i2].t[:, k, :]) for k in range(8)], reads=[w3, xgT[i2]])
                        kb.act(sl[i2].t[:, :], ph1[i2].t[:, :], AF.Silu, [ph1[i2]], [sl[i2]])
                        kb.tt("dve", hid[i2].t[:, :, :].rearrange("p c t -> p (c t)"), sl[i2].t[:, :], ph3[i2].t[:, :], ALU.mult,
                              [sl[i2], ph3[i2]], [hid[i2]])
                        if j + 1 < NT:
                            transp(j + 1)
                        for dh in range(2):
                            kb.mmg(py[dh], py[dh].t[:, :], [(hid[i2].t[:, fc, :], w2.t[:, fc, dh * 512:(dh + 1) * 512]) for fc in range(4)],
                                   reads=[hid[i2], w2])
                        kb.copy("act", ysb[i2].t[:, 0:512], py[0].t[:, :], [py[0]], [ysb[i2]])
                        kb.copy("dve", ysb[i2].t[:, 512:1024], py[1].t[:, :], [py[1]], [ysb[i2]])
                        kb.idma(y2_d[:, :], ysb[i2].t[:, :], lst[i3].t[:, 1:2], True, 2 * T - 1, ysb[i2], reads=[ysb[i2], lst[i3]])

                    fetch(0)
                    fetch(1)
                    transp(0)
                    NFLAT = 6
                    for j in range(NFLAT):
                        kb.region(4096 - j * 128, lambda j=j: slot(j))

                    def rest():
                        for j in range(NFLAT, NT):
                            kb.region(4096 - j * 128, lambda j=j: slot(j))
                    kb.region(4096 - NFLAT * 128, rest)
                kb.barrier()
                for gt in range(NT):
                    yl = y2l[gt % 2]
                    xl = x1l[gt % 2]
                    r = rr[gt % 2]
                    kb.dma("sp", yl.t[:, :, :], y2_d[gt * 256:(gt + 1) * 256, :].rearrange("(p r) d -> p r d", r=2), yl, writes=[yl])
                    kb.dma("sp", xl.t[:, :], xa_d[gt * 128:(gt + 1) * 128, :], xl, reads=[xa_B[gt]], writes=[xl])
                    kb.ts("dve", yl.t[:, 0, :], yl.t[:, 0, :], cAB.t[:, gt, 0:1], None, ALU.mult, None, [yl, cAB], [yl])
                    kb.stt("dve", yl.t[:, 0, :], yl.t[:, 1, :], cAB.t[:, gt, 1:2], yl.t[:, 0, :], ALU.mult, ALU.add, [yl, cAB], [yl])
                    kb.stt("dve", r.t[:, :], xl.t[:, :], ALPHA, yl.t[:, 0, :], ALU.mult, ALU.add, [xl, yl], [r])
                    layernorm(ps_, r, D, fg, fb, xl, xl.t[:, :], st, mv, tmp)
                    kb.dma("pool", dst_d[gt * 128:(gt + 1) * 128, :], xl.t[:, :], xl, reads=[xl], writes=[dst_B[gt]])
                kb.barrier()

        def attention():
            with contextlib.ExitStack() as ps_:
                Wq = load_w(ps_, "a_Wqkv", w_qkv_d, 3072)
                xin = kb.sb("a_xin", [128, 4, D], F32, ps_)
                xbf = [kb.sb("a_xbf%d" % i, [128, D], BF16, ps_) for i in range(2)]
                xT = kb.sb("a_xT", [128, 8, 512], BF16, ps_)
                qm = [kb.sb("a_qm%d" % i, [128, 8, 512], BF16, ps_) for i in range(2)]
                km = [kb.sb("a_km%d" % i, [128, 8, 512], BF16, ps_) for i in range(2)]
                vm = [kb.sb("a_vm%d" % i, [128, 4, D], BF16, ps_) for i in range(2)]
                pT = kb.ps("a_pT", [128, 1024], BF16, ps_)
                pp = [kb.ps("a_pp%d" % i, [128, 512], F32, ps_) for i in range(4)]
                ppi = 0
                for mt in range(8):
                    load_xT(xb_d, xb_B, mt, xin, xbf, pT, xT)
                    q_ = qm[mt % 2]
                    k_ = km[mt % 2]
                    v_ = vm[mt % 2]
                    for hp in range(8):
                        p = pp[ppi % 4]; ppi += 1
                        kb.mmg(p, p.t[:, :], [(Wq.t[:, k, hp * 128:(hp + 1) * 128], xT.t[:, k, :]) for k in range(8)], reads=[Wq, xT])
                        kb.op("act", lambda e, p=p, hp=hp, q_=q_: e.mul(out=q_.t[:, hp, :], in_=p.t[:, :], mul=0.125), reads=[p], writes=[q_])
                        p = pp[ppi % 4]; ppi += 1
                        kb.mmg(p, p.t[:, :], [(Wq.t[:, k, 1024 + hp * 128:1024 + (hp + 1) * 128], xT.t[:, k, :]) for k in range(8)], reads=[Wq, xT])
                        kb.copy("dve", k_.t[:, hp, :], p.t[:, :], [p], [k_])
                    for s in range(4):
                        for dh in range(2):
                            p = pp[ppi % 4]; ppi += 1
                            kb.mmg(p, p.t[:, :], [(xT.t[:, k, s * 128:(s + 1) * 128], Wq.t[:, k, 2048 + dh * 512:2048 + (dh + 1) * 512]) for k in range(8)],
                                   reads=[Wq, xT])
                            kb.copy("dve" if dh == 0 else "act", v_.t[:, s, dh * 512:(dh + 1) * 512], p.t[:, :], [p], [v_])
                    cs = slice(mt * 512, (mt + 1) * 512)
                    kb.dma("pool", qT_d.rearrange("h p t -> p h t")[:, :, cs], q_.t[:, :, :], q_, reads=[q_], writes=[q_B[mt]])
                    kb.dma("pool", kT_d.rearrange("h p t -> p h t")[:, :, cs], k_.t[:, :, :], k_, reads=[k_], writes=[k_B[mt]])
                    kb.dma("pool", v_d[mt * 512:(mt + 1) * 512, :].rearrange("(s p) d -> p s d", p=128), v_.t[:, :, :], v_, reads=[v_], writes=[v_B[mt]])
                kb.barrier()

            with contextlib.ExitStack() as ps_:
                oT = kb.sb("a_oT", [128, 8, T], BF16, ps_)
                masks = kb.sb("a_mask", [128, 4, 512], BF16, ps_)
                negU = kb.sb("a_negU", [128, 128], BF16, ps_)
                negO = kb.sb("a_negO", [128, 128], BF16, ps_)
                zer = kb.sb("a_zero", [128, 128], BF16, ps_)
                kb.op("pool", lambda e: e.memset(masks.t[:, :, :], 1.0), writes=[masks])
                for m in range(4):
                    kb.op("pool", lambda e, m=m: e.affine_select(out=masks.t[:, m, :], in_=masks.t[:, m, :], pattern=[[1, 512]],
                                                                compare_op=ALU.is_gt, fill=0.0, base=-128 * m, channel_multiplier=-1),
                          reads=[masks], writes=[masks])
                kb.op("pool", lambda e: e.memset(negU.t[:, :], -1.0), writes=[negU])
                kb.op("pool", lambda e: e.affine_select(out=negU.t[:, :], in_=negU.t[:, :], pattern=[[-1, 128]],
                                                        compare_op=ALU.is_ge, fill=0.0, base=0, channel_multiplier=1),
                      reads=[negU], writes=[negU])
                kb.op("pool", lambda e: e.memset(negO.t[:, :], -1.0), writes=[negO])
                kb.op("pool", lambda e: e.memset(zer.t[:, :], 0.0), writes=[zer])
                with contextlib.ExitStack() as ps2:
                    qh = [kb.sb("a_qh%d" % i, [128, T], BF16, ps2) for i in range(2)]
                    kh = [kb.sb("a_kh%d" % i, [128, T], BF16, ps2) for i in range(2)]
                    vh = [kb.sb("a_vh%d" % i, [128, NT, 128], BF16, ps2) for i in range(2)]
                    ex = [[kb.sb("a_ex", [128, 512], F32, ps2) for _ in range(2)] for c in range(2)]
                    lnu = [[kb.sb("a_lnu", [128, 512], BF16, ps2) for _ in range(3)] for c in range(2)]
                    Sb = [[kb.sb("a_S", [128, 512], BF16, ps2) for _ in range(4)] for c in range(2)]
                    att = [[kb.sb("a_att", [128, 512], BF16, ps2) for _ in range(2)] for c in range(2)]
                    pz = [[kb.ps("a_pz", [128, 512], F32, ps2) for _ in range(2)] for c in range(2)]
                    pM = [kb.ps("a_pM", [128, 512], F32, ps2) for c in range(2)]
                    pc = [kb.ps("a_pc", [128, 512], F32, ps2) for c in range(2)]

                    def load_hp(hp):
                        i2 = hp % 2
                        kb.dma("sp", qh[i2].t[:, :], qT_d[hp], qh[i2], reads=q_B, writes=[qh[i2]])
                        kb.dma("sp", kh[i2].t[:, :], kT_d[hp], kh[i2], reads=k_B, writes=[kh[i2]])
                        kb.dma("sp", vh[i2].t[:, :, :], v_d.rearrange("(b p) d -> p b d", p=128)[:, :, hp * 128:(hp + 1) * 128], vh[i2],
                               reads=v_B, writes=[vh[i2]])

                    class It:
                        pass

                    nctr = [0, 0]

                    def make_items(hp, c):
                        items = []
                        for i in range(8):
                            for b in range(4 * i + 3, -1, -1):
                                it = It()
                                it.c = c
                                it.hp = hp
                                it.i = i
                                it.b = b
                                it.first = (b == 4 * i + 3)
                                it.last = (b == 0)
                                it.m = b - 4 * i
                                it.qlo = 128 * it.m if it.m > 0 else 0
                                it.n = nctr[c]
                                nctr[c] += 1
                                items.append(it)
                        return items

                    def stageA(it):
                        c = it.c
                        q_ = qh[it.hp % 2]
                        k_ = kh[it.hp % 2]
                        pr = slice(c * 64, (c + 1) * 64)
                        qlo = it.qlo
                        n = 512 - qlo
                        qs = slice(it.i * 512 + qlo, (it.i + 1) * 512)
                        ks = slice(it.b * 128, (it.b + 1) * 128)
                        z = pz[c][it.n % 2]
                        e_ = ex[c][it.n % 2]
                        l_ = lnu[c][it.n % 3]
                        kb.mmg(z, z.t[:, 0:n], [(k_.t[pr, ks], q_.t[pr, qs])], reads=[k_, q_])
                        kb.act(e_.t[:, 0:n], z.t[:, 0:n], AF.Exp, [z], [e_])
                        kb.act(l_.t[:, 0:n], e_.t[:, 0:n], AF.Ln, [e_], [l_], bias=1.0, scale=1.0)
                        if it.m >= 0:
                            kb.tt("pool", l_.t[:, 0:n], l_.t[:, 0:n], masks.t[:, it.m, qlo:512], ALU.mult, [l_, masks], [l_])
                        if not it.last:
                            Sn = Sb[c][it.n % 4]
                            if it.first:
                                if qlo > 0:
                                    kb.op("dve", lambda e: e.memset(Sn.t[:, 0:qlo], 0.0), writes=[Sn])
                                kb.copy("dve", Sn.t[:, qlo:512], l_.t[:, 0:n], [l_], [Sn])
                            else:
                                Sp = Sb[c][(it.n - 1) % 4]
                                if qlo > 0:
                                    kb.copy("dve", Sn.t[:, 0:qlo], Sp.t[:, 0:qlo], [Sp], [Sn])
                                kb.tt("dve", Sn.t[:, qlo:512], Sp.t[:, qlo:512], l_.t[:, 0:n], ALU.add, [Sp, l_], [Sn])

                    def stageB(it):
                        c = it.c
                        q_ = qh[it.hp % 2]
                        k_ = kh[it.hp % 2]
                        pr = slice(c * 64, (c + 1) * 64)
                        qlo = it.qlo
                        n = 512 - qlo
                        qs = slice(it.i * 512 + qlo, (it.i + 1) * 512)
                        ks = slice(it.b * 128, (it.b + 1) * 128)
                        l_ = lnu[c][it.n % 3]
                        a_ = att[c][it.n % 2]
                        M = pM[c]
                        prs = [(k_.t[pr, ks], q_.t[pr, qs]), (negU.t[:, :], l_.t[:, 0:n])]
                        rds = [k_, q_, negU, l_]
                        if not it.first:
                            Sp = Sb[c][(it.n - 1) % 4]
                            prs.append((negO.t[:, :], Sp.t[:, qlo:512]))
                            rds.append(Sp)
                        kb.mmg(M, M.t[:, 0:n], prs, reads=rds)
                        kb.act(a_.t[:, 0:n], M.t[:, 0:n], AF.Exp, [M], [a_])
                        if it.m >= 0:
                            kb.tt("pool", a_.t[:, 0:n], a_.t[:, 0:n], masks.t[:, it.m, qlo:512], ALU.mult, [a_, masks], [a_])

                    def stageC(it):
                        c = it.c
                        v_ = vh[it.hp % 2]
                        pr = slice(c * 64, (c + 1) * 64)
                        qlo = it.qlo
                        n = 512 - qlo
                        a_ = att[c][it.n % 2]
                        pcb = pc[c]
                        if it.first:
                            kb.op("pe", lambda e: e.matmul(pcb.t[:, :], lhsT=zer.t[:, :], rhs=masks.t[:, 0, :], start=True, stop=False),
                                  reads=[zer, masks], writes=[pcb], mark=False)
                        kb.op("pe", lambda e: e.matmul(pcb.t[:, qlo:512], lhsT=v_.t[:, it.b, :], rhs=a_.t[:, 0:n], start=False, stop=it.last),
                              reads=[v_, a_], writes=[pcb], mark=True)
                        if it.last:
                            kb.copy("dve", oT.t[pr, it.hp, it.i * 512:(it.i + 1) * 512], pcb.t[pr, :], [pcb], [oT])

                    load_hp(0)
                    for hp in range(8):
                        if hp + 1 < 8:
                            load_hp(hp + 1)
                        l0 = make_items(hp, 0)
                        l1 = make_items(hp, 1)
                        L = []
                        for a, b_ in zip(l0, l1):
                            L.append(a)
                            L.append(b_)
                        for g in range(len(L) + 4):
                            if g < len(L):
                                stageA(L[g])
                            if 0 <= g - 2 < len(L):
                                stageB(L[g - 2])
                            if 0 <= g - 4 < len(L):
                                stageC(L[g - 4])
                    kb.barrier()
                with contextlib.ExitStack() as ps3:
                    P = post_mixer_allocs(ps3, 1)
                    Wo = load_w(ps3, "a_Wo", w_out1_d, 1024)
                    xres = [kb.sb("a_xres%d" % i, [128, D], F32, ps3) for i in range(2)]
                    pso = [kb.ps("a_pso%d" % i, [128, 512], F32, ps3) for i in range(2)]
                    for gt in range(NT):
                        xr = xres[gt % 2]
                        kb.dma("sp", xr.t[:, :], xb_d[gt * 128:(gt + 1) * 128, :], xr, reads=[xb_B[gt]], writes=[xr])
                        post_mixer(ps3, 1, gt, xr.t[:, :], xr, pso, oT, slice(gt * 128, (gt + 1) * 128), Wo, P)
                    kb.barrier()

        moe_fn = moe_sparse if SPARSE else moe
        mixer0()
        if stop_after != "m0":
            moe_fn(0, xb_d, xb_B)
            if stop_after != "moe0":
                attention()
                if stop_after != "attn":
                    moe_fn(1, out_d, out_B)
        kb.barrier()

    es.close()
    return nc


_NC_CACHE = {}


def kernel(x, even_w_in, even_conv_w, even_sgu_ln_g, even_sgu_ln_b, even_sgu_w_s, even_sgu_b_s, even_w_out,
           odd_w_qkv, odd_w_out, mix_ln_g, mix_ln_b, moe_w_group, moe_b_group, moe_w_router, moe_b_router,
           moe_w1, moe_w3, moe_w2, ffn_ln_g, ffn_ln_b):
    f = lambda a: np.ascontiguousarray(np.asarray(a, dtype=np.float32))
    convw = f(np.asarray(even_conv_w)[0].reshape(3, 4, 128).transpose(2, 1, 0).reshape(128, 12))
    wsT = f(np.asarray(even_sgu_w_s)[0].transpose(0, 2, 1))
    bs = f(np.asarray(even_sgu_b_s)[0].reshape(1, 1024))
    wr = f(np.concatenate([np.asarray(moe_w_group),
                           np.asarray(moe_w_router).transpose(0, 2, 1, 3).reshape(2, D, 16)], axis=2))
    br = f(np.concatenate([np.asarray(moe_b_group), np.asarray(moe_b_router).reshape(2, 16)], axis=1))
    shared = {
        "w_in": f(even_w_in[0]), "convw": convw, "sgu_g": f(even_sgu_ln_g), "sgu_b": f(even_sgu_ln_b),
        "wsT": wsT, "bs": bs, "w_out0": f(even_w_out[0]), "w_qkv": f(odd_w_qkv[0]), "w_out1": f(odd_w_out[0]),
        "mix_g": f(mix_ln_g), "mix_b": f(mix_ln_b), "wr": wr, "br": br,
        "w1": f(np.asarray(moe_w1).reshape(2, 16, D, 512)), "w3": f(np.asarray(moe_w3).reshape(2, 16, D, 512)),
        "w2": f(np.asarray(moe_w2).reshape(2, 16, 512, D)), "ffn_g": f(ffn_ln_g), "ffn_b": f(ffn_ln_b),
    }
    xs = f(x)
    if "nc" not in _NC_CACHE:
        _NC_CACHE["nc"] = build()
    nc = _NC_CACHE["nc"]
    in_maps = [dict(shared, x=xs[c]) for c in range(NCORES)]
    res = run_bass_kernel_spmd(nc, in_maps, core_ids=list(range(NCORES)))
    return np.stack([res.results[c]["out"] for c in range(NCORES)], axis=0)
```

```python
import contextlib
import numpy as np
import concourse.bass as bass
import concourse.mybir as mybir
from concourse.bass_utils import run_bass_kernel_spmd

F32 = mybir.dt.float32
BF16 = mybir.dt.bfloat16
I32 = mybir.dt.int32
OOB = 1 << 20
AF = mybir.ActivationFunctionType
ALU = mybir.AluOpType

T = 4096
D = 1024
NT = 32
ALPHA = float(4 ** 0.25)
EPS = 1e-5
NCORES = 8
SPARSE = True


class B:
    __slots__ = ("t", "w", "r", "dsem", "dcnt")

    def __init__(self, t=None):
        self.t = t
        self.w = None
        self.r = {}
        self.dsem = None
        self.dcnt = 0


class Eng:
    pass


class KB:
    def __init__(self):
        self.nc = bass.Bass("TRN2", target_bir_lowering=False)
        self.es = contextlib.ExitStack()
        self.nsem = 0
        self.dma_bufs = []
        self.rstack = []
        self.bregs = {}
        self.free_sems = []

    def sem(self, name):
        self.nsem += 1
        return self.es.enter_context(self.nc.semaphore(name))

    def sb(self, name, shape, dt, stack=None):
        st = stack if stack is not None else self.es
        self.nt = getattr(self, "nt", 0) + 1
        return B(st.enter_context(self.nc.sbuf_tensor("%s_%d" % (name, self.nt), shape, dt)))

    def ps(self, name, shape, dt, stack=None):
        st = stack if stack is not None else self.es
        self.nt = getattr(self, "nt", 0) + 1
        return B(st.enter_context(self.nc.psum_tensor("%s_%d" % (name, self.nt), shape, dt)))

    def start(self):
        nc = self.nc
        self.E = {}
        for name, eng in (("pe", nc.tensor), ("act", nc.scalar), ("dve", nc.vector),
                          ("pool", nc.gpsimd), ("sp", nc.sync)):
            e = Eng()
            e.name = name
            e.eng = eng
            e.sem = self.sem("s_" + name)
            e.count = 0
            e.seen = {}
            self.E[name] = e
        ET = mybir.EngineType
        self.etmap = {ET.PE: "pe", ET.Activation: "act", ET.DVE: "dve", ET.Pool: "pool", ET.SP: "sp"}
        self.mregs = nc.alloc_registers("mr", [ET.PE, ET.Activation, ET.DVE, ET.Pool, ET.SP])

    def _waits(self, E, reads, writes):
        need = {}

        def acc(tok):
            k = id(tok[0])
            if k not in need or need[k][1] < tok[1]:
                need[k] = tok

        for b in reads:
            if b.w is not None:
                acc(b.w)
        for b in writes:
            if b.w is not None:
                acc(b.w)
            for tok in b.r.values():
                acc(tok)
        for k, (sem, val) in need.items():
            if E.name == "pe" and sem is E.sem:
                continue
            if E.seen.get(k, 0) >= val:
                continue
            E.eng.wait_ge(sem, val)
            E.seen[k] = val

    def _commit(self, tok, reads, writes):
        k = id(tok[0])
        for b in reads:
            b.r[k] = tok
        for b in writes:
            b.w = tok
            b.r = {}

    def op(self, en, fn, reads=(), writes=(), mark=True):
        E = self.E[en]
        self._waits(E, reads, writes)
        inst = fn(E.eng)
        if mark:
            E.count += 1
            inst.then_inc(E.sem, 1)
            tok = (E.sem, E.count)
        else:
            tok = (E.sem, E.count + 1)
        self._commit(tok, reads, writes)
        return inst

    def dma(self, q, out_ap, in_ap, sbufB, reads=(), writes=()):
        E = self.E[q]
        self._waits(E, reads, writes)
        self._get_dsem(sbufB)
        for rd in self.rstack:
            rec = rd[q].setdefault(id(sbufB.dsem), [sbufB.dsem, sbufB.dcnt, 0])
            rec[2] += 16
        sbufB.dcnt += 16
        E.eng.dma_start(out=out_ap, in_=in_ap).then_inc(sbufB.dsem, 16)
        tok = (sbufB.dsem, sbufB.dcnt)
        self._commit(tok, reads, writes)

    def idma(self, out_ap, in_ap, idx_ap, scatter, bound, sbufB, reads=(), writes=()):
        E = self.E["pool"]
        self._waits(E, reads, writes)
        self._get_dsem(sbufB)
        for rd in self.rstack:
            rec = rd["pool"].setdefault(id(sbufB.dsem), [sbufB.dsem, sbufB.dcnt, 0])
            rec[2] += 16
        sbufB.dcnt += 16
        off = bass.IndirectOffsetOnAxis(ap=idx_ap, axis=0)
        if bound not in self.bregs:
            r = self.es.enter_context(E.eng.register("rb%d" % bound))
            E.eng.reg_mov(r, bound)
            self.bregs[bound] = r
        bound = self.bregs[bound]
        if scatter:
            E.eng.indirect_dma_start(out=out_ap, out_offset=off, in_=in_ap, in_offset=None,
                                     bounds_check=bound, oob_is_err=False).then_inc(sbufB.dsem, 16)
        else:
            E.eng.indirect_dma_start(out=out_ap, out_offset=None, in_=in_ap, in_offset=off,
                                     bounds_check=bound, oob_is_err=False).then_inc(sbufB.dsem, 16)
        tok = (sbufB.dsem, sbufB.dcnt)
        self._commit(tok, reads, writes)

    def region(self, thr, body):
        engs = list(self.E.values())
        snap = {E.name: dict(E.seen) for E in engs}
        before = {E.name: E.count for E in engs}
        rd = {E.name: {} for E in engs}
        self.rstack.append(rd)
        with self.nc.If_cmp(self.mregs, thr, "IS_LT"):
            body()
        self.rstack.pop()
        with self.nc.Else():
            for E in engs:
                nm = E.count - before[E.name]
                if nm > 0:
                    E.eng.wait_ge(E.sem, before[E.name])
                    E.eng.sem_inc(E.sem, nm)
                for (sem, bt, add) in rd[E.name].values():
                    E.eng.wait_ge(sem, bt)
                    E.eng.sem_inc(sem, add)
        for E in engs:
            E.seen = snap[E.name]

    def _get_dsem(self, b):
        if b.dsem is None:
            if self.free_sems:
                b.dsem, b.dcnt = self.free_sems.pop()
            else:
                b.dsem = self.sem("d%d" % self.nsem)
                b.dcnt = 0
            self.dma_bufs.append(b)

    def release_sems(self):
        for b in self.dma_bufs:
            if b.dsem is not None:
                self.free_sems.append((b.dsem, b.dcnt))
                b.dsem = None
        self.dma_bufs = []

    def barrier(self):
        toks = []
        for e in self.E.values():
            if e.count > 0:
                toks.append((e.sem, e.count))
        for b in self.dma_bufs:
            if b.dcnt > 0:
                toks.append((b.dsem, b.dcnt))
        for E in self.E.values():
            for (sem, val) in toks:
                if sem is E.sem:
                    continue
                k = id(sem)
                if E.seen.get(k, 0) >= val:
                    continue
                E.eng.wait_ge(sem, val)
                E.seen[k] = val

    def mmg(self, outB, out_ap, pairs, reads):
        n = len(pairs)
        for i, (l, r) in enumerate(pairs):
            self.op("pe", lambda e, l=l, r=r, i=i: e.matmul(out_ap, lhsT=l, rhs=r, start=(i == 0), stop=(i == n - 1)),
                    reads=reads, writes=[outB], mark=(i == n - 1))

    def act(self, out_ap, in_ap, func, reads, writes, bias=None, scale=None):
        kw = {}
        if bias is not None:
            kw["bias"] = bias
        if scale is not None:
            kw["scale"] = scale
        self.op("act", lambda e: e.activation(out=out_ap, in_=in_ap, func=func, **kw), reads=reads, writes=writes)

    def copy(self, en, out_ap, in_ap, reads, writes):
        if en == "act":
            self.op("act", lambda e: e.copy(out=out_ap, in_=in_ap), reads=reads, writes=writes)
        else:
            self.op(en, lambda e: e.tensor_copy(out=out_ap, in_=in_ap), reads=reads, writes=writes)

    def tt(self, en, out_ap, a_ap, b_ap, op, reads, writes):
        self.op(en, lambda e: e.tensor_tensor(out=out_ap, in0=a_ap, in1=b_ap, op=op), reads=reads, writes=writes)

    def ts(self, en, out_ap, in_ap, s1, s2, op0, op1, reads, writes):
        if op1 is None:
            self.op(en, lambda e: e.tensor_scalar(out=out_ap, in0=in_ap, scalar1=s1, scalar2=None, op0=op0),
                    reads=reads, writes=writes)
        else:
            self.op(en, lambda e: e.tensor_scalar(out=out_ap, in0=in_ap, scalar1=s1, scalar2=s2, op0=op0, op1=op1),
                    reads=reads, writes=writes)

    def stt(self, en, out_ap, in0, scalar, in1, op0, op1, reads, writes):
        self.op(en, lambda e: e.scalar_tensor_tensor(out=out_ap, in0=in0, scalar=scalar, in1=in1, op0=op0, op1=op1),
                reads=reads, writes=writes)


def build(stop_after=None, dbg=False):
    kb = KB()
    dk = {"kind": "ExternalOutput"} if dbg else {}
    nc = kb.nc

    def din(name, shape):
        return nc.dram_tensor(name, shape, F32, kind="ExternalInput").ap()

    x_d = din("x", [T, D])
    w_in_d = din("w_in", [D, 2560])
    convw_d = din("convw", [128, 12])
    sgu_g_d = din("sgu_g", [1, 512])
    sgu_b_d = din("sgu_b", [1, 512])
    wsT_d = din("wsT", [8, 128, 128])
    bs_d = din("bs", [1, 1024])
    w_out0_d = din("w_out0", [D, D])
    w_qkv_d = din("w_qkv", [D, 3072])
    w_out1_d = din("w_out1", [D, D])
    mix_g_d = din("mix_g", [2, D])
    mix_b_d = din("mix_b", [2, D])
    wr_d = din("wr", [2, D, 20])
    br_d = din("br", [2, 20])
    w1_d = din("w1", [2, 16, D, 512])
    w3_d = din("w3", [2, 16, D, 512])
    w2_d = din("w2", [2, 16, 512, D])
    ffn_g_d = din("ffn_g", [2, D])
    ffn_b_d = din("ffn_b", [2, D])
    out_d = nc.dram_tensor("out", [T, D], F32, kind="ExternalOutput").ap()

    xa_d = nc.dram_tensor("xa_s", [T, D], F32, **dk).ap()
    xb_d = nc.dram_tensor("xb_s", [T, D], F32, **dk).ap()
    xT_d = nc.dram_tensor("xT_s", [128, 8, T], BF16, **dk).ap()
    qT_d = nc.dram_tensor("qT_s", [8, 128, T], BF16, **dk).ap()
    kT_d = nc.dram_tensor("kT_s", [8, 128, T], BF16, **dk).ap()
    v_d = nc.dram_tensor("v_s", [T, D], BF16, **dk).ap()

    x1b_d = nc.dram_tensor("x1b_s", [T, D], BF16, **dk).ap()
    list_d = nc.dram_tensor("list_s", [16 * 4096, 2], I32, **dk).ap()
    y2_d = nc.dram_tensor("y2_s", [2 * T, D], F32, **dk).ap()
    x1bd_B = [B() for _ in range(NT)]
    xa_B = [B() for _ in range(NT)]
    xb_B = [B() for _ in range(NT)]
    xT_B = [B() for _ in range(8)]
    q_B = [B() for _ in range(8)]
    k_B = [B() for _ in range(8)]
    v_B = [B() for _ in range(8)]
    out_B = [B() for _ in range(NT)]

    kb.start()
    es = kb.es

    ident = kb.sb("ident", [128, 128], BF16)
    c_all = kb.sb("c_all", [128, NT, 16], F32)
    cAB = kb.sb("cAB", [128, NT, 2], F32)
    off_t = kb.sb("off_t", [128, 16], F32)
    ebase1 = kb.sb("ebase1", [128, 16], F32)
    Lstr = kb.sb("Lstr", [128, 128], BF16)
    onesb = kb.sb("onesb", [128, 128], BF16)
    cnt_i = kb.sb("cnt_i", [128, 16], I32)
    tl2 = kb.sb("tl2", [128, NT, 2, 2], I32)
    oobt = kb.sb("oobt", [128, 1024], I32)

    block = es.enter_context(nc.Block())

    @block.sync
    def _(sync):
        kb.op("pool", lambda e: e.memset(ident.t[:], 0.0), writes=[ident])
        kb.op("pool", lambda e: e.affine_select(out=ident.t[:], in_=ident.t[:], pattern=[[-1, 128]],
                                                compare_op=ALU.not_equal, fill=1.0, base=0, channel_multiplier=1),
              reads=[ident], writes=[ident])

        kb.op("pool", lambda e: e.memset(onesb.t[:], 1.0), writes=[onesb])
        kb.op("pool", lambda e: e.memset(Lstr.t[:], 1.0), writes=[Lstr])
        kb.op("pool", lambda e: e.affine_select(out=Lstr.t[:], in_=Lstr.t[:], pattern=[[1, 128]],
                                                compare_op=ALU.is_gt, fill=0.0, base=0, channel_multiplier=-1),
              reads=[Lstr], writes=[Lstr])
        kb.op("pool", lambda e: e.iota(ebase1.t[:], [[4096, 16]], base=1, channel_multiplier=0,
                                       allow_small_or_imprecise_dtypes=True), writes=[ebase1])
        kb.op("pool", lambda e: e.memset(oobt.t[:], OOB), writes=[oobt])
        for r_ in range(2):
            kb.op("pool", lambda e, r_=r_: e.iota(tl2.t[:, :, r_, 0], [[128, NT]], base=0, channel_multiplier=1), writes=[tl2])
            kb.op("pool", lambda e, r_=r_: e.iota(tl2.t[:, :, r_, 1], [[256, NT]], base=r_, channel_multiplier=2), writes=[tl2])

        def layernorm(ps_, r, D_, g_bc, b_bc, outB, out_ap, stats, mv, tmp, mul_eng="pool"):
            nch = D_ // 512
            for c in range(nch):
                kb.op("dve", lambda e, c=c: e.bn_stats(out=stats.t[:, c * 6:(c + 1) * 6], in_=r.t[:, c * 512:(c + 1) * 512]),
                      reads=[r], writes=[stats])
            kb.op("dve", lambda e: e.bn_aggr(out=mv.t[:, 0:2], in_=stats.t[:, 0:nch * 6]), reads=[stats], writes=[mv])
            kb.act(tmp.t[:, 0:1], mv.t[:, 1:2], AF.Ln, [mv], [tmp], bias=EPS, scale=1.0)
            kb.act(tmp.t[:, 1:2], tmp.t[:, 0:1], AF.Exp, [tmp], [tmp], scale=-0.5)
            if D_ == D:
                kb.ts("dve", tmp.t[:, 0:1], mv.t[:, 0:1], -1.0, tmp.t[:, 1:2], ALU.mult, ALU.mult, [mv, tmp], [tmp])
                kb.act(r.t[:, 0:D_], r.t[:, 0:D_], AF.Identity, [r, tmp], [r], bias=tmp.t[:, 0:1], scale=tmp.t[:, 1:2])
            else:
                kb.ts("dve", r.t[:, 0:D_], r.t[:, 0:D_], mv.t[:, 0:1], tmp.t[:, 1:2], ALU.subtract, ALU.mult, [r, mv, tmp], [r])
            kb.tt(mul_eng, r.t[:, 0:D_], r.t[:, 0:D_], g_bc.t[:, 0:D_], ALU.mult, [r, g_bc], [r])
            kb.tt(mul_eng, out_ap, r.t[:, 0:D_], b_bc.t[:, 0:D_], ALU.add, [r, b_bc], [outB])

        def bcast_load(dst, src_row_ap):
            kb.dma("sp", dst.t[:], src_row_ap.partition_broadcast(128), dst, writes=[dst])

        def post_mixer(ps_, lyr, gt, x_res_ap, x_resB, pso, yT, yT_cols, Wo, P):
            for dh in range(2):
                kb.mmg(pso[dh], pso[dh].t[:, :],
                       [(yT.t[:, k, yT_cols], Wo.t[:, k, dh * 512:(dh + 1) * 512]) for k in range(8)],
                       reads=[yT, Wo])
            r = P["r"][gt % 2]
            for dh in range(2):
                kb.stt("dve", r.t[:, dh * 512:(dh + 1) * 512], x_res_ap[:, dh * 512:(dh + 1) * 512], ALPHA,
                       pso[dh].t[:, :], ALU.mult, ALU.add, [x_resB, pso[dh]], [r])
            x1 = P["x1"][gt % 2]
            layernorm(ps_, r, D, P["mg"], P["mb"], x1, x1.t[:, :], P["stats"], P["mv"], P["tmp"])
            kb.dma("pool", xa_d[gt * 128:(gt + 1) * 128, :], x1.t[:, :], x1, reads=[x1], writes=[xa_B[gt]])
            x1b = P["x1b"][gt % 2]
            kb.copy("pool", x1b.t[:, :], x1.t[:, :], [x1], [x1b])
            kb.dma("pool", x1b_d[gt * 128:(gt + 1) * 128, :], x1b.t[:, :], x1b, reads=[x1b], writes=[x1bd_B[gt]])
            pT = P["pT"]
            for k in range(8):
                kb.op("pe", lambda e, k=k: e.transpose(pT.t[:, k * 128:(k + 1) * 128], x1b.t[:, k * 128:(k + 1) * 128], ident.t[:]),
                      reads=[x1b, ident], writes=[pT], mark=(k == 7))
            x1T = P["x1T"][gt % 2]
            s = gt % 4
            kb.copy("act", x1T.t[:, :, :], pT.t[:, :].rearrange("p (k t) -> p k t", k=8), [pT], [x1T])
            prt = P["prt"]
            kb.mmg(prt, prt.t[:, 0:20], [(x1T.t[:, k, :], P["Wr"].t[:, k, :]) for k in range(8)],
                   reads=[x1T, P["Wr"]])
            rt = P["rt"]
            lg = rt.t[:, 0:20]
            kb.tt("dve", lg, prt.t[:, 0:20], P["brb"].t[:, :], ALU.add, [prt, P["brb"]], [rt])
            R_ = [rt]
            kb.op("dve", lambda e: e.reduce_max(out=rt.t[:, 20:21], in_=rt.t[:, 0:4], axis=mybir.AxisListType.X), reads=R_, writes=R_)
            kb.ts("dve", rt.t[:, 24:28], rt.t[:, 0:4], rt.t[:, 20:21], None, ALU.is_equal, None, R_, R_)
            kb.ts("dve", rt.t[:, 21:22], rt.t[:, 20:21], -1.0, None, ALU.mult, None, R_, R_)
            kb.act(rt.t[:, 28:32], rt.t[:, 0:4], AF.Exp, R_, R_, bias=rt.t[:, 21:22], scale=1.0)
            kb.op("dve", lambda e: e.reduce_sum(out=rt.t[:, 22:23], in_=rt.t[:, 28:32], axis=mybir.AxisListType.X), reads=R_, writes=R_)
            kb.ts("dve", rt.t[:, 32:36], rt.t[:, 4:8], rt.t[:, 24:25], None, ALU.mult, None, R_, R_)
            for g in range(1, 4):
                kb.stt("dve", rt.t[:, 32:36], rt.t[:, 4 + 4 * g:8 + 4 * g], rt.t[:, 24 + g:25 + g], rt.t[:, 32:36],
                       ALU.mult, ALU.add, R_, R_)
            kb.op("dve", lambda e: e.reduce_max(out=rt.t[:, 36:37], in_=rt.t[:, 32:36], axis=mybir.AxisListType.X), reads=R_, writes=R_)
            kb.ts("dve", rt.t[:, 40:44], rt.t[:, 32:36], rt.t[:, 36:37], None, ALU.is_equal, None, R_, R_)
            kb.ts("dve", rt.t[:, 37:38], rt.t[:, 36:37], -1.0, None, ALU.mult, None, R_, R_)
            kb.act(rt.t[:, 44:48], rt.t[:, 32:36], AF.Exp, R_, R_, bias=rt.t[:, 37:38], scale=1.0)
            kb.op("dve", lambda e: e.reduce_max(out=rt.t[:, 38:39], in_=rt.t[:, 44:48], axis=mybir.AxisListType.X), reads=R_, writes=R_)
            kb.tt("dve", rt.t[:, 48:52], rt.t[:, 44:48], rt.t[:, 40:44], ALU.mult, R_, R_)
            kb.tt("dve", rt.t[:, 48:52], rt.t[:, 44:48], rt.t[:, 48:52], ALU.subtract, R_, R_)
            kb.op("dve", lambda e: e.reduce_max(out=rt.t[:, 39:40], in_=rt.t[:, 48:52], axis=mybir.AxisListType.X), reads=R_, writes=R_)
            kb.ts("dve", rt.t[:, 52:56], rt.t[:, 44:48], rt.t[:, 39:40], None, ALU.is_ge, None, R_, R_)
            kb.tt("dve", rt.t[:, 52:56], rt.t[:, 52:56], rt.t[:, 44:48], ALU.mult, R_, R_)
            kb.tt("dve", rt.t[:, 56:57], rt.t[:, 38:39], rt.t[:, 39:40], ALU.add, R_, R_)
            kb.tt("dve", rt.t[:, 56:57], rt.t[:, 56:57], rt.t[:, 22:23], ALU.mult, R_, R_)
            kb.op("dve", lambda e: e.reciprocal(out=rt.t[:, 57:58], in_=rt.t[:, 56:57]), reads=R_, writes=R_)
            kb.ts("dve", rt.t[:, 60:64], rt.t[:, 24:28], rt.t[:, 57:58], None, ALU.mult, None, R_, R_)
            for g in range(4):
                kb.ts("dve", c_all.t[:, gt, 4 * g:4 * g + 4], rt.t[:, 52:56], rt.t[:, 60 + g:61 + g], None, ALU.mult, None,
                      R_, [c_all])
            if s == 3 and not SPARSE:
                mt = gt // 4
                kb.dma("pool", xT_d[:, :, mt * 512:(mt + 1) * 512], x1T.t[:, :, :], x1T, reads=[x1T], writes=[xT_B[mt]])
            if SPARSE:
                rt2 = P["rt2"]
                mkb = P["mkb"]
                idx2 = P["idx2"][gt % 2]
                Q_ = [rt2]
                cg = c_all.t[:, gt, :]
                kb.ts("dve", rt2.t[:, 0:16], cg, 0.0, None, ALU.is_gt, None, [c_all], Q_)
                kb.copy("dve", mkb.t[:, :], rt2.t[:, 0:16], Q_, [mkb])
                kb.mmg(prt, prt.t[:, 32:48], [(Lstr.t[:, :], mkb.t[:, :])], reads=[Lstr, mkb])
                kb.mmg(prt, prt.t[:, 48:64], [(onesb.t[:, :], mkb.t[:, :])], reads=[onesb, mkb])
                kb.tt("dve", rt2.t[:, 16:32], prt.t[:, 32:48], ebase1.t[:, :], ALU.add, [prt, ebase1], Q_)
                kb.tt("dve", rt2.t[:, 16:32], rt2.t[:, 16:32], off_t.t[:, :], ALU.add, Q_ + [off_t], Q_)
                kb.tt("dve", rt2.t[:, 16:32], rt2.t[:, 16:32], rt2.t[:, 0:16], ALU.mult, Q_, Q_)
                kb.tt("dve", off_t.t[:, :], off_t.t[:, :], prt.t[:, 48:64], ALU.add, [off_t, prt], [off_t])
                kb.op("dve", lambda e: e.reduce_max(out=rt2.t[:, 64:65], in_=rt2.t[:, 16:32], axis=mybir.AxisListType.X), reads=Q_, writes=Q_)
                kb.ts("dve", rt2.t[:, 32:48], rt2.t[:, 16:32], rt2.t[:, 64:65], None, ALU.is_equal, None, Q_, Q_)
                kb.tt("dve", rt2.t[:, 48:64], rt2.t[:, 32:48], cg, ALU.mult, Q_ + [c_all], Q_)
                kb.op("dve", lambda e: e.reduce_sum(out=cAB.t[:, gt, 0:1], in_=rt2.t[:, 48:64], axis=mybir.AxisListType.X), reads=Q_, writes=[cAB])
                kb.op("dve", lambda e: e.reduce_sum(out=rt2.t[:, 66:67], in_=cg, axis=mybir.AxisListType.X), reads=[c_all], writes=Q_)
                kb.tt("dve", cAB.t[:, gt, 1:2], rt2.t[:, 66:67], cAB.t[:, gt, 0:1], ALU.subtract, Q_ + [cAB], [cAB])
                kb.tt("dve", rt2.t[:, 48:64], rt2.t[:, 16:32], rt2.t[:, 32:48], ALU.mult, Q_, Q_)
                kb.tt("dve", rt2.t[:, 48:64], rt2.t[:, 16:32], rt2.t[:, 48:64], ALU.subtract, Q_, Q_)
                kb.op("dve", lambda e: e.reduce_max(out=rt2.t[:, 67:68], in_=rt2.t[:, 48:64], axis=mybir.AxisListType.X), reads=Q_, writes=Q_)
                kb.ts("dve", rt2.t[:, 68:69], rt2.t[:, 67:68], 0.0, float(OOB), ALU.is_equal, ALU.mult, Q_, Q_)
                kb.tt("dve", rt2.t[:, 67:68], rt2.t[:, 67:68], rt2.t[:, 68:69], ALU.add, Q_, Q_)
                kb.ts("dve", idx2.t[:, 0:1], rt2.t[:, 64:65], -1.0, None, ALU.add, None, Q_, [idx2])
                kb.ts("dve", idx2.t[:, 1:2], rt2.t[:, 67:68], -1.0, None, ALU.add, None, Q_, [idx2])
                for r_ in range(2):
                    kb.idma(list_d[:, :], tl2.t[:, gt, r_, :], idx2.t[:, r_:r_ + 1], True, 16 * 4096 - 1, idx2,
                            reads=[idx2, tl2, P["listB"]])
                if gt == NT - 1:
                    kb.ts("dve", cnt_i.t[:, :], off_t.t[:, :], -1.0, 4096.0, ALU.mult, ALU.add, [off_t], [cnt_i])

        def post_mixer_allocs(ps_, lyr):
            P = {}
            P["r"] = [kb.sb("pm_r%d" % i, [128, D], F32, ps_) for i in range(2)]
            P["x1"] = [kb.sb("pm_x1%d" % i, [128, D], F32, ps_) for i in range(2)]
            P["x1b"] = [kb.sb("pm_x1b", [128, D], BF16, ps_) for _ in range(2)]
            P["rt2"] = kb.sb("pm_rt2", [128, 128], F32, ps_)
            P["mkb"] = kb.sb("pm_mkb", [128, 16], BF16, ps_)
            P["idx2"] = [kb.sb("pm_idx2", [128, 2], I32, ps_) for _ in range(2)]
            P["listB"] = B()
            kb.op("dve", lambda e: e.memset(off_t.t[:], 0.0), writes=[off_t])
            kb.dma("pool", list_d.rearrange("(p a) c -> p (a c)", p=128), oobt.t[:, :], oobt, reads=[oobt], writes=[P["listB"]])
            P["x1T"] = [kb.sb("pm_x1T", [128, 8, 128], BF16, ps_) for _ in range(2)]
            P["stats"] = kb.sb("pm_stats", [128, 12], F32, ps_)
            P["mv"] = kb.sb("pm_mv", [128, 2], F32, ps_)
            P["tmp"] = kb.sb("pm_tmp", [128, 2], F32, ps_)
            P["rt"] = kb.sb("pm_rt", [128, 64], F32, ps_)
            P["mg"] = kb.sb("pm_mg", [128, D], F32, ps_)
            P["mb"] = kb.sb("pm_mb", [128, D], F32, ps_)
            P["brb"] = kb.sb("pm_brb", [128, 20], F32, ps_)
            P["Wr"] = kb.sb("pm_Wr", [128, 8, 20], BF16, ps_)
            P["pT"] = kb.ps("pm_pT", [128, 1024], BF16, ps_)
            P["prt"] = kb.ps("pm_prt", [128, 512], F32, ps_)
            bcast_load(P["mg"], mix_g_d[lyr:lyr + 1, :])
            bcast_load(P["mb"], mix_b_d[lyr:lyr + 1, :])
            bcast_load(P["brb"], br_d[lyr:lyr + 1, :])
            kb.dma("pool", P["Wr"].t[:, :, :], wr_d[lyr].rearrange("(k p) n -> p k n", p=128), P["Wr"], writes=[P["Wr"]])
            return P

        def load_w(ps_, name, src_ap, ncols):
            W = kb.sb(name, [128, 8, ncols], BF16, ps_)
            v = src_ap.rearrange("(k p) n -> p k n", p=128)
            for k in range(8):
                kb.dma("pool", W.t[:, k, :], v[:, k, :], W, writes=[W])
            return W

        def load_xT(xsrc_d, xsrc_B, mt, xin, xbf, pT, xT):
            rd = [xsrc_B[mt * 4 + s] for s in range(4)] if xsrc_B is not None else []
            kb.dma("sp", xin.t[:, :, :], xsrc_d[mt * 512:(mt + 1) * 512, :].rearrange("(s p) d -> p s d", p=128), xin,
                   reads=rd, writes=[xin])
            for s in range(4):
                xb = xbf[s % 2]
                kb.copy("pool", xb.t[:, :], xin.t[:, s, :], [xin], [xb])
                for k in range(8):
                    kb.op("pe", lambda e, k=k, xb=xb: e.transpose(pT.t[:, k * 128:(k + 1) * 128], xb.t[:, k * 128:(k + 1) * 128], ident.t[:]),
                          reads=[xb, ident], writes=[pT], mark=(k == 7))
                kb.copy("act", xT.t[:, :, s * 128:(s + 1) * 128], pT.t[:, :].rearrange("p (k t) -> p k t", k=8), [pT], [xT])

        def mixer0():
            with contextlib.ExitStack() as ps_:
                P = post_mixer_allocs(ps_, 0)
                Wi = load_w(ps_, "m0_Wi", w_in_d, 2560)
                Wo = load_w(ps_, "m0_Wo", w_out0_d, 1024)
                WsT = kb.sb("m0_WsT", [128, 8, 128], BF16, ps_)
                kb.dma("pool", WsT.t[:, :, :], wsT_d.rearrange("h s t -> s h t"), WsT, writes=[WsT])
                kb.op("pool", lambda e: e.memset(WsT.t[64:128, :, 0:64], 0.0), reads=[WsT], writes=[WsT])
                bsr = kb.sb("m0_bsr", [1, 1024], BF16, ps_)
                kb.dma("pool", bsr.t[:, :], bs_d[:, :], bsr, writes=[bsr])
                onesr = kb.sb("m0_ones", [1, 128], BF16, ps_)
                kb.op("pool", lambda e: e.memset(onesr.t[:, :], 1.0), writes=[onesr])
                cw = kb.sb("m0_cw", [128, 12], F32, ps_)
                kb.dma("sp", cw.t[:, :], convw_d[:, :], cw, writes=[cw])
                sg = kb.sb("m0_sg", [128, 512], F32, ps_)
                sbb = kb.sb("m0_sbb", [128, 512], F32, ps_)
                bcast_load(sg, sgu_g_d[0:1, :])
                bcast_load(sbb, sgu_b_d[0:1, :])
                xin = kb.sb("m0_xin", [128, 4, D], F32, ps_)
                xbf = [kb.sb("m0_xbf%d" % i, [128, D], BF16, ps_) for i in range(2)]
                xres = [kb.sb("m0_xres%d" % i, [128, D], F32, ps_) for i in range(2)]
                xT = [kb.sb("m0_xT", [128, 8, 512], BF16, ps_) for _ in range(2)]
                yT = [kb.sb("m0_yT", [128, 8, 512], BF16, ps_) for _ in range(2)]
                ub = kb.sb("m0_ub", [128, 4, 514], F32, ps_)
                Cs = kb.sb("m0_Cs", [128, 512], F32, ps_)
                acc = kb.sb("m0_acc", [128, 512], F32, ps_)
                zu = kb.sb("m0_zu", [128, 4, 512], F32, ps_)
                g1 = kb.sb("m0_g1", [128, 512], F32, ps_)
                g2 = kb.sb("m0_g2", [128, 512], F32, ps_)
                gv = kb.sb("m0_gv", [128, 512], F32, ps_)
                vt = kb.sb("m0_vt", [128, 4, 512], BF16, ps_)
                st2 = kb.sb("m0_st2", [128, 6], F32, ps_)
                mv2 = kb.sb("m0_mv2", [128, 2], F32, ps_)
                tmp2 = kb.sb("m0_tmp2", [128, 2], F32, ps_)
                pp = [kb.ps("m0_pp%d" % i, [128, 512], F32, ps_) for i in range(2)]
                psg = [kb.ps("m0_psg%d" % i, [128, 512], F32, ps_) for i in range(2)]
                pso = [kb.ps("m0_pso%d" % i, [128, 512], F32, ps_) for i in range(2)]
                pT = P["pT"]
                kb.op("pool", lambda e: e.memset(ub.t[:, :, :], 0.0), writes=[ub])
                ppi = [0]

                def proj_fm(c, xT):
                    p = pp[ppi[0] % 2]
                    ppi[0] += 1
                    kb.mmg(p, p.t[:, :], [(Wi.t[:, k, c * 128:(c + 1) * 128], xT.t[:, k, :]) for k in range(8)], reads=[Wi, xT])
                    return p

                def gelu(p, p_ap, outB, out_ap, n):
                    kb.act(g1.t[:, 0:n], p_ap, AF.Square, [p], [g1])
                    kb.ts("pool", g1.t[:, 0:n], g1.t[:, 0:n], 0.044715, 1.0, ALU.mult, ALU.add, [g1], [g1])
                    kb.tt("dve", g2.t[:, 0:n], g1.t[:, 0:n], p_ap, ALU.mult, [g1, p], [g2])
                    kb.act(g1.t[:, 0:n], g2.t[:, 0:n], AF.Exp, [g2], [g1], scale=-1.5957691216057308)
                    kb.ts("pool", g1.t[:, 0:n], g1.t[:, 0:n], 1.0, None, ALU.add, None, [g1], [g1])
                    kb.op("dve", lambda e: e.reciprocal(out=g2.t[:, 0:n], in_=g1.t[:, 0:n]), reads=[g1], writes=[g2])
                    kb.tt("dve", out_ap, g2.t[:, 0:n], p_ap, ALU.mult, [g2, p], [outB])

                def piece(mt, j):
                    xTb = xT[mt % 2]
                    yTb = yT[mt % 2]
                    u = ub
                    pC = proj_fm(4 + j, xTb)
                    kb.copy("act", Cs.t[:, :], pC.t[:, :], [pC], [Cs])
                    pH = proj_fm(8 + j, xTb)
                    kb.copy("dve", u.t[:, j, 0:2], u.t[:, j, 512:514], [u], [u])
                    kb.tt("dve", u.t[:, j, 2:514], Cs.t[:, :], pH.t[:, :], ALU.mult, [Cs, pH], [u])
                    kb.ts("dve", acc.t[:, :], u.t[:, j, 2:514], cw.t[:, j * 3 + 2:j * 3 + 3], None, ALU.mult, None, [u, cw], [acc])
                    kb.stt("dve", acc.t[:, :], u.t[:, j, 1:513], cw.t[:, j * 3 + 1:j * 3 + 2], acc.t[:, :], ALU.mult, ALU.add, [u, cw, acc], [acc])
                    kb.stt("dve", acc.t[:, :], u.t[:, j, 0:512], cw.t[:, j * 3:j * 3 + 1], acc.t[:, :], ALU.mult, ALU.add, [u, cw, acc], [acc])
                    pB = proj_fm(j, xTb)
                    kb.tt("dve", yTb.t[:, j, :], acc.t[:, :], pB.t[:, :], ALU.mult, [acc, pB], [yTb])
                    pZ = proj_fm(12 + j, xTb)
                    gelu(pZ, pZ.t[:, :], zu, zu.t[:, j, :], 512)
                    p = pp[ppi[0] % 2]
                    ppi[0] += 1
                    kb.mmg(p, p.t[:, :], [(xTb.t[:, k, j * 128:(j + 1) * 128], Wi.t[:, k, 2048:2560]) for k in range(8)], reads=[Wi, xTb])
                    gelu(p, p.t[:, :], gv, gv.t[:, :], 512)
                    layernorm(ps_, gv, 512, sg, sbb, vt, vt.t[:, j, :], st2, mv2, tmp2, mul_eng="pool")

                def sgu(mt):
                    yTb = yT[mt % 2]
                    for hp in range(4):
                        for j in range(2):
                            h = 2 * hp + j
                            pg = psg[j]
                            for s in range(4):
                                kb.op("pe", lambda e, s=s, pg=pg, h=h, hp=hp: e.matmul(pg.t[:, s * 128:(s + 1) * 128], lhsT=vt.t[:, s, hp * 128:(hp + 1) * 128],
                                                                         rhs=WsT.t[:, h, :], start=True, stop=False),
                                      reads=[vt, WsT], writes=[pg], mark=False)
                                kb.op("pe", lambda e, s=s, pg=pg, h=h: e.matmul(pg.t[:, s * 128:(s + 1) * 128], lhsT=onesr.t[0:1, :],
                                                                   rhs=bsr.t[0:1, h * 128:(h + 1) * 128], start=False, stop=True),
                                      reads=[onesr, bsr], writes=[pg], mark=(s == 3))
                            kb.tt("dve", yTb.t[j * 64:(j + 1) * 64, 4 + hp, :], zu.t[j * 64:(j + 1) * 64, hp, :], pg.t[j * 64:(j + 1) * 64, :],
                                  ALU.mult, [zu, pg], [yTb])

                def back(mt, s):
                    gt = mt * 4 + s
                    xr = xres[gt % 2]
                    kb.dma("sp", xr.t[:, :], x_d[gt * 128:(gt + 1) * 128, :], xr, writes=[xr])
                    post_mixer(ps_, 0, gt, xr.t[:, :], xr, pso, yT[mt % 2], slice(s * 128, (s + 1) * 128), Wo, P)

                load_xT(x_d, None, 0, xin, xbf, pT, xT[0])
                for j in range(4):
                    piece(0, j)
                sgu(0)
                for mt in range(8):
                    if mt + 1 < 8:
                        load_xT(x_d, None, mt + 1, xin, xbf, pT, xT[(mt + 1) % 2])
                    for j in range(4):
                        if mt + 1 < 8:
                            piece(mt + 1, j)
                        back(mt, j)
                    if mt + 1 < 8:
                        sgu(mt + 1)
                kb.barrier()
                kb.release_sems()

        def moe(lyr, dst_d, dst_B):
            with contextlib.ExitStack() as ps_:
                fg = kb.sb("f_g", [128, D], F32, ps_)
                fb = kb.sb("f_b", [128, D], F32, ps_)
                bcast_load(fg, ffn_g_d[lyr:lyr + 1, :])
                bcast_load(fb, ffn_b_d[lyr:lyr + 1, :])
                xTh = kb.sb("f_xTh", [128, 8, 2048], BF16, ps_)
                yacc = kb.sb("f_yacc", [128, 16, D], F32, ps_)
                w1s = [kb.sb("f_w1_%d" % i, [128, 8, 512], BF16, ps_) for i in range(2)]
                w3s = [kb.sb("f_w3_%d" % i, [128, 8, 512], BF16, ps_) for i in range(2)]
                w2s = [kb.sb("f_w2_%d" % i, [128, 4, D], BF16, ps_) for i in range(2)]
                hid = [kb.sb("f_hid%d" % i, [128, 4, 512], BF16, ps_) for i in range(2)]
                sl = [kb.sb("f_sl%d" % i, [128, 512], BF16, ps_) for i in range(2)]
                x1l = [kb.sb("f_x1l%d" % i, [128, D], F32, ps_) for i in range(2)]
                rr = [kb.sb("f_r%d" % i, [128, D], F32, ps_) for i in range(2)]
                st = kb.sb("f_st", [128, 12], F32, ps_)
                mv = kb.sb("f_mv", [128, 2], F32, ps_)
                tmp = kb.sb("f_tmp", [128, 2], F32, ps_)
                ph1 = [kb.ps("f_ph1_%d" % i, [128, 512], F32, ps_) for i in range(2)]
                ph3 = [kb.ps("f_ph3_%d" % i, [128, 512], F32, ps_) for i in range(2)]
                py = [kb.ps("f_py%d" % i, [128, 512], F32, ps_) for i in range(2)]

                def load_expert(e):
                    sl_ = e % 2
                    kb.dma("pool", w1s[sl_].t[:, :, :], w1_d[lyr, e].rearrange("(k p) f -> p k f", p=128), w1s[sl_], writes=[w1s[sl_]])
                    kb.dma("pool", w3s[sl_].t[:, :, :], w3_d[lyr, e].rearrange("(k p) f -> p k f", p=128), w3s[sl_], writes=[w3s[sl_]])
                    kb.dma("pool", w2s[sl_].t[:, :, :], w2_d[lyr, e].rearrange("(c p) d -> p c d", p=128), w2s[sl_], writes=[w2s[sl_]])

                cnt = [0]
                for hf in range(2):
                    kb.dma("sp", xTh.t[:, :, :], xT_d[:, :, hf * 2048:(hf + 1) * 2048], xTh,
                           reads=[xT_B[hf * 4 + i] for i in range(4)], writes=[xTh])
                    load_expert(0)
                    for e in range(16):
                        if e + 1 < 16:
                            load_expert(e + 1)
                        w1 = w1s[e % 2]
                        w3 = w3s[e % 2]
                        w2 = w2s[e % 2]
                        for mt in range(4):
                            hd = hid[(e * 4 + mt) % 2]
                            for fc in range(4):
                                i2 = cnt[0] % 2
                                cnt[0] += 1
                                xs = xTh.t[:, :, mt * 512:(mt + 1) * 512]
                                kb.mmg(ph1[i2], ph1[i2].t[:, :], [(w1.t[:, k, fc * 128:(fc + 1) * 128], xTh.t[:, k, mt * 512:(mt + 1) * 512]) for k in range(8)],
                                       reads=[w1, xTh])
                                kb.mmg(ph3[i2], ph3[i2].t[:, :], [(w3.t[:, k, fc * 128:(fc + 1) * 128], xTh.t[:, k, mt * 512:(mt + 1) * 512]) for k in range(8)],
                                       reads=[w3, xTh])
                                kb.act(sl[i2].t[:, :], ph1[i2].t[:, :], AF.Silu, [ph1[i2]], [sl[i2]])
                                kb.tt("dve", hd.t[:, fc, :], sl[i2].t[:, :], ph3[i2].t[:, :], ALU.mult, [sl[i2], ph3[i2]], [hd])
                            for s in range(4):
                                ti = mt * 4 + s
                                gt = hf * 16 + ti
                                for dh in range(2):
                                    p = py[dh]
                                    kb.mmg(p, p.t[:, :], [(hd.t[:, fc, s * 128:(s + 1) * 128], w2.t[:, fc, dh * 512:(dh + 1) * 512]) for fc in range(4)],
                                           reads=[hd, w2])
                                    ya = yacc.t[:, ti, dh * 512:(dh + 1) * 512]
                                    if e == 0:
                                        kb.ts("dve", ya, p.t[:, :], c_all.t[:, gt, e:e + 1], None, ALU.mult, None, [p, c_all], [yacc])
                                    else:
                                        kb.stt("dve", ya, p.t[:, :], c_all.t[:, gt, e:e + 1], ya, ALU.mult, ALU.add, [p, c_all, yacc], [yacc])
                    for ti in range(16):
                        gt = hf * 16 + ti
                        xl = x1l[ti % 2]
                        r = rr[ti % 2]
                        kb.dma("sp", xl.t[:, :], xa_d[gt * 128:(gt + 1) * 128, :], xl, reads=[xa_B[gt]], writes=[xl])
                        kb.stt("dve", r.t[:, :], xl.t[:, :], ALPHA, yacc.t[:, ti, :], ALU.mult, ALU.add, [xl, yacc], [r])
                        layernorm(ps_, r, D, fg, fb, xl, xl.t[:, :], st, mv, tmp)
                        kb.dma("pool", dst_d[gt * 128:(gt + 1) * 128, :], xl.t[:, :], xl, reads=[xl], writes=[dst_B[gt]])
                kb.barrier()
                kb.release_sems()

        def moe_sparse(lyr, dst_d, dst_B):
            with contextlib.ExitStack() as ps_:
                fg = kb.sb("f_g", [128, D], F32, ps_)
                fb = kb.sb("f_b", [128, D], F32, ps_)
                bcast_load(fg, ffn_g_d[lyr:lyr + 1, :])
                bcast_load(fb, ffn_b_d[lyr:lyr + 1, :])
                w1s = [kb.sb("f_w1", [128, 8, 512], BF16, ps_) for i in range(2)]
                w3s = [kb.sb("f_w3", [128, 8, 512], BF16, ps_) for i in range(2)]
                w2s = [kb.sb("f_w2", [128, 4, D], BF16, ps_) for i in range(2)]
                xg = [kb.sb("f_xg", [128, D], BF16, ps_) for i in range(8)]
                lst = [kb.sb("f_lst", [128, 2], I32, ps_) for i in range(8)]
                xgT = [kb.sb("f_xgT", [128, 8, 128], BF16, ps_) for i in range(8)]
                sl = [kb.sb("f_sl", [128, 512], BF16, ps_) for i in range(2)]
                hid = [kb.sb("f_hid", [128, 4, 128], BF16, ps_) for i in range(2)]
                ysb = [kb.sb("f_ysb", [128, D], F32, ps_) for i in range(4)]
                y2l = [kb.sb("f_y2l", [128, 2, D], F32, ps_) for i in range(2)]
                x1l = [kb.sb("f_x1l", [128, D], F32, ps_) for i in range(2)]
                rr = [kb.sb("f_r", [128, D], F32, ps_) for i in range(2)]
                st = kb.sb("f_st", [128, 12], F32, ps_)
                mv = kb.sb("f_mv", [128, 2], F32, ps_)
                tmp = kb.sb("f_tmp", [128, 2], F32, ps_)
                pT = kb.ps("f_pT", [128, 1024], BF16, ps_)
                ph1 = [kb.ps("f_ph1", [128, 512], F32, ps_) for i in range(2)]
                ph3 = [kb.ps("f_ph3", [128, 512], F32, ps_) for i in range(2)]
                py = [kb.ps("f_py", [128, 512], F32, ps_) for i in range(2)]
                for i in range(8):
                    kb.op("dve", lambda e, i=i: e.memset(xg[i].t[:, :], 0.0), writes=[xg[i]])

                def load_expert(e):
                    sl_ = e % 2
                    kb.dma("pool", w1s[sl_].t[:, :, :], w1_d[lyr, e].rearrange("(k p) f -> p k f", p=128), w1s[sl_], writes=[w1s[sl_]])
                    kb.dma("pool", w3s[sl_].t[:, :, :], w3_d[lyr, e].rearrange("(k p) f -> p k f", p=128), w3s[sl_], writes=[w3s[sl_]])
                    kb.dma("pool", w2s[sl_].t[:, :, :], w2_d[lyr, e].rearrange("(c p) d -> p c d", p=128), w2s[sl_], writes=[w2s[sl_]])

                load_expert(0)
                for e in range(16):
                    if e + 1 < 16:
                        load_expert(e + 1)
                    w1 = w1s[e % 2]
                    w3 = w3s[e % 2]
                    w2 = w2s[e % 2]
                    for reg in kb.mregs:
                        E_ = kb.E[kb.etmap[reg.engine]]
                        kb._waits(E_, [cnt_i], [])
                        E_.eng.reg_load(reg, cnt_i.t[0:1, e:e + 1])

                    def fetch(j, e=e):
                        i4 = (e % 2) * 4 + j % 4
                        base = e * 4096 + j * 128
                        kb.dma("sp", lst[i4].t[:, :], list_d[base:base + 128, :], lst[i4], writes=[lst[i4]])
                        kb.idma(xg[i4].t[:, :], x1b_d[:, :], lst[i4].t[:, 0:1], False, T - 1, xg[i4], reads=[lst[i4]], writes=[xg[i4]])

                    def transp(j, e=e):
                        i4 = (e % 2) * 4 + j % 4
                        for k in range(8):
                            kb.op("pe", lambda e_, k=k: e_.transpose(pT.t[:, k * 128:(k + 1) * 128], xg[i4].t[:, k * 128:(k + 1) * 128], ident.t[:]),
                                  reads=[xg[i4], ident], writes=[pT], mark=(k == 7))
                        kb.copy("act", xgT[i4].t[:, :, :], pT.t[:, :].rearrange("p (k t) -> p k t", k=8), [pT], [xgT[i4]])

                    def hpart(j, w1=w1, w3=w3, e=e):
                        i2 = j % 2
                        i4 = (e % 2) * 4 + j % 4
                        for fc in range(4):
                            kb.mmg(ph1[i2], ph1[i2].t[:, fc * 128:(fc + 1) * 128],
                                   [(w1.t[:, k, fc * 128:(fc + 1) * 128], xgT[i4].t[:, k, :]) for k in range(8)], reads=[w1, xgT[i4]])
                        for fc in range(4):
                            kb.mmg(ph3[i2], ph3[i2].t[:, fc * 128:(fc + 1) * 128],
                                   [(w3.t[:, k, fc * 128:(fc + 1) * 128], xgT[i4].t[:, k, :]) for k in range(8)], reads=[w3, xgT[i4]])
                        kb.act(sl[i2].t[:, :], ph1[i2].t[:, :], AF.Silu, [ph1[i2]], [sl[i2]])
                        kb.tt("dve", hid[i2].t[:, :, :].rearrange("p c t -> p (c t)"), sl[i2].t[:, :], ph3[i2].t[:, :], ALU.mult,
                              [sl[i2], ph3[i2]], [hid[i2]])

                    def ypart(j, w2=w2, e=e):
                        i2 = j % 2
                        i4 = j % 4
                        il = (e % 2) * 4 + j % 4
                        for dh in range(2):
                            kb.mmg(py[dh], py[dh].t[:, :], [(hid[i2].t[:, fc, :], w2.t[:, fc, dh * 512:(dh + 1) * 512]) for fc in range(4)],
                                   reads=[hid[i2], w2])
                        kb.copy("act", ysb[i4].t[:, 0:512], py[0].t[:, :], [py[0]], [ysb[i4]])
                        kb.copy("dve", ysb[i4].t[:, 512:1024], py[1].t[:, :], [py[1]], [ysb[i4]])
                        kb.idma(y2_d[:, :], ysb[i4].t[:, :], lst[il].t[:, 1:2], True, 2 * T - 1, ysb[i4], reads=[ysb[i4], lst[il]])

                    def slot2(k):
                        A = 2 * k
                        Bq = 2 * k + 1
                        hpart(A)
                        hpart(Bq)
                        ypart(A)
                        if A + 2 < NT:
                            fetch(A + 2)
                            transp(A + 2) if False else None
                        ypart(Bq)
                        if Bq + 2 < NT:
                            fetch(Bq + 2)

                    if e == 0:
                        for j_ in range(4):
                            fetch(j_)
                        transp(0)
                        transp(1)
                    if e + 1 < 16:
                        for j_ in range(4):
                            fetch(j_, e + 1)
                    NFLAT = 3

                    def slot2t(k):
                        A = 2 * k
                        hpart(A)
                        hpart(A + 1)
                        if A + 2 < NT:
                            transp(A + 2)
                        ypart(A)
                        if A + 3 < NT:
                            transp(A + 3)
                        ypart(A + 1)
                        if A + 4 < NT:
                            fetch(A + 4)
                        if A + 5 < NT:
                            fetch(A + 5)

                    for k in range(NFLAT):
                        kb.region(4096 - k * 256, lambda k=k: slot2t(k))
                        if k == 0 and e + 1 < 16:
                            transp(0, e + 1)
                            transp(1, e + 1)

                    def rest():
                        for k in range(NFLAT, NT // 2):
                            kb.region(4096 - k * 256, lambda k=k: slot2t(k))
                    kb.region(4096 - NFLAT * 256, rest)
                kb.barrier()
                kb.release_sems()
                for gt in range(NT):
                    yl = y2l[gt % 2]
                    xl = x1l[gt % 2]
                    r = rr[gt % 2]
                    kb.dma("sp", yl.t[:, :, :], y2_d[gt * 256:(gt + 1) * 256, :].rearrange("(p r) d -> p r d", r=2), yl, writes=[yl])
                    kb.dma("sp", xl.t[:, :], xa_d[gt * 128:(gt + 1) * 128, :], xl, reads=[xa_B[gt]], writes=[xl])
                    kb.op("act", lambda e, yl=yl, gt=gt: e.mul(out=yl.t[:, 0, :], in_=yl.t[:, 0, :], mul=cAB.t[:, gt, 0:1]), reads=[yl, cAB], writes=[yl])
                    kb.stt("dve", yl.t[:, 0, :], yl.t[:, 1, :], cAB.t[:, gt, 1:2], yl.t[:, 0, :], ALU.mult, ALU.add, [yl, cAB], [yl])
                    kb.stt("dve", r.t[:, :], xl.t[:, :], ALPHA, yl.t[:, 0, :], ALU.mult, ALU.add, [xl, yl], [r])
                    layernorm(ps_, r, D, fg, fb, xl, xl.t[:, :], st, mv, tmp)
                    kb.dma("pool", dst_d[gt * 128:(gt + 1) * 128, :], xl.t[:, :], xl, reads=[xl], writes=[dst_B[gt]])
                kb.barrier()
                kb.release_sems()

        def attention():
            with contextlib.ExitStack() as ps_:
                Wq = load_w(ps_, "a_Wqkv", w_qkv_d, 3072)
                xin = kb.sb("a_xin", [128, 4, D], F32, ps_)
                xbf = [kb.sb("a_xbf%d" % i, [128, D], BF16, ps_) for i in range(2)]
                xT = kb.sb("a_xT", [128, 8, 512], BF16, ps_)
                qm = [kb.sb("a_qm%d" % i, [128, 8, 512], BF16, ps_) for i in range(2)]
                km = [kb.sb("a_km%d" % i, [128, 8, 512], BF16, ps_) for i in range(2)]
                vm = [kb.sb("a_vm%d" % i, [128, 4, D], BF16, ps_) for i in range(2)]
                pT = kb.ps("a_pT", [128, 1024], BF16, ps_)
                pp = [kb.ps("a_pp%d" % i, [128, 512], F32, ps_) for i in range(4)]
                ppi = 0
                for mt in range(8):
                    load_xT(xb_d, xb_B, mt, xin, xbf, pT, xT)
                    q_ = qm[mt % 2]
                    k_ = km[mt % 2]
                    v_ = vm[mt % 2]
                    for hp in range(8):
                        p = pp[ppi % 4]; ppi += 1
                        kb.mmg(p, p.t[:, :], [(Wq.t[:, k, hp * 128:(hp + 1) * 128], xT.t[:, k, :]) for k in range(8)], reads=[Wq, xT])
                        kb.op("act", lambda e, p=p, hp=hp, q_=q_: e.mul(out=q_.t[:, hp, :], in_=p.t[:, :], mul=0.125), reads=[p], writes=[q_])
                        p = pp[ppi % 4]; ppi += 1
                        kb.mmg(p, p.t[:, :], [(Wq.t[:, k, 1024 + hp * 128:1024 + (hp + 1) * 128], xT.t[:, k, :]) for k in range(8)], reads=[Wq, xT])
                        kb.copy("dve", k_.t[:, hp, :], p.t[:, :], [p], [k_])
                    for s in range(4):
                        for dh in range(2):
                            p = pp[ppi % 4]; ppi += 1
                            kb.mmg(p, p.t[:, :], [(xT.t[:, k, s * 128:(s + 1) * 128], Wq.t[:, k, 2048 + dh * 512:2048 + (dh + 1) * 512]) for k in range(8)],
                                   reads=[Wq, xT])
                            kb.copy("dve" if dh == 0 else "act", v_.t[:, s, dh * 512:(dh + 1) * 512], p.t[:, :], [p], [v_])
                    cs = slice(mt * 512, (mt + 1) * 512)
                    kb.dma("pool", qT_d.rearrange("h p t -> p h t")[:, :, cs], q_.t[:, :, :], q_, reads=[q_], writes=[q_B[mt]])
                    kb.dma("pool", kT_d.rearrange("h p t -> p h t")[:, :, cs], k_.t[:, :, :], k_, reads=[k_], writes=[k_B[mt]])
                    kb.dma("pool", v_d[mt * 512:(mt + 1) * 512, :].rearrange("(s p) d -> p s d", p=128), v_.t[:, :, :], v_, reads=[v_], writes=[v_B[mt]])
                kb.barrier()
                kb.release_sems()

            with contextlib.ExitStack() as ps_:
                oT = kb.sb("a_oT", [128, 8, T], BF16, ps_)
                masks = kb.sb("a_mask", [128, 4, 512], BF16, ps_)
                negU = kb.sb("a_negU", [128, 128], BF16, ps_)
                negO = kb.sb("a_negO", [128, 128], BF16, ps_)
                zer = kb.sb("a_zero", [128, 128], BF16, ps_)
                kb.op("pool", lambda e: e.memset(masks.t[:, :, :], 1.0), writes=[masks])
                for m in range(4):
                    kb.op("pool", lambda e, m=m: e.affine_select(out=masks.t[:, m, :], in_=masks.t[:, m, :], pattern=[[1, 512]],
                                                                compare_op=ALU.is_gt, fill=0.0, base=-128 * m, channel_multiplier=-1),
                          reads=[masks], writes=[masks])
                kb.op("pool", lambda e: e.memset(negU.t[:, :], -1.0), writes=[negU])
                kb.op("pool", lambda e: e.affine_select(out=negU.t[:, :], in_=negU.t[:, :], pattern=[[-1, 128]],
                                                        compare_op=ALU.is_ge, fill=0.0, base=0, channel_multiplier=1),
                      reads=[negU], writes=[negU])
                kb.op("pool", lambda e: e.memset(negO.t[:, :], -1.0), writes=[negO])
                kb.op("pool", lambda e: e.memset(zer.t[:, :], 0.0), writes=[zer])
                with contextlib.ExitStack() as ps2:
                    qh = [kb.sb("a_qh%d" % i, [128, T], BF16, ps2) for i in range(2)]
                    kh = [kb.sb("a_kh%d" % i, [128, T], BF16, ps2) for i in range(2)]
                    vh = [kb.sb("a_vh%d" % i, [128, NT, 128], BF16, ps2) for i in range(2)]
                    ex = [[kb.sb("a_ex", [128, 512], F32, ps2) for _ in range(2)] for c in range(2)]
                    lnu = [[kb.sb("a_lnu", [128, 512], BF16, ps2) for _ in range(3)] for c in range(2)]
                    Sb = [[kb.sb("a_S", [128, 512], BF16, ps2) for _ in range(4)] for c in range(2)]
                    att = [[kb.sb("a_att", [128, 512], BF16, ps2) for _ in range(2)] for c in range(2)]
                    pz = [[kb.ps("a_pz", [128, 512], F32, ps2) for _ in range(1)] for c in range(2)]
                    pM = [kb.ps("a_pM", [128, 512], F32, ps2) for c in range(2)]
                    pc = [kb.ps("a_pc", [128, 512], F32, ps2) for c in range(2)]
                    pR = kb.ps("a_pR", [128, 512], F32, ps2)
                    pcC = kb.ps("a_pcC", [128, 512], F32, ps2)
                    exC = [kb.sb("a_exC", [128, 512], F32, ps2) for _ in range(2)]
                    lnuC = [kb.sb("a_lnuC", [128, 512], BF16, ps2) for _ in range(2)]
                    SC = [kb.sb("a_SC", [128, 512], BF16, ps2) for _ in range(2)]
                    attC = [kb.sb("a_attC", [128, 512], BF16, ps2) for _ in range(2)]
                    Ssave = [kb.sb("a_Ssave", [128, 512], BF16, ps2) for _ in range(2)]
                    flg = kb.sb("a_flg", [128, 128], I32, ps2)
                    flagB = [B() for _ in range(128)]
                    rmin = kb.sb("a_rmin", [128, 2], F32, ps2)
                    THR = 120.0
                    NU = 6

                    def load_hp(hp):
                        i2 = hp % 2
                        kb.dma("sp", qh[i2].t[:, :], qT_d[hp], qh[i2], reads=q_B, writes=[qh[i2]])
                        kb.dma("sp", kh[i2].t[:, :], kT_d[hp], kh[i2], reads=k_B, writes=[kh[i2]])
                        kb.dma("sp", vh[i2].t[:, :, :], v_d.rearrange("(b p) d -> p b d", p=128)[:, :, hp * 128:(hp + 1) * 128], vh[i2],
                               reads=v_B, writes=[vh[i2]])

                    class It:
                        pass

                    nctr = [0, 0]

                    def make_items(hp, c):
                        items = []
                        for i in range(8):
                            blo = max(4 * i + 3 - (NU - 1), 0)
                            for b in range(4 * i + 3, blo - 1, -1):
                                it = It()
                                it.c = c
                                it.hp = hp
                                it.i = i
                                it.b = b
                                it.first = (b == 4 * i + 3)
                                it.last = (b == 0)
                                it.ulast = (b == blo)
                                it.hasC = (blo > 0)
                                it.fcol = (hp * 2 + c) * 8 + i
                                it.m = b - 4 * i
                                it.qlo = 128 * it.m if it.m > 0 else 0
                                it.n = nctr[c]
                                nctr[c] += 1
                                items.append(it)
                        return items

                    def stageA(it):
                        c = it.c
                        q_ = qh[it.hp % 2]
                        k_ = kh[it.hp % 2]
                        pr = slice(c * 64, (c + 1) * 64)
                        qlo = it.qlo
                        n = 512 - qlo
                        qs = slice(it.i * 512 + qlo, (it.i + 1) * 512)
                        ks = slice(it.b * 128, (it.b + 1) * 128)
                        z = pz[c][0]
                        e_ = ex[c][it.n % 2]
                        l_ = lnu[c][it.n % 3]
                        kb.mmg(z, z.t[:, 0:n], [(k_.t[pr, ks], q_.t[pr, qs])], reads=[k_, q_])
                        kb.act(e_.t[:, 0:n], z.t[:, 0:n], AF.Exp, [z], [e_])
                        kb.act(l_.t[:, 0:n], e_.t[:, 0:n], AF.Ln, [e_], [l_], bias=1.0, scale=1.0)
                        if it.m >= 0:
                            kb.tt("pool", l_.t[:, 0:n], l_.t[:, 0:n], masks.t[:, it.m, qlo:512], ALU.mult, [l_, masks], [l_])
                        if not it.last:
                            Sn = Sb[c][it.n % 4]
                            if it.first:
                                if qlo > 0:
                                    kb.op("dve", lambda e: e.memset(Sn.t[:, 0:qlo], 0.0), writes=[Sn])
                                kb.copy("dve", Sn.t[:, qlo:512], l_.t[:, 0:n], [l_], [Sn])
                            else:
                                Sp = Sb[c][(it.n - 1) % 4]
                                if qlo > 0:
                                    kb.copy("dve", Sn.t[:, 0:qlo], Sp.t[:, 0:qlo], [Sp], [Sn])
                                kb.tt("dve", Sn.t[:, qlo:512], Sp.t[:, qlo:512], l_.t[:, 0:n], ALU.add, [Sp, l_], [Sn])
                            if it.ulast and it.hasC:
                                kb.mmg(pR, pR.t[:, :], [(onesb.t[:, :], Sn.t[:, :])], reads=[onesb, Sn])
                                kb.op("dve", lambda e: e.tensor_reduce(out=rmin.t[:, c:c + 1], in_=pR.t[:, :], axis=mybir.AxisListType.X, op=ALU.min),
                                      reads=[pR], writes=[rmin])
                                kb.ts("dve", flg.t[:, it.fcol:it.fcol + 1], rmin.t[:, c:c + 1], THR, None, ALU.is_gt, None, [rmin], [flagB[it.fcol]])
                                kb.copy("dve", Ssave[c].t[:, :], Sn.t[:, :], [Sn], [Ssave[c]])

                    def stageB(it):
                        c = it.c
                        q_ = qh[it.hp % 2]
                        k_ = kh[it.hp % 2]
                        pr = slice(c * 64, (c + 1) * 64)
                        qlo = it.qlo
                        n = 512 - qlo
                        qs = slice(it.i * 512 + qlo, (it.i + 1) * 512)
                        ks = slice(it.b * 128, (it.b + 1) * 128)
                        l_ = lnu[c][it.n % 3]
                        a_ = att[c][it.n % 2]
                        M = pM[c]
                        prs = [(k_.t[pr, ks], q_.t[pr, qs]), (negU.t[:, :], l_.t[:, 0:n])]
                        rds = [k_, q_, negU, l_]
                        if not it.first:
                            Sp = Sb[c][(it.n - 1) % 4]
                            prs.append((negO.t[:, :], Sp.t[:, qlo:512]))
                            rds.append(Sp)
                        kb.mmg(M, M.t[:, 0:n], prs, reads=rds)
                        kb.act(a_.t[:, 0:n], M.t[:, 0:n], AF.Exp, [M], [a_])
                        if it.m >= 0:
                            kb.tt("pool", a_.t[:, 0:n], a_.t[:, 0:n], masks.t[:, it.m, qlo:512], ALU.mult, [a_, masks], [a_])

                    def stageC(it):
                        c = it.c
                        v_ = vh[it.hp % 2]
                        pr = slice(c * 64, (c + 1) * 64)
                        qlo = it.qlo
                        n = 512 - qlo
                        a_ = att[c][it.n % 2]
                        pcb = pc[c]
                        if it.first:
                            kb.op("pe", lambda e: e.matmul(pcb.t[:, :], lhsT=zer.t[:, :], rhs=masks.t[:, 0, :], start=True, stop=False),
                                  reads=[zer, masks], writes=[pcb], mark=False)
                        kb.op("pe", lambda e: e.matmul(pcb.t[:, qlo:512], lhsT=v_.t[:, it.b, :], rhs=a_.t[:, 0:n], start=False, stop=it.ulast),
                              reads=[v_, a_], writes=[pcb], mark=True)
                        if it.ulast:
                            kb.copy("dve", oT.t[pr, it.hp, it.i * 512:(it.i + 1) * 512], pcb.t[pr, :], [pcb], [oT])

                    def cond_rest(it):
                        c = it.c
                        q_ = qh[it.hp % 2]
                        k_ = kh[it.hp % 2]
                        v_ = vh[it.hp % 2]
                        pr = slice(c * 64, (c + 1) * 64)
                        qs = slice(it.i * 512, (it.i + 1) * 512)
                        for reg in kb.mregs:
                            E_ = kb.E[kb.etmap[reg.engine]]
                            kb._waits(E_, [flagB[it.fcol]], [])
                            E_.eng.reg_load(reg, flg.t[0:1, it.fcol:it.fcol + 1])

                        def cbody():
                            kb.op("pe", lambda e: e.matmul(pcC.t[:, :], lhsT=zer.t[:, :], rhs=masks.t[:, 0, :], start=True, stop=False),
                                  reads=[zer, masks], writes=[pcC], mark=False)
                            Sprev = Ssave[c]
                            for n2, b in enumerate(range(it.b - 1, -1, -1)):
                                ks = slice(b * 128, (b + 1) * 128)
                                z = pz[c][0]
                                M = pM[c]
                                e_ = exC[n2 % 2]
                                l_ = lnuC[n2 % 2]
                                a_ = attC[n2 % 2]
                                kb.mmg(z, z.t[:, :], [(k_.t[pr, ks], q_.t[pr, qs])], reads=[k_, q_])
                                kb.act(e_.t[:, :], z.t[:, :], AF.Exp, [z], [e_])
                                kb.act(l_.t[:, :], e_.t[:, :], AF.Ln, [e_], [l_], bias=1.0, scale=1.0)
                                kb.mmg(M, M.t[:, :], [(k_.t[pr, ks], q_.t[pr, qs]), (negU.t[:, :], l_.t[:, :]), (negO.t[:, :], Sprev.t[:, :])],
                                       reads=[k_, q_, negU, l_, negO, Sprev])
                                kb.act(a_.t[:, :], M.t[:, :], AF.Exp, [M], [a_])
                                if b > 0:
                                    Sn = SC[n2 % 2]
                                    kb.tt("dve", Sn.t[:, :], Sprev.t[:, :], l_.t[:, :], ALU.add, [Sprev, l_], [Sn])
                                    Sprev = Sn
                                kb.op("pe", lambda e, b=b, a_=a_: e.matmul(pcC.t[:, :], lhsT=v_.t[:, b, :], rhs=a_.t[:, :], start=False, stop=(b == 0)),
                                      reads=[v_, a_], writes=[pcC], mark=True)
                            oc = oT.t[pr, it.hp, it.i * 512:(it.i + 1) * 512]
                            kb.tt("dve", oc, oc, pcC.t[pr, :], ALU.add, [oT, pcC], [oT])

                        kb.region(1, cbody)

                    load_hp(0)
                    for hp in range(8):
                        if hp + 1 < 8:
                            load_hp(hp + 1)
                        l0 = make_items(hp, 0)
                        l1 = make_items(hp, 1)
                        L = []
                        for a, b_ in zip(l0, l1):
                            L.append(a)
                            L.append(b_)
                        for g in range(len(L) + 4):
                            if g < len(L):
                                stageA(L[g])
                            if 0 <= g - 2 < len(L):
                                stageB(L[g - 2])
                            if 0 <= g - 4 < len(L):
                                stageC(L[g - 4])
                                if L[g - 4].ulast and L[g - 4].hasC:
                                    cond_rest(L[g - 4])
                    kb.barrier()
                    kb.release_sems()
                with contextlib.ExitStack() as ps3:
                    P = post_mixer_allocs(ps3, 1)
                    Wo = load_w(ps3, "a_Wo", w_out1_d, 1024)
                    xres = [kb.sb("a_xres%d" % i, [128, D], F32, ps3) for i in range(2)]
                    pso = [kb.ps("a_pso%d" % i, [128, 512], F32, ps3) for i in range(2)]
                    for gt in range(NT):
                        xr = xres[gt % 2]
                        kb.dma("sp", xr.t[:, :], xb_d[gt * 128:(gt + 1) * 128, :], xr, reads=[xb_B[gt]], writes=[xr])
                        post_mixer(ps3, 1, gt, xr.t[:, :], xr, pso, oT, slice(gt * 128, (gt + 1) * 128), Wo, P)
                    kb.barrier()
                    kb.release_sems()

        moe_fn = moe_sparse if SPARSE else moe
        mixer0()
        if stop_after != "m0":
            moe_fn(0, xb_d, xb_B)
            if stop_after != "moe0":
                attention()
                if stop_after != "attn":
                    moe_fn(1, out_d, out_B)
        kb.barrier()
        kb.release_sems()

    es.close()
    return nc


_NC_CACHE = {}


def kernel(x, even_w_in, even_conv_w, even_sgu_ln_g, even_sgu_ln_b, even_sgu_w_s, even_sgu_b_s, even_w_out,
           odd_w_qkv, odd_w_out, mix_ln_g, mix_ln_b, moe_w_group, moe_b_group, moe_w_router, moe_b_router,
           moe_w1, moe_w3, moe_w2, ffn_ln_g, ffn_ln_b):
    f = lambda a: np.ascontiguousarray(np.asarray(a, dtype=np.float32))
    convw = f(np.asarray(even_conv_w)[0].reshape(3, 4, 128).transpose(2, 1, 0).reshape(128, 12))
    wsT = f(np.asarray(even_sgu_w_s)[0].transpose(0, 2, 1))
    bs = f(np.asarray(even_sgu_b_s)[0].reshape(1, 1024))
    wr = f(np.concatenate([np.asarray(moe_w_group),
                           np.asarray(moe_w_router).transpose(0, 2, 1, 3).reshape(2, D, 16)], axis=2))
    br = f(np.concatenate([np.asarray(moe_b_group), np.asarray(moe_b_router).reshape(2, 16)], axis=1))
    shared = {
        "w_in": f(even_w_in[0]), "convw": convw, "sgu_g": f(even_sgu_ln_g), "sgu_b": f(even_sgu_ln_b),
        "wsT": wsT, "bs": bs, "w_out0": f(even_w_out[0]), "w_qkv": f(odd_w_qkv[0]), "w_out1": f(odd_w_out[0]),
        "mix_g": f(mix_ln_g), "mix_b": f(mix_ln_b), "wr": wr, "br": br,
        "w1": f(np.asarray(moe_w1).reshape(2, 16, D, 512)), "w3": f(np.asarray(moe_w3).reshape(2, 16, D, 512)),
        "w2": f(np.asarray(moe_w2).reshape(2, 16, 512, D)), "ffn_g": f(ffn_ln_g), "ffn_b": f(ffn_ln_b),
    }
    xs = f(x)
    if "nc" not in _NC_CACHE:
        _NC_CACHE["nc"] = build()
    nc = _NC_CACHE["nc"]
    in_maps = [dict(shared, x=xs[c]) for c in range(NCORES)]
    res = run_bass_kernel_spmd(nc, in_maps, core_ids=list(range(NCORES)))
    return np.stack([res.results[c]["out"] for c in range(NCORES)], axis=0)
```

```python
import contextlib
import numpy as np
import concourse.bass as bass
import concourse.mybir as mybir
from concourse.bass_utils import run_bass_kernel_spmd

F32 = mybir.dt.float32
BF16 = mybir.dt.bfloat16
I32 = mybir.dt.int32
OOB = 1 << 20
AF = mybir.ActivationFunctionType
ALU = mybir.AluOpType

T = 4096
D = 1024
NT = 32
ALPHA = float(4 ** 0.25)
EPS = 1e-5
NCORES = 8
SPARSE = True


class B:
    __slots__ = ("t", "w", "r", "dsem", "dcnt")

    def __init__(self, t=None):
        self.t = t
        self.w = None
        self.r = {}
        self.dsem = None
        self.dcnt = 0


class Eng:
    pass


class KB:
    def __init__(self):
        self.nc = bass.Bass("TRN2", target_bir_lowering=False)
        self.es = contextlib.ExitStack()
        self.nsem = 0
        self.dma_bufs = []
        self.rstack = []
        self.bregs = {}
        self.free_sems = []

    def sem(self, name):
        self.nsem += 1
        return self.es.enter_context(self.nc.semaphore(name))

    def sb(self, name, shape, dt, stack=None):
        st = stack if stack is not None else self.es
        self.nt = getattr(self, "nt", 0) + 1
        return B(st.enter_context(self.nc.sbuf_tensor("%s_%d" % (name, self.nt), shape, dt)))

    def ps(self, name, shape, dt, stack=None):
        st = stack if stack is not None else self.es
        self.nt = getattr(self, "nt", 0) + 1
        return B(st.enter_context(self.nc.psum_tensor("%s_%d" % (name, self.nt), shape, dt)))

    def start(self):
        nc = self.nc
        self.E = {}
        for name, eng in (("pe", nc.tensor), ("act", nc.scalar), ("dve", nc.vector),
                          ("pool", nc.gpsimd), ("sp", nc.sync)):
            e = Eng()
            e.name = name
            e.eng = eng
            e.sem = self.sem("s_" + name)
            e.count = 0
            e.seen = {}
            self.E[name] = e
        ET = mybir.EngineType
        self.etmap = {ET.PE: "pe", ET.Activation: "act", ET.DVE: "dve", ET.Pool: "pool", ET.SP: "sp"}
        self.mregs = nc.alloc_registers("mr", [ET.PE, ET.Activation, ET.DVE, ET.Pool, ET.SP])

    def _waits(self, E, reads, writes):
        need = {}

        def acc(tok):
            k = id(tok[0])
            if k not in need or need[k][1] < tok[1]:
                need[k] = tok

        for b in reads:
            if b.w is not None:
                acc(b.w)
        for b in writes:
            if b.w is not None:
                acc(b.w)
            for tok in b.r.values():
                acc(tok)
        for k, (sem, val) in need.items():
            if E.name == "pe" and sem is E.sem:
                continue
            if E.seen.get(k, 0) >= val:
                continue
            E.eng.wait_ge(sem, val)
            E.seen[k] = val

    def _commit(self, tok, reads, writes):
        k = id(tok[0])
        for b in reads:
            b.r[k] = tok
        for b in writes:
            b.w = tok
            b.r = {}

    def op(self, en, fn, reads=(), writes=(), mark=True):
        E = self.E[en]
        self._waits(E, reads, writes)
        inst = fn(E.eng)
        if mark:
            E.count += 1
            inst.then_inc(E.sem, 1)
            tok = (E.sem, E.count)
        else:
            tok = (E.sem, E.count + 1)
        self._commit(tok, reads, writes)
        return inst

    def dma(self, q, out_ap, in_ap, sbufB, reads=(), writes=()):
        E = self.E[q]
        self._waits(E, reads, writes)
        self._get_dsem(sbufB)
        for rd in self.rstack:
            rec = rd[q].setdefault(id(sbufB.dsem), [sbufB.dsem, sbufB.dcnt, 0])
            rec[2] += 16
        sbufB.dcnt += 16
        E.eng.dma_start(out=out_ap, in_=in_ap).then_inc(sbufB.dsem, 16)
        tok = (sbufB.dsem, sbufB.dcnt)
        self._commit(tok, reads, writes)

    def idma(self, out_ap, in_ap, idx_ap, scatter, bound, sbufB, reads=(), writes=()):
        E = self.E["pool"]
        self._waits(E, reads, writes)
        self._get_dsem(sbufB)
        for rd in self.rstack:
            rec = rd["pool"].setdefault(id(sbufB.dsem), [sbufB.dsem, sbufB.dcnt, 0])
            rec[2] += 16
        sbufB.dcnt += 16
        off = bass.IndirectOffsetOnAxis(ap=idx_ap, axis=0)
        if bound not in self.bregs:
            r = self.es.enter_context(E.eng.register("rb%d" % bound))
            E.eng.reg_mov(r, bound)
            self.bregs[bound] = r
        bound = self.bregs[bound]
        if scatter:
            E.eng.indirect_dma_start(out=out_ap, out_offset=off, in_=in_ap, in_offset=None,
                                     bounds_check=bound, oob_is_err=False).then_inc(sbufB.dsem, 16)
        else:
            E.eng.indirect_dma_start(out=out_ap, out_offset=None, in_=in_ap, in_offset=off,
                                     bounds_check=bound, oob_is_err=False).then_inc(sbufB.dsem, 16)
        tok = (sbufB.dsem, sbufB.dcnt)
        self._commit(tok, reads, writes)

    def region(self, thr, body):
        engs = list(self.E.values())
        snap = {E.name: dict(E.seen) for E in engs}
        before = {E.name: E.count for E in engs}
        rd = {E.name: {} for E in engs}
        self.rstack.append(rd)
        with self.nc.If_cmp(self.mregs, thr, "IS_LT"):
            body()
        self.rstack.pop()
        with self.nc.Else():
            for E in engs:
                nm = E.count - before[E.name]
                if nm > 0:
                    E.eng.wait_ge(E.sem, before[E.name])
                    E.eng.sem_inc(E.sem, nm)
                for (sem, bt, add) in rd[E.name].values():
                    E.eng.wait_ge(sem, bt)
                    E.eng.sem_inc(sem, add)
        for E in engs:
            E.seen = snap[E.name]

    def _get_dsem(self, b):
        if b.dsem is None:
            if self.free_sems:
                b.dsem, b.dcnt = self.free_sems.pop()
            else:
                b.dsem = self.sem("d%d" % self.nsem)
                b.dcnt = 0
            self.dma_bufs.append(b)

    def release_sems(self):
        for b in self.dma_bufs:
            if b.dsem is not None:
                self.free_sems.append((b.dsem, b.dcnt))
                b.dsem = None
        self.dma_bufs = []

    def barrier(self):
        toks = []
        for e in self.E.values():
            if e.count > 0:
                toks.append((e.sem, e.count))
        for b in self.dma_bufs:
            if b.dcnt > 0:
                toks.append((b.dsem, b.dcnt))
        for E in self.E.values():
            for (sem, val) in toks:
                if sem is E.sem:
                    continue
                k = id(sem)
                if E.seen.get(k, 0) >= val:
                    continue
                E.eng.wait_ge(sem, val)
                E.seen[k] = val

    def mmg(self, outB, out_ap, pairs, reads):
        n = len(pairs)
        for i, (l, r) in enumerate(pairs):
            self.op("pe", lambda e, l=l, r=r, i=i: e.matmul(out_ap, lhsT=l, rhs=r, start=(i == 0), stop=(i == n - 1)),
                    reads=reads, writes=[outB], mark=(i == n - 1))

    def act(self, out_ap, in_ap, func, reads, writes, bias=None, scale=None):
        kw = {}
        if bias is not None:
            kw["bias"] = bias
        if scale is not None:
            kw["scale"] = scale
        self.op("act", lambda e: e.activation(out=out_ap, in_=in_ap, func=func, **kw), reads=reads, writes=writes)

    def copy(self, en, out_ap, in_ap, reads, writes):
        if en == "act":
            self.op("act", lambda e: e.copy(out=out_ap, in_=in_ap), reads=reads, writes=writes)
        else:
            self.op(en, lambda e: e.tensor_copy(out=out_ap, in_=in_ap), reads=reads, writes=writes)

    def tt(self, en, out_ap, a_ap, b_ap, op, reads, writes):
        self.op(en, lambda e: e.tensor_tensor(out=out_ap, in0=a_ap, in1=b_ap, op=op), reads=reads, writes=writes)

    def ts(self, en, out_ap, in_ap, s1, s2, op0, op1, reads, writes):
        if op1 is None:
            self.op(en, lambda e: e.tensor_scalar(out=out_ap, in0=in_ap, scalar1=s1, scalar2=None, op0=op0),
                    reads=reads, writes=writes)
        else:
            self.op(en, lambda e: e.tensor_scalar(out=out_ap, in0=in_ap, scalar1=s1, scalar2=s2, op0=op0, op1=op1),
                    reads=reads, writes=writes)

    def stt(self, en, out_ap, in0, scalar, in1, op0, op1, reads, writes):
        self.op(en, lambda e: e.scalar_tensor_tensor(out=out_ap, in0=in0, scalar=scalar, in1=in1, op0=op0, op1=op1),
                reads=reads, writes=writes)


def build(stop_after=None, dbg=False):
    kb = KB()
    dk = {"kind": "ExternalOutput"} if dbg else {}
    nc = kb.nc

    def din(name, shape):
        return nc.dram_tensor(name, shape, F32, kind="ExternalInput").ap()

    x_d = din("x", [T, D])
    w_in_d = din("w_in", [D, 2560])
    convw_d = din("convw", [128, 12])
    sgu_g_d = din("sgu_g", [1, 512])
    sgu_b_d = din("sgu_b", [1, 512])
    wsT_d = din("wsT", [8, 128, 128])
    bs_d = din("bs", [1, 1024])
    w_out0_d = din("w_out0", [D, D])
    w_qkv_d = din("w_qkv", [D, 3072])
    w_out1_d = din("w_out1", [D, D])
    mix_g_d = din("mix_g", [2, D])
    mix_b_d = din("mix_b", [2, D])
    wr_d = din("wr", [2, D, 20])
    br_d = din("br", [2, 20])
    w1_d = din("w1", [2, 16, D, 512])
    w3_d = din("w3", [2, 16, D, 512])
    w2_d = din("w2", [2, 16, 512, D])
    ffn_g_d = din("ffn_g", [2, D])
    ffn_b_d = din("ffn_b", [2, D])
    out_d = nc.dram_tensor("out", [T, D], F32, kind="ExternalOutput").ap()

    xa_d = nc.dram_tensor("xa_s", [T, D], F32, **dk).ap()
    xb_d = nc.dram_tensor("xb_s", [T, D], F32, **dk).ap()
    xT_d = nc.dram_tensor("xT_s", [128, 8, T], BF16, **dk).ap()
    qT_d = nc.dram_tensor("qT_s", [8, 128, T], BF16, **dk).ap()
    kT_d = nc.dram_tensor("kT_s", [8, 128, T], BF16, **dk).ap()
    v_d = nc.dram_tensor("v_s", [T, D], BF16, **dk).ap()

    x1b_d = nc.dram_tensor("x1b_s", [T, D], BF16, **dk).ap()
    list_d = nc.dram_tensor("list_s", [16 * 4096, 2], I32, **dk).ap()
    y2_d = nc.dram_tensor("y2_s", [2 * T, D], F32, **dk).ap()
    x1bd_B = [B() for _ in range(NT)]
    xa_B = [B() for _ in range(NT)]
    xb_B = [B() for _ in range(NT)]
    xT_B = [B() for _ in range(8)]
    q_B = [B() for _ in range(8)]
    k_B = [B() for _ in range(8)]
    v_B = [B() for _ in range(8)]
    out_B = [B() for _ in range(NT)]

    kb.start()
    es = kb.es

    ident = kb.sb("ident", [128, 128], BF16)
    c_all = kb.sb("c_all", [128, NT, 16], F32)
    cAB = kb.sb("cAB", [128, NT, 2], F32)
    off_t = kb.sb("off_t", [128, 16], F32)
    ebase1 = kb.sb("ebase1", [128, 16], F32)
    Lstr = kb.sb("Lstr", [128, 128], BF16)
    onesb = kb.sb("onesb", [128, 128], BF16)
    cnt_i = kb.sb("cnt_i", [128, 16], I32)
    tl2 = kb.sb("tl2", [128, NT, 2, 2], I32)
    oobt = kb.sb("oobt", [128, 1024], I32)

    block = es.enter_context(nc.Block())

    @block.sync
    def _(sync):
        kb.op("pool", lambda e: e.memset(ident.t[:], 0.0), writes=[ident])
        kb.op("pool", lambda e: e.affine_select(out=ident.t[:], in_=ident.t[:], pattern=[[-1, 128]],
                                                compare_op=ALU.not_equal, fill=1.0, base=0, channel_multiplier=1),
              reads=[ident], writes=[ident])

        kb.op("pool", lambda e: e.memset(onesb.t[:], 1.0), writes=[onesb])
        kb.op("pool", lambda e: e.memset(Lstr.t[:], 1.0), writes=[Lstr])
        kb.op("pool", lambda e: e.affine_select(out=Lstr.t[:], in_=Lstr.t[:], pattern=[[1, 128]],
                                                compare_op=ALU.is_gt, fill=0.0, base=0, channel_multiplier=-1),
              reads=[Lstr], writes=[Lstr])
        kb.op("pool", lambda e: e.iota(ebase1.t[:], [[4096, 16]], base=1, channel_multiplier=0,
                                       allow_small_or_imprecise_dtypes=True), writes=[ebase1])
        kb.op("pool", lambda e: e.memset(oobt.t[:], OOB), writes=[oobt])
        for r_ in range(2):
            kb.op("pool", lambda e, r_=r_: e.iota(tl2.t[:, :, r_, 0], [[128, NT]], base=0, channel_multiplier=1), writes=[tl2])
            kb.op("pool", lambda e, r_=r_: e.iota(tl2.t[:, :, r_, 1], [[256, NT]], base=r_, channel_multiplier=2), writes=[tl2])

        def layernorm(ps_, r, D_, g_bc, b_bc, outB, out_ap, stats, mv, tmp, mul_eng="pool"):
            nch = D_ // 512
            for c in range(nch):
                kb.op("dve", lambda e, c=c: e.bn_stats(out=stats.t[:, c * 6:(c + 1) * 6], in_=r.t[:, c * 512:(c + 1) * 512]),
                      reads=[r], writes=[stats])
            kb.op("dve", lambda e: e.bn_aggr(out=mv.t[:, 0:2], in_=stats.t[:, 0:nch * 6]), reads=[stats], writes=[mv])
            kb.act(tmp.t[:, 0:1], mv.t[:, 1:2], AF.Ln, [mv], [tmp], bias=EPS, scale=1.0)
            kb.act(tmp.t[:, 1:2], tmp.t[:, 0:1], AF.Exp, [tmp], [tmp], scale=-0.5)
            if D_ == D:
                kb.ts("dve", tmp.t[:, 0:1], mv.t[:, 0:1], -1.0, tmp.t[:, 1:2], ALU.mult, ALU.mult, [mv, tmp], [tmp])
                kb.act(r.t[:, 0:D_], r.t[:, 0:D_], AF.Identity, [r, tmp], [r], bias=tmp.t[:, 0:1], scale=tmp.t[:, 1:2])
            else:
                kb.ts("dve", r.t[:, 0:D_], r.t[:, 0:D_], mv.t[:, 0:1], tmp.t[:, 1:2], ALU.subtract, ALU.mult, [r, mv, tmp], [r])
            kb.tt(mul_eng, r.t[:, 0:D_], r.t[:, 0:D_], g_bc.t[:, 0:D_], ALU.mult, [r, g_bc], [r])
            kb.tt(mul_eng, out_ap, r.t[:, 0:D_], b_bc.t[:, 0:D_], ALU.add, [r, b_bc], [outB])

        def bcast_load(dst, src_row_ap):
            kb.dma("sp", dst.t[:], src_row_ap.partition_broadcast(128), dst, writes=[dst])

        def post_mixer(ps_, lyr, gt, x_res_ap, x_resB, pso, yT, yT_cols, Wo, P, stage=0):
            x1 = P["x1"][gt % 3]
            if stage in (0, 1):
                for dh in range(2):
                    kb.mmg(pso[dh], pso[dh].t[:, :],
                           [(yT.t[:, k, yT_cols], Wo.t[:, k, dh * 512:(dh + 1) * 512]) for k in range(8)],
                           reads=[yT, Wo])
                r = P["r"][gt % 2]
                for dh in range(2):
                    kb.stt("dve", r.t[:, dh * 512:(dh + 1) * 512], x_res_ap[:, dh * 512:(dh + 1) * 512], ALPHA,
                           pso[dh].t[:, :], ALU.mult, ALU.add, [x_resB, pso[dh]], [r])
                layernorm(ps_, r, D, P["mg"], P["mb"], x1, x1.t[:, :], P["stats"], P["mv"], P["tmp"])
                if stage == 1:
                    return
            kb.dma("pool", xa_d[gt * 128:(gt + 1) * 128, :], x1.t[:, :], x1, reads=[x1], writes=[xa_B[gt]])
            x1b = P["x1b"][gt % 2]
            kb.copy("act", x1b.t[:, :], x1.t[:, :], [x1], [x1b])
            kb.dma("pool", x1b_d[gt * 128:(gt + 1) * 128, :], x1b.t[:, :], x1b, reads=[x1b], writes=[x1bd_B[gt]])
            pT = P["pT"]
            for k in range(8):
                kb.op("pe", lambda e, k=k: e.transpose(pT.t[:, k * 128:(k + 1) * 128], x1b.t[:, k * 128:(k + 1) * 128], ident.t[:]),
                      reads=[x1b, ident], writes=[pT], mark=(k == 7))
            x1T = P["x1T"][gt % 2]
            s = gt % 4
            kb.copy("act", x1T.t[:, :, :], pT.t[:, :].rearrange("p (k t) -> p k t", k=8), [pT], [x1T])
            prt = P["prt"]
            kb.mmg(prt, prt.t[:, 0:20], [(x1T.t[:, k, :], P["Wr"].t[:, k, :]) for k in range(8)],
                   reads=[x1T, P["Wr"]])
            rt = P["rt"]
            lg = rt.t[:, 0:20]
            kb.tt("dve", lg, prt.t[:, 0:20], P["brb"].t[:, :], ALU.add, [prt, P["brb"]], [rt])
            R_ = [rt]
            kb.op("dve", lambda e: e.reduce_max(out=rt.t[:, 20:21], in_=rt.t[:, 0:4], axis=mybir.AxisListType.X), reads=R_, writes=R_)
            kb.ts("dve", rt.t[:, 24:28], rt.t[:, 0:4], rt.t[:, 20:21], None, ALU.is_equal, None, R_, R_)
            kb.ts("dve", rt.t[:, 21:22], rt.t[:, 20:21], -1.0, None, ALU.mult, None, R_, R_)
            kb.act(rt.t[:, 28:32], rt.t[:, 0:4], AF.Exp, R_, R_, bias=rt.t[:, 21:22], scale=1.0)
            kb.op("dve", lambda e: e.reduce_sum(out=rt.t[:, 22:23], in_=rt.t[:, 28:32], axis=mybir.AxisListType.X), reads=R_, writes=R_)
            kb.ts("dve", rt.t[:, 32:36], rt.t[:, 4:8], rt.t[:, 24:25], None, ALU.mult, None, R_, R_)
            for g in range(1, 4):
                kb.stt("dve", rt.t[:, 32:36], rt.t[:, 4 + 4 * g:8 + 4 * g], rt.t[:, 24 + g:25 + g], rt.t[:, 32:36],
                       ALU.mult, ALU.add, R_, R_)
            kb.op("dve", lambda e: e.reduce_max(out=rt.t[:, 36:37], in_=rt.t[:, 32:36], axis=mybir.AxisListType.X), reads=R_, writes=R_)
            kb.ts("dve", rt.t[:, 40:44], rt.t[:, 32:36], rt.t[:, 36:37], None, ALU.is_equal, None, R_, R_)
            kb.ts("dve", rt.t[:, 37:38], rt.t[:, 36:37], -1.0, None, ALU.mult, None, R_, R_)
            kb.act(rt.t[:, 44:48], rt.t[:, 32:36], AF.Exp, R_, R_, bias=rt.t[:, 37:38], scale=1.0)
            kb.op("dve", lambda e: e.reduce_max(out=rt.t[:, 38:39], in_=rt.t[:, 44:48], axis=mybir.AxisListType.X), reads=R_, writes=R_)
            kb.tt("dve", rt.t[:, 48:52], rt.t[:, 44:48], rt.t[:, 40:44], ALU.mult, R_, R_)
            kb.tt("dve", rt.t[:, 48:52], rt.t[:, 44:48], rt.t[:, 48:52], ALU.subtract, R_, R_)
            kb.op("dve", lambda e: e.reduce_max(out=rt.t[:, 39:40], in_=rt.t[:, 48:52], axis=mybir.AxisListType.X), reads=R_, writes=R_)
            kb.ts("dve", rt.t[:, 52:56], rt.t[:, 44:48], rt.t[:, 39:40], None, ALU.is_ge, None, R_, R_)
            kb.tt("dve", rt.t[:, 52:56], rt.t[:, 52:56], rt.t[:, 44:48], ALU.mult, R_, R_)
            kb.tt("dve", rt.t[:, 56:57], rt.t[:, 38:39], rt.t[:, 39:40], ALU.add, R_, R_)
            kb.tt("dve", rt.t[:, 56:57], rt.t[:, 56:57], rt.t[:, 22:23], ALU.mult, R_, R_)
            kb.op("dve", lambda e: e.reciprocal(out=rt.t[:, 57:58], in_=rt.t[:, 56:57]), reads=R_, writes=R_)
            kb.ts("dve", rt.t[:, 60:64], rt.t[:, 24:28], rt.t[:, 57:58], None, ALU.mult, None, R_, R_)
            for g in range(4):
                kb.ts("dve", c_all.t[:, gt, 4 * g:4 * g + 4], rt.t[:, 52:56], rt.t[:, 60 + g:61 + g], None, ALU.mult, None,
                      R_, [c_all])
            if s == 3 and not SPARSE:
                mt = gt // 4
                kb.dma("pool", xT_d[:, :, mt * 512:(mt + 1) * 512], x1T.t[:, :, :], x1T, reads=[x1T], writes=[xT_B[mt]])
            if SPARSE:
                rt2 = P["rt2"]
                mkb = P["mkb"]
                idx2 = P["idx2"][gt % 2]
                Q_ = [rt2]
                cg = c_all.t[:, gt, :]
                kb.ts("dve", rt2.t[:, 0:16], cg, 0.0, None, ALU.is_gt, None, [c_all], Q_)
                kb.copy("dve", mkb.t[:, :], rt2.t[:, 0:16], Q_, [mkb])
                kb.mmg(prt, prt.t[:, 32:48], [(Lstr.t[:, :], mkb.t[:, :])], reads=[Lstr, mkb])
                kb.mmg(prt, prt.t[:, 48:64], [(onesb.t[:, :], mkb.t[:, :])], reads=[onesb, mkb])
                kb.tt("dve", rt2.t[:, 16:32], prt.t[:, 32:48], ebase1.t[:, :], ALU.add, [prt, ebase1], Q_)
                kb.tt("dve", rt2.t[:, 16:32], rt2.t[:, 16:32], off_t.t[:, :], ALU.add, Q_ + [off_t], Q_)
                kb.tt("dve", rt2.t[:, 16:32], rt2.t[:, 16:32], rt2.t[:, 0:16], ALU.mult, Q_, Q_)
                kb.tt("dve", off_t.t[:, :], off_t.t[:, :], prt.t[:, 48:64], ALU.add, [off_t, prt], [off_t])
                kb.op("dve", lambda e: e.reduce_max(out=rt2.t[:, 64:65], in_=rt2.t[:, 16:32], axis=mybir.AxisListType.X), reads=Q_, writes=Q_)
                kb.ts("dve", rt2.t[:, 32:48], rt2.t[:, 16:32], rt2.t[:, 64:65], None, ALU.is_equal, None, Q_, Q_)
                kb.tt("dve", rt2.t[:, 48:64], rt2.t[:, 32:48], cg, ALU.mult, Q_ + [c_all], Q_)
                kb.op("dve", lambda e: e.reduce_sum(out=cAB.t[:, gt, 0:1], in_=rt2.t[:, 48:64], axis=mybir.AxisListType.X), reads=Q_, writes=[cAB])
                kb.op("dve", lambda e: e.reduce_sum(out=rt2.t[:, 66:67], in_=cg, axis=mybir.AxisListType.X), reads=[c_all], writes=Q_)
                kb.tt("dve", cAB.t[:, gt, 1:2], rt2.t[:, 66:67], cAB.t[:, gt, 0:1], ALU.subtract, Q_ + [cAB], [cAB])
                kb.tt("dve", rt2.t[:, 48:64], rt2.t[:, 16:32], rt2.t[:, 32:48], ALU.mult, Q_, Q_)
                kb.tt("dve", rt2.t[:, 48:64], rt2.t[:, 16:32], rt2.t[:, 48:64], ALU.subtract, Q_, Q_)
                kb.op("dve", lambda e: e.reduce_max(out=rt2.t[:, 67:68], in_=rt2.t[:, 48:64], axis=mybir.AxisListType.X), reads=Q_, writes=Q_)
                kb.ts("dve", rt2.t[:, 68:69], rt2.t[:, 67:68], 0.0, float(OOB), ALU.is_equal, ALU.mult, Q_, Q_)
                kb.tt("dve", rt2.t[:, 67:68], rt2.t[:, 67:68], rt2.t[:, 68:69], ALU.add, Q_, Q_)
                kb.ts("dve", idx2.t[:, 0:1], rt2.t[:, 64:65], -1.0, None, ALU.add, None, Q_, [idx2])
                kb.ts("dve", idx2.t[:, 1:2], rt2.t[:, 67:68], -1.0, None, ALU.add, None, Q_, [idx2])
                for r_ in range(2):
                    kb.idma(list_d[:, :], tl2.t[:, gt, r_, :], idx2.t[:, r_:r_ + 1], True, 16 * 4096 - 1, idx2,
                            reads=[idx2, tl2, P["listB"]])
                if gt == NT - 1:
                    kb.ts("dve", cnt_i.t[:, :], off_t.t[:, :], -1.0, 4096.0, ALU.mult, ALU.add, [off_t], [cnt_i])

        def post_mixer_allocs(ps_, lyr):
            P = {}
            P["r"] = [kb.sb("pm_r%d" % i, [128, D], F32, ps_) for i in range(2)]
            P["x1"] = [kb.sb("pm_x1%d" % i, [128, D], F32, ps_) for i in range(3)]
            P["x1b"] = [kb.sb("pm_x1b", [128, D], BF16, ps_) for _ in range(2)]
            P["rt2"] = kb.sb("pm_rt2", [128, 128], F32, ps_)
            P["mkb"] = kb.sb("pm_mkb", [128, 16], BF16, ps_)
            P["idx2"] = [kb.sb("pm_idx2", [128, 2], I32, ps_) for _ in range(2)]
            P["listB"] = B()
            kb.op("dve", lambda e: e.memset(off_t.t[:], 0.0), writes=[off_t])
            kb.dma("pool", list_d.rearrange("(p a) c -> p (a c)", p=128), oobt.t[:, :], oobt, reads=[oobt], writes=[P["listB"]])
            P["x1T"] = [kb.sb("pm_x1T", [128, 8, 128], BF16, ps_) for _ in range(2)]
            P["stats"] = kb.sb("pm_stats", [128, 12], F32, ps_)
            P["mv"] = kb.sb("pm_mv", [128, 2], F32, ps_)
            P["tmp"] = kb.sb("pm_tmp", [128, 2], F32, ps_)
            P["rt"] = kb.sb("pm_rt", [128, 64], F32, ps_)
            P["mg"] = kb.sb("pm_mg", [128, D], F32, ps_)
            P["mb"] = kb.sb("pm_mb", [128, D], F32, ps_)
            P["brb"] = kb.sb("pm_brb", [128, 20], F32, ps_)
            P["Wr"] = kb.sb("pm_Wr", [128, 8, 20], BF16, ps_)
            P["pT"] = kb.ps("pm_pT", [128, 1024], BF16, ps_)
            P["prt"] = kb.ps("pm_prt", [128, 512], F32, ps_)
            bcast_load(P["mg"], mix_g_d[lyr:lyr + 1, :])
            bcast_load(P["mb"], mix_b_d[lyr:lyr + 1, :])
            bcast_load(P["brb"], br_d[lyr:lyr + 1, :])
            kb.dma("pool", P["Wr"].t[:, :, :], wr_d[lyr].rearrange("(k p) n -> p k n", p=128), P["Wr"], writes=[P["Wr"]])
            return P

        def load_w(ps_, name, src_ap, ncols):
            W = kb.sb(name, [128, 8, ncols], BF16, ps_)
            v = src_ap.rearrange("(k p) n -> p k n", p=128)
            for k in range(8):
                kb.dma("pool", W.t[:, k, :], v[:, k, :], W, writes=[W])
            return W

        def load_xT(xsrc_d, xsrc_B, mt, xin, xbf, pT, xT):
            rd = [xsrc_B[mt * 4 + s] for s in range(4)] if xsrc_B is not None else []
            kb.dma("sp", xin.t[:, :, :], xsrc_d[mt * 512:(mt + 1) * 512, :].rearrange("(s p) d -> p s d", p=128), xin,
                   reads=rd, writes=[xin])
            for s in range(4):
                xb = xbf[s % 2]
                kb.copy("act", xb.t[:, :], xin.t[:, s, :], [xin], [xb])
                for k in range(8):
                    kb.op("pe", lambda e, k=k, xb=xb: e.transpose(pT.t[:, k * 128:(k + 1) * 128], xb.t[:, k * 128:(k + 1) * 128], ident.t[:]),
                          reads=[xb, ident], writes=[pT], mark=(k == 7))
                kb.copy("act", xT.t[:, :, s * 128:(s + 1) * 128], pT.t[:, :].rearrange("p (k t) -> p k t", k=8), [pT], [xT])

        def mixer0():
            with contextlib.ExitStack() as ps_:
                P = post_mixer_allocs(ps_, 0)
                Wi = load_w(ps_, "m0_Wi", w_in_d, 2560)
                Wo = load_w(ps_, "m0_Wo", w_out0_d, 1024)
                WsT = kb.sb("m0_WsT", [128, 8, 128], BF16, ps_)
                kb.dma("pool", WsT.t[:, :, :], wsT_d.rearrange("h s t -> s h t"), WsT, writes=[WsT])
                kb.op("pool", lambda e: e.memset(WsT.t[64:128, :, 0:64], 0.0), reads=[WsT], writes=[WsT])
                bsr = kb.sb("m0_bsr", [1, 1024], BF16, ps_)
                kb.dma("pool", bsr.t[:, :], bs_d[:, :], bsr, writes=[bsr])
                onesr = kb.sb("m0_ones", [1, 128], BF16, ps_)
                kb.op("pool", lambda e: e.memset(onesr.t[:, :], 1.0), writes=[onesr])
                cw = kb.sb("m0_cw", [128, 12], F32, ps_)
                kb.dma("sp", cw.t[:, :], convw_d[:, :], cw, writes=[cw])
                sg = kb.sb("m0_sg", [128, 512], F32, ps_)
                sbb = kb.sb("m0_sbb", [128, 512], F32, ps_)
                bcast_load(sg, sgu_g_d[0:1, :])
                bcast_load(sbb, sgu_b_d[0:1, :])
                xin = kb.sb("m0_xin", [128, 4, D], F32, ps_)
                xbf = [kb.sb("m0_xbf%d" % i, [128, D], BF16, ps_) for i in range(2)]
                xres = [kb.sb("m0_xres%d" % i, [128, D], F32, ps_) for i in range(2)]
                xT = [kb.sb("m0_xT", [128, 8, 512], BF16, ps_) for _ in range(2)]
                yT = [kb.sb("m0_yT", [128, 8, 512], BF16, ps_) for _ in range(2)]
                ub = kb.sb("m0_ub", [128, 4, 514], F32, ps_)
                Cs = kb.sb("m0_Cs", [128, 512], F32, ps_)
                acc = kb.sb("m0_acc", [128, 512], F32, ps_)
                zu = kb.sb("m0_zu", [128, 4, 512], F32, ps_)
                g1 = kb.sb("m0_g1", [128, 512], F32, ps_)
                g2 = kb.sb("m0_g2", [128, 512], F32, ps_)
                gv = kb.sb("m0_gv", [128, 512], F32, ps_)
                vt = kb.sb("m0_vt", [128, 4, 512], BF16, ps_)
                st2 = kb.sb("m0_st2", [128, 6], F32, ps_)
                mv2 = kb.sb("m0_mv2", [128, 2], F32, ps_)
                tmp2 = kb.sb("m0_tmp2", [128, 2], F32, ps_)
                pp = [kb.ps("m0_pp%d" % i, [128, 512], F32, ps_) for i in range(2)]
                psg = [kb.ps("m0_psg%d" % i, [128, 512], F32, ps_) for i in range(2)]
                pso = [kb.ps("m0_pso%d" % i, [128, 512], F32, ps_) for i in range(2)]
                pT = P["pT"]
                kb.op("pool", lambda e: e.memset(ub.t[:, :, :], 0.0), writes=[ub])
                ppi = [0]

                def proj_fm(c, xT):
                    p = pp[ppi[0] % 2]
                    ppi[0] += 1
                    kb.mmg(p, p.t[:, :], [(Wi.t[:, k, c * 128:(c + 1) * 128], xT.t[:, k, :]) for k in range(8)], reads=[Wi, xT])
                    return p

                def gelu(p, p_ap, outB, out_ap, n):
                    kb.act(g1.t[:, 0:n], p_ap, AF.Square, [p], [g1])
                    kb.act(g1.t[:, 0:n], g1.t[:, 0:n], AF.Identity, [g1], [g1], bias=1.0, scale=0.044715)
                    kb.tt("dve", g2.t[:, 0:n], g1.t[:, 0:n], p_ap, ALU.mult, [g1, p], [g2])
                    kb.act(g1.t[:, 0:n], g2.t[:, 0:n], AF.Exp, [g2], [g1], scale=-1.5957691216057308)
                    kb.act(g1.t[:, 0:n], g1.t[:, 0:n], AF.Identity, [g1], [g1], bias=1.0, scale=1.0)
                    kb.op("dve", lambda e: e.reciprocal(out=g2.t[:, 0:n], in_=g1.t[:, 0:n]), reads=[g1], writes=[g2])
                    kb.tt("dve", out_ap, g2.t[:, 0:n], p_ap, ALU.mult, [g2, p], [outB])

                def piece(mt, j):
                    xTb = xT[mt % 2]
                    yTb = yT[mt % 2]
                    u = ub
                    pC = proj_fm(4 + j, xTb)
                    kb.copy("act", Cs.t[:, :], pC.t[:, :], [pC], [Cs])
                    pH = proj_fm(8 + j, xTb)
                    kb.copy("dve", u.t[:, j, 0:2], u.t[:, j, 512:514], [u], [u])
                    kb.tt("dve", u.t[:, j, 2:514], Cs.t[:, :], pH.t[:, :], ALU.mult, [Cs, pH], [u])
                    kb.ts("dve", acc.t[:, :], u.t[:, j, 2:514], cw.t[:, j * 3 + 2:j * 3 + 3], None, ALU.mult, None, [u, cw], [acc])
                    kb.stt("dve", acc.t[:, :], u.t[:, j, 1:513], cw.t[:, j * 3 + 1:j * 3 + 2], acc.t[:, :], ALU.mult, ALU.add, [u, cw, acc], [acc])
                    kb.stt("dve", acc.t[:, :], u.t[:, j, 0:512], cw.t[:, j * 3:j * 3 + 1], acc.t[:, :], ALU.mult, ALU.add, [u, cw, acc], [acc])
                    pB = proj_fm(j, xTb)
                    kb.tt("dve", yTb.t[:, j, :], acc.t[:, :], pB.t[:, :], ALU.mult, [acc, pB], [yTb])
                    pZ = proj_fm(12 + j, xTb)
                    gelu(pZ, pZ.t[:, :], zu, zu.t[:, j, :], 512)
                    p = pp[ppi[0] % 2]
                    ppi[0] += 1
                    kb.mmg(p, p.t[:, :], [(xTb.t[:, k, j * 128:(j + 1) * 128], Wi.t[:, k, 2048:2560]) for k in range(8)], reads=[Wi, xTb])
                    gelu(p, p.t[:, :], gv, gv.t[:, :], 512)
                    layernorm(ps_, gv, 512, sg, sbb, vt, vt.t[:, j, :], st2, mv2, tmp2, mul_eng="pool")

                def sgu(mt):
                    yTb = yT[mt % 2]
                    for hp in range(4):
                        for j in range(2):
                            h = 2 * hp + j
                            pg = psg[j]
                            for s in range(4):
                                kb.op("pe", lambda e, s=s, pg=pg, h=h, hp=hp: e.matmul(pg.t[:, s * 128:(s + 1) * 128], lhsT=vt.t[:, s, hp * 128:(hp + 1) * 128],
                                                                         rhs=WsT.t[:, h, :], start=True, stop=False),
                                      reads=[vt, WsT], writes=[pg], mark=False)
                                kb.op("pe", lambda e, s=s, pg=pg, h=h: e.matmul(pg.t[:, s * 128:(s + 1) * 128], lhsT=onesr.t[0:1, :],
                                                                   rhs=bsr.t[0:1, h * 128:(h + 1) * 128], start=False, stop=True),
                                      reads=[onesr, bsr], writes=[pg], mark=(s == 3))
                            kb.tt("dve", yTb.t[j * 64:(j + 1) * 64, 4 + hp, :], zu.t[j * 64:(j + 1) * 64, hp, :], pg.t[j * 64:(j + 1) * 64, :],
                                  ALU.mult, [zu, pg], [yTb])

                def back(mt, s):
                    gt = mt * 4 + s
                    xr = xres[gt % 2]
                    kb.dma("sp", xr.t[:, :], x_d[gt * 128:(gt + 1) * 128, :], xr, writes=[xr])
                    post_mixer(ps_, 0, gt, xr.t[:, :], xr, pso, yT[mt % 2], slice(s * 128, (s + 1) * 128), Wo, P, stage=1)
                    if gt > 0:
                        post_mixer(ps_, 0, gt - 1, None, None, pso, None, None, Wo, P, stage=2)

                load_xT(x_d, None, 0, xin, xbf, pT, xT[0])
                for j in range(4):
                    piece(0, j)
                sgu(0)
                for mt in range(8):
                    if mt + 1 < 8:
                        load_xT(x_d, None, mt + 1, xin, xbf, pT, xT[(mt + 1) % 2])
                    for j in range(4):
                        if mt + 1 < 8:
                            piece(mt + 1, j)
                        back(mt, j)
                    if mt + 1 < 8:
                        sgu(mt + 1)
                post_mixer(ps_, 0, NT - 1, None, None, pso, None, None, Wo, P, stage=2)
                kb.barrier()
                kb.release_sems()

        def moe(lyr, dst_d, dst_B):
            with contextlib.ExitStack() as ps_:
                fg = kb.sb("f_g", [128, D], F32, ps_)
                fb = kb.sb("f_b", [128, D], F32, ps_)
                bcast_load(fg, ffn_g_d[lyr:lyr + 1, :])
                bcast_load(fb, ffn_b_d[lyr:lyr + 1, :])
                xTh = kb.sb("f_xTh", [128, 8, 2048], BF16, ps_)
                yacc = kb.sb("f_yacc", [128, 16, D], F32, ps_)
                w1s = [kb.sb("f_w1_%d" % i, [128, 8, 512], BF16, ps_) for i in range(2)]
                w3s = [kb.sb("f_w3_%d" % i, [128, 8, 512], BF16, ps_) for i in range(2)]
                w2s = [kb.sb("f_w2_%d" % i, [128, 4, D], BF16, ps_) for i in range(2)]
                hid = [kb.sb("f_hid%d" % i, [128, 4, 512], BF16, ps_) for i in range(2)]
                sl = [kb.sb("f_sl%d" % i, [128, 512], BF16, ps_) for i in range(2)]
                x1l = [kb.sb("f_x1l%d" % i, [128, D], F32, ps_) for i in range(2)]
                rr = [kb.sb("f_r%d" % i, [128, D], F32, ps_) for i in range(2)]
                st = kb.sb("f_st", [128, 12], F32, ps_)
                mv = kb.sb("f_mv", [128, 2], F32, ps_)
                tmp = kb.sb("f_tmp", [128, 2], F32, ps_)
                ph1 = [kb.ps("f_ph1_%d" % i, [128, 512], F32, ps_) for i in range(2)]
                ph3 = [kb.ps("f_ph3_%d" % i, [128, 512], F32, ps_) for i in range(2)]
                py = [kb.ps("f_py%d" % i, [128, 512], F32, ps_) for i in range(2)]

                def load_expert(e):
                    sl_ = e % 2
                    kb.dma("pool", w1s[sl_].t[:, :, :], w1_d[lyr, e].rearrange("(k p) f -> p k f", p=128), w1s[sl_], writes=[w1s[sl_]])
                    kb.dma("pool", w3s[sl_].t[:, :, :], w3_d[lyr, e].rearrange("(k p) f -> p k f", p=128), w3s[sl_], writes=[w3s[sl_]])
                    kb.dma("pool", w2s[sl_].t[:, :, :], w2_d[lyr, e].rearrange("(c p) d -> p c d", p=128), w2s[sl_], writes=[w2s[sl_]])

                cnt = [0]
                for hf in range(2):
                    kb.dma("sp", xTh.t[:, :, :], xT_d[:, :, hf * 2048:(hf + 1) * 2048], xTh,
                           reads=[xT_B[hf * 4 + i] for i in range(4)], writes=[xTh])
                    load_expert(0)
                    for e in range(16):
                        if e + 1 < 16:
                            load_expert(e + 1)
                        w1 = w1s[e % 2]
                        w3 = w3s[e % 2]
                        w2 = w2s[e % 2]
                        for mt in range(4):
                            hd = hid[(e * 4 + mt) % 2]
                            for fc in range(4):
                                i2 = cnt[0] % 2
                                cnt[0] += 1
                                xs = xTh.t[:, :, mt * 512:(mt + 1) * 512]
                                kb.mmg(ph1[i2], ph1[i2].t[:, :], [(w1.t[:, k, fc * 128:(fc + 1) * 128], xTh.t[:, k, mt * 512:(mt + 1) * 512]) for k in range(8)],
                                       reads=[w1, xTh])
                                kb.mmg(ph3[i2], ph3[i2].t[:, :], [(w3.t[:, k, fc * 128:(fc + 1) * 128], xTh.t[:, k, mt * 512:(mt + 1) * 512]) for k in range(8)],
                                       reads=[w3, xTh])
                                kb.act(sl[i2].t[:, :], ph1[i2].t[:, :], AF.Silu, [ph1[i2]], [sl[i2]])
                                kb.tt("dve", hd.t[:, fc, :], sl[i2].t[:, :], ph3[i2].t[:, :], ALU.mult, [sl[i2], ph3[i2]], [hd])
                            for s in range(4):
                                ti = mt * 4 + s
                                gt = hf * 16 + ti
                                for dh in range(2):
                                    p = py[dh]
                                    kb.mmg(p, p.t[:, :], [(hd.t[:, fc, s * 128:(s + 1) * 128], w2.t[:, fc, dh * 512:(dh + 1) * 512]) for fc in range(4)],
                                           reads=[hd, w2])
                                    ya = yacc.t[:, ti, dh * 512:(dh + 1) * 512]
                                    if e == 0:
                                        kb.ts("dve", ya, p.t[:, :], c_all.t[:, gt, e:e + 1], None, ALU.mult, None, [p, c_all], [yacc])
                                    else:
                                        kb.stt("dve", ya, p.t[:, :], c_all.t[:, gt, e:e + 1], ya, ALU.mult, ALU.add, [p, c_all, yacc], [yacc])
                    for ti in range(16):
                        gt = hf * 16 + ti
                        xl = x1l[ti % 2]
                        r = rr[ti % 2]
                        kb.dma("sp", xl.t[:, :], xa_d[gt * 128:(gt + 1) * 128, :], xl, reads=[xa_B[gt]], writes=[xl])
                        kb.stt("dve", r.t[:, :], xl.t[:, :], ALPHA, yacc.t[:, ti, :], ALU.mult, ALU.add, [xl, yacc], [r])
                        layernorm(ps_, r, D, fg, fb, xl, xl.t[:, :], st, mv, tmp)
                        kb.dma("pool", dst_d[gt * 128:(gt + 1) * 128, :], xl.t[:, :], xl, reads=[xl], writes=[dst_B[gt]])
                kb.barrier()
                kb.release_sems()

        def moe_sparse(lyr, dst_d, dst_B):
            with contextlib.ExitStack() as ps_:
                fg = kb.sb("f_g", [128, D], F32, ps_)
                fb = kb.sb("f_b", [128, D], F32, ps_)
                bcast_load(fg, ffn_g_d[lyr:lyr + 1, :])
                bcast_load(fb, ffn_b_d[lyr:lyr + 1, :])
                w1s = [kb.sb("f_w1", [128, 8, 512], BF16, ps_) for i in range(2)]
                w3s = [kb.sb("f_w3", [128, 8, 512], BF16, ps_) for i in range(2)]
                w2s = [kb.sb("f_w2", [128, 4, D], BF16, ps_) for i in range(2)]
                xg = [kb.sb("f_xg", [128, D], BF16, ps_) for i in range(8)]
                lst = [kb.sb("f_lst", [128, 2], I32, ps_) for i in range(8)]
                xgT = [kb.sb("f_xgT", [128, 8, 128], BF16, ps_) for i in range(8)]
                sl = [kb.sb("f_sl", [128, 512], BF16, ps_) for i in range(2)]
                hid = [kb.sb("f_hid", [128, 4, 128], BF16, ps_) for i in range(2)]
                ysb = [kb.sb("f_ysb", [128, D], F32, ps_) for i in range(4)]
                y2l = [kb.sb("f_y2l", [128, 2, D], F32, ps_) for i in range(2)]
                x1l = [kb.sb("f_x1l", [128, D], F32, ps_) for i in range(2)]
                rr = [kb.sb("f_r", [128, D], F32, ps_) for i in range(2)]
                st = kb.sb("f_st", [128, 12], F32, ps_)
                mv = kb.sb("f_mv", [128, 2], F32, ps_)
                tmp = kb.sb("f_tmp", [128, 2], F32, ps_)
                pT = kb.ps("f_pT", [128, 1024], BF16, ps_)
                ph1 = [kb.ps("f_ph1", [128, 512], F32, ps_) for i in range(2)]
                ph3 = [kb.ps("f_ph3", [128, 512], F32, ps_) for i in range(2)]
                py = [kb.ps("f_py", [128, 512], F32, ps_) for i in range(2)]
                for i in range(8):
                    kb.op("dve", lambda e, i=i: e.memset(xg[i].t[:, :], 0.0), writes=[xg[i]])

                def load_expert(e):
                    sl_ = e % 2
                    kb.dma("pool", w1s[sl_].t[:, :, :], w1_d[lyr, e].rearrange("(k p) f -> p k f", p=128), w1s[sl_], writes=[w1s[sl_]])
                    kb.dma("pool", w3s[sl_].t[:, :, :], w3_d[lyr, e].rearrange("(k p) f -> p k f", p=128), w3s[sl_], writes=[w3s[sl_]])
                    kb.dma("pool", w2s[sl_].t[:, :, :], w2_d[lyr, e].rearrange("(c p) d -> p c d", p=128), w2s[sl_], writes=[w2s[sl_]])

                load_expert(0)
                for e in range(16):
                    if e + 1 < 16:
                        load_expert(e + 1)
                    w1 = w1s[e % 2]
                    w3 = w3s[e % 2]
                    w2 = w2s[e % 2]
                    for reg in kb.mregs:
                        E_ = kb.E[kb.etmap[reg.engine]]
                        kb._waits(E_, [cnt_i], [])
                        E_.eng.reg_load(reg, cnt_i.t[0:1, e:e + 1])

                    def fetch(j, e=e):
                        i4 = (e % 2) * 4 + j % 4
                        base = e * 4096 + j * 128
                        kb.dma("sp", lst[i4].t[:, :], list_d[base:base + 128, :], lst[i4], writes=[lst[i4]])
                        kb.idma(xg[i4].t[:, :], x1b_d[:, :], lst[i4].t[:, 0:1], False, T - 1, xg[i4], reads=[lst[i4]], writes=[xg[i4]])

                    def transp(j, e=e):
                        i4 = (e % 2) * 4 + j % 4
                        for k in range(8):
                            kb.op("pe", lambda e_, k=k: e_.transpose(pT.t[:, k * 128:(k + 1) * 128], xg[i4].t[:, k * 128:(k + 1) * 128], ident.t[:]),
                                  reads=[xg[i4], ident], writes=[pT], mark=(k == 7))
                        kb.copy("act", xgT[i4].t[:, :, :], pT.t[:, :].rearrange("p (k t) -> p k t", k=8), [pT], [xgT[i4]])

                    def hpart(j, w1=w1, w3=w3, e=e):
                        i2 = j % 2
                        i4 = (e % 2) * 4 + j % 4
                        for fc in range(4):
                            kb.mmg(ph1[i2], ph1[i2].t[:, fc * 128:(fc + 1) * 128],
                                   [(w1.t[:, k, fc * 128:(fc + 1) * 128], xgT[i4].t[:, k, :]) for k in range(8)], reads=[w1, xgT[i4]])
                        for fc in range(4):
                            kb.mmg(ph3[i2], ph3[i2].t[:, fc * 128:(fc + 1) * 128],
                                   [(w3.t[:, k, fc * 128:(fc + 1) * 128], xgT[i4].t[:, k, :]) for k in range(8)], reads=[w3, xgT[i4]])
                        kb.act(sl[i2].t[:, :], ph1[i2].t[:, :], AF.Silu, [ph1[i2]], [sl[i2]])
                        kb.tt("dve", hid[i2].t[:, :, :].rearrange("p c t -> p (c t)"), sl[i2].t[:, :], ph3[i2].t[:, :], ALU.mult,
                              [sl[i2], ph3[i2]], [hid[i2]])

                    def ypart(j, w2=w2, e=e):
                        i2 = j % 2
                        i4 = j % 4
                        il = (e % 2) * 4 + j % 4
                        for dh in range(2):
                            kb.mmg(py[dh], py[dh].t[:, :], [(hid[i2].t[:, fc, :], w2.t[:, fc, dh * 512:(dh + 1) * 512]) for fc in range(4)],
                                   reads=[hid[i2], w2])
                        kb.copy("act", ysb[i4].t[:, 0:512], py[0].t[:, :], [py[0]], [ysb[i4]])
                        kb.copy("dve", ysb[i4].t[:, 512:1024], py[1].t[:, :], [py[1]], [ysb[i4]])
                        kb.idma(y2_d[:, :], ysb[i4].t[:, :], lst[il].t[:, 1:2], True, 2 * T - 1, ysb[i4], reads=[ysb[i4], lst[il]])

                    def slot2(k):
                        A = 2 * k
                        Bq = 2 * k + 1
                        hpart(A)
                        hpart(Bq)
                        ypart(A)
                        if A + 2 < NT:
                            fetch(A + 2)
                            transp(A + 2) if False else None
                        ypart(Bq)
                        if Bq + 2 < NT:
                            fetch(Bq + 2)

                    if e == 0:
                        for j_ in range(4):
                            fetch(j_)
                        transp(0)
                        transp(1)
                    if e + 1 < 16:
                        for j_ in range(4):
                            fetch(j_, e + 1)
                    NFLAT = 3

                    def slot2t(k):
                        A = 2 * k
                        hpart(A)
                        hpart(A + 1)
                        if A + 2 < NT:
                            transp(A + 2)
                        ypart(A)
                        if A + 3 < NT:
                            transp(A + 3)
                        ypart(A + 1)
                        if A + 4 < NT:
                            fetch(A + 4)
                        if A + 5 < NT:
                            fetch(A + 5)

                    for k in range(NFLAT):
                        kb.region(4096 - k * 256, lambda k=k: slot2t(k))
                        if k == 0 and e + 1 < 16:
                            transp(0, e + 1)
                            transp(1, e + 1)

                    def rest():
                        for k in range(NFLAT, NT // 2):
                            kb.region(4096 - k * 256, lambda k=k: slot2t(k))
                    kb.region(4096 - NFLAT * 256, rest)
                kb.barrier()
                kb.release_sems()
                for gt in range(NT):
                    yl = y2l[gt % 2]
                    xl = x1l[gt % 2]
                    r = rr[gt % 2]
                    kb.dma("sp", yl.t[:, :, :], y2_d[gt * 256:(gt + 1) * 256, :].rearrange("(p r) d -> p r d", r=2), yl, writes=[yl])
                    kb.dma("sp", xl.t[:, :], xa_d[gt * 128:(gt + 1) * 128, :], xl, reads=[xa_B[gt]], writes=[xl])
                    kb.op("act", lambda e, yl=yl, gt=gt: e.mul(out=yl.t[:, 0, :], in_=yl.t[:, 0, :], mul=cAB.t[:, gt, 0:1]), reads=[yl, cAB], writes=[yl])
                    kb.stt("dve", yl.t[:, 0, :], yl.t[:, 1, :], cAB.t[:, gt, 1:2], yl.t[:, 0, :], ALU.mult, ALU.add, [yl, cAB], [yl])
                    kb.stt("dve", r.t[:, :], xl.t[:, :], ALPHA, yl.t[:, 0, :], ALU.mult, ALU.add, [xl, yl], [r])
                    layernorm(ps_, r, D, fg, fb, xl, xl.t[:, :], st, mv, tmp)
                    kb.dma("pool", dst_d[gt * 128:(gt + 1) * 128, :], xl.t[:, :], xl, reads=[xl], writes=[dst_B[gt]])
                kb.barrier()
                kb.release_sems()

        def attention():
            with contextlib.ExitStack() as ps_:
                Wq = load_w(ps_, "a_Wqkv", w_qkv_d, 3072)
                xin = kb.sb("a_xin", [128, 4, D], F32, ps_)
                xbf = [kb.sb("a_xbf%d" % i, [128, D], BF16, ps_) for i in range(2)]
                xT = kb.sb("a_xT", [128, 8, 512], BF16, ps_)
                qm = [kb.sb("a_qm%d" % i, [128, 8, 512], BF16, ps_) for i in range(2)]
                km = [kb.sb("a_km%d" % i, [128, 8, 512], BF16, ps_) for i in range(2)]
                vm = [kb.sb("a_vm%d" % i, [128, 4, D], BF16, ps_) for i in range(2)]
                pT = kb.ps("a_pT", [128, 1024], BF16, ps_)
                pp = [kb.ps("a_pp%d" % i, [128, 512], F32, ps_) for i in range(4)]
                ppi = 0
                for mt in range(8):
                    load_xT(xb_d, xb_B, mt, xin, xbf, pT, xT)
                    q_ = qm[mt % 2]
                    k_ = km[mt % 2]
                    v_ = vm[mt % 2]
                    for hp in range(8):
                        p = pp[ppi % 4]; ppi += 1
                        kb.mmg(p, p.t[:, :], [(Wq.t[:, k, hp * 128:(hp + 1) * 128], xT.t[:, k, :]) for k in range(8)], reads=[Wq, xT])
                        kb.op("act", lambda e, p=p, hp=hp, q_=q_: e.mul(out=q_.t[:, hp, :], in_=p.t[:, :], mul=0.125), reads=[p], writes=[q_])
                        p = pp[ppi % 4]; ppi += 1
                        kb.mmg(p, p.t[:, :], [(Wq.t[:, k, 1024 + hp * 128:1024 + (hp + 1) * 128], xT.t[:, k, :]) for k in range(8)], reads=[Wq, xT])
                        kb.copy("dve", k_.t[:, hp, :], p.t[:, :], [p], [k_])
                    for s in range(4):
                        for dh in range(2):
                            p = pp[ppi % 4]; ppi += 1
                            kb.mmg(p, p.t[:, :], [(xT.t[:, k, s * 128:(s + 1) * 128], Wq.t[:, k, 2048 + dh * 512:2048 + (dh + 1) * 512]) for k in range(8)],
                                   reads=[Wq, xT])
                            kb.copy("dve" if dh == 0 else "act", v_.t[:, s, dh * 512:(dh + 1) * 512], p.t[:, :], [p], [v_])
                    cs = slice(mt * 512, (mt + 1) * 512)
                    kb.dma("pool", qT_d.rearrange("h p t -> p h t")[:, :, cs], q_.t[:, :, :], q_, reads=[q_], writes=[q_B[mt]])
                    kb.dma("pool", kT_d.rearrange("h p t -> p h t")[:, :, cs], k_.t[:, :, :], k_, reads=[k_], writes=[k_B[mt]])
                    kb.dma("pool", v_d[mt * 512:(mt + 1) * 512, :].rearrange("(s p) d -> p s d", p=128), v_.t[:, :, :], v_, reads=[v_], writes=[v_B[mt]])
                kb.barrier()
                kb.release_sems()

            with contextlib.ExitStack() as ps_:
                oT = kb.sb("a_oT", [128, 8, T], BF16, ps_)
                masks = kb.sb("a_mask", [128, 4, 512], BF16, ps_)
                negU = kb.sb("a_negU", [128, 128], BF16, ps_)
                negO = kb.sb("a_negO", [128, 128], BF16, ps_)
                zer = kb.sb("a_zero", [128, 128], BF16, ps_)
                kb.op("pool", lambda e: e.memset(masks.t[:, :, :], 1.0), writes=[masks])
                for m in range(4):
                    kb.op("pool", lambda e, m=m: e.affine_select(out=masks.t[:, m, :], in_=masks.t[:, m, :], pattern=[[1, 512]],
                                                                compare_op=ALU.is_gt, fill=0.0, base=-128 * m, channel_multiplier=-1),
                          reads=[masks], writes=[masks])
                kb.op("pool", lambda e: e.memset(negU.t[:, :], -1.0), writes=[negU])
                kb.op("pool", lambda e: e.affine_select(out=negU.t[:, :], in_=negU.t[:, :], pattern=[[-1, 128]],
                                                        compare_op=ALU.is_ge, fill=0.0, base=0, channel_multiplier=1),
                      reads=[negU], writes=[negU])
                kb.op("pool", lambda e: e.memset(negO.t[:, :], -1.0), writes=[negO])
                kb.op("pool", lambda e: e.memset(zer.t[:, :], 0.0), writes=[zer])
                with contextlib.ExitStack() as ps2:
                    qh = [kb.sb("a_qh%d" % i, [128, T], BF16, ps2) for i in range(2)]
                    kh = [kb.sb("a_kh%d" % i, [128, T], BF16, ps2) for i in range(2)]
                    vh = [kb.sb("a_vh%d" % i, [128, NT, 128], BF16, ps2) for i in range(2)]
                    ex = [[kb.sb("a_ex", [128, 512], F32, ps2) for _ in range(2)] for c in range(2)]
                    lnu = [[kb.sb("a_lnu", [128, 512], BF16, ps2) for _ in range(3)] for c in range(2)]
                    Sb = [[kb.sb("a_S", [128, 512], BF16, ps2) for _ in range(4)] for c in range(2)]
                    att = [[kb.sb("a_att", [128, 512], BF16, ps2) for _ in range(2)] for c in range(2)]
                    pz = [[kb.ps("a_pz", [128, 512], F32, ps2) for _ in range(1)] for c in range(2)]
                    pM = [kb.ps("a_pM", [128, 512], F32, ps2) for c in range(2)]
                    pc = [kb.ps("a_pc", [128, 512], F32, ps2) for c in range(2)]
                    pR = kb.ps("a_pR", [128, 512], F32, ps2)
                    pcC = kb.ps("a_pcC", [128, 512], F32, ps2)
                    exC = [kb.sb("a_exC", [128, 512], F32, ps2) for _ in range(2)]
                    lnuC = [kb.sb("a_lnuC", [128, 512], BF16, ps2) for _ in range(2)]
                    SC = [kb.sb("a_SC", [128, 512], BF16, ps2) for _ in range(2)]
                    attC = [kb.sb("a_attC", [128, 512], BF16, ps2) for _ in range(2)]
                    Ssave = [kb.sb("a_Ssave", [128, 512], BF16, ps2) for _ in range(2)]
                    flg = kb.sb("a_flg", [128, 128], I32, ps2)
                    flagB = [B() for _ in range(128)]
                    rmin = kb.sb("a_rmin", [128, 2], F32, ps2)
                    THR = 120.0
                    NU = 6

                    def load_hp(hp):
                        i2 = hp % 2
                        kb.dma("sp", qh[i2].t[:, :], qT_d[hp], qh[i2], reads=q_B, writes=[qh[i2]])
                        kb.dma("sp", kh[i2].t[:, :], kT_d[hp], kh[i2], reads=k_B, writes=[kh[i2]])
                        kb.dma("sp", vh[i2].t[:, :, :], v_d.rearrange("(b p) d -> p b d", p=128)[:, :, hp * 128:(hp + 1) * 128], vh[i2],
                               reads=v_B, writes=[vh[i2]])

                    class It:
                        pass

                    nctr = [0, 0]

                    def make_items(hp, c):
                        items = []
                        for i in range(8):
                            blo = max(4 * i + 3 - (NU - 1), 0)
                            for b in range(4 * i + 3, blo - 1, -1):
                                it = It()
                                it.c = c
                                it.hp = hp
                                it.i = i
                                it.b = b
                                it.first = (b == 4 * i + 3)
                                it.last = (b == 0)
                                it.ulast = (b == blo)
                                it.hasC = (blo > 0)
                                it.fcol = (hp * 2 + c) * 8 + i
                                it.m = b - 4 * i
                                it.qlo = 128 * it.m if it.m > 0 else 0
                                it.n = nctr[c]
                                nctr[c] += 1
                                items.append(it)
                        return items

                    def stageA(it):
                        c = it.c
                        q_ = qh[it.hp % 2]
                        k_ = kh[it.hp % 2]
                        pr = slice(c * 64, (c + 1) * 64)
                        qlo = it.qlo
                        n = 512 - qlo
                        qs = slice(it.i * 512 + qlo, (it.i + 1) * 512)
                        ks = slice(it.b * 128, (it.b + 1) * 128)
                        z = pz[c][0]
                        e_ = ex[c][it.n % 2]
                        l_ = lnu[c][it.n % 3]
                        kb.mmg(z, z.t[:, 0:n], [(k_.t[pr, ks], q_.t[pr, qs])], reads=[k_, q_])
                        kb.act(e_.t[:, 0:n], z.t[:, 0:n], AF.Exp, [z], [e_])
                        kb.act(l_.t[:, 0:n], e_.t[:, 0:n], AF.Ln, [e_], [l_], bias=1.0, scale=1.0)
                        if it.m >= 0:
                            kb.tt("pool", l_.t[:, 0:n], l_.t[:, 0:n], masks.t[:, it.m, qlo:512], ALU.mult, [l_, masks], [l_])
                        if not it.last:
                            Sn = Sb[c][it.n % 4]
                            if it.first:
                                if qlo > 0:
                                    kb.op("dve", lambda e: e.memset(Sn.t[:, 0:qlo], 0.0), writes=[Sn])
                                kb.copy("dve", Sn.t[:, qlo:512], l_.t[:, 0:n], [l_], [Sn])
                            else:
                                Sp = Sb[c][(it.n - 1) % 4]
                                if qlo > 0:
                                    kb.copy("dve", Sn.t[:, 0:qlo], Sp.t[:, 0:qlo], [Sp], [Sn])
                                kb.tt("dve", Sn.t[:, qlo:512], Sp.t[:, qlo:512], l_.t[:, 0:n], ALU.add, [Sp, l_], [Sn])
                            if it.ulast and it.hasC:
                                kb.mmg(pR, pR.t[:, :], [(onesb.t[:, :], Sn.t[:, :])], reads=[onesb, Sn])
                                kb.op("dve", lambda e: e.tensor_reduce(out=rmin.t[:, c:c + 1], in_=pR.t[:, :], axis=mybir.AxisListType.X, op=ALU.min),
                                      reads=[pR], writes=[rmin])
                                kb.ts("dve", flg.t[:, it.fcol:it.fcol + 1], rmin.t[:, c:c + 1], THR, None, ALU.is_gt, None, [rmin], [flagB[it.fcol]])
                                kb.copy("dve", Ssave[c].t[:, :], Sn.t[:, :], [Sn], [Ssave[c]])

                    def stageB(it):
                        c = it.c
                        q_ = qh[it.hp % 2]
                        k_ = kh[it.hp % 2]
                        pr = slice(c * 64, (c + 1) * 64)
                        qlo = it.qlo
                        n = 512 - qlo
                        qs = slice(it.i * 512 + qlo, (it.i + 1) * 512)
                        ks = slice(it.b * 128, (it.b + 1) * 128)
                        l_ = lnu[c][it.n % 3]
                        a_ = att[c][it.n % 2]
                        M = pM[c]
                        prs = [(k_.t[pr, ks], q_.t[pr, qs]), (negU.t[:, :], l_.t[:, 0:n])]
                        rds = [k_, q_, negU, l_]
                        if not it.first:
                            Sp = Sb[c][(it.n - 1) % 4]
                            prs.append((negO.t[:, :], Sp.t[:, qlo:512]))
                            rds.append(Sp)
                        kb.mmg(M, M.t[:, 0:n], prs, reads=rds)
                        kb.act(a_.t[:, 0:n], M.t[:, 0:n], AF.Exp, [M], [a_])
                        if it.m >= 0:
                            kb.tt("pool", a_.t[:, 0:n], a_.t[:, 0:n], masks.t[:, it.m, qlo:512], ALU.mult, [a_, masks], [a_])

                    def stageC(it):
                        c = it.c
                        v_ = vh[it.hp % 2]
                        pr = slice(c * 64, (c + 1) * 64)
                        qlo = it.qlo
                        n = 512 - qlo
                        a_ = att[c][it.n % 2]
                        pcb = pc[c]
                        if it.first:
                            kb.op("pe", lambda e: e.matmul(pcb.t[:, :], lhsT=zer.t[:, :], rhs=masks.t[:, 0, :], start=True, stop=False),
                                  reads=[zer, masks], writes=[pcb], mark=False)
                        kb.op("pe", lambda e: e.matmul(pcb.t[:, qlo:512], lhsT=v_.t[:, it.b, :], rhs=a_.t[:, 0:n], start=False, stop=it.ulast),
                              reads=[v_, a_], writes=[pcb], mark=True)
                        if it.ulast:
                            kb.copy("dve", oT.t[pr, it.hp, it.i * 512:(it.i + 1) * 512], pcb.t[pr, :], [pcb], [oT])

                    def cond_rest(it):
                        c = it.c
                        q_ = qh[it.hp % 2]
                        k_ = kh[it.hp % 2]
                        v_ = vh[it.hp % 2]
                        pr = slice(c * 64, (c + 1) * 64)
                        qs = slice(it.i * 512, (it.i + 1) * 512)
                        for reg in kb.mregs:
                            E_ = kb.E[kb.etmap[reg.engine]]
                            kb._waits(E_, [flagB[it.fcol]], [])
                            E_.eng.reg_load(reg, flg.t[0:1, it.fcol:it.fcol + 1])

                        def cbody():
                            kb.op("pe", lambda e: e.matmul(pcC.t[:, :], lhsT=zer.t[:, :], rhs=masks.t[:, 0, :], start=True, stop=False),
                                  reads=[zer, masks], writes=[pcC], mark=False)
                            Sprev = Ssave[c]
                            for n2, b in enumerate(range(it.b - 1, -1, -1)):
                                ks = slice(b * 128, (b + 1) * 128)
                                z = pz[c][0]
                                M = pM[c]
                                e_ = exC[n2 % 2]
                                l_ = lnuC[n2 % 2]
                                a_ = attC[n2 % 2]
                                kb.mmg(z, z.t[:, :], [(k_.t[pr, ks], q_.t[pr, qs])], reads=[k_, q_])
                                kb.act(e_.t[:, :], z.t[:, :], AF.Exp, [z], [e_])
                                kb.act(l_.t[:, :], e_.t[:, :], AF.Ln, [e_], [l_], bias=1.0, scale=1.0)
                                kb.mmg(M, M.t[:, :], [(k_.t[pr, ks], q_.t[pr, qs]), (negU.t[:, :], l_.t[:, :]), (negO.t[:, :], Sprev.t[:, :])],
                                       reads=[k_, q_, negU, l_, negO, Sprev])
                                kb.act(a_.t[:, :], M.t[:, :], AF.Exp, [M], [a_])
                                if b > 0:
                                    Sn = SC[n2 % 2]
                                    kb.tt("dve", Sn.t[:, :], Sprev.t[:, :], l_.t[:, :], ALU.add, [Sprev, l_], [Sn])
                                    Sprev = Sn
                                kb.op("pe", lambda e, b=b, a_=a_: e.matmul(pcC.t[:, :], lhsT=v_.t[:, b, :], rhs=a_.t[:, :], start=False, stop=(b == 0)),
                                      reads=[v_, a_], writes=[pcC], mark=True)
                            oc = oT.t[pr, it.hp, it.i * 512:(it.i + 1) * 512]
                            kb.tt("dve", oc, oc, pcC.t[pr, :], ALU.add, [oT, pcC], [oT])

                        kb.region(1, cbody)

                    load_hp(0)
                    for hp in range(8):
                        if hp + 1 < 8:
                            load_hp(hp + 1)
                        l0 = make_items(hp, 0)
                        l1 = make_items(hp, 1)
                        L = []
                        for a, b_ in zip(l0, l1):
                            L.append(a)
                            L.append(b_)
                        for g in range(len(L) + 4):
                            if g < len(L):
                                stageA(L[g])
                            if 0 <= g - 2 < len(L):
                                stageB(L[g - 2])
                            if 0 <= g - 4 < len(L):
                                stageC(L[g - 4])
                                if L[g - 4].ulast and L[g - 4].hasC:
                                    cond_rest(L[g - 4])
                    kb.barrier()
                    kb.release_sems()
                with contextlib.ExitStack() as ps3:
                    P = post_mixer_allocs(ps3, 1)
                    Wo = load_w(ps3, "a_Wo", w_out1_d, 1024)
                    xres = [kb.sb("a_xres%d" % i, [128, D], F32, ps3) for i in range(2)]
                    pso = [kb.ps("a_pso%d" % i, [128, 512], F32, ps3) for i in range(2)]
                    def s1(gt):
                        xr = xres[gt % 2]
                        kb.dma("sp", xr.t[:, :], xb_d[gt * 128:(gt + 1) * 128, :], xr, reads=[xb_B[gt]], writes=[xr])
                        post_mixer(ps3, 1, gt, xr.t[:, :], xr, pso, oT, slice(gt * 128, (gt + 1) * 128), Wo, P, stage=1)

                    s1(0)
                    for gt in range(NT):
                        if gt + 1 < NT:
                            s1(gt + 1)
                        post_mixer(ps3, 1, gt, None, None, pso, oT, None, Wo, P, stage=2)
                    kb.barrier()
                    kb.release_sems()

        moe_fn = moe_sparse if SPARSE else moe
        mixer0()
        if stop_after != "m0":
            moe_fn(0, xb_d, xb_B)
            if stop_after != "moe0":
                attention()
                if stop_after != "attn":
                    moe_fn(1, out_d, out_B)
        kb.barrier()
        kb.release_sems()

    es.close()
    return nc


_NC_CACHE = {}


def kernel(x, even_w_in, even_conv_w, even_sgu_ln_g, even_sgu_ln_b, even_sgu_w_s, even_sgu_b_s, even_w_out,
           odd_w_qkv, odd_w_out, mix_ln_g, mix_ln_b, moe_w_group, moe_b_group, moe_w_router, moe_b_router,
           moe_w1, moe_w3, moe_w2, ffn_ln_g, ffn_ln_b):
    f = lambda a: np.ascontiguousarray(np.asarray(a, dtype=np.float32))
    convw = f(np.asarray(even_conv_w)[0].reshape(3, 4, 128).transpose(2, 1, 0).reshape(128, 12))
    wsT = f(np.asarray(even_sgu_w_s)[0].transpose(0, 2, 1))
    bs = f(np.asarray(even_sgu_b_s)[0].reshape(1, 1024))
    wr = f(np.concatenate([np.asarray(moe_w_group),
                           np.asarray(moe_w_router).transpose(0, 2, 1, 3).reshape(2, D, 16)], axis=2))
    br = f(np.concatenate([np.asarray(moe_b_group), np.asarray(moe_b_router).reshape(2, 16)], axis=1))
    shared = {
        "w_in": f(even_w_in[0]), "convw": convw, "sgu_g": f(even_sgu_ln_g), "sgu_b": f(even_sgu_ln_b),
        "wsT": wsT, "bs": bs, "w_out0": f(even_w_out[0]), "w_qkv": f(odd_w_qkv[0]), "w_out1": f(odd_w_out[0]),
        "mix_g": f(mix_ln_g), "mix_b": f(mix_ln_b), "wr": wr, "br": br,
        "w1": f(np.asarray(moe_w1).reshape(2, 16, D, 512)), "w3": f(np.asarray(moe_w3).reshape(2, 16, D, 512)),
        "w2": f(np.asarray(moe_w2).reshape(2, 16, 512, D)), "ffn_g": f(ffn_ln_g), "ffn_b": f(ffn_ln_b),
    }
    xs = f(x)
    if "nc" not in _NC_CACHE:
        _NC_CACHE["nc"] = build()
    nc = _NC_CACHE["nc"]
    in_maps = [dict(shared, x=xs[c]) for c in range(NCORES)]
    res = run_bass_kernel_spmd(nc, in_maps, core_ids=list(range(NCORES)))
    return np.stack([res.results[c]["out"] for c in range(NCORES)], axis=0)
```

```python
import contextlib
import numpy as np
import concourse.bass as bass
import concourse.mybir as mybir
from concourse.bass_utils import run_bass_kernel_spmd

F32 = mybir.dt.float32
BF16 = mybir.dt.bfloat16
I32 = mybir.dt.int32
OOB = 1 << 20
AF = mybir.ActivationFunctionType
ALU = mybir.AluOpType

T = 4096
D = 1024
NT = 32
ALPHA = float(4 ** 0.25)
EPS = 1e-5
NCORES = 8
SPARSE = True


class B:
    __slots__ = ("t", "w", "r", "dsem", "dcnt")

    def __init__(self, t=None):
        self.t = t
        self.w = None
        self.r = {}
        self.dsem = None
        self.dcnt = 0


class Eng:
    pass


class KB:
    def __init__(self):
        self.nc = bass.Bass("TRN2", target_bir_lowering=False)
        self.es = contextlib.ExitStack()
        self.nsem = 0
        self.dma_bufs = []
        self.rstack = []
        self.bregs = {}
        self.free_sems = []

    def sem(self, name):
        self.nsem += 1
        return self.es.enter_context(self.nc.semaphore(name))

    def sb(self, name, shape, dt, stack=None):
        st = stack if stack is not None else self.es
        self.nt = getattr(self, "nt", 0) + 1
        return B(st.enter_context(self.nc.sbuf_tensor("%s_%d" % (name, self.nt), shape, dt)))

    def ps(self, name, shape, dt, stack=None):
        st = stack if stack is not None else self.es
        self.nt = getattr(self, "nt", 0) + 1
        return B(st.enter_context(self.nc.psum_tensor("%s_%d" % (name, self.nt), shape, dt)))

    def start(self):
        nc = self.nc
        self.E = {}
        for name, eng in (("pe", nc.tensor), ("act", nc.scalar), ("dve", nc.vector),
                          ("pool", nc.gpsimd), ("sp", nc.sync)):
            e = Eng()
            e.name = name
            e.eng = eng
            e.sem = self.sem("s_" + name)
            e.count = 0
            e.seen = {}
            self.E[name] = e
        ET = mybir.EngineType
        self.etmap = {ET.PE: "pe", ET.Activation: "act", ET.DVE: "dve", ET.Pool: "pool", ET.SP: "sp"}
        self.mregs = nc.alloc_registers("mr", [ET.PE, ET.Activation, ET.DVE, ET.Pool, ET.SP])

    def _waits(self, E, reads, writes):
        need = {}

        def acc(tok):
            k = id(tok[0])
            if k not in need or need[k][1] < tok[1]:
                need[k] = tok

        for b in reads:
            if b.w is not None:
                acc(b.w)
        for b in writes:
            if b.w is not None:
                acc(b.w)
            for tok in b.r.values():
                acc(tok)
        for k, (sem, val) in need.items():
            if E.name == "pe" and sem is E.sem:
                continue
            if E.seen.get(k, 0) >= val:
                continue
            E.eng.wait_ge(sem, val)
            E.seen[k] = val

    def _commit(self, tok, reads, writes):
        k = id(tok[0])
        for b in reads:
            b.r[k] = tok
        for b in writes:
            b.w = tok
            b.r = {}

    def op(self, en, fn, reads=(), writes=(), mark=True):
        E = self.E[en]
        self._waits(E, reads, writes)
        inst = fn(E.eng)
        if mark:
            E.count += 1
            inst.then_inc(E.sem, 1)
            tok = (E.sem, E.count)
        else:
            tok = (E.sem, E.count + 1)
        self._commit(tok, reads, writes)
        return inst

    def dma(self, q, out_ap, in_ap, sbufB, reads=(), writes=()):
        E = self.E[q]
        self._waits(E, reads, writes)
        self._get_dsem(sbufB)
        for rd in self.rstack:
            rec = rd[q].setdefault(id(sbufB.dsem), [sbufB.dsem, sbufB.dcnt, 0])
            rec[2] += 16
        sbufB.dcnt += 16
        E.eng.dma_start(out=out_ap, in_=in_ap).then_inc(sbufB.dsem, 16)
        tok = (sbufB.dsem, sbufB.dcnt)
        self._commit(tok, reads, writes)

    def idma(self, out_ap, in_ap, idx_ap, scatter, bound, sbufB, reads=(), writes=()):
        E = self.E["pool"]
        self._waits(E, reads, writes)
        self._get_dsem(sbufB)
        for rd in self.rstack:
            rec = rd["pool"].setdefault(id(sbufB.dsem), [sbufB.dsem, sbufB.dcnt, 0])
            rec[2] += 16
        sbufB.dcnt += 16
        off = bass.IndirectOffsetOnAxis(ap=idx_ap, axis=0)
        if bound not in self.bregs:
            r = self.es.enter_context(E.eng.register("rb%d" % bound))
            E.eng.reg_mov(r, bound)
            self.bregs[bound] = r
        bound = self.bregs[bound]
        if scatter:
            E.eng.indirect_dma_start(out=out_ap, out_offset=off, in_=in_ap, in_offset=None,
                                     bounds_check=bound, oob_is_err=False).then_inc(sbufB.dsem, 16)
        else:
            E.eng.indirect_dma_start(out=out_ap, out_offset=None, in_=in_ap, in_offset=off,
                                     bounds_check=bound, oob_is_err=False).then_inc(sbufB.dsem, 16)
        tok = (sbufB.dsem, sbufB.dcnt)
        self._commit(tok, reads, writes)

    def region(self, thr, body):
        engs = list(self.E.values())
        snap = {E.name: dict(E.seen) for E in engs}
        before = {E.name: E.count for E in engs}
        rd = {E.name: {} for E in engs}
        self.rstack.append(rd)
        with self.nc.If_cmp(self.mregs, thr, "IS_LT"):
            body()
        self.rstack.pop()
        with self.nc.Else():
            for E in engs:
                nm = E.count - before[E.name]
                if nm > 0:
                    E.eng.wait_ge(E.sem, before[E.name])
                    E.eng.sem_inc(E.sem, nm)
                for (sem, bt, add) in rd[E.name].values():
                    E.eng.wait_ge(sem, bt)
                    E.eng.sem_inc(sem, add)
        for E in engs:
            E.seen = snap[E.name]

    def _get_dsem(self, b):
        if b.dsem is None:
            if self.free_sems:
                b.dsem, b.dcnt = self.free_sems.pop()
            else:
                b.dsem = self.sem("d%d" % self.nsem)
                b.dcnt = 0
            self.dma_bufs.append(b)

    def release_sems(self):
        for b in self.dma_bufs:
            if b.dsem is not None:
                self.free_sems.append((b.dsem, b.dcnt))
                b.dsem = None
        self.dma_bufs = []

    def barrier(self):
        toks = []
        for e in self.E.values():
            if e.count > 0:
                toks.append((e.sem, e.count))
        for b in self.dma_bufs:
            if b.dcnt > 0:
                toks.append((b.dsem, b.dcnt))
        for E in self.E.values():
            for (sem, val) in toks:
                if sem is E.sem:
                    continue
                k = id(sem)
                if E.seen.get(k, 0) >= val:
                    continue
                E.eng.wait_ge(sem, val)
                E.seen[k] = val

    def mmg(self, outB, out_ap, pairs, reads):
        n = len(pairs)
        for i, (l, r) in enumerate(pairs):
            self.op("pe", lambda e, l=l, r=r, i=i: e.matmul(out_ap, lhsT=l, rhs=r, start=(i == 0), stop=(i == n - 1)),
                    reads=reads, writes=[outB], mark=(i == n - 1))

    def act(self, out_ap, in_ap, func, reads, writes, bias=None, scale=None):
        kw = {}
        if bias is not None:
            kw["bias"] = bias
        if scale is not None:
            kw["scale"] = scale
        self.op("act", lambda e: e.activation(out=out_ap, in_=in_ap, func=func, **kw), reads=reads, writes=writes)

    def copy(self, en, out_ap, in_ap, reads, writes):
        if en == "act":
            self.op("act", lambda e: e.copy(out=out_ap, in_=in_ap), reads=reads, writes=writes)
        else:
            self.op(en, lambda e: e.tensor_copy(out=out_ap, in_=in_ap), reads=reads, writes=writes)

    def tt(self, en, out_ap, a_ap, b_ap, op, reads, writes):
        self.op(en, lambda e: e.tensor_tensor(out=out_ap, in0=a_ap, in1=b_ap, op=op), reads=reads, writes=writes)

    def ts(self, en, out_ap, in_ap, s1, s2, op0, op1, reads, writes):
        if op1 is None:
            self.op(en, lambda e: e.tensor_scalar(out=out_ap, in0=in_ap, scalar1=s1, scalar2=None, op0=op0),
                    reads=reads, writes=writes)
        else:
            self.op(en, lambda e: e.tensor_scalar(out=out_ap, in0=in_ap, scalar1=s1, scalar2=s2, op0=op0, op1=op1),
                    reads=reads, writes=writes)

    def stt(self, en, out_ap, in0, scalar, in1, op0, op1, reads, writes):
        self.op(en, lambda e: e.scalar_tensor_tensor(out=out_ap, in0=in0, scalar=scalar, in1=in1, op0=op0, op1=op1),
                reads=reads, writes=writes)


def build(stop_after=None, dbg=False):
    kb = KB()
    dk = {"kind": "ExternalOutput"} if dbg else {}
    nc = kb.nc

    def din(name, shape):
        return nc.dram_tensor(name, shape, F32, kind="ExternalInput").ap()

    x_d = din("x", [T, D])
    w_in_d = din("w_in", [D, 2560])
    convw_d = din("convw", [128, 12])
    sgu_g_d = din("sgu_g", [1, 512])
    sgu_b_d = din("sgu_b", [1, 512])
    wsT_d = din("wsT", [8, 128, 128])
    bs_d = din("bs", [1, 1024])
    w_out0_d = din("w_out0", [D, D])
    w_qkv_d = din("w_qkv", [D, 3072])
    w_out1_d = din("w_out1", [D, D])
    mix_g_d = din("mix_g", [2, D])
    mix_b_d = din("mix_b", [2, D])
    wr_d = din("wr", [2, D, 20])
    br_d = din("br", [2, 20])
    w1_d = din("w1", [2, 16, D, 512])
    w3_d = din("w3", [2, 16, D, 512])
    w2_d = din("w2", [2, 16, 512, D])
    ffn_g_d = din("ffn_g", [2, D])
    ffn_b_d = din("ffn_b", [2, D])
    out_d = nc.dram_tensor("out", [T, D], F32, kind="ExternalOutput").ap()

    xa_d = nc.dram_tensor("xa_s", [T, D], F32, **dk).ap()
    xb_d = nc.dram_tensor("xb_s", [T, D], F32, **dk).ap()
    xT_d = nc.dram_tensor("xT_s", [128, 8, T], BF16, **dk).ap()
    qT_d = nc.dram_tensor("qT_s", [8, 128, T], BF16, **dk).ap()
    kT_d = nc.dram_tensor("kT_s", [8, 128, T], BF16, **dk).ap()
    v_d = nc.dram_tensor("v_s", [T, D], BF16, **dk).ap()

    x1b_d = nc.dram_tensor("x1b_s", [T, D], BF16, **dk).ap()
    list_d = nc.dram_tensor("list_s", [16 * 4096, 2], I32, **dk).ap()
    y2_d = nc.dram_tensor("y2_s", [2 * T, D], F32, **dk).ap()
    x1bd_B = [B() for _ in range(NT)]
    xa_B = [B() for _ in range(NT)]
    xb_B = [B() for _ in range(NT)]
    xT_B = [B() for _ in range(8)]
    q_B = [B() for _ in range(8)]
    k_B = [B() for _ in range(8)]
    v_B = [B() for _ in range(8)]
    out_B = [B() for _ in range(NT)]

    kb.start()
    es = kb.es

    ident = kb.sb("ident", [128, 128], BF16)
    c_all = kb.sb("c_all", [128, NT, 16], F32)
    cAB = kb.sb("cAB", [128, NT, 2], F32)
    off_t = kb.sb("off_t", [128, 16], F32)
    ebase1 = kb.sb("ebase1", [128, 16], F32)
    Lstr = kb.sb("Lstr", [128, 128], BF16)
    onesb = kb.sb("onesb", [128, 128], BF16)
    cnt_i = kb.sb("cnt_i", [128, 16], I32)
    tl2 = kb.sb("tl2", [128, NT, 2, 2], I32)
    oobt = kb.sb("oobt", [128, 1024], I32)

    block = es.enter_context(nc.Block())

    @block.sync
    def _(sync):
        kb.op("pool", lambda e: e.memset(ident.t[:], 0.0), writes=[ident])
        kb.op("pool", lambda e: e.affine_select(out=ident.t[:], in_=ident.t[:], pattern=[[-1, 128]],
                                                compare_op=ALU.not_equal, fill=1.0, base=0, channel_multiplier=1),
              reads=[ident], writes=[ident])

        kb.op("pool", lambda e: e.memset(onesb.t[:], 1.0), writes=[onesb])
        kb.op("pool", lambda e: e.memset(Lstr.t[:], 1.0), writes=[Lstr])
        kb.op("pool", lambda e: e.affine_select(out=Lstr.t[:], in_=Lstr.t[:], pattern=[[1, 128]],
                                                compare_op=ALU.is_gt, fill=0.0, base=0, channel_multiplier=-1),
              reads=[Lstr], writes=[Lstr])
        kb.op("pool", lambda e: e.iota(ebase1.t[:], [[4096, 16]], base=1, channel_multiplier=0,
                                       allow_small_or_imprecise_dtypes=True), writes=[ebase1])
        kb.op("pool", lambda e: e.memset(oobt.t[:], OOB), writes=[oobt])
        for r_ in range(2):
            kb.op("pool", lambda e, r_=r_: e.iota(tl2.t[:, :, r_, 0], [[128, NT]], base=0, channel_multiplier=1), writes=[tl2])
            kb.op("pool", lambda e, r_=r_: e.iota(tl2.t[:, :, r_, 1], [[256, NT]], base=r_, channel_multiplier=2), writes=[tl2])

        def layernorm(ps_, r, D_, g_bc, b_bc, outB, out_ap, stats, mv, tmp, mul_eng="pool"):
            nch = D_ // 512
            for c in range(nch):
                kb.op("dve", lambda e, c=c: e.bn_stats(out=stats.t[:, c * 6:(c + 1) * 6], in_=r.t[:, c * 512:(c + 1) * 512]),
                      reads=[r], writes=[stats])
            kb.op("dve", lambda e: e.bn_aggr(out=mv.t[:, 0:2], in_=stats.t[:, 0:nch * 6]), reads=[stats], writes=[mv])
            kb.act(tmp.t[:, 0:1], mv.t[:, 1:2], AF.Ln, [mv], [tmp], bias=EPS, scale=1.0)
            kb.act(tmp.t[:, 1:2], tmp.t[:, 0:1], AF.Exp, [tmp], [tmp], scale=-0.5)
            if D_ == D:
                kb.ts("dve", tmp.t[:, 0:1], mv.t[:, 0:1], -1.0, tmp.t[:, 1:2], ALU.mult, ALU.mult, [mv, tmp], [tmp])
                kb.act(r.t[:, 0:D_], r.t[:, 0:D_], AF.Identity, [r, tmp], [r], bias=tmp.t[:, 0:1], scale=tmp.t[:, 1:2])
            else:
                kb.ts("dve", r.t[:, 0:D_], r.t[:, 0:D_], mv.t[:, 0:1], tmp.t[:, 1:2], ALU.subtract, ALU.mult, [r, mv, tmp], [r])
            kb.tt(mul_eng, r.t[:, 0:D_], r.t[:, 0:D_], g_bc.t[:, 0:D_], ALU.mult, [r, g_bc], [r])
            kb.tt(mul_eng, out_ap, r.t[:, 0:D_], b_bc.t[:, 0:D_], ALU.add, [r, b_bc], [outB])

        def bcast_load(dst, src_row_ap):
            kb.dma("sp", dst.t[:], src_row_ap.partition_broadcast(128), dst, writes=[dst])

        def post_mixer(ps_, lyr, gt, x_res_ap, x_resB, pso, yT, yT_cols, Wo, P, stage=0):
            x1 = P["x1"][gt % 3]
            if stage in (0, 1):
                for dh in range(2):
                    kb.mmg(pso[dh], pso[dh].t[:, :],
                           [(yT.t[:, k, yT_cols], Wo.t[:, k, dh * 512:(dh + 1) * 512]) for k in range(8)],
                           reads=[yT, Wo])
                r = P["r"][gt % 2]
                for dh in range(2):
                    kb.stt("dve", r.t[:, dh * 512:(dh + 1) * 512], x_res_ap[:, dh * 512:(dh + 1) * 512], ALPHA,
                           pso[dh].t[:, :], ALU.mult, ALU.add, [x_resB, pso[dh]], [r])
                layernorm(ps_, r, D, P["mg"], P["mb"], x1, x1.t[:, :], P["stats"], P["mv"], P["tmp"])
                if stage == 1:
                    return
            kb.dma("pool", xa_d[gt * 128:(gt + 1) * 128, :], x1.t[:, :], x1, reads=[x1], writes=[xa_B[gt]])
            x1b = P["x1b"][gt % 2]
            kb.copy("act", x1b.t[:, :], x1.t[:, :], [x1], [x1b])
            kb.dma("pool", x1b_d[gt * 128:(gt + 1) * 128, :], x1b.t[:, :], x1b, reads=[x1b], writes=[x1bd_B[gt]])
            pT = P["pT"]
            for k in range(8):
                kb.op("pe", lambda e, k=k: e.transpose(pT.t[:, k * 128:(k + 1) * 128], x1b.t[:, k * 128:(k + 1) * 128], ident.t[:]),
                      reads=[x1b, ident], writes=[pT], mark=(k == 7))
            x1T = P["x1T"][gt % 2]
            s = gt % 4
            kb.copy("act", x1T.t[:, :, :], pT.t[:, :].rearrange("p (k t) -> p k t", k=8), [pT], [x1T])
            prt = P["prt"]
            kb.mmg(prt, prt.t[:, 0:20], [(x1T.t[:, k, :], P["Wr"].t[:, k, :]) for k in range(8)],
                   reads=[x1T, P["Wr"]])
            rt = P["rt"]
            lg = rt.t[:, 0:20]
            kb.tt("dve", lg, prt.t[:, 0:20], P["brb"].t[:, :], ALU.add, [prt, P["brb"]], [rt])
            R_ = [rt]
            kb.op("dve", lambda e: e.reduce_max(out=rt.t[:, 20:21], in_=rt.t[:, 0:4], axis=mybir.AxisListType.X), reads=R_, writes=R_)
            kb.ts("dve", rt.t[:, 24:28], rt.t[:, 0:4], rt.t[:, 20:21], None, ALU.is_equal, None, R_, R_)
            kb.ts("dve", rt.t[:, 21:22], rt.t[:, 20:21], -1.0, None, ALU.mult, None, R_, R_)
            kb.act(rt.t[:, 28:32], rt.t[:, 0:4], AF.Exp, R_, R_, bias=rt.t[:, 21:22], scale=1.0)
            kb.op("dve", lambda e: e.reduce_sum(out=rt.t[:, 22:23], in_=rt.t[:, 28:32], axis=mybir.AxisListType.X), reads=R_, writes=R_)
            kb.ts("dve", rt.t[:, 32:36], rt.t[:, 4:8], rt.t[:, 24:25], None, ALU.mult, None, R_, R_)
            for g in range(1, 4):
                kb.stt("dve", rt.t[:, 32:36], rt.t[:, 4 + 4 * g:8 + 4 * g], rt.t[:, 24 + g:25 + g], rt.t[:, 32:36],
                       ALU.mult, ALU.add, R_, R_)
            kb.op("dve", lambda e: e.reduce_max(out=rt.t[:, 36:37], in_=rt.t[:, 32:36], axis=mybir.AxisListType.X), reads=R_, writes=R_)
            kb.ts("dve", rt.t[:, 40:44], rt.t[:, 32:36], rt.t[:, 36:37], None, ALU.is_equal, None, R_, R_)
            kb.ts("dve", rt.t[:, 37:38], rt.t[:, 36:37], -1.0, None, ALU.mult, None, R_, R_)
            kb.act(rt.t[:, 44:48], rt.t[:, 32:36], AF.Exp, R_, R_, bias=rt.t[:, 37:38], scale=1.0)
            kb.op("dve", lambda e: e.reduce_max(out=rt.t[:, 38:39], in_=rt.t[:, 44:48], axis=mybir.AxisListType.X), reads=R_, writes=R_)
            kb.tt("dve", rt.t[:, 48:52], rt.t[:, 44:48], rt.t[:, 40:44], ALU.mult, R_, R_)
            kb.tt("dve", rt.t[:, 48:52], rt.t[:, 44:48], rt.t[:, 48:52], ALU.subtract, R_, R_)
            kb.op("dve", lambda e: e.reduce_max(out=rt.t[:, 39:40], in_=rt.t[:, 48:52], axis=mybir.AxisListType.X), reads=R_, writes=R_)
            kb.ts("dve", rt.t[:, 52:56], rt.t[:, 44:48], rt.t[:, 39:40], None, ALU.is_ge, None, R_, R_)
            kb.tt("dve", rt.t[:, 52:56], rt.t[:, 52:56], rt.t[:, 44:48], ALU.mult, R_, R_)
            kb.tt("dve", rt.t[:, 56:57], rt.t[:, 38:39], rt.t[:, 39:40], ALU.add, R_, R_)
            kb.tt("dve", rt.t[:, 56:57], rt.t[:, 56:57], rt.t[:, 22:23], ALU.mult, R_, R_)
            kb.op("dve", lambda e: e.reciprocal(out=rt.t[:, 57:58], in_=rt.t[:, 56:57]), reads=R_, writes=R_)
            kb.ts("dve", rt.t[:, 60:64], rt.t[:, 24:28], rt.t[:, 57:58], None, ALU.mult, None, R_, R_)
            for g in range(4):
                kb.ts("dve", c_all.t[:, gt, 4 * g:4 * g + 4], rt.t[:, 52:56], rt.t[:, 60 + g:61 + g], None, ALU.mult, None,
                      R_, [c_all])
            if s == 3 and not SPARSE:
                mt = gt // 4
                kb.dma("pool", xT_d[:, :, mt * 512:(mt + 1) * 512], x1T.t[:, :, :], x1T, reads=[x1T], writes=[xT_B[mt]])
            if SPARSE:
                rt2 = P["rt2"]
                mkb = P["mkb"]
                idx2 = P["idx2"][gt % 2]
                Q_ = [rt2]
                cg = c_all.t[:, gt, :]
                kb.ts("dve", rt2.t[:, 0:16], cg, 0.0, None, ALU.is_gt, None, [c_all], Q_)
                kb.copy("dve", mkb.t[:, :], rt2.t[:, 0:16], Q_, [mkb])
                kb.mmg(prt, prt.t[:, 32:48], [(Lstr.t[:, :], mkb.t[:, :])], reads=[Lstr, mkb])
                kb.mmg(prt, prt.t[:, 48:64], [(onesb.t[:, :], mkb.t[:, :])], reads=[onesb, mkb])
                kb.tt("dve", rt2.t[:, 16:32], prt.t[:, 32:48], ebase1.t[:, :], ALU.add, [prt, ebase1], Q_)
                kb.tt("dve", rt2.t[:, 16:32], rt2.t[:, 16:32], off_t.t[:, :], ALU.add, Q_ + [off_t], Q_)
                kb.tt("dve", rt2.t[:, 16:32], rt2.t[:, 16:32], rt2.t[:, 0:16], ALU.mult, Q_, Q_)
                kb.tt("dve", off_t.t[:, :], off_t.t[:, :], prt.t[:, 48:64], ALU.add, [off_t, prt], [off_t])
                kb.op("dve", lambda e: e.reduce_max(out=rt2.t[:, 64:65], in_=rt2.t[:, 16:32], axis=mybir.AxisListType.X), reads=Q_, writes=Q_)
                kb.ts("dve", rt2.t[:, 32:48], rt2.t[:, 16:32], rt2.t[:, 64:65], None, ALU.is_equal, None, Q_, Q_)
                kb.tt("dve", rt2.t[:, 48:64], rt2.t[:, 32:48], cg, ALU.mult, Q_ + [c_all], Q_)
                kb.op("dve", lambda e: e.reduce_sum(out=cAB.t[:, gt, 0:1], in_=rt2.t[:, 48:64], axis=mybir.AxisListType.X), reads=Q_, writes=[cAB])
                kb.op("dve", lambda e: e.reduce_sum(out=rt2.t[:, 66:67], in_=cg, axis=mybir.AxisListType.X), reads=[c_all], writes=Q_)
                kb.tt("dve", cAB.t[:, gt, 1:2], rt2.t[:, 66:67], cAB.t[:, gt, 0:1], ALU.subtract, Q_ + [cAB], [cAB])
                kb.tt("dve", rt2.t[:, 48:64], rt2.t[:, 16:32], rt2.t[:, 32:48], ALU.mult, Q_, Q_)
                kb.tt("dve", rt2.t[:, 48:64], rt2.t[:, 16:32], rt2.t[:, 48:64], ALU.subtract, Q_, Q_)
                kb.op("dve", lambda e: e.reduce_max(out=rt2.t[:, 67:68], in_=rt2.t[:, 48:64], axis=mybir.AxisListType.X), reads=Q_, writes=Q_)
                kb.ts("dve", rt2.t[:, 68:69], rt2.t[:, 67:68], 0.0, float(OOB), ALU.is_equal, ALU.mult, Q_, Q_)
                kb.tt("dve", rt2.t[:, 67:68], rt2.t[:, 67:68], rt2.t[:, 68:69], ALU.add, Q_, Q_)
                kb.ts("dve", idx2.t[:, 0:1], rt2.t[:, 64:65], -1.0, None, ALU.add, None, Q_, [idx2])
                kb.ts("dve", idx2.t[:, 1:2], rt2.t[:, 67:68], -1.0, None, ALU.add, None, Q_, [idx2])
                for r_ in range(2):
                    kb.idma(list_d[:, :], tl2.t[:, gt, r_, :], idx2.t[:, r_:r_ + 1], True, 16 * 4096 - 1, idx2,
                            reads=[idx2, tl2, P["listB"]])
                if gt == NT - 1:
                    kb.ts("dve", cnt_i.t[:, :], off_t.t[:, :], -1.0, 4096.0, ALU.mult, ALU.add, [off_t], [cnt_i])

        def post_mixer_allocs(ps_, lyr):
            P = {}
            P["r"] = [kb.sb("pm_r%d" % i, [128, D], F32, ps_) for i in range(2)]
            P["x1"] = [kb.sb("pm_x1%d" % i, [128, D], F32, ps_) for i in range(3)]
            P["x1b"] = [kb.sb("pm_x1b", [128, D], BF16, ps_) for _ in range(2)]
            P["rt2"] = kb.sb("pm_rt2", [128, 128], F32, ps_)
            P["mkb"] = kb.sb("pm_mkb", [128, 16], BF16, ps_)
            P["idx2"] = [kb.sb("pm_idx2", [128, 2], I32, ps_) for _ in range(2)]
            P["listB"] = B()
            kb.op("dve", lambda e: e.memset(off_t.t[:], 0.0), writes=[off_t])
            kb.dma("pool", list_d.rearrange("(p a) c -> p (a c)", p=128), oobt.t[:, :], oobt, reads=[oobt], writes=[P["listB"]])
            P["x1T"] = [kb.sb("pm_x1T", [128, 8, 128], BF16, ps_) for _ in range(2)]
            P["stats"] = kb.sb("pm_stats", [128, 12], F32, ps_)
            P["mv"] = kb.sb("pm_mv", [128, 2], F32, ps_)
            P["tmp"] = kb.sb("pm_tmp", [128, 2], F32, ps_)
            P["rt"] = kb.sb("pm_rt", [128, 64], F32, ps_)
            P["mg"] = kb.sb("pm_mg", [128, D], F32, ps_)
            P["mb"] = kb.sb("pm_mb", [128, D], F32, ps_)
            P["brb"] = kb.sb("pm_brb", [128, 20], F32, ps_)
            P["Wr"] = kb.sb("pm_Wr", [128, 8, 20], BF16, ps_)
            P["pT"] = kb.ps("pm_pT", [128, 1024], BF16, ps_)
            P["prt"] = kb.ps("pm_prt", [128, 512], F32, ps_)
            bcast_load(P["mg"], mix_g_d[lyr:lyr + 1, :])
            bcast_load(P["mb"], mix_b_d[lyr:lyr + 1, :])
            bcast_load(P["brb"], br_d[lyr:lyr + 1, :])
            kb.dma("pool", P["Wr"].t[:, :, :], wr_d[lyr].rearrange("(k p) n -> p k n", p=128), P["Wr"], writes=[P["Wr"]])
            return P

        def load_w(ps_, name, src_ap, ncols):
            W = kb.sb(name, [128, 8, ncols], BF16, ps_)
            v = src_ap.rearrange("(k p) n -> p k n", p=128)
            for k in range(8):
                kb.dma("pool", W.t[:, k, :], v[:, k, :], W, writes=[W])
            return W

        def load_xT(xsrc_d, xsrc_B, mt, xin, xbf, pT, xT):
            rd = [xsrc_B[mt * 4 + s] for s in range(4)] if xsrc_B is not None else []
            kb.dma("sp", xin.t[:, :, :], xsrc_d[mt * 512:(mt + 1) * 512, :].rearrange("(s p) d -> p s d", p=128), xin,
                   reads=rd, writes=[xin])
            for s in range(4):
                xb = xbf[s % 2]
                kb.copy("act", xb.t[:, :], xin.t[:, s, :], [xin], [xb])
                for k in range(8):
                    kb.op("pe", lambda e, k=k, xb=xb: e.transpose(pT.t[:, k * 128:(k + 1) * 128], xb.t[:, k * 128:(k + 1) * 128], ident.t[:]),
                          reads=[xb, ident], writes=[pT], mark=(k == 7))
                kb.copy("act", xT.t[:, :, s * 128:(s + 1) * 128], pT.t[:, :].rearrange("p (k t) -> p k t", k=8), [pT], [xT])

        def mixer0():
            with contextlib.ExitStack() as ps_:
                P = post_mixer_allocs(ps_, 0)
                Wi = load_w(ps_, "m0_Wi", w_in_d, 2560)
                Wo = load_w(ps_, "m0_Wo", w_out0_d, 1024)
                WsT = kb.sb("m0_WsT", [128, 8, 128], BF16, ps_)
                kb.dma("pool", WsT.t[:, :, :], wsT_d.rearrange("h s t -> s h t"), WsT, writes=[WsT])
                kb.op("pool", lambda e: e.memset(WsT.t[64:128, :, 0:64], 0.0), reads=[WsT], writes=[WsT])
                bsr = kb.sb("m0_bsr", [1, 1024], BF16, ps_)
                kb.dma("pool", bsr.t[:, :], bs_d[:, :], bsr, writes=[bsr])
                onesr = kb.sb("m0_ones", [1, 128], BF16, ps_)
                kb.op("pool", lambda e: e.memset(onesr.t[:, :], 1.0), writes=[onesr])
                cw = kb.sb("m0_cw", [128, 12], F32, ps_)
                kb.dma("sp", cw.t[:, :], convw_d[:, :], cw, writes=[cw])
                sg = kb.sb("m0_sg", [128, 512], F32, ps_)
                sbb = kb.sb("m0_sbb", [128, 512], F32, ps_)
                bcast_load(sg, sgu_g_d[0:1, :])
                bcast_load(sbb, sgu_b_d[0:1, :])
                xin = kb.sb("m0_xin", [128, 4, D], F32, ps_)
                xbf = [kb.sb("m0_xbf%d" % i, [128, D], BF16, ps_) for i in range(2)]
                xres = [kb.sb("m0_xres%d" % i, [128, D], F32, ps_) for i in range(2)]
                xT = [kb.sb("m0_xT", [128, 8, 512], BF16, ps_) for _ in range(2)]
                yT = [kb.sb("m0_yT", [128, 8, 512], BF16, ps_) for _ in range(2)]
                ub = kb.sb("m0_ub", [128, 4, 514], F32, ps_)
                Cs = kb.sb("m0_Cs", [128, 512], F32, ps_)
                acc = kb.sb("m0_acc", [128, 512], F32, ps_)
                zu = kb.sb("m0_zu", [128, 4, 512], F32, ps_)
                g1 = kb.sb("m0_g1", [128, 512], F32, ps_)
                g2 = kb.sb("m0_g2", [128, 512], F32, ps_)
                gv = kb.sb("m0_gv", [128, 512], F32, ps_)
                vt = kb.sb("m0_vt", [128, 4, 512], BF16, ps_)
                st2 = kb.sb("m0_st2", [128, 6], F32, ps_)
                mv2 = kb.sb("m0_mv2", [128, 2], F32, ps_)
                tmp2 = kb.sb("m0_tmp2", [128, 2], F32, ps_)
                pp = [kb.ps("m0_pp%d" % i, [128, 512], F32, ps_) for i in range(2)]
                psg = [kb.ps("m0_psg%d" % i, [128, 512], F32, ps_) for i in range(2)]
                pso = [kb.ps("m0_pso%d" % i, [128, 512], F32, ps_) for i in range(2)]
                pT = P["pT"]
                kb.op("pool", lambda e: e.memset(ub.t[:, :, :], 0.0), writes=[ub])
                ppi = [0]

                def proj_fm(c, xT):
                    p = pp[ppi[0] % 2]
                    ppi[0] += 1
                    kb.mmg(p, p.t[:, :], [(Wi.t[:, k, c * 128:(c + 1) * 128], xT.t[:, k, :]) for k in range(8)], reads=[Wi, xT])
                    return p

                def gelu(p, p_ap, outB, out_ap, n):
                    kb.act(g1.t[:, 0:n], p_ap, AF.Square, [p], [g1])
                    kb.act(g1.t[:, 0:n], g1.t[:, 0:n], AF.Identity, [g1], [g1], bias=1.0, scale=0.044715)
                    kb.tt("dve", g2.t[:, 0:n], g1.t[:, 0:n], p_ap, ALU.mult, [g1, p], [g2])
                    kb.act(g1.t[:, 0:n], g2.t[:, 0:n], AF.Exp, [g2], [g1], scale=-1.5957691216057308)
                    kb.act(g1.t[:, 0:n], g1.t[:, 0:n], AF.Identity, [g1], [g1], bias=1.0, scale=1.0)
                    kb.op("dve", lambda e: e.reciprocal(out=g2.t[:, 0:n], in_=g1.t[:, 0:n]), reads=[g1], writes=[g2])
                    kb.tt("dve", out_ap, g2.t[:, 0:n], p_ap, ALU.mult, [g2, p], [outB])

                def piece(mt, j):
                    xTb = xT[mt % 2]
                    yTb = yT[mt % 2]
                    u = ub
                    pC = proj_fm(4 + j, xTb)
                    kb.copy("act", Cs.t[:, :], pC.t[:, :], [pC], [Cs])
                    pH = proj_fm(8 + j, xTb)
                    kb.copy("dve", u.t[:, j, 0:2], u.t[:, j, 512:514], [u], [u])
                    kb.tt("dve", u.t[:, j, 2:514], Cs.t[:, :], pH.t[:, :], ALU.mult, [Cs, pH], [u])
                    kb.ts("dve", acc.t[:, :], u.t[:, j, 2:514], cw.t[:, j * 3 + 2:j * 3 + 3], None, ALU.mult, None, [u, cw], [acc])
                    kb.stt("dve", acc.t[:, :], u.t[:, j, 1:513], cw.t[:, j * 3 + 1:j * 3 + 2], acc.t[:, :], ALU.mult, ALU.add, [u, cw, acc], [acc])
                    kb.stt("dve", acc.t[:, :], u.t[:, j, 0:512], cw.t[:, j * 3:j * 3 + 1], acc.t[:, :], ALU.mult, ALU.add, [u, cw, acc], [acc])
                    pB = proj_fm(j, xTb)
                    kb.tt("dve", yTb.t[:, j, :], acc.t[:, :], pB.t[:, :], ALU.mult, [acc, pB], [yTb])
                    pZ = proj_fm(12 + j, xTb)
                    gelu(pZ, pZ.t[:, :], zu, zu.t[:, j, :], 512)
                    p = pp[ppi[0] % 2]
                    ppi[0] += 1
                    kb.mmg(p, p.t[:, :], [(xTb.t[:, k, j * 128:(j + 1) * 128], Wi.t[:, k, 2048:2560]) for k in range(8)], reads=[Wi, xTb])
                    gelu(p, p.t[:, :], gv, gv.t[:, :], 512)
                    layernorm(ps_, gv, 512, sg, sbb, vt, vt.t[:, j, :], st2, mv2, tmp2, mul_eng="pool")

                def sgu(mt):
                    yTb = yT[mt % 2]
                    for hp in range(4):
                        for j in range(2):
                            h = 2 * hp + j
                            pg = psg[j]
                            for s in range(4):
                                kb.op("pe", lambda e, s=s, pg=pg, h=h, hp=hp: e.matmul(pg.t[:, s * 128:(s + 1) * 128], lhsT=vt.t[:, s, hp * 128:(hp + 1) * 128],
                                                                         rhs=WsT.t[:, h, :], start=True, stop=False),
                                      reads=[vt, WsT], writes=[pg], mark=False)
                                kb.op("pe", lambda e, s=s, pg=pg, h=h: e.matmul(pg.t[:, s * 128:(s + 1) * 128], lhsT=onesr.t[0:1, :],
                                                                   rhs=bsr.t[0:1, h * 128:(h + 1) * 128], start=False, stop=True),
                                      reads=[onesr, bsr], writes=[pg], mark=(s == 3))
                            kb.tt("dve", yTb.t[j * 64:(j + 1) * 64, 4 + hp, :], zu.t[j * 64:(j + 1) * 64, hp, :], pg.t[j * 64:(j + 1) * 64, :],
                                  ALU.mult, [zu, pg], [yTb])

                def back(mt, s):
                    gt = mt * 4 + s
                    xr = xres[gt % 2]
                    kb.dma("sp", xr.t[:, :], x_d[gt * 128:(gt + 1) * 128, :], xr, writes=[xr])
                    post_mixer(ps_, 0, gt, xr.t[:, :], xr, pso, yT[mt % 2], slice(s * 128, (s + 1) * 128), Wo, P, stage=1)
                    if gt > 0:
                        post_mixer(ps_, 0, gt - 1, None, None, pso, None, None, Wo, P, stage=2)

                load_xT(x_d, None, 0, xin, xbf, pT, xT[0])
                for j in range(4):
                    piece(0, j)
                sgu(0)
                for mt in range(8):
                    if mt + 1 < 8:
                        load_xT(x_d, None, mt + 1, xin, xbf, pT, xT[(mt + 1) % 2])
                    for j in range(4):
                        if mt + 1 < 8:
                            piece(mt + 1, j)
                        back(mt, j)
                    if mt + 1 < 8:
                        sgu(mt + 1)
                post_mixer(ps_, 0, NT - 1, None, None, pso, None, None, Wo, P, stage=2)
                kb.barrier()
                kb.release_sems()

        def moe(lyr, dst_d, dst_B):
            with contextlib.ExitStack() as ps_:
                fg = kb.sb("f_g", [128, D], F32, ps_)
                fb = kb.sb("f_b", [128, D], F32, ps_)
                bcast_load(fg, ffn_g_d[lyr:lyr + 1, :])
                bcast_load(fb, ffn_b_d[lyr:lyr + 1, :])
                xTh = kb.sb("f_xTh", [128, 8, 2048], BF16, ps_)
                yacc = kb.sb("f_yacc", [128, 16, D], F32, ps_)
                w1s = [kb.sb("f_w1_%d" % i, [128, 8, 512], BF16, ps_) for i in range(2)]
                w3s = [kb.sb("f_w3_%d" % i, [128, 8, 512], BF16, ps_) for i in range(2)]
                w2s = [kb.sb("f_w2_%d" % i, [128, 4, D], BF16, ps_) for i in range(2)]
                hid = [kb.sb("f_hid%d" % i, [128, 4, 512], BF16, ps_) for i in range(2)]
                sl = [kb.sb("f_sl%d" % i, [128, 512], BF16, ps_) for i in range(2)]
                x1l = [kb.sb("f_x1l%d" % i, [128, D], F32, ps_) for i in range(2)]
                rr = [kb.sb("f_r%d" % i, [128, D], F32, ps_) for i in range(2)]
                st = kb.sb("f_st", [128, 12], F32, ps_)
                mv = kb.sb("f_mv", [128, 2], F32, ps_)
                tmp = kb.sb("f_tmp", [128, 2], F32, ps_)
                ph1 = [kb.ps("f_ph1_%d" % i, [128, 512], F32, ps_) for i in range(2)]
                ph3 = [kb.ps("f_ph3_%d" % i, [128, 512], F32, ps_) for i in range(2)]
                py = [kb.ps("f_py%d" % i, [128, 512], F32, ps_) for i in range(2)]

                def load_expert(e):
                    sl_ = e % 2
                    kb.dma("pool", w1s[sl_].t[:, :, :], w1_d[lyr, e].rearrange("(k p) f -> p k f", p=128), w1s[sl_], writes=[w1s[sl_]])
                    kb.dma("pool", w3s[sl_].t[:, :, :], w3_d[lyr, e].rearrange("(k p) f -> p k f", p=128), w3s[sl_], writes=[w3s[sl_]])
                    kb.dma("pool", w2s[sl_].t[:, :, :], w2_d[lyr, e].rearrange("(c p) d -> p c d", p=128), w2s[sl_], writes=[w2s[sl_]])

                cnt = [0]
                for hf in range(2):
                    kb.dma("sp", xTh.t[:, :, :], xT_d[:, :, hf * 2048:(hf + 1) * 2048], xTh,
                           reads=[xT_B[hf * 4 + i] for i in range(4)], writes=[xTh])
                    load_expert(0)
                    for e in range(16):
                        if e + 1 < 16:
                            load_expert(e + 1)
                        w1 = w1s[e % 2]
                        w3 = w3s[e % 2]
                        w2 = w2s[e % 2]
                        for mt in range(4):
                            hd = hid[(e * 4 + mt) % 2]
                            for fc in range(4):
                                i2 = cnt[0] % 2
                                cnt[0] += 1
                                xs = xTh.t[:, :, mt * 512:(mt + 1) * 512]
                                kb.mmg(ph1[i2], ph1[i2].t[:, :], [(w1.t[:, k, fc * 128:(fc + 1) * 128], xTh.t[:, k, mt * 512:(mt + 1) * 512]) for k in range(8)],
                                       reads=[w1, xTh])
                                kb.mmg(ph3[i2], ph3[i2].t[:, :], [(w3.t[:, k, fc * 128:(fc + 1) * 128], xTh.t[:, k, mt * 512:(mt + 1) * 512]) for k in range(8)],
                                       reads=[w3, xTh])
                                kb.act(sl[i2].t[:, :], ph1[i2].t[:, :], AF.Silu, [ph1[i2]], [sl[i2]])
                                kb.tt("dve", hd.t[:, fc, :], sl[i2].t[:, :], ph3[i2].t[:, :], ALU.mult, [sl[i2], ph3[i2]], [hd])
                            for s in range(4):
                                ti = mt * 4 + s
                                gt = hf * 16 + ti
                                for dh in range(2):
                                    p = py[dh]
                                    kb.mmg(p, p.t[:, :], [(hd.t[:, fc, s * 128:(s + 1) * 128], w2.t[:, fc, dh * 512:(dh + 1) * 512]) for fc in range(4)],
                                           reads=[hd, w2])
                                    ya = yacc.t[:, ti, dh * 512:(dh + 1) * 512]
                                    if e == 0:
                                        kb.ts("dve", ya, p.t[:, :], c_all.t[:, gt, e:e + 1], None, ALU.mult, None, [p, c_all], [yacc])
                                    else:
                                        kb.stt("dve", ya, p.t[:, :], c_all.t[:, gt, e:e + 1], ya, ALU.mult, ALU.add, [p, c_all, yacc], [yacc])
                    for ti in range(16):
                        gt = hf * 16 + ti
                        xl = x1l[ti % 2]
                        r = rr[ti % 2]
                        kb.dma("sp", xl.t[:, :], xa_d[gt * 128:(gt + 1) * 128, :], xl, reads=[xa_B[gt]], writes=[xl])
                        kb.stt("dve", r.t[:, :], xl.t[:, :], ALPHA, yacc.t[:, ti, :], ALU.mult, ALU.add, [xl, yacc], [r])
                        layernorm(ps_, r, D, fg, fb, xl, xl.t[:, :], st, mv, tmp)
                        kb.dma("pool", dst_d[gt * 128:(gt + 1) * 128, :], xl.t[:, :], xl, reads=[xl], writes=[dst_B[gt]])
                kb.barrier()
                kb.release_sems()

        def moe_sparse(lyr, dst_d, dst_B):
            with contextlib.ExitStack() as ps_:
                fg = kb.sb("f_g", [128, D], F32, ps_)
                fb = kb.sb("f_b", [128, D], F32, ps_)
                bcast_load(fg, ffn_g_d[lyr:lyr + 1, :])
                bcast_load(fb, ffn_b_d[lyr:lyr + 1, :])
                w1s = [kb.sb("f_w1", [128, 8, 512], BF16, ps_) for i in range(2)]
                w3s = [kb.sb("f_w3", [128, 8, 512], BF16, ps_) for i in range(2)]
                w2s = [kb.sb("f_w2", [128, 4, D], BF16, ps_) for i in range(2)]
                xg = [kb.sb("f_xg", [128, D], BF16, ps_) for i in range(8)]
                lst = [kb.sb("f_lst", [128, 2], I32, ps_) for i in range(8)]
                xgT = [kb.sb("f_xgT", [128, 8, 128], BF16, ps_) for i in range(8)]
                sl = [kb.sb("f_sl", [128, 512], BF16, ps_) for i in range(2)]
                hid = [kb.sb("f_hid", [128, 4, 128], BF16, ps_) for i in range(2)]
                ysb = [kb.sb("f_ysb", [128, D], F32, ps_) for i in range(4)]
                y2l = [kb.sb("f_y2l", [128, 2, D], F32, ps_) for i in range(2)]
                x1l = [kb.sb("f_x1l", [128, D], F32, ps_) for i in range(2)]
                rr = [kb.sb("f_r", [128, D], F32, ps_) for i in range(2)]
                st = kb.sb("f_st", [128, 12], F32, ps_)
                mv = kb.sb("f_mv", [128, 2], F32, ps_)
                tmp = kb.sb("f_tmp", [128, 2], F32, ps_)
                pT = kb.ps("f_pT", [128, 1024], BF16, ps_)
                ph1 = [kb.ps("f_ph1", [128, 512], F32, ps_) for i in range(2)]
                ph3 = [kb.ps("f_ph3", [128, 512], F32, ps_) for i in range(2)]
                py = [kb.ps("f_py", [128, 512], F32, ps_) for i in range(2)]
                for i in range(8):
                    kb.op("dve", lambda e, i=i: e.memset(xg[i].t[:, :], 0.0), writes=[xg[i]])

                def load_expert(e):
                    sl_ = e % 2
                    kb.dma("pool", w1s[sl_].t[:, :, :], w1_d[lyr, e].rearrange("(k p) f -> p k f", p=128), w1s[sl_], writes=[w1s[sl_]])
                    kb.dma("pool", w3s[sl_].t[:, :, :], w3_d[lyr, e].rearrange("(k p) f -> p k f", p=128), w3s[sl_], writes=[w3s[sl_]])
                    kb.dma("pool", w2s[sl_].t[:, :, :], w2_d[lyr, e].rearrange("(c p) d -> p c d", p=128), w2s[sl_], writes=[w2s[sl_]])

                load_expert(0)
                for e in range(16):
                    if e + 1 < 16:
                        load_expert(e + 1)
                    w1 = w1s[e % 2]
                    w3 = w3s[e % 2]
                    w2 = w2s[e % 2]
                    for reg in kb.mregs:
                        E_ = kb.E[kb.etmap[reg.engine]]
                        kb._waits(E_, [cnt_i], [])
                        E_.eng.reg_load(reg, cnt_i.t[0:1, e:e + 1])

                    def fetch(j, e=e):
                        i4 = (e % 2) * 4 + j % 4
                        base = e * 4096 + j * 128
                        kb.dma("sp", lst[i4].t[:, :], list_d[base:base + 128, :], lst[i4], writes=[lst[i4]])
                        kb.idma(xg[i4].t[:, :], x1b_d[:, :], lst[i4].t[:, 0:1], False, T - 1, xg[i4], reads=[lst[i4]], writes=[xg[i4]])

                    def transp(j, e=e):
                        i4 = (e % 2) * 4 + j % 4
                        for k in range(8):
                            kb.op("pe", lambda e_, k=k: e_.transpose(pT.t[:, k * 128:(k + 1) * 128], xg[i4].t[:, k * 128:(k + 1) * 128], ident.t[:]),
                                  reads=[xg[i4], ident], writes=[pT], mark=(k == 7))
                        kb.copy("act", xgT[i4].t[:, :, :], pT.t[:, :].rearrange("p (k t) -> p k t", k=8), [pT], [xgT[i4]])

                    def hpart(j, w1=w1, w3=w3, e=e):
                        i2 = j % 2
                        i4 = (e % 2) * 4 + j % 4
                        for fc in range(4):
                            kb.mmg(ph1[i2], ph1[i2].t[:, fc * 128:(fc + 1) * 128],
                                   [(w1.t[:, k, fc * 128:(fc + 1) * 128], xgT[i4].t[:, k, :]) for k in range(8)], reads=[w1, xgT[i4]])
                        for fc in range(4):
                            kb.mmg(ph3[i2], ph3[i2].t[:, fc * 128:(fc + 1) * 128],
                                   [(w3.t[:, k, fc * 128:(fc + 1) * 128], xgT[i4].t[:, k, :]) for k in range(8)], reads=[w3, xgT[i4]])
                        kb.act(sl[i2].t[:, :], ph1[i2].t[:, :], AF.Silu, [ph1[i2]], [sl[i2]])
                        kb.tt("dve", hid[i2].t[:, :, :].rearrange("p c t -> p (c t)"), sl[i2].t[:, :], ph3[i2].t[:, :], ALU.mult,
                              [sl[i2], ph3[i2]], [hid[i2]])

                    def ypart(j, w2=w2, e=e):
                        i2 = j % 2
                        i4 = j % 4
                        il = (e % 2) * 4 + j % 4
                        for dh in range(2):
                            kb.mmg(py[dh], py[dh].t[:, :], [(hid[i2].t[:, fc, :], w2.t[:, fc, dh * 512:(dh + 1) * 512]) for fc in range(4)],
                                   reads=[hid[i2], w2])
                        kb.copy("act", ysb[i4].t[:, 0:512], py[0].t[:, :], [py[0]], [ysb[i4]])
                        kb.copy("dve", ysb[i4].t[:, 512:1024], py[1].t[:, :], [py[1]], [ysb[i4]])
                        kb.idma(y2_d[:, :], ysb[i4].t[:, :], lst[il].t[:, 1:2], True, 2 * T - 1, ysb[i4], reads=[ysb[i4], lst[il]])

                    def slot2(k):
                        A = 2 * k
                        Bq = 2 * k + 1
                        hpart(A)
                        hpart(Bq)
                        ypart(A)
                        if A + 2 < NT:
                            fetch(A + 2)
                            transp(A + 2) if False else None
                        ypart(Bq)
                        if Bq + 2 < NT:
                            fetch(Bq + 2)

                    if e == 0:
                        for j_ in range(4):
                            fetch(j_)
                        transp(0)
                        transp(1)
                    if e + 1 < 16:
                        for j_ in range(4):
                            fetch(j_, e + 1)
                    NFLAT = 3

                    def slot2t(k):
                        A = 2 * k
                        hpart(A)
                        hpart(A + 1)
                        if A + 2 < NT:
                            transp(A + 2)
                        ypart(A)
                        if A + 3 < NT:
                            transp(A + 3)
                        ypart(A + 1)
                        if A + 4 < NT:
                            fetch(A + 4)
                        if A + 5 < NT:
                            fetch(A + 5)

                    for k in range(NFLAT):
                        kb.region(4096 - k * 256, lambda k=k: slot2t(k))
                        if k == 0 and e + 1 < 16:
                            transp(0, e + 1)
                            transp(1, e + 1)

                    def rest():
                        for k in range(NFLAT, NT // 2):
                            kb.region(4096 - k * 256, lambda k=k: slot2t(k))
                    kb.region(4096 - NFLAT * 256, rest)
                kb.barrier()
                kb.release_sems()
                for gt in range(NT):
                    yl = y2l[gt % 2]
                    xl = x1l[gt % 2]
                    r = rr[gt % 2]
                    kb.dma("sp", yl.t[:, :, :], y2_d[gt * 256:(gt + 1) * 256, :].rearrange("(p r) d -> p r d", r=2), yl, writes=[yl])
                    kb.dma("sp", xl.t[:, :], xa_d[gt * 128:(gt + 1) * 128, :], xl, reads=[xa_B[gt]], writes=[xl])
                    kb.op("act", lambda e, yl=yl, gt=gt: e.mul(out=yl.t[:, 0, :], in_=yl.t[:, 0, :], mul=cAB.t[:, gt, 0:1]), reads=[yl, cAB], writes=[yl])
                    kb.stt("dve", yl.t[:, 0, :], yl.t[:, 1, :], cAB.t[:, gt, 1:2], yl.t[:, 0, :], ALU.mult, ALU.add, [yl, cAB], [yl])
                    kb.stt("dve", r.t[:, :], xl.t[:, :], ALPHA, yl.t[:, 0, :], ALU.mult, ALU.add, [xl, yl], [r])
                    layernorm(ps_, r, D, fg, fb, xl, xl.t[:, :], st, mv, tmp)
                    kb.dma("pool", dst_d[gt * 128:(gt + 1) * 128, :], xl.t[:, :], xl, reads=[xl], writes=[dst_B[gt]])
                kb.barrier()
                kb.release_sems()

        def attention():
            with contextlib.ExitStack() as ps_:
                Wq = load_w(ps_, "a_Wqkv", w_qkv_d, 3072)
                xin = kb.sb("a_xin", [128, 4, D], F32, ps_)
                xbf = [kb.sb("a_xbf%d" % i, [128, D], BF16, ps_) for i in range(2)]
                xT = kb.sb("a_xT", [128, 8, 512], BF16, ps_)
                qm = [kb.sb("a_qm%d" % i, [128, 8, 512], BF16, ps_) for i in range(2)]
                km = [kb.sb("a_km%d" % i, [128, 8, 512], BF16, ps_) for i in range(2)]
                vm = [kb.sb("a_vm%d" % i, [128, 4, D], BF16, ps_) for i in range(2)]
                pT = kb.ps("a_pT", [128, 1024], BF16, ps_)
                pp = [kb.ps("a_pp%d" % i, [128, 512], F32, ps_) for i in range(4)]
                ppi = 0
                for mt in range(8):
                    load_xT(xb_d, xb_B, mt, xin, xbf, pT, xT)
                    q_ = qm[mt % 2]
                    k_ = km[mt % 2]
                    v_ = vm[mt % 2]
                    for hp in range(8):
                        p = pp[ppi % 4]; ppi += 1
                        kb.mmg(p, p.t[:, :], [(Wq.t[:, k, hp * 128:(hp + 1) * 128], xT.t[:, k, :]) for k in range(8)], reads=[Wq, xT])
                        kb.op("act", lambda e, p=p, hp=hp, q_=q_: e.mul(out=q_.t[:, hp, :], in_=p.t[:, :], mul=0.125), reads=[p], writes=[q_])
                        p = pp[ppi % 4]; ppi += 1
                        kb.mmg(p, p.t[:, :], [(Wq.t[:, k, 1024 + hp * 128:1024 + (hp + 1) * 128], xT.t[:, k, :]) for k in range(8)], reads=[Wq, xT])
                        kb.copy("dve", k_.t[:, hp, :], p.t[:, :], [p], [k_])
                    for s in range(4):
                        for dh in range(2):
                            p = pp[ppi % 4]; ppi += 1
                            kb.mmg(p, p.t[:, :], [(xT.t[:, k, s * 128:(s + 1) * 128], Wq.t[:, k, 2048 + dh * 512:2048 + (dh + 1) * 512]) for k in range(8)],
                                   reads=[Wq, xT])
                            kb.copy("dve" if dh == 0 else "act", v_.t[:, s, dh * 512:(dh + 1) * 512], p.t[:, :], [p], [v_])
                    cs = slice(mt * 512, (mt + 1) * 512)
                    kb.dma("pool", qT_d.rearrange("h p t -> p h t")[:, :, cs], q_.t[:, :, :], q_, reads=[q_], writes=[q_B[mt]])
                    kb.dma("pool", kT_d.rearrange("h p t -> p h t")[:, :, cs], k_.t[:, :, :], k_, reads=[k_], writes=[k_B[mt]])
                    kb.dma("pool", v_d[mt * 512:(mt + 1) * 512, :].rearrange("(s p) d -> p s d", p=128), v_.t[:, :, :], v_, reads=[v_], writes=[v_B[mt]])
                kb.barrier()
                kb.release_sems()

            with contextlib.ExitStack() as ps_:
                oT = kb.sb("a_oT", [128, 8, T], BF16, ps_)
                masks = kb.sb("a_mask", [128, 4, 512], BF16, ps_)
                negU = kb.sb("a_negU", [128, 128], BF16, ps_)
                negO = kb.sb("a_negO", [128, 128], BF16, ps_)
                zer = kb.sb("a_zero", [128, 128], BF16, ps_)
                kb.op("pool", lambda e: e.memset(masks.t[:, :, :], 1.0), writes=[masks])
                for m in range(4):
                    kb.op("pool", lambda e, m=m: e.affine_select(out=masks.t[:, m, :], in_=masks.t[:, m, :], pattern=[[1, 512]],
                                                                compare_op=ALU.is_gt, fill=0.0, base=-128 * m, channel_multiplier=-1),
                          reads=[masks], writes=[masks])
                kb.op("pool", lambda e: e.memset(negU.t[:, :], -1.0), writes=[negU])
                kb.op("pool", lambda e: e.affine_select(out=negU.t[:, :], in_=negU.t[:, :], pattern=[[-1, 128]],
                                                        compare_op=ALU.is_ge, fill=0.0, base=0, channel_multiplier=1),
                      reads=[negU], writes=[negU])
                kb.op("pool", lambda e: e.memset(negO.t[:, :], -1.0), writes=[negO])
                kb.op("pool", lambda e: e.memset(zer.t[:, :], 0.0), writes=[zer])
                with contextlib.ExitStack() as ps2:
                    qh = [kb.sb("a_qh%d" % i, [128, T], BF16, ps2) for i in range(2)]
                    kh = [kb.sb("a_kh%d" % i, [128, T], BF16, ps2) for i in range(2)]
                    vh = [kb.sb("a_vh%d" % i, [128, NT, 128], BF16, ps2) for i in range(2)]
                    ex = [[kb.sb("a_ex", [128, 512], F32, ps2) for _ in range(2)] for c in range(2)]
                    lnu = [[kb.sb("a_lnu", [128, 512], BF16, ps2) for _ in range(3)] for c in range(2)]
                    Sb = [[kb.sb("a_S", [128, 512], BF16, ps2) for _ in range(4)] for c in range(2)]
                    att = [[kb.sb("a_att", [128, 512], BF16, ps2) for _ in range(2)] for c in range(2)]
                    pz = [[kb.ps("a_pz", [128, 512], F32, ps2) for _ in range(2)] for c in range(2)]
                    pc = [kb.ps("a_pc", [128, 512], F32, ps2) for c in range(2)]
                    pR = kb.ps("a_pR", [128, 512], F32, ps2)
                    pcC = kb.ps("a_pcC", [128, 512], F32, ps2)
                    exC = [kb.sb("a_exC", [128, 512], F32, ps2) for _ in range(2)]
                    lnuC = [kb.sb("a_lnuC", [128, 512], BF16, ps2) for _ in range(2)]
                    SC = [kb.sb("a_SC", [128, 512], BF16, ps2) for _ in range(2)]
                    attC = [kb.sb("a_attC", [128, 512], BF16, ps2) for _ in range(2)]
                    Ssave = [kb.sb("a_Ssave", [128, 512], BF16, ps2) for _ in range(2)]
                    flg = kb.sb("a_flg", [128, 128], I32, ps2)
                    flagB = [B() for _ in range(128)]
                    rmin = kb.sb("a_rmin", [128, 2], F32, ps2)
                    THR = 120.0
                    NU = 6

                    def load_hp(hp):
                        i2 = hp % 2
                        kb.dma("sp", qh[i2].t[:, :], qT_d[hp], qh[i2], reads=q_B, writes=[qh[i2]])
                        kb.dma("sp", kh[i2].t[:, :], kT_d[hp], kh[i2], reads=k_B, writes=[kh[i2]])
                        kb.dma("sp", vh[i2].t[:, :, :], v_d.rearrange("(b p) d -> p b d", p=128)[:, :, hp * 128:(hp + 1) * 128], vh[i2],
                               reads=v_B, writes=[vh[i2]])

                    class It:
                        pass

                    nctr = [0, 0]

                    def make_items(hp, c):
                        items = []
                        for i in range(8):
                            blo = max(4 * i + 3 - (NU - 1), 0)
                            for b in range(4 * i + 3, blo - 1, -1):
                                it = It()
                                it.c = c
                                it.hp = hp
                                it.i = i
                                it.b = b
                                it.first = (b == 4 * i + 3)
                                it.last = (b == 0)
                                it.ulast = (b == blo)
                                it.hasC = (blo > 0)
                                it.fcol = (hp * 2 + c) * 8 + i
                                it.m = b - 4 * i
                                it.qlo = 128 * it.m if it.m > 0 else 0
                                it.n = nctr[c]
                                nctr[c] += 1
                                items.append(it)
                        return items

                    def stageA(it):
                        c = it.c
                        q_ = qh[it.hp % 2]
                        k_ = kh[it.hp % 2]
                        pr = slice(c * 64, (c + 1) * 64)
                        qlo = it.qlo
                        n = 512 - qlo
                        qs = slice(it.i * 512 + qlo, (it.i + 1) * 512)
                        ks = slice(it.b * 128, (it.b + 1) * 128)
                        z = pz[c][it.n % 2]
                        e_ = ex[c][it.n % 2]
                        l_ = lnu[c][it.n % 3]
                        kb.op("pe", lambda e: e.matmul(z.t[:, 0:n], lhsT=k_.t[pr, ks], rhs=q_.t[pr, qs], start=True, stop=False),
                              reads=[k_, q_], writes=[z], mark=True)
                        kb.act(e_.t[:, 0:n], z.t[:, 0:n], AF.Exp, [z], [e_])
                        kb.act(l_.t[:, 0:n], e_.t[:, 0:n], AF.Ln, [e_], [l_], bias=1.0, scale=1.0)
                        if it.m >= 0:
                            kb.tt("pool", l_.t[:, 0:n], l_.t[:, 0:n], masks.t[:, it.m, qlo:512], ALU.mult, [l_, masks], [l_])
                        if not it.last:
                            Sn = Sb[c][it.n % 4]
                            if it.first:
                                if qlo > 0:
                                    kb.op("dve", lambda e: e.memset(Sn.t[:, 0:qlo], 0.0), writes=[Sn])
                                kb.copy("dve", Sn.t[:, qlo:512], l_.t[:, 0:n], [l_], [Sn])
                            else:
                                Sp = Sb[c][(it.n - 1) % 4]
                                if qlo > 0:
                                    kb.copy("dve", Sn.t[:, 0:qlo], Sp.t[:, 0:qlo], [Sp], [Sn])
                                kb.tt("dve", Sn.t[:, qlo:512], Sp.t[:, qlo:512], l_.t[:, 0:n], ALU.add, [Sp, l_], [Sn])
                            if it.ulast and it.hasC:
                                kb.mmg(pR, pR.t[:, :], [(onesb.t[:, :], Sn.t[:, :])], reads=[onesb, Sn])
                                kb.op("dve", lambda e: e.tensor_reduce(out=rmin.t[:, c:c + 1], in_=pR.t[:, :], axis=mybir.AxisListType.X, op=ALU.min),
                                      reads=[pR], writes=[rmin])
                                kb.ts("dve", flg.t[:, it.fcol:it.fcol + 1], rmin.t[:, c:c + 1], THR, None, ALU.is_gt, None, [rmin], [flagB[it.fcol]])
                                kb.copy("dve", Ssave[c].t[:, :], Sn.t[:, :], [Sn], [Ssave[c]])

                    def stageB(it):
                        c = it.c
                        q_ = qh[it.hp % 2]
                        k_ = kh[it.hp % 2]
                        pr = slice(c * 64, (c + 1) * 64)
                        qlo = it.qlo
                        n = 512 - qlo
                        qs = slice(it.i * 512 + qlo, (it.i + 1) * 512)
                        ks = slice(it.b * 128, (it.b + 1) * 128)
                        l_ = lnu[c][it.n % 3]
                        a_ = att[c][it.n % 2]
                        M = pz[c][it.n % 2]
                        prs = [(negU.t[:, :], l_.t[:, 0:n])]
                        rds = [negU, l_]
                        if not it.first:
                            Sp = Sb[c][(it.n - 1) % 4]
                            prs.append((negO.t[:, :], Sp.t[:, qlo:512]))
                            rds.append(Sp)
                        for pi, (l__, r__) in enumerate(prs):
                            kb.op("pe", lambda e, l__=l__, r__=r__, pi=pi: e.matmul(M.t[:, 0:n], lhsT=l__, rhs=r__, start=False, stop=(pi == len(prs) - 1)),
                                  reads=rds, writes=[M], mark=(pi == len(prs) - 1))
                        kb.act(a_.t[:, 0:n], M.t[:, 0:n], AF.Exp, [M], [a_])
                        if it.m >= 0:
                            kb.tt("pool", a_.t[:, 0:n], a_.t[:, 0:n], masks.t[:, it.m, qlo:512], ALU.mult, [a_, masks], [a_])

                    def stageC(it):
                        c = it.c
                        v_ = vh[it.hp % 2]
                        pr = slice(c * 64, (c + 1) * 64)
                        qlo = it.qlo
                        n = 512 - qlo
                        a_ = att[c][it.n % 2]
                        pcb = pc[c]
                        if it.first:
                            kb.op("pe", lambda e: e.matmul(pcb.t[:, :], lhsT=zer.t[:, :], rhs=masks.t[:, 0, :], start=True, stop=False),
                                  reads=[zer, masks], writes=[pcb], mark=False)
                        kb.op("pe", lambda e: e.matmul(pcb.t[:, qlo:512], lhsT=v_.t[:, it.b, :], rhs=a_.t[:, 0:n], start=False, stop=it.ulast),
                              reads=[v_, a_], writes=[pcb], mark=True)
                        if it.ulast:
                            kb.copy("dve", oT.t[pr, it.hp, it.i * 512:(it.i + 1) * 512], pcb.t[pr, :], [pcb], [oT])

                    def cond_rest(it):
                        c = it.c
                        q_ = qh[it.hp % 2]
                        k_ = kh[it.hp % 2]
                        v_ = vh[it.hp % 2]
                        pr = slice(c * 64, (c + 1) * 64)
                        qs = slice(it.i * 512, (it.i + 1) * 512)
                        for reg in kb.mregs:
                            E_ = kb.E[kb.etmap[reg.engine]]
                            kb._waits(E_, [flagB[it.fcol]], [])
                            E_.eng.reg_load(reg, flg.t[0:1, it.fcol:it.fcol + 1])

                        def cbody():
                            kb.op("pe", lambda e: e.matmul(pcC.t[:, :], lhsT=zer.t[:, :], rhs=masks.t[:, 0, :], start=True, stop=False),
                                  reads=[zer, masks], writes=[pcC], mark=False)
                            Sprev = Ssave[c]
                            for n2, b in enumerate(range(it.b - 1, -1, -1)):
                                ks = slice(b * 128, (b + 1) * 128)
                                z = pR
                                M = pR
                                e_ = exC[n2 % 2]
                                l_ = lnuC[n2 % 2]
                                a_ = attC[n2 % 2]
                                kb.op("pe", lambda e, ks=ks: e.matmul(z.t[:, :], lhsT=k_.t[pr, ks], rhs=q_.t[pr, qs], start=True, stop=False),
                                      reads=[k_, q_], writes=[z], mark=True)
                                kb.act(e_.t[:, :], z.t[:, :], AF.Exp, [z], [e_])
                                kb.act(l_.t[:, :], e_.t[:, :], AF.Ln, [e_], [l_], bias=1.0, scale=1.0)
                                kb.op("pe", lambda e, l_=l_: e.matmul(M.t[:, :], lhsT=negU.t[:, :], rhs=l_.t[:, :], start=False, stop=False),
                                      reads=[negU, l_], writes=[M], mark=False)
                                kb.op("pe", lambda e, Sprev=Sprev: e.matmul(M.t[:, :], lhsT=negO.t[:, :], rhs=Sprev.t[:, :], start=False, stop=True),
                                      reads=[negO, Sprev], writes=[M], mark=True)
                                kb.act(a_.t[:, :], M.t[:, :], AF.Exp, [M], [a_])
                                if b > 0:
                                    Sn = SC[n2 % 2]
                                    kb.tt("dve", Sn.t[:, :], Sprev.t[:, :], l_.t[:, :], ALU.add, [Sprev, l_], [Sn])
                                    Sprev = Sn
                                kb.op("pe", lambda e, b=b, a_=a_: e.matmul(pcC.t[:, :], lhsT=v_.t[:, b, :], rhs=a_.t[:, :], start=False, stop=(b == 0)),
                                      reads=[v_, a_], writes=[pcC], mark=True)
                            oc = oT.t[pr, it.hp, it.i * 512:(it.i + 1) * 512]
                            kb.tt("dve", oc, oc, pcC.t[pr, :], ALU.add, [oT, pcC], [oT])

                        kb.region(1, cbody)

                    load_hp(0)
                    for hp in range(8):
                        if hp + 1 < 8:
                            load_hp(hp + 1)
                        l0 = make_items(hp, 0)
                        l1 = make_items(hp, 1)
                        L = []
                        for a, b_ in zip(l0, l1):
                            L.append(a)
                            L.append(b_)
                        for g in range(len(L) + 4):
                            if g < len(L):
                                stageA(L[g])
                            if 0 <= g - 2 < len(L):
                                stageB(L[g - 2])
                            if 0 <= g - 4 < len(L):
                                stageC(L[g - 4])
                                if L[g - 4].ulast and L[g - 4].hasC:
                                    cond_rest(L[g - 4])
                    kb.barrier()
                    kb.release_sems()
                with contextlib.ExitStack() as ps3:
                    P = post_mixer_allocs(ps3, 1)
                    Wo = load_w(ps3, "a_Wo", w_out1_d, 1024)
                    xres = [kb.sb("a_xres%d" % i, [128, D], F32, ps3) for i in range(2)]
                    pso = [kb.ps("a_pso%d" % i, [128, 512], F32, ps3) for i in range(2)]
                    def s1(gt):
                        xr = xres[gt % 2]
                        kb.dma("sp", xr.t[:, :], xb_d[gt * 128:(gt + 1) * 128, :], xr, reads=[xb_B[gt]], writes=[xr])
                        post_mixer(ps3, 1, gt, xr.t[:, :], xr, pso, oT, slice(gt * 128, (gt + 1) * 128), Wo, P, stage=1)

                    s1(0)
                    for gt in range(NT):
                        if gt + 1 < NT:
                            s1(gt + 1)
                        post_mixer(ps3, 1, gt, None, None, pso, oT, None, Wo, P, stage=2)
                    kb.barrier()
                    kb.release_sems()

        moe_fn = moe_sparse if SPARSE else moe
        mixer0()
        if stop_after != "m0":
            moe_fn(0, xb_d, xb_B)
            if stop_after != "moe0":
                attention()
                if stop_after != "attn":
                    moe_fn(1, out_d, out_B)
        kb.barrier()
        kb.release_sems()

    es.close()
    return nc


_NC_CACHE = {}


def kernel(x, even_w_in, even_conv_w, even_sgu_ln_g, even_sgu_ln_b, even_sgu_w_s, even_sgu_b_s, even_w_out,
           odd_w_qkv, odd_w_out, mix_ln_g, mix_ln_b, moe_w_group, moe_b_group, moe_w_router, moe_b_router,
           moe_w1, moe_w3, moe_w2, ffn_ln_g, ffn_ln_b):
    f = lambda a: np.ascontiguousarray(np.asarray(a, dtype=np.float32))
    convw = f(np.asarray(even_conv_w)[0].reshape(3, 4, 128).transpose(2, 1, 0).reshape(128, 12))
    wsT = f(np.asarray(even_sgu_w_s)[0].transpose(0, 2, 1))
    bs = f(np.asarray(even_sgu_b_s)[0].reshape(1, 1024))
    wr = f(np.concatenate([np.asarray(moe_w_group),
                           np.asarray(moe_w_router).transpose(0, 2, 1, 3).reshape(2, D, 16)], axis=2))
    br = f(np.concatenate([np.asarray(moe_b_group), np.asarray(moe_b_router).reshape(2, 16)], axis=1))
    shared = {
        "w_in": f(even_w_in[0]), "convw": convw, "sgu_g": f(even_sgu_ln_g), "sgu_b": f(even_sgu_ln_b),
        "wsT": wsT, "bs": bs, "w_out0": f(even_w_out[0]), "w_qkv": f(odd_w_qkv[0]), "w_out1": f(odd_w_out[0]),
        "mix_g": f(mix_ln_g), "mix_b": f(mix_ln_b), "wr": wr, "br": br,
        "w1": f(np.asarray(moe_w1).reshape(2, 16, D, 512)), "w3": f(np.asarray(moe_w3).reshape(2, 16, D, 512)),
        "w2": f(np.asarray(moe_w2).reshape(2, 16, 512, D)), "ffn_g": f(ffn_ln_g), "ffn_b": f(ffn_ln_b),
    }
    xs = f(x)
    if "nc" not in _NC_CACHE:
        _NC_CACHE["nc"] = build()
    nc = _NC_CACHE["nc"]
    in_maps = [dict(shared, x=xs[c]) for c in range(NCORES)]
    res = run_bass_kernel_spmd(nc, in_maps, core_ids=list(range(NCORES)))
    return np.stack([res.results[c]["out"] for c in range(NCORES)], axis=0)
```

```python
import contextlib
import numpy as np
import concourse.bass as bass
import concourse.mybir as mybir
from concourse.bass_utils import run_bass_kernel_spmd

F32 = mybir.dt.float32
BF16 = mybir.dt.bfloat16
I32 = mybir.dt.int32
OOB = 1 << 20
AF = mybir.ActivationFunctionType
ALU = mybir.AluOpType

T = 4096
D = 1024
NT = 32
ALPHA = float(4 ** 0.25)
EPS = 1e-5
NCORES = 8
SPARSE = True


class B:
    __slots__ = ("t", "w", "r", "dsem", "dcnt")

    def __init__(self, t=None):
        self.t = t
        self.w = None
        self.r = {}
        self.dsem = None
        self.dcnt = 0


class Eng:
    pass


class KB:
    def __init__(self):
        self.nc = bass.Bass("TRN2", target_bir_lowering=False)
        self.es = contextlib.ExitStack()
        self.nsem = 0
        self.dma_bufs = []
        self.rstack = []
        self.bregs = {}
        self.free_sems = []

    def sem(self, name):
        self.nsem += 1
        return self.es.enter_context(self.nc.semaphore(name))

    def sb(self, name, shape, dt, stack=None):
        st = stack if stack is not None else self.es
        self.nt = getattr(self, "nt", 0) + 1
        return B(st.enter_context(self.nc.sbuf_tensor("%s_%d" % (name, self.nt), shape, dt)))

    def ps(self, name, shape, dt, stack=None):
        st = stack if stack is not None else self.es
        self.nt = getattr(self, "nt", 0) + 1
        return B(st.enter_context(self.nc.psum_tensor("%s_%d" % (name, self.nt), shape, dt)))

    def start(self):
        nc = self.nc
        self.E = {}
        for name, eng in (("pe", nc.tensor), ("act", nc.scalar), ("dve", nc.vector),
                          ("pool", nc.gpsimd), ("sp", nc.sync)):
            e = Eng()
            e.name = name
            e.eng = eng
            e.sem = self.sem("s_" + name)
            e.count = 0
            e.seen = {}
            self.E[name] = e
        ET = mybir.EngineType
        self.etmap = {ET.PE: "pe", ET.Activation: "act", ET.DVE: "dve", ET.Pool: "pool", ET.SP: "sp"}
        self.mregs = nc.alloc_registers("mr", [ET.PE, ET.Activation, ET.DVE, ET.Pool, ET.SP])

    def _waits(self, E, reads, writes):
        need = {}

        def acc(tok):
            k = id(tok[0])
            if k not in need or need[k][1] < tok[1]:
                need[k] = tok

        for b in reads:
            if b.w is not None:
                acc(b.w)
        for b in writes:
            if b.w is not None:
                acc(b.w)
            for tok in b.r.values():
                acc(tok)
        for k, (sem, val) in need.items():
            if E.name == "pe" and sem is E.sem:
                continue
            if E.seen.get(k, 0) >= val:
                continue
            E.eng.wait_ge(sem, val)
            E.seen[k] = val

    def _commit(self, tok, reads, writes):
        k = id(tok[0])
        for b in reads:
            b.r[k] = tok
        for b in writes:
            b.w = tok
            b.r = {}

    def op(self, en, fn, reads=(), writes=(), mark=True):
        E = self.E[en]
        self._waits(E, reads, writes)
        inst = fn(E.eng)
        if mark:
            E.count += 1
            inst.then_inc(E.sem, 1)
            tok = (E.sem, E.count)
        else:
            tok = (E.sem, E.count + 1)
        self._commit(tok, reads, writes)
        return inst

    def dma(self, q, out_ap, in_ap, sbufB, reads=(), writes=()):
        E = self.E[q]
        self._waits(E, reads, writes)
        self._get_dsem(sbufB)
        for rd in self.rstack:
            rec = rd[q].setdefault(id(sbufB.dsem), [sbufB.dsem, sbufB.dcnt, 0])
            rec[2] += 16
        sbufB.dcnt += 16
        E.eng.dma_start(out=out_ap, in_=in_ap).then_inc(sbufB.dsem, 16)
        tok = (sbufB.dsem, sbufB.dcnt)
        self._commit(tok, reads, writes)

    def idma(self, out_ap, in_ap, idx_ap, scatter, bound, sbufB, reads=(), writes=()):
        E = self.E["pool"]
        self._waits(E, reads, writes)
        self._get_dsem(sbufB)
        for rd in self.rstack:
            rec = rd["pool"].setdefault(id(sbufB.dsem), [sbufB.dsem, sbufB.dcnt, 0])
            rec[2] += 16
        sbufB.dcnt += 16
        off = bass.IndirectOffsetOnAxis(ap=idx_ap, axis=0)
        if bound not in self.bregs:
            r = self.es.enter_context(E.eng.register("rb%d" % bound))
            E.eng.reg_mov(r, bound)
            self.bregs[bound] = r
        bound = self.bregs[bound]
        if scatter:
            E.eng.indirect_dma_start(out=out_ap, out_offset=off, in_=in_ap, in_offset=None,
                                     bounds_check=bound, oob_is_err=False).then_inc(sbufB.dsem, 16)
        else:
            E.eng.indirect_dma_start(out=out_ap, out_offset=None, in_=in_ap, in_offset=off,
                                     bounds_check=bound, oob_is_err=False).then_inc(sbufB.dsem, 16)
        tok = (sbufB.dsem, sbufB.dcnt)
        self._commit(tok, reads, writes)

    def region(self, thr, body):
        engs = list(self.E.values())
        snap = {E.name: dict(E.seen) for E in engs}
        before = {E.name: E.count for E in engs}
        rd = {E.name: {} for E in engs}
        self.rstack.append(rd)
        with self.nc.If_cmp(self.mregs, thr, "IS_LT"):
            body()
        self.rstack.pop()
        with self.nc.Else():
            for E in engs:
                nm = E.count - before[E.name]
                if nm > 0:
                    E.eng.wait_ge(E.sem, before[E.name])
                    E.eng.sem_inc(E.sem, nm)
                for (sem, bt, add) in rd[E.name].values():
                    E.eng.wait_ge(sem, bt)
                    E.eng.sem_inc(sem, add)
        for E in engs:
            E.seen = snap[E.name]

    def _get_dsem(self, b):
        if b.dsem is None:
            if self.free_sems:
                b.dsem, b.dcnt = self.free_sems.pop()
            else:
                b.dsem = self.sem("d%d" % self.nsem)
                b.dcnt = 0
            self.dma_bufs.append(b)

    def release_sems(self):
        for b in self.dma_bufs:
            if b.dsem is not None:
                self.free_sems.append((b.dsem, b.dcnt))
                b.dsem = None
        self.dma_bufs = []

    def barrier(self):
        toks = []
        for e in self.E.values():
            if e.count > 0:
                toks.append((e.sem, e.count))
        for b in self.dma_bufs:
            if b.dcnt > 0:
                toks.append((b.dsem, b.dcnt))
        for E in self.E.values():
            for (sem, val) in toks:
                if sem is E.sem:
                    continue
                k = id(sem)
                if E.seen.get(k, 0) >= val:
                    continue
                E.eng.wait_ge(sem, val)
                E.seen[k] = val

    def mmg(self, outB, out_ap, pairs, reads):
        n = len(pairs)
        for i, (l, r) in enumerate(pairs):
            self.op("pe", lambda e, l=l, r=r, i=i: e.matmul(out_ap, lhsT=l, rhs=r, start=(i == 0), stop=(i == n - 1)),
                    reads=reads, writes=[outB], mark=(i == n - 1))

    def act(self, out_ap, in_ap, func, reads, writes, bias=None, scale=None):
        kw = {}
        if bias is not None:
            kw["bias"] = bias
        if scale is not None:
            kw["scale"] = scale
        self.op("act", lambda e: e.activation(out=out_ap, in_=in_ap, func=func, **kw), reads=reads, writes=writes)

    def copy(self, en, out_ap, in_ap, reads, writes):
        if en == "act":
            self.op("act", lambda e: e.copy(out=out_ap, in_=in_ap), reads=reads, writes=writes)
        else:
            self.op(en, lambda e: e.tensor_copy(out=out_ap, in_=in_ap), reads=reads, writes=writes)

    def tt(self, en, out_ap, a_ap, b_ap, op, reads, writes):
        self.op(en, lambda e: e.tensor_tensor(out=out_ap, in0=a_ap, in1=b_ap, op=op), reads=reads, writes=writes)

    def ts(self, en, out_ap, in_ap, s1, s2, op0, op1, reads, writes):
        if op1 is None:
            self.op(en, lambda e: e.tensor_scalar(out=out_ap, in0=in_ap, scalar1=s1, scalar2=None, op0=op0),
                    reads=reads, writes=writes)
        else:
            self.op(en, lambda e: e.tensor_scalar(out=out_ap, in0=in_ap, scalar1=s1, scalar2=s2, op0=op0, op1=op1),
                    reads=reads, writes=writes)

    def stt(self, en, out_ap, in0, scalar, in1, op0, op1, reads, writes):
        self.op(en, lambda e: e.scalar_tensor_tensor(out=out_ap, in0=in0, scalar=scalar, in1=in1, op0=op0, op1=op1),
                reads=reads, writes=writes)


def build(stop_after=None, dbg=False):
    kb = KB()
    dk = {"kind": "ExternalOutput"} if dbg else {}
    nc = kb.nc

    def din(name, shape):
        return nc.dram_tensor(name, shape, F32, kind="ExternalInput").ap()

    x_d = din("x", [T, D])
    w_in_d = din("w_in", [D, 2560])
    convw_d = din("convw", [128, 12])
    sgu_g_d = din("sgu_g", [1, 512])
    sgu_b_d = din("sgu_b", [1, 512])
    wsT_d = din("wsT", [8, 128, 128])
    bs_d = din("bs", [1, 1024])
    w_out0_d = din("w_out0", [D, D])
    w_qkv_d = din("w_qkv", [D, 3072])
    w_out1_d = din("w_out1", [D, D])
    mix_g_d = din("mix_g", [2, D])
    mix_b_d = din("mix_b", [2, D])
    wr_d = din("wr", [2, D, 20])
    br_d = din("br", [2, 20])
    w1_d = din("w1", [2, 16, D, 512])
    w3_d = din("w3", [2, 16, D, 512])
    w2_d = din("w2", [2, 16, 512, D])
    ffn_g_d = din("ffn_g", [2, D])
    ffn_b_d = din("ffn_b", [2, D])
    out_d = nc.dram_tensor("out", [T, D], F32, kind="ExternalOutput").ap()

    xa_d = nc.dram_tensor("xa_s", [T, D], F32, **dk).ap()
    xb_d = nc.dram_tensor("xb_s", [T, D], F32, **dk).ap()
    xT_d = nc.dram_tensor("xT_s", [128, 8, T], BF16, **dk).ap()
    qT_d = nc.dram_tensor("qT_s", [8, 128, T], BF16, **dk).ap()
    kT_d = nc.dram_tensor("kT_s", [8, 128, T], BF16, **dk).ap()
    v_d = nc.dram_tensor("v_s", [T, D], BF16, **dk).ap()

    x1b_d = nc.dram_tensor("x1b_s", [T, D], BF16, **dk).ap()
    list_d = nc.dram_tensor("list_s", [16 * 4096, 2], I32, **dk).ap()
    y2_d = nc.dram_tensor("y2_s", [2 * T, D], F32, **dk).ap()
    x1bd_B = [B() for _ in range(NT)]
    xa_B = [B() for _ in range(NT)]
    xb_B = [B() for _ in range(NT)]
    xT_B = [B() for _ in range(8)]
    q_B = [B() for _ in range(8)]
    k_B = [B() for _ in range(8)]
    v_B = [B() for _ in range(8)]
    out_B = [B() for _ in range(NT)]

    kb.start()
    es = kb.es

    ident = kb.sb("ident", [128, 128], BF16)
    c_all = kb.sb("c_all", [128, NT, 16], F32)
    cAB = kb.sb("cAB", [128, NT, 2], F32)
    off_t = kb.sb("off_t", [128, 16], F32)
    ebase1 = kb.sb("ebase1", [128, 16], F32)
    Lstr = kb.sb("Lstr", [128, 128], BF16)
    onesb = kb.sb("onesb", [128, 128], BF16)
    cnt_i = kb.sb("cnt_i", [128, 16], I32)
    tl2 = kb.sb("tl2", [128, NT, 2, 2], I32)
    oobt = kb.sb("oobt", [128, 1024], I32)

    block = es.enter_context(nc.Block())

    @block.sync
    def _(sync):
        kb.op("pool", lambda e: e.memset(ident.t[:], 0.0), writes=[ident])
        kb.op("pool", lambda e: e.affine_select(out=ident.t[:], in_=ident.t[:], pattern=[[-1, 128]],
                                                compare_op=ALU.not_equal, fill=1.0, base=0, channel_multiplier=1),
              reads=[ident], writes=[ident])

        kb.op("pool", lambda e: e.memset(onesb.t[:], 1.0), writes=[onesb])
        kb.op("pool", lambda e: e.memset(Lstr.t[:], 1.0), writes=[Lstr])
        kb.op("pool", lambda e: e.affine_select(out=Lstr.t[:], in_=Lstr.t[:], pattern=[[1, 128]],
                                                compare_op=ALU.is_gt, fill=0.0, base=0, channel_multiplier=-1),
              reads=[Lstr], writes=[Lstr])
        kb.op("pool", lambda e: e.iota(ebase1.t[:], [[4096, 16]], base=1, channel_multiplier=0,
                                       allow_small_or_imprecise_dtypes=True), writes=[ebase1])
        kb.op("pool", lambda e: e.memset(oobt.t[:], OOB), writes=[oobt])
        for r_ in range(2):
            kb.op("pool", lambda e, r_=r_: e.iota(tl2.t[:, :, r_, 0], [[128, NT]], base=0, channel_multiplier=1), writes=[tl2])
            kb.op("pool", lambda e, r_=r_: e.iota(tl2.t[:, :, r_, 1], [[256, NT]], base=r_, channel_multiplier=2), writes=[tl2])

        def layernorm(ps_, r, D_, g_bc, b_bc, outB, out_ap, stats, mv, tmp, mul_eng="pool"):
            nch = D_ // 512
            for c in range(nch):
                kb.op("dve", lambda e, c=c: e.bn_stats(out=stats.t[:, c * 6:(c + 1) * 6], in_=r.t[:, c * 512:(c + 1) * 512]),
                      reads=[r], writes=[stats])
            kb.op("dve", lambda e: e.bn_aggr(out=mv.t[:, 0:2], in_=stats.t[:, 0:nch * 6]), reads=[stats], writes=[mv])
            kb.act(tmp.t[:, 0:1], mv.t[:, 1:2], AF.Ln, [mv], [tmp], bias=EPS, scale=1.0)
            kb.act(tmp.t[:, 1:2], tmp.t[:, 0:1], AF.Exp, [tmp], [tmp], scale=-0.5)
            if D_ == D:
                kb.ts("dve", tmp.t[:, 0:1], mv.t[:, 0:1], -1.0, tmp.t[:, 1:2], ALU.mult, ALU.mult, [mv, tmp], [tmp])
                kb.act(r.t[:, 0:D_], r.t[:, 0:D_], AF.Identity, [r, tmp], [r], bias=tmp.t[:, 0:1], scale=tmp.t[:, 1:2])
            else:
                kb.ts("dve", r.t[:, 0:D_], r.t[:, 0:D_], mv.t[:, 0:1], tmp.t[:, 1:2], ALU.subtract, ALU.mult, [r, mv, tmp], [r])
            kb.tt(mul_eng, r.t[:, 0:D_], r.t[:, 0:D_], g_bc.t[:, 0:D_], ALU.mult, [r, g_bc], [r])
            kb.tt(mul_eng, out_ap, r.t[:, 0:D_], b_bc.t[:, 0:D_], ALU.add, [r, b_bc], [outB])

        def bcast_load(dst, src_row_ap):
            kb.dma("sp", dst.t[:], src_row_ap.partition_broadcast(128), dst, writes=[dst])

        def post_mixer(ps_, lyr, gt, x_res_ap, x_resB, pso, yT, yT_cols, Wo, P, stage=0):
            x1 = P["x1"][gt % 3]
            if stage in (0, 1):
                for dh in range(2):
                    kb.mmg(pso[dh], pso[dh].t[:, :],
                           [(yT.t[:, k, yT_cols], Wo.t[:, k, dh * 512:(dh + 1) * 512]) for k in range(8)],
                           reads=[yT, Wo])
                r = P["r"][gt % 2]
                for dh in range(2):
                    kb.stt("dve", r.t[:, dh * 512:(dh + 1) * 512], x_res_ap[:, dh * 512:(dh + 1) * 512], ALPHA,
                           pso[dh].t[:, :], ALU.mult, ALU.add, [x_resB, pso[dh]], [r])
                layernorm(ps_, r, D, P["mg"], P["mb"], x1, x1.t[:, :], P["stats"], P["mv"], P["tmp"])
                if stage == 1:
                    return
            kb.dma("pool", xa_d[gt * 128:(gt + 1) * 128, :], x1.t[:, :], x1, reads=[x1], writes=[xa_B[gt]])
            x1b = P["x1b"][gt % 2]
            kb.copy("act", x1b.t[:, :], x1.t[:, :], [x1], [x1b])
            kb.dma("pool", x1b_d[gt * 128:(gt + 1) * 128, :], x1b.t[:, :], x1b, reads=[x1b], writes=[x1bd_B[gt]])
            pT = P["pT"]
            for k in range(8):
                kb.op("pe", lambda e, k=k: e.transpose(pT.t[:, k * 128:(k + 1) * 128], x1b.t[:, k * 128:(k + 1) * 128], ident.t[:]),
                      reads=[x1b, ident], writes=[pT], mark=(k == 7))
            x1T = P["x1T"][gt % 2]
            s = gt % 4
            kb.copy("act", x1T.t[:, :, :], pT.t[:, :].rearrange("p (k t) -> p k t", k=8), [pT], [x1T])
            prt = P["prt"]
            kb.mmg(prt, prt.t[:, 0:20], [(x1T.t[:, k, :], P["Wr"].t[:, k, :]) for k in range(8)],
                   reads=[x1T, P["Wr"]])
            rt = P["rt"]
            lg = rt.t[:, 0:20]
            kb.tt("dve", lg, prt.t[:, 0:20], P["brb"].t[:, :], ALU.add, [prt, P["brb"]], [rt])
            R_ = [rt]
            kb.op("dve", lambda e: e.reduce_max(out=rt.t[:, 20:21], in_=rt.t[:, 0:4], axis=mybir.AxisListType.X), reads=R_, writes=R_)
            kb.ts("dve", rt.t[:, 24:28], rt.t[:, 0:4], rt.t[:, 20:21], None, ALU.is_equal, None, R_, R_)
            kb.ts("dve", rt.t[:, 21:22], rt.t[:, 20:21], -1.0, None, ALU.mult, None, R_, R_)
            kb.act(rt.t[:, 28:32], rt.t[:, 0:4], AF.Exp, R_, R_, bias=rt.t[:, 21:22], scale=1.0)
            kb.op("dve", lambda e: e.reduce_sum(out=rt.t[:, 22:23], in_=rt.t[:, 28:32], axis=mybir.AxisListType.X), reads=R_, writes=R_)
            kb.ts("dve", rt.t[:, 32:36], rt.t[:, 4:8], rt.t[:, 24:25], None, ALU.mult, None, R_, R_)
            for g in range(1, 4):
                kb.stt("dve", rt.t[:, 32:36], rt.t[:, 4 + 4 * g:8 + 4 * g], rt.t[:, 24 + g:25 + g], rt.t[:, 32:36],
                       ALU.mult, ALU.add, R_, R_)
            kb.op("dve", lambda e: e.reduce_max(out=rt.t[:, 36:37], in_=rt.t[:, 32:36], axis=mybir.AxisListType.X), reads=R_, writes=R_)
            kb.ts("dve", rt.t[:, 40:44], rt.t[:, 32:36], rt.t[:, 36:37], None, ALU.is_equal, None, R_, R_)
            kb.ts("dve", rt.t[:, 37:38], rt.t[:, 36:37], -1.0, None, ALU.mult, None, R_, R_)
            kb.act(rt.t[:, 44:48], rt.t[:, 32:36], AF.Exp, R_, R_, bias=rt.t[:, 37:38], scale=1.0)
            kb.op("dve", lambda e: e.reduce_max(out=rt.t[:, 38:39], in_=rt.t[:, 44:48], axis=mybir.AxisListType.X), reads=R_, writes=R_)
            kb.tt("dve", rt.t[:, 48:52], rt.t[:, 44:48], rt.t[:, 40:44], ALU.mult, R_, R_)
            kb.tt("dve", rt.t[:, 48:52], rt.t[:, 44:48], rt.t[:, 48:52], ALU.subtract, R_, R_)
            kb.op("dve", lambda e: e.reduce_max(out=rt.t[:, 39:40], in_=rt.t[:, 48:52], axis=mybir.AxisListType.X), reads=R_, writes=R_)
            kb.ts("dve", rt.t[:, 52:56], rt.t[:, 44:48], rt.t[:, 39:40], None, ALU.is_ge, None, R_, R_)
            kb.tt("dve", rt.t[:, 52:56], rt.t[:, 52:56], rt.t[:, 44:48], ALU.mult, R_, R_)
            kb.tt("dve", rt.t[:, 56:57], rt.t[:, 38:39], rt.t[:, 39:40], ALU.add, R_, R_)
            kb.tt("dve", rt.t[:, 56:57], rt.t[:, 56:57], rt.t[:, 22:23], ALU.mult, R_, R_)
            kb.op("dve", lambda e: e.reciprocal(out=rt.t[:, 57:58], in_=rt.t[:, 56:57]), reads=R_, writes=R_)
            kb.ts("dve", rt.t[:, 60:64], rt.t[:, 24:28], rt.t[:, 57:58], None, ALU.mult, None, R_, R_)
            for g in range(4):
                kb.ts("dve", c_all.t[:, gt, 4 * g:4 * g + 4], rt.t[:, 52:56], rt.t[:, 60 + g:61 + g], None, ALU.mult, None,
                      R_, [c_all])
            if s == 3 and not SPARSE:
                mt = gt // 4
                kb.dma("pool", xT_d[:, :, mt * 512:(mt + 1) * 512], x1T.t[:, :, :], x1T, reads=[x1T], writes=[xT_B[mt]])
            if SPARSE:
                rt2 = P["rt2"]
                mkb = P["mkb"]
                idx2 = P["idx2"][gt % 2]
                Q_ = [rt2]
                cg = c_all.t[:, gt, :]
                kb.ts("dve", rt2.t[:, 0:16], cg, 0.0, None, ALU.is_gt, None, [c_all], Q_)
                kb.copy("dve", mkb.t[:, :], rt2.t[:, 0:16], Q_, [mkb])
                kb.mmg(prt, prt.t[:, 32:48], [(Lstr.t[:, :], mkb.t[:, :])], reads=[Lstr, mkb])
                kb.mmg(prt, prt.t[:, 48:64], [(onesb.t[:, :], mkb.t[:, :])], reads=[onesb, mkb])
                kb.tt("dve", rt2.t[:, 16:32], prt.t[:, 32:48], ebase1.t[:, :], ALU.add, [prt, ebase1], Q_)
                kb.tt("dve", rt2.t[:, 16:32], rt2.t[:, 16:32], off_t.t[:, :], ALU.add, Q_ + [off_t], Q_)
                kb.tt("dve", rt2.t[:, 16:32], rt2.t[:, 16:32], rt2.t[:, 0:16], ALU.mult, Q_, Q_)
                kb.tt("dve", off_t.t[:, :], off_t.t[:, :], prt.t[:, 48:64], ALU.add, [off_t, prt], [off_t])
                kb.op("dve", lambda e: e.reduce_max(out=rt2.t[:, 64:65], in_=rt2.t[:, 16:32], axis=mybir.AxisListType.X), reads=Q_, writes=Q_)
                kb.ts("dve", rt2.t[:, 32:48], rt2.t[:, 16:32], rt2.t[:, 64:65], None, ALU.is_equal, None, Q_, Q_)
                kb.tt("dve", rt2.t[:, 48:64], rt2.t[:, 32:48], cg, ALU.mult, Q_ + [c_all], Q_)
                kb.op("dve", lambda e: e.reduce_sum(out=cAB.t[:, gt, 0:1], in_=rt2.t[:, 48:64], axis=mybir.AxisListType.X), reads=Q_, writes=[cAB])
                kb.op("dve", lambda e: e.reduce_sum(out=rt2.t[:, 66:67], in_=cg, axis=mybir.AxisListType.X), reads=[c_all], writes=Q_)
                kb.tt("dve", cAB.t[:, gt, 1:2], rt2.t[:, 66:67], cAB.t[:, gt, 0:1], ALU.subtract, Q_ + [cAB], [cAB])
                kb.tt("dve", rt2.t[:, 48:64], rt2.t[:, 16:32], rt2.t[:, 32:48], ALU.mult, Q_, Q_)
                kb.tt("dve", rt2.t[:, 48:64], rt2.t[:, 16:32], rt2.t[:, 48:64], ALU.subtract, Q_, Q_)
                kb.op("dve", lambda e: e.reduce_max(out=rt2.t[:, 67:68], in_=rt2.t[:, 48:64], axis=mybir.AxisListType.X), reads=Q_, writes=Q_)
                kb.ts("dve", rt2.t[:, 68:69], rt2.t[:, 67:68], 0.0, float(OOB), ALU.is_equal, ALU.mult, Q_, Q_)
                kb.tt("dve", rt2.t[:, 67:68], rt2.t[:, 67:68], rt2.t[:, 68:69], ALU.add, Q_, Q_)
                kb.ts("dve", idx2.t[:, 0:1], rt2.t[:, 64:65], -1.0, None, ALU.add, None, Q_, [idx2])
                kb.ts("dve", idx2.t[:, 1:2], rt2.t[:, 67:68], -1.0, None, ALU.add, None, Q_, [idx2])
                for r_ in range(2):
                    kb.idma(list_d[:, :], tl2.t[:, gt, r_, :], idx2.t[:, r_:r_ + 1], True, 16 * 4096 - 1, idx2,
                            reads=[idx2, tl2, P["listB"]])
                if gt == NT - 1:
                    kb.ts("dve", cnt_i.t[:, :], off_t.t[:, :], -1.0, 4096.0, ALU.mult, ALU.add, [off_t], [cnt_i])

        def post_mixer_allocs(ps_, lyr):
            P = {}
            P["r"] = [kb.sb("pm_r%d" % i, [128, D], F32, ps_) for i in range(2)]
            P["x1"] = [kb.sb("pm_x1%d" % i, [128, D], F32, ps_) for i in range(3)]
            P["x1b"] = [kb.sb("pm_x1b", [128, D], BF16, ps_) for _ in range(2)]
            P["rt2"] = kb.sb("pm_rt2", [128, 128], F32, ps_)
            P["mkb"] = kb.sb("pm_mkb", [128, 16], BF16, ps_)
            P["idx2"] = [kb.sb("pm_idx2", [128, 2], I32, ps_) for _ in range(2)]
            P["listB"] = B()
            kb.op("dve", lambda e: e.memset(off_t.t[:], 0.0), writes=[off_t])
            kb.dma("pool", list_d.rearrange("(p a) c -> p (a c)", p=128), oobt.t[:, :], oobt, reads=[oobt], writes=[P["listB"]])
            P["x1T"] = [kb.sb("pm_x1T", [128, 8, 128], BF16, ps_) for _ in range(2)]
            P["stats"] = kb.sb("pm_stats", [128, 12], F32, ps_)
            P["mv"] = kb.sb("pm_mv", [128, 2], F32, ps_)
            P["tmp"] = kb.sb("pm_tmp", [128, 2], F32, ps_)
            P["rt"] = kb.sb("pm_rt", [128, 64], F32, ps_)
            P["mg"] = kb.sb("pm_mg", [128, D], F32, ps_)
            P["mb"] = kb.sb("pm_mb", [128, D], F32, ps_)
            P["brb"] = kb.sb("pm_brb", [128, 20], F32, ps_)
            P["Wr"] = kb.sb("pm_Wr", [128, 8, 20], BF16, ps_)
            P["pT"] = kb.ps("pm_pT", [128, 1024], BF16, ps_)
            P["prt"] = kb.ps("pm_prt", [128, 512], F32, ps_)
            bcast_load(P["mg"], mix_g_d[lyr:lyr + 1, :])
            bcast_load(P["mb"], mix_b_d[lyr:lyr + 1, :])
            bcast_load(P["brb"], br_d[lyr:lyr + 1, :])
            kb.dma("pool", P["Wr"].t[:, :, :], wr_d[lyr].rearrange("(k p) n -> p k n", p=128), P["Wr"], writes=[P["Wr"]])
            return P

        def load_w(ps_, name, src_ap, ncols):
            W = kb.sb(name, [128, 8, ncols], BF16, ps_)
            v = src_ap.rearrange("(k p) n -> p k n", p=128)
            for k in range(8):
                kb.dma("pool", W.t[:, k, :], v[:, k, :], W, writes=[W])
            return W

        def load_xT(xsrc_d, xsrc_B, mt, xin, xbf, pT, xT):
            rd = [xsrc_B[mt * 4 + s] for s in range(4)] if xsrc_B is not None else []
            kb.dma("sp", xin.t[:, :, :], xsrc_d[mt * 512:(mt + 1) * 512, :].rearrange("(s p) d -> p s d", p=128), xin,
                   reads=rd, writes=[xin])
            for s in range(4):
                xb = xbf[s % 2]
                kb.copy("act", xb.t[:, :], xin.t[:, s, :], [xin], [xb])
                for k in range(8):
                    kb.op("pe", lambda e, k=k, xb=xb: e.transpose(pT.t[:, k * 128:(k + 1) * 128], xb.t[:, k * 128:(k + 1) * 128], ident.t[:]),
                          reads=[xb, ident], writes=[pT], mark=(k == 7))
                kb.copy("act", xT.t[:, :, s * 128:(s + 1) * 128], pT.t[:, :].rearrange("p (k t) -> p k t", k=8), [pT], [xT])

        def mixer0():
            with contextlib.ExitStack() as ps_:
                P = post_mixer_allocs(ps_, 0)
                Wi = load_w(ps_, "m0_Wi", w_in_d, 2560)
                Wo = load_w(ps_, "m0_Wo", w_out0_d, 1024)
                WsT = kb.sb("m0_WsT", [128, 8, 128], BF16, ps_)
                kb.dma("pool", WsT.t[:, :, :], wsT_d.rearrange("h s t -> s h t"), WsT, writes=[WsT])
                kb.op("pool", lambda e: e.memset(WsT.t[64:128, :, 0:64], 0.0), reads=[WsT], writes=[WsT])
                bsr = kb.sb("m0_bsr", [1, 1024], BF16, ps_)
                kb.dma("pool", bsr.t[:, :], bs_d[:, :], bsr, writes=[bsr])
                onesr = kb.sb("m0_ones", [1, 128], BF16, ps_)
                kb.op("pool", lambda e: e.memset(onesr.t[:, :], 1.0), writes=[onesr])
                cw = kb.sb("m0_cw", [128, 12], F32, ps_)
                kb.dma("sp", cw.t[:, :], convw_d[:, :], cw, writes=[cw])
                sg = kb.sb("m0_sg", [128, 512], F32, ps_)
                sbb = kb.sb("m0_sbb", [128, 512], F32, ps_)
                bcast_load(sg, sgu_g_d[0:1, :])
                bcast_load(sbb, sgu_b_d[0:1, :])
                xin = kb.sb("m0_xin", [128, 4, D], F32, ps_)
                xbf = [kb.sb("m0_xbf%d" % i, [128, D], BF16, ps_) for i in range(2)]
                xres = [kb.sb("m0_xres%d" % i, [128, D], F32, ps_) for i in range(2)]
                xT = [kb.sb("m0_xT", [128, 8, 512], BF16, ps_) for _ in range(2)]
                yT = [kb.sb("m0_yT", [128, 8, 512], BF16, ps_) for _ in range(2)]
                ub = kb.sb("m0_ub", [128, 4, 514], F32, ps_)
                Cs = kb.sb("m0_Cs", [128, 512], F32, ps_)
                acc = kb.sb("m0_acc", [128, 512], F32, ps_)
                zu = kb.sb("m0_zu", [128, 4, 512], F32, ps_)
                g1 = kb.sb("m0_g1", [128, 512], F32, ps_)
                g2 = kb.sb("m0_g2", [128, 512], F32, ps_)
                gv = kb.sb("m0_gv", [128, 512], F32, ps_)
                vt = kb.sb("m0_vt", [128, 4, 512], BF16, ps_)
                st2 = kb.sb("m0_st2", [128, 6], F32, ps_)
                mv2 = kb.sb("m0_mv2", [128, 2], F32, ps_)
                tmp2 = kb.sb("m0_tmp2", [128, 2], F32, ps_)
                pp = [kb.ps("m0_pp%d" % i, [128, 512], F32, ps_) for i in range(2)]
                psg = [kb.ps("m0_psg%d" % i, [128, 512], F32, ps_) for i in range(2)]
                pso = [kb.ps("m0_pso%d" % i, [128, 512], F32, ps_) for i in range(2)]
                pT = P["pT"]
                kb.op("pool", lambda e: e.memset(ub.t[:, :, :], 0.0), writes=[ub])
                ppi = [0]

                def proj_fm(c, xT):
                    p = pp[ppi[0] % 2]
                    ppi[0] += 1
                    kb.mmg(p, p.t[:, :], [(Wi.t[:, k, c * 128:(c + 1) * 128], xT.t[:, k, :]) for k in range(8)], reads=[Wi, xT])
                    return p

                def gelu(p, p_ap, outB, out_ap, n):
                    kb.act(g1.t[:, 0:n], p_ap, AF.Square, [p], [g1])
                    kb.act(g1.t[:, 0:n], g1.t[:, 0:n], AF.Identity, [g1], [g1], bias=1.0, scale=0.044715)
                    kb.tt("dve", g2.t[:, 0:n], g1.t[:, 0:n], p_ap, ALU.mult, [g1, p], [g2])
                    kb.act(g1.t[:, 0:n], g2.t[:, 0:n], AF.Exp, [g2], [g1], scale=-1.5957691216057308)
                    kb.act(g1.t[:, 0:n], g1.t[:, 0:n], AF.Identity, [g1], [g1], bias=1.0, scale=1.0)
                    kb.op("dve", lambda e: e.reciprocal(out=g2.t[:, 0:n], in_=g1.t[:, 0:n]), reads=[g1], writes=[g2])
                    kb.tt("dve", out_ap, g2.t[:, 0:n], p_ap, ALU.mult, [g2, p], [outB])

                def piece(mt, j):
                    xTb = xT[mt % 2]
                    yTb = yT[mt % 2]
                    u = ub
                    pC = proj_fm(4 + j, xTb)
                    kb.copy("act", Cs.t[:, :], pC.t[:, :], [pC], [Cs])
                    pH = proj_fm(8 + j, xTb)
                    kb.copy("dve", u.t[:, j, 0:2], u.t[:, j, 512:514], [u], [u])
                    kb.tt("dve", u.t[:, j, 2:514], Cs.t[:, :], pH.t[:, :], ALU.mult, [Cs, pH], [u])
                    kb.ts("dve", acc.t[:, :], u.t[:, j, 2:514], cw.t[:, j * 3 + 2:j * 3 + 3], None, ALU.mult, None, [u, cw], [acc])
                    kb.stt("dve", acc.t[:, :], u.t[:, j, 1:513], cw.t[:, j * 3 + 1:j * 3 + 2], acc.t[:, :], ALU.mult, ALU.add, [u, cw, acc], [acc])
                    kb.stt("dve", acc.t[:, :], u.t[:, j, 0:512], cw.t[:, j * 3:j * 3 + 1], acc.t[:, :], ALU.mult, ALU.add, [u, cw, acc], [acc])
                    pB = proj_fm(j, xTb)
                    kb.tt("dve", yTb.t[:, j, :], acc.t[:, :], pB.t[:, :], ALU.mult, [acc, pB], [yTb])
                    pZ = proj_fm(12 + j, xTb)
                    gelu(pZ, pZ.t[:, :], zu, zu.t[:, j, :], 512)
                    p = pp[ppi[0] % 2]
                    ppi[0] += 1
                    kb.mmg(p, p.t[:, :], [(xTb.t[:, k, j * 128:(j + 1) * 128], Wi.t[:, k, 2048:2560]) for k in range(8)], reads=[Wi, xTb])
                    gelu(p, p.t[:, :], gv, gv.t[:, :], 512)
                    layernorm(ps_, gv, 512, sg, sbb, vt, vt.t[:, j, :], st2, mv2, tmp2, mul_eng="pool")

                def sgu(mt):
                    yTb = yT[mt % 2]
                    for hp in range(4):
                        for j in range(2):
                            h = 2 * hp + j
                            pg = psg[j]
                            for s in range(4):
                                kb.op("pe", lambda e, s=s, pg=pg, h=h, hp=hp: e.matmul(pg.t[:, s * 128:(s + 1) * 128], lhsT=vt.t[:, s, hp * 128:(hp + 1) * 128],
                                                                         rhs=WsT.t[:, h, :], start=True, stop=False),
                                      reads=[vt, WsT], writes=[pg], mark=False)
                                kb.op("pe", lambda e, s=s, pg=pg, h=h: e.matmul(pg.t[:, s * 128:(s + 1) * 128], lhsT=onesr.t[0:1, :],
                                                                   rhs=bsr.t[0:1, h * 128:(h + 1) * 128], start=False, stop=True),
                                      reads=[onesr, bsr], writes=[pg], mark=(s == 3))
                            kb.tt("dve", yTb.t[j * 64:(j + 1) * 64, 4 + hp, :], zu.t[j * 64:(j + 1) * 64, hp, :], pg.t[j * 64:(j + 1) * 64, :],
                                  ALU.mult, [zu, pg], [yTb])

                def back(mt, s):
                    gt = mt * 4 + s
                    xr = xres[gt % 2]
                    kb.dma("sp", xr.t[:, :], x_d[gt * 128:(gt + 1) * 128, :], xr, writes=[xr])
                    post_mixer(ps_, 0, gt, xr.t[:, :], xr, pso, yT[mt % 2], slice(s * 128, (s + 1) * 128), Wo, P, stage=1)
                    if gt > 0:
                        post_mixer(ps_, 0, gt - 1, None, None, pso, None, None, Wo, P, stage=2)

                load_xT(x_d, None, 0, xin, xbf, pT, xT[0])
                for j in range(4):
                    piece(0, j)
                sgu(0)
                for mt in range(8):
                    if mt + 1 < 8:
                        load_xT(x_d, None, mt + 1, xin, xbf, pT, xT[(mt + 1) % 2])
                    for j in range(4):
                        if mt + 1 < 8:
                            piece(mt + 1, j)
                        back(mt, j)
                    if mt + 1 < 8:
                        sgu(mt + 1)
                post_mixer(ps_, 0, NT - 1, None, None, pso, None, None, Wo, P, stage=2)
                kb.barrier()
                kb.release_sems()

        def moe(lyr, dst_d, dst_B):
            with contextlib.ExitStack() as ps_:
                fg = kb.sb("f_g", [128, D], F32, ps_)
                fb = kb.sb("f_b", [128, D], F32, ps_)
                bcast_load(fg, ffn_g_d[lyr:lyr + 1, :])
                bcast_load(fb, ffn_b_d[lyr:lyr + 1, :])
                xTh = kb.sb("f_xTh", [128, 8, 2048], BF16, ps_)
                yacc = kb.sb("f_yacc", [128, 16, D], F32, ps_)
                w1s = [kb.sb("f_w1_%d" % i, [128, 8, 512], BF16, ps_) for i in range(2)]
                w3s = [kb.sb("f_w3_%d" % i, [128, 8, 512], BF16, ps_) for i in range(2)]
                w2s = [kb.sb("f_w2_%d" % i, [128, 4, D], BF16, ps_) for i in range(2)]
                hid = [kb.sb("f_hid%d" % i, [128, 4, 512], BF16, ps_) for i in range(2)]
                sl = [kb.sb("f_sl%d" % i, [128, 512], BF16, ps_) for i in range(2)]
                x1l = [kb.sb("f_x1l%d" % i, [128, D], F32, ps_) for i in range(2)]
                rr = [kb.sb("f_r%d" % i, [128, D], F32, ps_) for i in range(2)]
                st = kb.sb("f_st", [128, 12], F32, ps_)
                mv = kb.sb("f_mv", [128, 2], F32, ps_)
                tmp = kb.sb("f_tmp", [128, 2], F32, ps_)
                ph1 = [kb.ps("f_ph1_%d" % i, [128, 512], F32, ps_) for i in range(2)]
                ph3 = [kb.ps("f_ph3_%d" % i, [128, 512], F32, ps_) for i in range(2)]
                py = [kb.ps("f_py%d" % i, [128, 512], F32, ps_) for i in range(2)]

                def load_expert(e):
                    sl_ = e % 2
                    kb.dma("pool", w1s[sl_].t[:, :, :], w1_d[lyr, e].rearrange("(k p) f -> p k f", p=128), w1s[sl_], writes=[w1s[sl_]])
                    kb.dma("pool", w3s[sl_].t[:, :, :], w3_d[lyr, e].rearrange("(k p) f -> p k f", p=128), w3s[sl_], writes=[w3s[sl_]])
                    kb.dma("pool", w2s[sl_].t[:, :, :], w2_d[lyr, e].rearrange("(c p) d -> p c d", p=128), w2s[sl_], writes=[w2s[sl_]])

                cnt = [0]
                for hf in range(2):
                    kb.dma("sp", xTh.t[:, :, :], xT_d[:, :, hf * 2048:(hf + 1) * 2048], xTh,
                           reads=[xT_B[hf * 4 + i] for i in range(4)], writes=[xTh])
                    load_expert(0)
                    for e in range(16):
                        if e + 1 < 16:
                            load_expert(e + 1)
                        w1 = w1s[e % 2]
                        w3 = w3s[e % 2]
                        w2 = w2s[e % 2]
                        for mt in range(4):
                            hd = hid[(e * 4 + mt) % 2]
                            for fc in range(4):
                                i2 = cnt[0] % 2
                                cnt[0] += 1
                                xs = xTh.t[:, :, mt * 512:(mt + 1) * 512]
                                kb.mmg(ph1[i2], ph1[i2].t[:, :], [(w1.t[:, k, fc * 128:(fc + 1) * 128], xTh.t[:, k, mt * 512:(mt + 1) * 512]) for k in range(8)],
                                       reads=[w1, xTh])
                                kb.mmg(ph3[i2], ph3[i2].t[:, :], [(w3.t[:, k, fc * 128:(fc + 1) * 128], xTh.t[:, k, mt * 512:(mt + 1) * 512]) for k in range(8)],
                                       reads=[w3, xTh])
                                kb.act(sl[i2].t[:, :], ph1[i2].t[:, :], AF.Silu, [ph1[i2]], [sl[i2]])
                                kb.tt("dve", hd.t[:, fc, :], sl[i2].t[:, :], ph3[i2].t[:, :], ALU.mult, [sl[i2], ph3[i2]], [hd])
                            for s in range(4):
                                ti = mt * 4 + s
                                gt = hf * 16 + ti
                                for dh in range(2):
                                    p = py[dh]
                                    kb.mmg(p, p.t[:, :], [(hd.t[:, fc, s * 128:(s + 1) * 128], w2.t[:, fc, dh * 512:(dh + 1) * 512]) for fc in range(4)],
                                           reads=[hd, w2])
                                    ya = yacc.t[:, ti, dh * 512:(dh + 1) * 512]
                                    if e == 0:
                                        kb.ts("dve", ya, p.t[:, :], c_all.t[:, gt, e:e + 1], None, ALU.mult, None, [p, c_all], [yacc])
                                    else:
                                        kb.stt("dve", ya, p.t[:, :], c_all.t[:, gt, e:e + 1], ya, ALU.mult, ALU.add, [p, c_all, yacc], [yacc])
                    for ti in range(16):
                        gt = hf * 16 + ti
                        xl = x1l[ti % 2]
                        r = rr[ti % 2]
                        kb.dma("sp", xl.t[:, :], xa_d[gt * 128:(gt + 1) * 128, :], xl, reads=[xa_B[gt]], writes=[xl])
                        kb.stt("dve", r.t[:, :], xl.t[:, :], ALPHA, yacc.t[:, ti, :], ALU.mult, ALU.add, [xl, yacc], [r])
                        layernorm(ps_, r, D, fg, fb, xl, xl.t[:, :], st, mv, tmp)
                        kb.dma("pool", dst_d[gt * 128:(gt + 1) * 128, :], xl.t[:, :], xl, reads=[xl], writes=[dst_B[gt]])
                kb.barrier()
                kb.release_sems()

        def moe_sparse(lyr, dst_d, dst_B):
            with contextlib.ExitStack() as ps_:
                fg = kb.sb("f_g", [128, D], F32, ps_)
                fb = kb.sb("f_b", [128, D], F32, ps_)
                bcast_load(fg, ffn_g_d[lyr:lyr + 1, :])
                bcast_load(fb, ffn_b_d[lyr:lyr + 1, :])
                w1s = [kb.sb("f_w1", [128, 8, 512], BF16, ps_) for i in range(2)]
                w3s = [kb.sb("f_w3", [128, 8, 512], BF16, ps_) for i in range(2)]
                w2s = [kb.sb("f_w2", [128, 4, D], BF16, ps_) for i in range(2)]
                xg = [kb.sb("f_xg", [128, D], BF16, ps_) for i in range(8)]
                lst = [kb.sb("f_lst", [128, 2], I32, ps_) for i in range(8)]
                xgT = [kb.sb("f_xgT", [128, 8, 128], BF16, ps_) for i in range(8)]
                sl = [kb.sb("f_sl", [128, 512], BF16, ps_) for i in range(2)]
                hid = [kb.sb("f_hid", [128, 4, 128], BF16, ps_) for i in range(2)]
                ysb = [kb.sb("f_ysb", [128, D], F32, ps_) for i in range(4)]
                y2l = [kb.sb("f_y2l", [128, 2, D], F32, ps_) for i in range(2)]
                x1l = [kb.sb("f_x1l", [128, D], F32, ps_) for i in range(2)]
                rr = [kb.sb("f_r", [128, D], F32, ps_) for i in range(2)]
                st = kb.sb("f_st", [128, 12], F32, ps_)
                mv = kb.sb("f_mv", [128, 2], F32, ps_)
                tmp = kb.sb("f_tmp", [128, 2], F32, ps_)
                pT = kb.ps("f_pT", [128, 1024], BF16, ps_)
                ph1 = [kb.ps("f_ph1", [128, 512], F32, ps_) for i in range(2)]
                ph3 = [kb.ps("f_ph3", [128, 512], F32, ps_) for i in range(2)]
                py = [kb.ps("f_py", [128, 512], F32, ps_) for i in range(2)]
                for i in range(8):
                    kb.op("dve", lambda e, i=i: e.memset(xg[i].t[:, :], 0.0), writes=[xg[i]])

                def load_expert(e, part=None):
                    sl_ = e % 2
                    if part in (None, 0):
                        kb.dma("pool", w1s[sl_].t[:, :, :], w1_d[lyr, e].rearrange("(k p) f -> p k f", p=128), w1s[sl_], writes=[w1s[sl_]])
                    if part in (None, 1):
                        kb.dma("pool", w3s[sl_].t[:, :, :], w3_d[lyr, e].rearrange("(k p) f -> p k f", p=128), w3s[sl_], writes=[w3s[sl_]])
                    if part in (None, 2):
                        kb.dma("pool", w2s[sl_].t[:, :, :], w2_d[lyr, e].rearrange("(c p) d -> p c d", p=128), w2s[sl_], writes=[w2s[sl_]])

                load_expert(0)
                for e in range(16):
                    w1 = w1s[e % 2]
                    w3 = w3s[e % 2]
                    w2 = w2s[e % 2]
                    for reg in kb.mregs:
                        E_ = kb.E[kb.etmap[reg.engine]]
                        kb._waits(E_, [cnt_i], [])
                        E_.eng.reg_load(reg, cnt_i.t[0:1, e:e + 1])

                    def fetch(j, e=e):
                        i4 = (e % 2) * 4 + j % 4
                        base = e * 4096 + j * 128
                        kb.dma("sp", lst[i4].t[:, :], list_d[base:base + 128, :], lst[i4], writes=[lst[i4]])
                        kb.idma(xg[i4].t[:, :], x1b_d[:, :], lst[i4].t[:, 0:1], False, T - 1, xg[i4], reads=[lst[i4]], writes=[xg[i4]])

                    def transp(j, e=e):
                        i4 = (e % 2) * 4 + j % 4
                        for k in range(8):
                            kb.op("pe", lambda e_, k=k: e_.transpose(pT.t[:, k * 128:(k + 1) * 128], xg[i4].t[:, k * 128:(k + 1) * 128], ident.t[:]),
                                  reads=[xg[i4], ident], writes=[pT], mark=(k == 7))
                        kb.copy("act", xgT[i4].t[:, :, :], pT.t[:, :].rearrange("p (k t) -> p k t", k=8), [pT], [xgT[i4]])

                    def hpart(j, w1=w1, w3=w3, e=e):
                        i2 = j % 2
                        i4 = (e % 2) * 4 + j % 4
                        for fc in range(4):
                            kb.mmg(ph1[i2], ph1[i2].t[:, fc * 128:(fc + 1) * 128],
                                   [(w1.t[:, k, fc * 128:(fc + 1) * 128], xgT[i4].t[:, k, :]) for k in range(8)], reads=[w1, xgT[i4]])
                        for fc in range(4):
                            kb.mmg(ph3[i2], ph3[i2].t[:, fc * 128:(fc + 1) * 128],
                                   [(w3.t[:, k, fc * 128:(fc + 1) * 128], xgT[i4].t[:, k, :]) for k in range(8)], reads=[w3, xgT[i4]])
                        kb.act(sl[i2].t[:, :], ph1[i2].t[:, :], AF.Silu, [ph1[i2]], [sl[i2]])
                        kb.tt("dve", hid[i2].t[:, :, :].rearrange("p c t -> p (c t)"), sl[i2].t[:, :], ph3[i2].t[:, :], ALU.mult,
                              [sl[i2], ph3[i2]], [hid[i2]])

                    def ypart(j, w2=w2, e=e):
                        i2 = j % 2
                        i4 = j % 4
                        il = (e % 2) * 4 + j % 4
                        for dh in range(2):
                            kb.mmg(py[dh], py[dh].t[:, :], [(hid[i2].t[:, fc, :], w2.t[:, fc, dh * 512:(dh + 1) * 512]) for fc in range(4)],
                                   reads=[hid[i2], w2])
                        kb.copy("act", ysb[i4].t[:, 0:512], py[0].t[:, :], [py[0]], [ysb[i4]])
                        kb.copy("dve", ysb[i4].t[:, 512:1024], py[1].t[:, :], [py[1]], [ysb[i4]])
                        kb.idma(y2_d[:, :], ysb[i4].t[:, :], lst[il].t[:, 1:2], True, 2 * T - 1, ysb[i4], reads=[ysb[i4], lst[il]])

                    def slot2(k):
                        A = 2 * k
                        Bq = 2 * k + 1
                        hpart(A)
                        hpart(Bq)
                        ypart(A)
                        if A + 2 < NT:
                            fetch(A + 2)
                            transp(A + 2) if False else None
                        ypart(Bq)
                        if Bq + 2 < NT:
                            fetch(Bq + 2)

                    if e == 0:
                        for j_ in range(4):
                            fetch(j_)
                        transp(0)
                        transp(1)
                    if e + 1 < 16:
                        for j_ in range(4):
                            fetch(j_, e + 1)
                    NFLAT = 3

                    def slot2t(k):
                        A = 2 * k
                        hpart(A)
                        hpart(A + 1)
                        if A + 2 < NT:
                            transp(A + 2)
                        ypart(A)
                        if A + 3 < NT:
                            transp(A + 3)
                        ypart(A + 1)
                        if A + 4 < NT:
                            fetch(A + 4)
                        if A + 5 < NT:
                            fetch(A + 5)

                    for k in range(NFLAT):
                        kb.region(4096 - k * 256, lambda k=k: slot2t(k))
                        if e + 1 < 16:
                            load_expert(e + 1, k)
                            if k == 0:
                                transp(0, e + 1)
                                transp(1, e + 1)

                    def rest():
                        for k in range(NFLAT, NT // 2):
                            kb.region(4096 - k * 256, lambda k=k: slot2t(k))
                    kb.region(4096 - NFLAT * 256, rest)
                kb.barrier()
                kb.release_sems()
                for gt in range(NT):
                    yl = y2l[gt % 2]
                    xl = x1l[gt % 2]
                    r = rr[gt % 2]
                    kb.dma("sp", yl.t[:, :, :], y2_d[gt * 256:(gt + 1) * 256, :].rearrange("(p r) d -> p r d", r=2), yl, writes=[yl])
                    kb.dma("sp", xl.t[:, :], xa_d[gt * 128:(gt + 1) * 128, :], xl, reads=[xa_B[gt]], writes=[xl])
                    kb.op("act", lambda e, yl=yl, gt=gt: e.mul(out=yl.t[:, 0, :], in_=yl.t[:, 0, :], mul=cAB.t[:, gt, 0:1]), reads=[yl, cAB], writes=[yl])
                    kb.stt("dve", yl.t[:, 0, :], yl.t[:, 1, :], cAB.t[:, gt, 1:2], yl.t[:, 0, :], ALU.mult, ALU.add, [yl, cAB], [yl])
                    kb.stt("dve", r.t[:, :], xl.t[:, :], ALPHA, yl.t[:, 0, :], ALU.mult, ALU.add, [xl, yl], [r])
                    layernorm(ps_, r, D, fg, fb, xl, xl.t[:, :], st, mv, tmp)
                    kb.dma("pool", dst_d[gt * 128:(gt + 1) * 128, :], xl.t[:, :], xl, reads=[xl], writes=[dst_B[gt]])
                kb.barrier()
                kb.release_sems()

        def attention():
            with contextlib.ExitStack() as ps_:
                Wq = load_w(ps_, "a_Wqkv", w_qkv_d, 3072)
                xin = kb.sb("a_xin", [128, 4, D], F32, ps_)
                xbf = [kb.sb("a_xbf%d" % i, [128, D], BF16, ps_) for i in range(2)]
                xT = kb.sb("a_xT", [128, 8, 512], BF16, ps_)
                qm = [kb.sb("a_qm%d" % i, [128, 8, 512], BF16, ps_) for i in range(2)]
                km = [kb.sb("a_km%d" % i, [128, 8, 512], BF16, ps_) for i in range(2)]
                vm = [kb.sb("a_vm%d" % i, [128, 4, D], BF16, ps_) for i in range(2)]
                pT = kb.ps("a_pT", [128, 1024], BF16, ps_)
                pp = [kb.ps("a_pp%d" % i, [128, 512], F32, ps_) for i in range(4)]
                ppi = 0
                for mt in range(8):
                    load_xT(xb_d, xb_B, mt, xin, xbf, pT, xT)
                    q_ = qm[mt % 2]
                    k_ = km[mt % 2]
                    v_ = vm[mt % 2]
                    for hp in range(8):
                        p = pp[ppi % 4]; ppi += 1
                        kb.mmg(p, p.t[:, :], [(Wq.t[:, k, hp * 128:(hp + 1) * 128], xT.t[:, k, :]) for k in range(8)], reads=[Wq, xT])
                        kb.op("act", lambda e, p=p, hp=hp, q_=q_: e.mul(out=q_.t[:, hp, :], in_=p.t[:, :], mul=0.125), reads=[p], writes=[q_])
                        p = pp[ppi % 4]; ppi += 1
                        kb.mmg(p, p.t[:, :], [(Wq.t[:, k, 1024 + hp * 128:1024 + (hp + 1) * 128], xT.t[:, k, :]) for k in range(8)], reads=[Wq, xT])
                        kb.copy("dve", k_.t[:, hp, :], p.t[:, :], [p], [k_])
                    for s in range(4):
                        for dh in range(2):
                            p = pp[ppi % 4]; ppi += 1
                            kb.mmg(p, p.t[:, :], [(xT.t[:, k, s * 128:(s + 1) * 128], Wq.t[:, k, 2048 + dh * 512:2048 + (dh + 1) * 512]) for k in range(8)],
                                   reads=[Wq, xT])
                            kb.copy("dve" if dh == 0 else "act", v_.t[:, s, dh * 512:(dh + 1) * 512], p.t[:, :], [p], [v_])
                    cs = slice(mt * 512, (mt + 1) * 512)
                    kb.dma("pool", qT_d.rearrange("h p t -> p h t")[:, :, cs], q_.t[:, :, :], q_, reads=[q_], writes=[q_B[mt]])
                    kb.dma("pool", kT_d.rearrange("h p t -> p h t")[:, :, cs], k_.t[:, :, :], k_, reads=[k_], writes=[k_B[mt]])
                    kb.dma("pool", v_d[mt * 512:(mt + 1) * 512, :].rearrange("(s p) d -> p s d", p=128), v_.t[:, :, :], v_, reads=[v_], writes=[v_B[mt]])
                kb.barrier()
                kb.release_sems()

            with contextlib.ExitStack() as ps_:
                oT = kb.sb("a_oT", [128, 8, T], BF16, ps_)
                masks = kb.sb("a_mask", [128, 4, 512], BF16, ps_)
                negU = kb.sb("a_negU", [128, 128], BF16, ps_)
                negO = kb.sb("a_negO", [128, 128], BF16, ps_)
                zer = kb.sb("a_zero", [128, 128], BF16, ps_)
                kb.op("pool", lambda e: e.memset(masks.t[:, :, :], 1.0), writes=[masks])
                for m in range(4):
                    kb.op("pool", lambda e, m=m: e.affine_select(out=masks.t[:, m, :], in_=masks.t[:, m, :], pattern=[[1, 512]],
                                                                compare_op=ALU.is_gt, fill=0.0, base=-128 * m, channel_multiplier=-1),
                          reads=[masks], writes=[masks])
                kb.op("pool", lambda e: e.memset(negU.t[:, :], -1.0), writes=[negU])
                kb.op("pool", lambda e: e.affine_select(out=negU.t[:, :], in_=negU.t[:, :], pattern=[[-1, 128]],
                                                        compare_op=ALU.is_ge, fill=0.0, base=0, channel_multiplier=1),
                      reads=[negU], writes=[negU])
                kb.op("pool", lambda e: e.memset(negO.t[:, :], -1.0), writes=[negO])
                kb.op("pool", lambda e: e.memset(zer.t[:, :], 0.0), writes=[zer])
                with contextlib.ExitStack() as ps2:
                    qh = [kb.sb("a_qh%d" % i, [128, T], BF16, ps2) for i in range(2)]
                    kh = [kb.sb("a_kh%d" % i, [128, T], BF16, ps2) for i in range(2)]
                    vh = [kb.sb("a_vh%d" % i, [128, NT, 128], BF16, ps2) for i in range(2)]
                    ex = [[kb.sb("a_ex", [128, 512], F32, ps2) for _ in range(2)] for c in range(2)]
                    lnu = [[kb.sb("a_lnu", [128, 512], BF16, ps2) for _ in range(3)] for c in range(2)]
                    Sb = [[kb.sb("a_S", [128, 512], BF16, ps2) for _ in range(4)] for c in range(2)]
                    att = [[kb.sb("a_att", [128, 512], BF16, ps2) for _ in range(2)] for c in range(2)]
                    pz = [[kb.ps("a_pz", [128, 512], F32, ps2) for _ in range(2)] for c in range(2)]
                    pc = [kb.ps("a_pc", [128, 512], F32, ps2) for c in range(2)]
                    pR = kb.ps("a_pR", [128, 512], F32, ps2)
                    pcC = kb.ps("a_pcC", [128, 512], F32, ps2)
                    exC = [kb.sb("a_exC", [128, 512], F32, ps2) for _ in range(2)]
                    lnuC = [kb.sb("a_lnuC", [128, 512], BF16, ps2) for _ in range(2)]
                    SC = [kb.sb("a_SC", [128, 512], BF16, ps2) for _ in range(2)]
                    attC = [kb.sb("a_attC", [128, 512], BF16, ps2) for _ in range(2)]
                    Ssave = [kb.sb("a_Ssave", [128, 512], BF16, ps2) for _ in range(2)]
                    flg = kb.sb("a_flg", [128, 128], I32, ps2)
                    flagB = [B() for _ in range(128)]
                    rmin = kb.sb("a_rmin", [128, 2], F32, ps2)
                    THR = 120.0
                    NU = 6

                    def load_hp(hp):
                        i2 = hp % 2
                        kb.dma("sp", qh[i2].t[:, :], qT_d[hp], qh[i2], reads=q_B, writes=[qh[i2]])
                        kb.dma("sp", kh[i2].t[:, :], kT_d[hp], kh[i2], reads=k_B, writes=[kh[i2]])
                        kb.dma("sp", vh[i2].t[:, :, :], v_d.rearrange("(b p) d -> p b d", p=128)[:, :, hp * 128:(hp + 1) * 128], vh[i2],
                               reads=v_B, writes=[vh[i2]])

                    class It:
                        pass

                    nctr = [0, 0]

                    def make_items(hp, c):
                        items = []
                        for i in range(8):
                            blo = max(4 * i + 3 - (NU - 1), 0)
                            for b in range(4 * i + 3, blo - 1, -1):
                                it = It()
                                it.c = c
                                it.hp = hp
                                it.i = i
                                it.b = b
                                it.first = (b == 4 * i + 3)
                                it.last = (b == 0)
                                it.ulast = (b == blo)
                                it.hasC = (blo > 0)
                                it.fcol = (hp * 2 + c) * 8 + i
                                it.m = b - 4 * i
                                it.qlo = 128 * it.m if it.m > 0 else 0
                                it.n = nctr[c]
                                nctr[c] += 1
                                items.append(it)
                        return items

                    def stageA(it):
                        c = it.c
                        q_ = qh[it.hp % 2]
                        k_ = kh[it.hp % 2]
                        pr = slice(c * 64, (c + 1) * 64)
                        qlo = it.qlo
                        n = 512 - qlo
                        qs = slice(it.i * 512 + qlo, (it.i + 1) * 512)
                        ks = slice(it.b * 128, (it.b + 1) * 128)
                        z = pz[c][it.n % 2]
                        e_ = ex[c][it.n % 2]
                        l_ = lnu[c][it.n % 3]
                        kb.op("pe", lambda e: e.matmul(z.t[:, 0:n], lhsT=k_.t[pr, ks], rhs=q_.t[pr, qs], start=True, stop=False),
                              reads=[k_, q_], writes=[z], mark=True)
                        kb.act(e_.t[:, 0:n], z.t[:, 0:n], AF.Exp, [z], [e_])
                        kb.act(l_.t[:, 0:n], e_.t[:, 0:n], AF.Ln, [e_], [l_], bias=1.0, scale=1.0)
                        if it.m >= 0:
                            kb.tt("pool", l_.t[:, 0:n], l_.t[:, 0:n], masks.t[:, it.m, qlo:512], ALU.mult, [l_, masks], [l_])
                        if not it.last:
                            Sn = Sb[c][it.n % 4]
                            if it.first:
                                if qlo > 0:
                                    kb.op("dve", lambda e: e.memset(Sn.t[:, 0:qlo], 0.0), writes=[Sn])
                                kb.copy("dve", Sn.t[:, qlo:512], l_.t[:, 0:n], [l_], [Sn])
                            else:
                                Sp = Sb[c][(it.n - 1) % 4]
                                if qlo > 0:
                                    kb.copy("dve", Sn.t[:, 0:qlo], Sp.t[:, 0:qlo], [Sp], [Sn])
                                kb.tt("dve", Sn.t[:, qlo:512], Sp.t[:, qlo:512], l_.t[:, 0:n], ALU.add, [Sp, l_], [Sn])
                            if it.ulast and it.hasC:
                                kb.mmg(pR, pR.t[:, :], [(onesb.t[:, :], Sn.t[:, :])], reads=[onesb, Sn])
                                kb.op("dve", lambda e: e.tensor_reduce(out=rmin.t[:, c:c + 1], in_=pR.t[:, :], axis=mybir.AxisListType.X, op=ALU.min),
                                      reads=[pR], writes=[rmin])
                                kb.ts("dve", flg.t[:, it.fcol:it.fcol + 1], rmin.t[:, c:c + 1], THR, None, ALU.is_gt, None, [rmin], [flagB[it.fcol]])
                                kb.copy("dve", Ssave[c].t[:, :], Sn.t[:, :], [Sn], [Ssave[c]])

                    def stageB(it):
                        c = it.c
                        q_ = qh[it.hp % 2]
                        k_ = kh[it.hp % 2]
                        pr = slice(c * 64, (c + 1) * 64)
                        qlo = it.qlo
                        n = 512 - qlo
                        qs = slice(it.i * 512 + qlo, (it.i + 1) * 512)
                        ks = slice(it.b * 128, (it.b + 1) * 128)
                        l_ = lnu[c][it.n % 3]
                        a_ = att[c][it.n % 2]
                        M = pz[c][it.n % 2]
                        prs = [(negU.t[:, :], l_.t[:, 0:n])]
                        rds = [negU, l_]
                        if not it.first:
                            Sp = Sb[c][(it.n - 1) % 4]
                            prs.append((negO.t[:, :], Sp.t[:, qlo:512]))
                            rds.append(Sp)
                        for pi, (l__, r__) in enumerate(prs):
                            kb.op("pe", lambda e, l__=l__, r__=r__, pi=pi: e.matmul(M.t[:, 0:n], lhsT=l__, rhs=r__, start=False, stop=(pi == len(prs) - 1)),
                                  reads=rds, writes=[M], mark=(pi == len(prs) - 1))
                        kb.act(a_.t[:, 0:n], M.t[:, 0:n], AF.Exp, [M], [a_])
                        if it.m >= 0:
                            kb.tt("pool", a_.t[:, 0:n], a_.t[:, 0:n], masks.t[:, it.m, qlo:512], ALU.mult, [a_, masks], [a_])

                    def stageC(it):
                        c = it.c
                        v_ = vh[it.hp % 2]
                        pr = slice(c * 64, (c + 1) * 64)
                        qlo = it.qlo
                        n = 512 - qlo
                        a_ = att[c][it.n % 2]
                        pcb = pc[c]
                        if it.first:
                            kb.op("pe", lambda e: e.matmul(pcb.t[:, :], lhsT=zer.t[:, :], rhs=masks.t[:, 0, :], start=True, stop=False),
                                  reads=[zer, masks], writes=[pcb], mark=False)
                        kb.op("pe", lambda e: e.matmul(pcb.t[:, qlo:512], lhsT=v_.t[:, it.b, :], rhs=a_.t[:, 0:n], start=False, stop=it.ulast),
                              reads=[v_, a_], writes=[pcb], mark=True)
                        if it.ulast:
                            kb.copy("dve", oT.t[pr, it.hp, it.i * 512:(it.i + 1) * 512], pcb.t[pr, :], [pcb], [oT])

                    def cond_rest(it):
                        c = it.c
                        q_ = qh[it.hp % 2]
                        k_ = kh[it.hp % 2]
                        v_ = vh[it.hp % 2]
                        pr = slice(c * 64, (c + 1) * 64)
                        qs = slice(it.i * 512, (it.i + 1) * 512)
                        for reg in kb.mregs:
                            E_ = kb.E[kb.etmap[reg.engine]]
                            kb._waits(E_, [flagB[it.fcol]], [])
                            E_.eng.reg_load(reg, flg.t[0:1, it.fcol:it.fcol + 1])

                        def cbody():
                            kb.op("pe", lambda e: e.matmul(pcC.t[:, :], lhsT=zer.t[:, :], rhs=masks.t[:, 0, :], start=True, stop=False),
                                  reads=[zer, masks], writes=[pcC], mark=False)
                            Sprev = Ssave[c]
                            for n2, b in enumerate(range(it.b - 1, -1, -1)):
                                ks = slice(b * 128, (b + 1) * 128)
                                z = pR
                                M = pR
                                e_ = exC[n2 % 2]
                                l_ = lnuC[n2 % 2]
                                a_ = attC[n2 % 2]
                                kb.op("pe", lambda e, ks=ks: e.matmul(z.t[:, :], lhsT=k_.t[pr, ks], rhs=q_.t[pr, qs], start=True, stop=False),
                                      reads=[k_, q_], writes=[z], mark=True)
                                kb.act(e_.t[:, :], z.t[:, :], AF.Exp, [z], [e_])
                                kb.act(l_.t[:, :], e_.t[:, :], AF.Ln, [e_], [l_], bias=1.0, scale=1.0)
                                kb.op("pe", lambda e, l_=l_: e.matmul(M.t[:, :], lhsT=negU.t[:, :], rhs=l_.t[:, :], start=False, stop=False),
                                      reads=[negU, l_], writes=[M], mark=False)
                                kb.op("pe", lambda e, Sprev=Sprev: e.matmul(M.t[:, :], lhsT=negO.t[:, :], rhs=Sprev.t[:, :], start=False, stop=True),
                                      reads=[negO, Sprev], writes=[M], mark=True)
                                kb.act(a_.t[:, :], M.t[:, :], AF.Exp, [M], [a_])
                                if b > 0:
                                    Sn = SC[n2 % 2]
                                    kb.tt("dve", Sn.t[:, :], Sprev.t[:, :], l_.t[:, :], ALU.add, [Sprev, l_], [Sn])
                                    Sprev = Sn
                                kb.op("pe", lambda e, b=b, a_=a_: e.matmul(pcC.t[:, :], lhsT=v_.t[:, b, :], rhs=a_.t[:, :], start=False, stop=(b == 0)),
                                      reads=[v_, a_], writes=[pcC], mark=True)
                            oc = oT.t[pr, it.hp, it.i * 512:(it.i + 1) * 512]
                            kb.tt("dve", oc, oc, pcC.t[pr, :], ALU.add, [oT, pcC], [oT])

                        kb.region(1, cbody)

                    load_hp(0)
                    for hp in range(8):
                        if hp + 1 < 8:
                            load_hp(hp + 1)
                        l0 = make_items(hp, 0)
                        l1 = make_items(hp, 1)
                        L = []
                        for a, b_ in zip(l0, l1):
                            L.append(a)
                            L.append(b_)
                        for g in range(len(L) + 4):
                            if g < len(L):
                                stageA(L[g])
                            if 0 <= g - 2 < len(L):
                                stageB(L[g - 2])
                            if 0 <= g - 4 < len(L):
                                stageC(L[g - 4])
                                if L[g - 4].ulast and L[g - 4].hasC:
                                    cond_rest(L[g - 4])
                    kb.barrier()
                    kb.release_sems()
                with contextlib.ExitStack() as ps3:
                    P = post_mixer_allocs(ps3, 1)
                    Wo = load_w(ps3, "a_Wo", w_out1_d, 1024)
                    xres = [kb.sb("a_xres%d" % i, [128, D], F32, ps3) for i in range(2)]
                    pso = [kb.ps("a_pso%d" % i, [128, 512], F32, ps3) for i in range(2)]
                    def s1(gt):
                        xr = xres[gt % 2]
                        kb.dma("sp", xr.t[:, :], xb_d[gt * 128:(gt + 1) * 128, :], xr, reads=[xb_B[gt]], writes=[xr])
                        post_mixer(ps3, 1, gt, xr.t[:, :], xr, pso, oT, slice(gt * 128, (gt + 1) * 128), Wo, P, stage=1)

                    s1(0)
                    for gt in range(NT):
                        if gt + 1 < NT:
                            s1(gt + 1)
                        post_mixer(ps3, 1, gt, None, None, pso, oT, None, Wo, P, stage=2)
                    kb.barrier()
                    kb.release_sems()

        moe_fn = moe_sparse if SPARSE else moe
        mixer0()
        if stop_after != "m0":
            moe_fn(0, xb_d, xb_B)
            if stop_after != "moe0":
                attention()
                if stop_after != "attn":
                    moe_fn(1, out_d, out_B)
        kb.barrier()
        kb.release_sems()

    es.close()
    return nc


_NC_CACHE = {}


def kernel(x, even_w_in, even_conv_w, even_sgu_ln_g, even_sgu_ln_b, even_sgu_w_s, even_sgu_b_s, even_w_out,
           odd_w_qkv, odd_w_out, mix_ln_g, mix_ln_b, moe_w_group, moe_b_group, moe_w_router, moe_b_router,
           moe_w1, moe_w3, moe_w2, ffn_ln_g, ffn_ln_b):
    f = lambda a: np.ascontiguousarray(np.asarray(a, dtype=np.float32))
    convw = f(np.asarray(even_conv_w)[0].reshape(3, 4, 128).transpose(2, 1, 0).reshape(128, 12))
    wsT = f(np.asarray(even_sgu_w_s)[0].transpose(0, 2, 1))
    bs = f(np.asarray(even_sgu_b_s)[0].reshape(1, 1024))
    wr = f(np.concatenate([np.asarray(moe_w_group),
                           np.asarray(moe_w_router).transpose(0, 2, 1, 3).reshape(2, D, 16)], axis=2))
    br = f(np.concatenate([np.asarray(moe_b_group), np.asarray(moe_b_router).reshape(2, 16)], axis=1))
    shared = {
        "w_in": f(even_w_in[0]), "convw": convw, "sgu_g": f(even_sgu_ln_g), "sgu_b": f(even_sgu_ln_b),
        "wsT": wsT, "bs": bs, "w_out0": f(even_w_out[0]), "w_qkv": f(odd_w_qkv[0]), "w_out1": f(odd_w_out[0]),
        "mix_g": f(mix_ln_g), "mix_b": f(mix_ln_b), "wr": wr, "br": br,
        "w1": f(np.asarray(moe_w1).reshape(2, 16, D, 512)), "w3": f(np.asarray(moe_w3).reshape(2, 16, D, 512)),
        "w2": f(np.asarray(moe_w2).reshape(2, 16, 512, D)), "ffn_g": f(ffn_ln_g), "ffn_b": f(ffn_ln_b),
    }
    xs = f(x)
    if "nc" not in _NC_CACHE:
        _NC_CACHE["nc"] = build()
    nc = _NC_CACHE["nc"]
    in_maps = [dict(shared, x=xs[c]) for c in range(NCORES)]
    res = run_bass_kernel_spmd(nc, in_maps, core_ids=list(range(NCORES)))
    return np.stack([res.results[c]["out"] for c in range(NCORES)], axis=0)
```
